# Optimizing a Trainium2 kernel written in Bass

```python
import math
import jax
import jax.numpy as jnp
from jax import lax
import numpy as np

D_MODEL = 1024
BATCH = 1
SEQ = 16384
DEPTH = 4

GRID_W = 64
CTX_LEN = 256
HEAD_DIM = 64
BRANCH_W = D_MODEL // 2
MIX_W = 2 * BRANCH_W
NA_HEADS = BRANCH_W // HEAD_DIM
NA_WIN_ROWS = 8
NA_WIN_COLS = 16
GQA_HEADS = BRANCH_W // HEAD_DIM
GQA_KV_HEADS = 2
GQA_GROUP = GQA_HEADS // GQA_KV_HEADS
GQA_KV_W = GQA_KV_HEADS * HEAD_DIM
ROPE_THETA = 10000.0
ROPE_FREQS = HEAD_DIM // 4
Q_BLOCK = 128
RWKV_HEADS = BRANCH_W // HEAD_DIM
DECAY_LORA = 32
ICLR_LORA = 32
HY_WIDTH = BRANCH_W
HY_ORDER = 2
HY_POS_BANDS = 16
HY_POS_DIM = 1 + 2 * HY_POS_BANDS
HY_FILTER_HIDDEN = 64
HY_DECAY_TARGET = 1e-2
HY_FAST_DECAY_PCT = 0.3
HY_SLOW_DECAY_PCT = 1.5
NORM_EPS = 1e-6
RWKV_GN_EPS = 64e-5
N_ATTN_LAYERS = (DEPTH + 1) // 2
N_REC_LAYERS = DEPTH // 2
ATTN_SPLITS = (BRANCH_W, BRANCH_W, BRANCH_W, BRANCH_W, BRANCH_W, GQA_KV_W, GQA_KV_W, BRANCH_W)
ATTN_IN = 6 * BRANCH_W + 2 * GQA_KV_W
RWKV_SPLITS = (BRANCH_W, BRANCH_W, BRANCH_W, DECAY_LORA, DECAY_LORA, ICLR_LORA, ICLR_LORA)
RWKV_SHIFT_W = 3 * BRANCH_W + 2 * DECAY_LORA + 2 * ICLR_LORA
HY_IN_W = (HY_ORDER + 1) * HY_WIDTH
REC_SPLITS = (RWKV_SHIFT_W, BRANCH_W, HY_IN_W, HY_WIDTH)
REC_IN = RWKV_SHIFT_W + BRANCH_W + HY_IN_W + HY_WIDTH

kernel_name = "hybrid_natten_gqa_rwkv7_hyena_dit"


def split_cols(u, sizes):
    parts, start = [], 0
    for s in sizes:
        parts.append(u[..., start:start + s])
        start += s
    return parts


def to_heads(t, n_heads):
    return t.reshape(t.shape[:-1] + (n_heads, HEAD_DIM))


def rms_norm(x, gain):
    xf = x.astype(jnp.float32)
    y = xf * lax.rsqrt(jnp.mean(xf * xf, axis=-1, keepdims=True) + NORM_EPS)
    return (y * gain.astype(jnp.float32)).astype(x.dtype)


def adaln(cond, ada_w, ada_b):
    m = jax.nn.silu(cond) @ ada_w + ada_b
    return jnp.split(m, 3, axis=-1)


def modulate(x, gain, shift, scale):
    return rms_norm(x, gain) * (1 + scale) + shift


def centred_pad(u):
    return jnp.pad(u, ((0, 0), (1, 1), (0, 0)))


def centred_conv3(u, taps):
    up = centred_pad(u)
    return up[:, :-2] * taps[0] + up[:, 1:-1] * taps[1] + up[:, 2:] * taps[2]


def centred_token_shift(u, mu):
    up = centred_pad(u)
    return u + (0.5 * (up[:, :-2] + up[:, 2:]) - u) * mu


def axial_rope_tables(n):
    t = jnp.arange(n, dtype=jnp.int32)
    pos = jnp.stack([t // GRID_W, t % GRID_W], axis=-1).astype(jnp.float32)
    inv_freq = ROPE_THETA ** (-jnp.arange(ROPE_FREQS, dtype=jnp.float32) / ROPE_FREQS)
    ang = pos[:, :, None] * inv_freq
    return jnp.cos(ang), jnp.sin(ang)


def apply_axial_rope(x, cos, sin):
    xs = x.astype(jnp.float32).reshape(x.shape[:-1] + (2, 2, ROPE_FREQS))
    x1, x2 = xs[..., 0, :], xs[..., 1, :]
    c, s = cos[None, :, None], sin[None, :, None]
    y = jnp.stack([x1 * c - x2 * s, x2 * c + x1 * s], axis=-2)
    return y.reshape(x.shape).astype(x.dtype)


def softmax_f32(s):
    return jax.nn.softmax(s.astype(jnp.float32), axis=-1)


def dense_gqa(q, k, v):
    s = jnp.einsum('bqhgd,bkhd->bhgqk', q, k) * (q.shape[-1] ** -0.5)
    p = softmax_f32(s).astype(v.dtype)
    return jnp.einsum('bhgqk,bkhd->bqhgd', p, v)


def blockwise_gqa(q, k, v, k_ctx, v_ctx):
    b, n, hk, g, dh = q.shape
    scale = dh ** -0.5

    def block(q_blk):
        s = jnp.concatenate([jnp.einsum('bqhgd,bkhd->bhgqk', q_blk, k),
                             jnp.einsum('bqhgd,bkhd->bhgqk', q_blk, k_ctx)], axis=-1) * scale
        p = softmax_f32(s).astype(v.dtype)
        return (jnp.einsum('bhgqk,bkhd->bqhgd', p[..., :n], v)
                + jnp.einsum('bhgqk,bkhd->bqhgd', p[..., n:], v_ctx))

    q_blocks = jnp.moveaxis(q.reshape(b, n // Q_BLOCK, Q_BLOCK, hk, g, dh), 1, 0)
    out = lax.map(block, q_blocks)
    return jnp.moveaxis(out, 0, 1).reshape(b, n, hk, g, dh)


def neighbourhood_layout(n, rows):
    kh, kw = min(NA_WIN_ROWS, rows), NA_WIN_COLS
    t = jnp.arange(n, dtype=jnp.int32)
    q_row, q_col = t // GRID_W, t % GRID_W
    row0 = jnp.clip(q_row - kh // 2, 0, rows - kh)
    col0 = jnp.clip(q_col - kw // 2, 0, GRID_W - kw)
    k_row = row0[:, None, None] + jnp.arange(kh, dtype=jnp.int32)[None, :, None]
    k_col = col0[:, None, None] + jnp.arange(kw, dtype=jnp.int32)[None, None, :]
    shape = (n, kh, kw)
    idx = jnp.broadcast_to(k_row * GRID_W + k_col, shape).reshape(n, kh * kw)
    rel_r = jnp.broadcast_to(k_row - q_row[:, None, None] + NA_WIN_ROWS - 1, shape).reshape(n, kh * kw)
    rel_c = jnp.broadcast_to(k_col - q_col[:, None, None] + NA_WIN_COLS - 1, shape).reshape(n, kh * kw)
    return idx, rel_r, rel_c


def neighbourhood_attention(q, k, v, k_ctx, v_ctx, rpb, rows):
    b, n, h, dh = q.shape
    idx, rel_r, rel_c = neighbourhood_layout(n, rows)
    n_win = idx.shape[-1]
    nblk = n // Q_BLOCK
    scale = dh ** -0.5

    def block(args):
        q_blk, idx_blk, rr_blk, rc_blk = args
        k_win = jnp.take(k, idx_blk, axis=1)
        v_win = jnp.take(v, idx_blk, axis=1)
        s_win = jnp.einsum('bqhd,bqkhd->bhqk', q_blk, k_win) * scale + rpb[:, rr_blk, rc_blk][None]
        s_ctx = jnp.einsum('bqhd,bkhd->bhqk', q_blk, k_ctx) * scale
        p = softmax_f32(jnp.concatenate([s_win, s_ctx], axis=-1)).astype(v.dtype)
        return (jnp.einsum('bhqk,bqkhd->bqhd', p[..., :n_win], v_win)
                + jnp.einsum('bhqk,bkhd->bqhd', p[..., n_win:], v_ctx))

    xs = (jnp.moveaxis(q.reshape(b, nblk, Q_BLOCK, h, dh), 1, 0),
          idx.reshape(nblk, Q_BLOCK, n_win),
          rel_r.reshape(nblk, Q_BLOCK, n_win),
          rel_c.reshape(nblk, Q_BLOCK, n_win))
    out = lax.map(block, xs)
    return jnp.moveaxis(out, 0, 1).reshape(b, n, h, dh)


def rwkv_features(u, w0, w_up, a0, a_up, k_k, k_a):
    r, k, v, dw_f, dw_b, da_f, da_b = split_cols(u, RWKV_SPLITS)
    kk = to_heads(k * k_k, RWKV_HEADS).astype(jnp.float32)
    kk = kk * lax.rsqrt(jnp.sum(kk * kk, axis=-1, keepdims=True) + 1e-12)
    dirs = []
    for d, (dw, da) in enumerate(((dw_f, da_f), (dw_b, da_b))):
        w_log = -jax.nn.softplus(-(w0[d] + jnp.tanh(dw) @ w_up[d])) - 0.5
        decay = jnp.exp(-jnp.exp(w_log.astype(jnp.float32)))
        a = jax.nn.sigmoid(a0[d] + da @ a_up[d])
        k_d = k * (1 + (a - 1) * k_a)
        dirs.append((to_heads(decay, RWKV_HEADS), to_heads(k_d, RWKV_HEADS), kk * to_heads(a, RWKV_HEADS)))
    return to_heads(r, RWKV_HEADS), to_heads(v, RWKV_HEADS), kk, dirs


def wkv_scan(s0, r, decay, k, v, kk, b, reverse, emit):
    xs = tuple(jnp.moveaxis(t, 1, 0) for t in (r, decay, k, v, kk, b))

    def step(s, inp):
        r_t, w_t, k_t, v_t, kk_t, b_t = inp
        sa = jnp.einsum('bhij,bhj->bhi', s, kk_t)
        s = s * w_t[:, :, None, :] - sa[..., None] * b_t[:, :, None, :] + v_t[..., None] * k_t[:, :, None, :]
        y = jnp.einsum('bhij,bhj->bhi', s, r_t) if emit else None
        return s, y

    s_final, ys = lax.scan(step, s0, xs, reverse=reverse)
    return s_final, (jnp.moveaxis(ys, 0, 1) if emit else None)


def rwkv_readout(y, r, k_bonus, v, r_k, gn_w, gn_b):
    yf = y.astype(jnp.float32)
    mean = jnp.mean(yf, axis=-1, keepdims=True)
    var = jnp.mean(jnp.square(yf - mean), axis=-1, keepdims=True)
    yn = ((yf - mean) * lax.rsqrt(var + RWKV_GN_EPS)).reshape(y.shape[:2] + (BRANCH_W,))
    yn = (yn * gn_w + gn_b).astype(v.dtype)
    bonus = jnp.sum(r * k_bonus * r_k, axis=-1, keepdims=True) * v
    return yn + bonus.reshape(yn.shape)


def rwkv_branch(u_lat, u_ctx, mu, w0, w_up, a0, a_up, k_k, k_a, r_k, gn_w, gn_b, ctx_out):
    r, v, kk, dirs = rwkv_features(centred_token_shift(u_lat, mu), w0, w_up, a0, a_up, k_k, k_a)
    rc, vc, kkc, dirs_c = rwkv_features(centred_token_shift(u_ctx, mu), w0, w_up, a0, a_up, k_k, k_a)
    s0 = jnp.zeros((u_lat.shape[0], RWKV_HEADS, HEAD_DIM, HEAD_DIM), jnp.float32)
    y_lat, y_ctx = [], []
    for d in range(2):
        reverse = d == 1
        decay_c, k_c, b_c = dirs_c[d]
        s_ctx, yc = wkv_scan(s0, rc, decay_c, k_c, vc, kkc, b_c, reverse, ctx_out)
        decay_l, k_l, b_l = dirs[d]
        _, yl = wkv_scan(s_ctx, r, decay_l, k_l, v, kk, b_l, reverse, True)
        y_lat.append(yl)
        y_ctx.append(yc)
    out_lat = rwkv_readout(y_lat[0] + y_lat[1], r, 0.5 * (dirs[0][1] + dirs[1][1]), v, r_k, gn_w, gn_b)
    out_ctx = None
    if ctx_out:
        out_ctx = rwkv_readout(y_ctx[0] + y_ctx[1], rc, 0.5 * (dirs_c[0][1] + dirs_c[1][1]), vc, r_k, gn_w, gn_b)
    return out_lat, out_ctx


def hyena_filters(L, w1, b1, w2, b2, w3, b3):
    t_idx = jnp.arange(L, dtype=jnp.float32)
    t = t_idx / max(L - 1, 1)
    bands = jnp.linspace(1e-4, HY_POS_BANDS - 1, HY_POS_BANDS, dtype=jnp.float32)
    ang = 2.0 * math.pi * bands[None, :] * t_idx[:, None] / L
    feats = jnp.concatenate([t[:, None], jnp.cos(ang), -jnp.sin(ang)], axis=-1)
    hid = jnp.sin(feats @ w1 + b1)
    hid = jnp.sin(hid @ w2 + b2)
    taps = (hid @ w3 + b3).reshape(L, 2, HY_ORDER, HY_WIDTH)
    deltas = jnp.linspace(math.log(HY_DECAY_TARGET) / HY_FAST_DECAY_PCT,
                          math.log(HY_DECAY_TARGET) / HY_SLOW_DECAY_PCT, HY_WIDTH, dtype=jnp.float32)
    window = jnp.exp(-t[:, None] * jnp.abs(deltas))
    taps = taps * window[:, None, None, :]
    fwd, bwd = taps[:, 0], taps[:, 1]
    kern = jnp.concatenate([fwd[:1] + bwd[:1], fwd[1:], jnp.zeros_like(fwd[:1]), bwd[:0:-1]], axis=0)
    kern = kern.astype(jnp.float32)
    return kern * lax.rsqrt(jnp.sum(kern * kern, axis=0, keepdims=True))


def fft_long_conv(z, kern, skip):
    L = z.shape[1]
    zf = jnp.fft.rfft(z.astype(jnp.float32), n=2 * L, axis=1)
    kf = jnp.fft.rfft(kern, n=2 * L, axis=0)
    y = jnp.fft.irfft(zf * kf[None], n=2 * L, axis=1)[:, :L]
    return (y + z * skip).astype(z.dtype)


def hyena_operator(u, short_taps, kern, skip):
    v, x1, x2 = split_cols(centred_conv3(u, short_taps), (HY_WIDTH, HY_WIDTH, HY_WIDTH))
    z = v
    for o, gate in enumerate((x1, x2)):
        z = gate * fft_long_conv(z, kern[:, o], skip[o])
    return z


def attn_layer(x, ctx, c, c_ctx, norm_g, ada_w, ada_b, w_in, rpb, q_gain, k_gain, w_out, cos, sin, rows, ctx_out):
    b, n, _ = x.shape
    lc = ctx.shape[1]
    shift, scale, gate = adaln(c, ada_w, ada_b)
    shift_c, scale_c, gate_c = adaln(c_ctx, ada_w, ada_b)
    u = modulate(x, norm_g, shift[:, None], scale[:, None]) @ w_in
    uc = modulate(ctx, norm_g, shift_c, scale_c) @ w_in
    qa, ka, va, ga, qb, kb, vb, gb = split_cols(u, ATTN_SPLITS)
    qa_c, ka_c, va_c, ga_c, qb_c, kb_c, vb_c, gb_c = split_cols(uc, ATTN_SPLITS)
    ka_c, va_c = to_heads(ka_c, NA_HEADS), to_heads(va_c, NA_HEADS)
    o_a = neighbourhood_attention(to_heads(qa, NA_HEADS), to_heads(ka, NA_HEADS), to_heads(va, NA_HEADS),
                                  ka_c, va_c, rpb, rows)
    kb_c = rms_norm(to_heads(kb_c, GQA_KV_HEADS), k_gain)
    vb_c = to_heads(vb_c, GQA_KV_HEADS)
    q_lat = apply_axial_rope(rms_norm(to_heads(qb, GQA_HEADS), q_gain), cos, sin)
    q_lat = q_lat.reshape(b, n, GQA_KV_HEADS, GQA_GROUP, HEAD_DIM)
    k_lat = apply_axial_rope(rms_norm(to_heads(kb, GQA_KV_HEADS), k_gain), cos, sin)
    o_b = blockwise_gqa(q_lat, k_lat, to_heads(vb, GQA_KV_HEADS), kb_c, vb_c)
    y = jnp.concatenate([o_a.reshape(b, n, BRANCH_W) * jax.nn.silu(ga),
                         o_b.reshape(b, n, BRANCH_W) * jax.nn.silu(gb)], axis=-1) @ w_out
    x = x + gate[:, None] * y
    if ctx_out:
        oa_c = dense_gqa(to_heads(qa_c, NA_HEADS)[:, :, :, None], ka_c, va_c)
        qb_c = rms_norm(to_heads(qb_c, GQA_HEADS), q_gain).reshape(b, lc, GQA_KV_HEADS, GQA_GROUP, HEAD_DIM)
        ob_c = dense_gqa(qb_c, kb_c, vb_c)
        yc = jnp.concatenate([oa_c.reshape(b, lc, BRANCH_W) * jax.nn.silu(ga_c),
                              ob_c.reshape(b, lc, BRANCH_W) * jax.nn.silu(gb_c)], axis=-1) @ w_out
        ctx = ctx + gate_c * yc
    return x, ctx


def rec_layer(x, ctx, c, c_ctx, norm_g, ada_w, ada_b, w_in, mu, w0, w_up, a0, a_up, k_k, k_a, r_k, gn_w, gn_b,
              hy_short, hy_w1, hy_b1, hy_w2, hy_b2, hy_w3, hy_b3, hy_skip, w_out, ctx_out):
    shift, scale, gate = adaln(c, ada_w, ada_b)
    shift_c, scale_c, gate_c = adaln(c_ctx, ada_w, ada_b)
    u = modulate(x, norm_g, shift[:, None], scale[:, None]) @ w_in
    uc = modulate(ctx, norm_g, shift_c, scale_c) @ w_in
    u_rw, g_rw, u_hy, g_hy = split_cols(u, REC_SPLITS)
    uc_rw, gc_rw, uc_hy, gc_hy = split_cols(uc, REC_SPLITS)
    y_rw, yc_rw = rwkv_branch(u_rw, uc_rw, mu, w0, w_up, a0, a_up, k_k, k_a, r_k, gn_w, gn_b, ctx_out)
    y_hy = hyena_operator(u_hy, hy_short, hyena_filters(x.shape[1], hy_w1, hy_b1, hy_w2, hy_b2, hy_w3, hy_b3), hy_skip)
    y = jnp.concatenate([y_rw * jax.nn.silu(g_rw), y_hy * jax.nn.silu(g_hy)], axis=-1) @ w_out
    x = x + gate[:, None] * y
    if ctx_out:
        kern_c = hyena_filters(ctx.shape[1], hy_w1, hy_b1, hy_w2, hy_b2, hy_w3, hy_b3)
        yc_hy = hyena_operator(uc_hy, hy_short, kern_c, hy_skip)
        yc = jnp.concatenate([yc_rw * jax.nn.silu(gc_rw), yc_hy * jax.nn.silu(gc_hy)], axis=-1) @ w_out
        ctx = ctx + gate_c * yc
    return x, ctx


def setup_inputs(seed: int = 0) -> dict:
    key = jax.random.key(seed)
    keys = iter(jax.random.split(key, 48))

    def normal(shape, scale=1.0):
        return scale * jax.random.normal(next(keys), shape, jnp.float32)

    na, nr, d = N_ATTN_LAYERS, N_REC_LAYERS, D_MODEL
    return {
        "x": normal((BATCH, SEQ, d)),
        "c": normal((BATCH, d)),
        "ctx": normal((BATCH, CTX_LEN, d)),
        "c_ctx": normal((d,)),
        "attn_norm": 1.0 + normal((na, d), 0.1),
        "attn_ada_w": normal((na, d, 3 * d), d ** -0.5),
        "attn_ada_b": normal((na, 3 * d), 0.1),
        "attn_w_in": normal((na, d, ATTN_IN), d ** -0.5),
        "na_rpb": normal((na, NA_HEADS, 2 * NA_WIN_ROWS - 1, 2 * NA_WIN_COLS - 1), 0.5),
        "gqa_q_gain": 1.0 + normal((na, HEAD_DIM), 0.1),
        "gqa_k_gain": 1.0 + normal((na, HEAD_DIM), 0.1),
        "attn_w_out": normal((na, MIX_W, d), MIX_W ** -0.5),
        "rec_norm": 1.0 + normal((nr, d), 0.1),
        "rec_ada_w": normal((nr, d, 3 * d), d ** -0.5),
        "rec_ada_b": normal((nr, 3 * d), 0.1),
        "rec_w_in": normal((nr, d, REC_IN), d ** -0.5),
        "rwkv_mu": jax.random.uniform(next(keys), (nr, RWKV_SHIFT_W), jnp.float32),
        "rwkv_w0": jnp.linspace(-6.0, -0.5, BRANCH_W, dtype=jnp.float32) + normal((nr, 2, BRANCH_W), 0.1),
        "rwkv_w_up": normal((nr, 2, DECAY_LORA, BRANCH_W), 0.1),
        "rwkv_a0": normal((nr, 2, BRANCH_W), 0.5),
        "rwkv_a_up": normal((nr, 2, ICLR_LORA, BRANCH_W), ICLR_LORA ** -0.5),
        "rwkv_k_k": 0.85 + normal((nr, BRANCH_W), 0.05),
        "rwkv_k_a": 1.0 + normal((nr, BRANCH_W), 0.05),
        "rwkv_r_k": normal((nr, RWKV_HEADS, HEAD_DIM), 0.1),
        "rwkv_gn_w": 1.0 + normal((nr, BRANCH_W), 0.1),
        "rwkv_gn_b": normal((nr, BRANCH_W), 0.1),
        "hy_short": normal((nr, 3, HY_IN_W), 3 ** -0.5),
        "hy_w1": normal((nr, HY_POS_DIM, HY_FILTER_HIDDEN), HY_POS_DIM ** -0.5),
        "hy_b1": normal((nr, HY_FILTER_HIDDEN), 0.1),
        "hy_w2": normal((nr, HY_FILTER_HIDDEN, HY_FILTER_HIDDEN), HY_FILTER_HIDDEN ** -0.5),
        "hy_b2": normal((nr, HY_FILTER_HIDDEN), 0.1),
        "hy_w3": normal((nr, HY_FILTER_HIDDEN, 2 * HY_ORDER * HY_WIDTH), HY_FILTER_HIDDEN ** -0.5),
        "hy_b3": normal((nr, 2 * HY_ORDER * HY_WIDTH), 0.1),
        "hy_skip": normal((nr, HY_ORDER, HY_WIDTH), 0.5),
        "rec_w_out": normal((nr, MIX_W, d), MIX_W ** -0.5),
        "final_norm": 1.0 + normal((d,), 0.1),
    }


def reference(x, c, ctx, c_ctx, attn_norm, attn_ada_w, attn_ada_b, attn_w_in, na_rpb, gqa_q_gain, gqa_k_gain,
              attn_w_out, rec_norm, rec_ada_w, rec_ada_b, rec_w_in, rwkv_mu, rwkv_w0, rwkv_w_up, rwkv_a0, rwkv_a_up,
              rwkv_k_k, rwkv_k_a, rwkv_r_k, rwkv_gn_w, rwkv_gn_b, hy_short, hy_w1, hy_b1, hy_w2, hy_b2, hy_w3, hy_b3,
              hy_skip, rec_w_out, final_norm):
    n = x.shape[1]
    rows = n // GRID_W
    cos, sin = axial_rope_tables(n)
    for layer in range(DEPTH):
        i = layer // 2
        ctx_out = layer < DEPTH - 1
        if layer % 2 == 0:
            x, ctx = attn_layer(x, ctx, c, c_ctx, attn_norm[i], attn_ada_w[i], attn_ada_b[i], attn_w_in[i],
                                na_rpb[i], gqa_q_gain[i], gqa_k_gain[i], attn_w_out[i], cos, sin, rows, ctx_out)
        else:
            x, ctx = rec_layer(x, ctx, c, c_ctx, rec_norm[i], rec_ada_w[i], rec_ada_b[i], rec_w_in[i],
                               rwkv_mu[i], rwkv_w0[i], rwkv_w_up[i], rwkv_a0[i], rwkv_a_up[i], rwkv_k_k[i],
                               rwkv_k_a[i], rwkv_r_k[i], rwkv_gn_w[i], rwkv_gn_b[i], hy_short[i], hy_w1[i],
                               hy_b1[i], hy_w2[i], hy_b2[i], hy_w3[i], hy_b3[i], hy_skip[i], rec_w_out[i], ctx_out)
    return rms_norm(x, final_norm)
```

```python
import numpy as np
import concourse.bass as bass
import concourse.mybir as mybir
from concourse.bass_utils import run_bass_kernel_spmd

F32 = mybir.dt.float32
BF16 = mybir.dt.bfloat16
I32 = mybir.dt.int32
ALU = mybir.AluOpType
AF = mybir.ActivationFunctionType


class Buf:
    __slots__ = ("w", "r", "name")

    def __init__(self, name=""):
        self.w = None
        self.r = {}
        self.name = name


class Prog:
    ENGS = ("tensor", "vector", "scalar", "gpsimd", "sync")
    NDSEM = 6

    def __init__(self, nc):
        self.nc = nc
        self.lists = {e: [] for e in self.ENGS}
        self.count = {e: 0 for e in self.ENGS}
        self.waited = {e: {} for e in self.ENGS}
        self.sems = {}
        self._ctx = []
        for e in self.ENGS:
            cm = nc.semaphore("s_" + e)
            self.sems[e] = cm.__enter__()
            self._ctx.append(cm)
        self.dsem = {}
        self.dcount = {}
        self.dnext = {}
        for q in ("sync", "scalar", "gpsimd"):
            self.dsem[q] = []
            for i in range(self.NDSEM):
                cm = nc.semaphore("d_%s%d" % (q, i))
                self.dsem[q].append(cm.__enter__())
                self._ctx.append(cm)
                self.sems["d_%s%d" % (q, i)] = self.dsem[q][-1]
            self.dcount[q] = [0] * self.NDSEM
            self.dnext[q] = 0

    def _need(self, eng, evs):
        best = {}
        for ev in evs:
            if ev is None:
                continue
            k, v = ev
            if best.get(k, 0) < v:
                best[k] = v
        for k, v in best.items():
            if self.waited[eng].get(k, 0) >= v:
                continue
            self.waited[eng][k] = v
            sem = self.sems[k]
            self.lists[eng].append(lambda E, sem=sem, v=v: E.wait_ge(sem, v))

    def _deps(self, reads, writes):
        evs = []
        for b in reads:
            evs.append(b.w)
        for b in writes:
            evs.append(b.w)
            evs.extend(b.r.items())
        return evs

    def op(self, eng, fn, reads=(), writes=()):
        self._need(eng, self._deps(reads, writes))
        self.count[eng] += 1
        c = self.count[eng]
        sem = self.sems[eng]
        self.lists[eng].append(lambda E, fn=fn, sem=sem: fn(E).then_inc(sem, 1))
        ev = (eng, c)
        for b in reads:
            b.r[eng] = c
        for b in writes:
            b.w = ev
            b.r = {}
        return ev

    def dma(self, q, out, in_, reads=(), writes=(), **kw):
        i = self.dnext[q]
        self.dnext[q] = (i + 1) % self.NDSEM
        key = "d_%s%d" % (q, i)
        evs = self._deps(reads, writes)
        if self.dcount[q][i] > 0:
            evs.append((key, self.dcount[q][i]))
        self._need(q, evs)
        self.dcount[q][i] += 16
        v = self.dcount[q][i]
        sem = self.sems[key]
        self.lists[q].append(lambda E, out=out, in_=in_, sem=sem, kw=kw: E.dma_start(out=out, in_=in_, **kw).then_inc(sem, 16))
        ev = (key, v)
        for b in reads:
            b.r[key] = v
        for b in writes:
            b.w = ev
            b.r = {}
        return ev

    def finish(self):
        evs = []
        for q in self.dsem:
            for i in range(self.NDSEM):
                if self.dcount[q][i]:
                    evs.append(("d_%s%d" % (q, i), self.dcount[q][i]))
        for e in self.ENGS:
            if e != "sync" and self.count[e]:
                evs.append((e, self.count[e]))
        self._need("sync", evs)
        nc = self.nc
        lists = self.lists
        with nc.Block() as block:
            @block.sync
            def _(E):
                for f in lists["sync"]:
                    f(E)

            @block.tensor
            def _(E):
                for f in lists["tensor"]:
                    f(E)

            @block.vector
            def _(E):
                for f in lists["vector"]:
                    f(E)

            @block.scalar
            def _(E):
                for f in lists["scalar"]:
                    f(E)

            @block.gpsimd
            def _(E):
                for f in lists["gpsimd"]:
                    f(E)
        for cm in reversed(self._ctx):
            cm.__exit__(None, None, None)


from contextlib import ExitStack

NB = 18
NT = NB * 128
D = 1024
EPS = 1e-6


class KB:
    def __init__(self, nc):
        self.nc = nc
        self.P = Prog(nc)
        self.es = ExitStack()

    def sb(self, name, shape, dt):
        return self.es.enter_context(self.nc.sbuf_tensor(name, shape, dt)), Buf(name)

    def ps(self, name, shape, dt):
        return self.es.enter_context(self.nc.psum_tensor(name, shape, dt)), Buf(name)

    def din(self, name, shape, dt):
        return self.nc.dram_tensor(name, list(shape), dt, kind="ExternalInput").ap()

    def dout(self, name, shape, dt):
        return self.nc.dram_tensor(name, list(shape), dt, kind="ExternalOutput").ap()

    def V(self, fn, r=(), w=()):
        return self.P.op("vector", fn, r, w)

    def A(self, fn, r=(), w=()):
        return self.P.op("scalar", fn, r, w)

    def G(self, fn, r=(), w=()):
        return self.P.op("gpsimd", fn, r, w)

    def T(self, fn, r=(), w=()):
        return self.P.op("tensor", fn, r, w)

    def dma(self, q, out, in_, r=(), w=(), **kw):
        return self.P.dma(q, out, in_, r, w, **kw)

    def done(self):
        self.P.finish()
        self.es.close()


def load_w_bf16(k, w_dram, wt, wb, ncols, rows=8):
    first = True
    for kc in range(rows):
        c0 = 0
        while c0 < ncols:
            c1 = min(ncols, c0 + 2048)
            k.dma("gpsimd", wt[:, kc, c0:c1], w_dram[kc * 128:(kc + 1) * 128, c0:c1], w=[] if not first else [wb])
            first = False
            c0 = c1
    return


def build_tok(in_w, has_prev, final, u_dt, NB=18, NLAT=16):
    NT = NB * 128
    nc = bass.Bass("TRN2", target_bir_lowering=False)
    k = KB(nc)
    P = k.P
    x_in = k.din("x_in", [NT, D], F32)
    ident_d = k.din("ident", [128, 128], F32)
    if has_prev:
        mixT = k.din("mixT", [D, NT], BF16)
        w_out = k.din("w_out", [D, D], F32)
        gates_in = k.din("gates_in", [2, 128, D], F32)
        x_out = k.dout("x_out", [NT, D], F32)
    if final:
        fn_g = k.din("final_norm", [1, D], F32)
        y_out = k.dout("y_out", [NT, D], F32)
    else:
        c_cols = k.din("c_cols", [128, 16], F32)
        norm_g = k.din("norm_g", [1, D], F32)
        ada_w = k.din("ada_w", [D, 3 * D], F32)
        ada_b = k.din("ada_b", [1, 3 * D], F32)
        w_in = k.din("w_in", [D, in_w], F32)
        u_out = k.dout("u_out", [NT, in_w], u_dt)
        gates_out = k.dout("gates_out", [2, 128, D], F32)

    ident, ident_b = k.sb("ident_sb", [128, 128], F32)
    identb, identb_b = k.sb("identb_sb", [128, 128], BF16)
    k.dma("sync", ident[:], ident_d, w=[ident_b])
    k.V(lambda E: E.tensor_copy(out=identb[:], in_=ident[:]), [ident_b], [identb_b])
    eps_t, eps_b = k.sb("eps_t", [128, 1], F32)
    k.V(lambda E: E.memset(eps_t[:], EPS), [], [eps_b])
    if has_prev:
        wo, wo_b = k.sb("wo", [128, 8, D], BF16)
        wo_evs = []
        for kc in range(8):
            wo_evs.append(k.dma("gpsimd", wo[:, kc, :], w_out[kc * 128:(kc + 1) * 128, :]))
        gp, gp_b = k.sb("gp", [128, 2, D], F32)
        k.dma("sync", gp[:], gates_in.rearrange("v p d -> p v d"), w=[gp_b])
    if final:
        fg, fg_b = k.sb("fg", [128, D], F32)
        k.dma("sync", fg[:], fn_g.partition_broadcast(128), w=[fg_b])
    else:
        wi, wi_b = k.sb("wi", [128, 8, in_w], BF16)
        wi_evs = []
        for kc in range(8):
            c0 = 0
            while c0 < in_w:
                c1 = min(in_w, c0 + 2048)
                wi_evs.append(k.dma("gpsimd", wi[:, kc, c0:c1], w_in[kc * 128:(kc + 1) * 128, c0:c1]))
                c0 = c1
        mod, mod_b = k.sb("mod", [128, 2, 3 * D], F32)
        Gt, G_b = k.sb("Gt", [128, 2, D], F32)
        with ExitStack() as es1:
            cc, cc_b = es1.enter_context(nc.sbuf_tensor("cc", [128, 16], F32)), Buf()
            rep, rep_b = es1.enter_context(nc.sbuf_tensor("rep", [128, 16, 128], F32)), Buf()
            ones, ones_b = es1.enter_context(nc.sbuf_tensor("ones", [128, 128], F32)), Buf()
            ab, ab_b = es1.enter_context(nc.sbuf_tensor("ab", [128, 3 * D], F32)), Buf()
            ng, ng_b = es1.enter_context(nc.sbuf_tensor("ng", [128, D], F32)), Buf()
            aw = [(es1.enter_context(nc.sbuf_tensor("aw%d" % i, [128, 8, 512], F32)), Buf()) for i in range(2)]
            pa = [(es1.enter_context(nc.psum_tensor("pa%d" % i, [128, 512], F32)), Buf()) for i in range(2)]
            k.dma("sync", cc[:], c_cols, w=[cc_b])
            k.dma("sync", ab[:], ada_b.partition_broadcast(128), w=[ab_b])
            k.dma("sync", ng[:], norm_g.partition_broadcast(128), w=[ng_b])
            k.A(lambda E: E.activation(out=cc[:], in_=cc[:], func=AF.Silu), [cc_b], [cc_b])
            k.V(lambda E: E.memset(ones[:], 1.0), [], [ones_b])
            for j in range(16):
                k.V(lambda E, j=j: E.tensor_scalar(out=rep[:, j, :], in0=ones[:], scalar1=cc[:, j:j + 1], scalar2=None, op0=ALU.mult),
                    [ones_b, cc_b], [rep_b])
            for g in range(6):
                awt, awb = aw[g % 2]
                k.dma("sync", awt[:], ada_w[:, g * 512:(g + 1) * 512].rearrange("(k p) n -> p k n", p=128), w=[awb])
                for v in range(2):
                    pt, pb = pa[v]
                    for kc in range(8):
                        k.T(lambda E, pt=pt, v=v, kc=kc, awt=awt: E.matmul(pt[:], rep[:, v * 8 + kc, :], awt[:, kc, :], start=(kc == 0), stop=(kc == 7)),
                            [rep_b, awb], [pb])
                    k.V(lambda E, pt=pt, v=v, g=g: E.tensor_tensor(out=mod[:, v, g * 512:(g + 1) * 512], in0=pt[:], in1=ab[:, g * 512:(g + 1) * 512], op=ALU.add),
                        [pb, ab_b], [mod_b])
            for v in range(2):
                k.V(lambda E, v=v: E.scalar_tensor_tensor(out=Gt[:, v, :], in0=mod[:, v, D:2 * D], scalar=1.0, in1=ng[:], op0=ALU.add, op1=ALU.mult),
                    [mod_b, ng_b], [G_b])
            k.dma("sync", gates_out.rearrange("v p d -> p v d"), mod[:, :, 2 * D:3 * D], r=[mod_b])
            scope_bufs = [cc_b, rep_b, ones_b, ab_b, ng_b, aw[0][1], aw[1][1], pa[0][1], pa[1][1]]

    xt = [k.sb("xt%d" % i, [128, D], F32) for i in range(2)]
    tmp = [k.sb("tmp%d" % i, [128, D], F32) for i in range(2)]
    ss = [k.sb("ss%d" % i, [128, 4], F32) for i in range(2)]
    junk, junk_b = k.sb("junk", [128, D], BF16)
    if has_prev:
        mt = [k.sb("mt%d" % i, [128, 8, 128], BF16) for i in range(2)]
        py = [k.ps("py%d" % i, [128, 512], F32) for i in range(2)]
    if not final:
        xm = [k.sb("xm%d" % i, [128, D], BF16) for i in range(2)]
        xmT = [k.sb("xmT%d" % i, [128, 8, 128], BF16) for i in range(2)]
        ptr = [k.ps("ptr%d" % i, [128, 8, 128], BF16) for i in range(2)]
        pu = [k.ps("pu%d" % i, [128, 512], F32) for i in range(3)]
        ut = [k.sb("ut%d" % i, [128, 512], u_dt) for i in range(4)]
    if not final:
        fence = Buf("fence")
        for b in scope_bufs:
            if b.w:
                fence.r[b.w[0]] = max(fence.r.get(b.w[0], 0), b.w[1])
            for kk, vv in b.r.items():
                fence.r[kk] = max(fence.r.get(kk, 0), vv)
    else:
        fence = Buf("fence")
    fence_evs = list(fence.r.items())
    for e in ("vector", "scalar", "gpsimd", "tensor", "sync"):
        P._need(e, fence_evs)
    if has_prev:
        for e in ("tensor",):
            P._need(e, wo_evs)
    if not final:
        P._need("tensor", wi_evs)

    ngroups = (in_w + 511) // 512 if not final else 0
    ucount = 0
    for b in range(NB):
        v = 0 if b < NLAT else 1
        xtt, xtb = xt[b % 2]
        tt, tb = tmp[b % 2]
        sst, ssb = ss[b % 2]
        k.dma("sync", xtt[:], x_in[b * 128:(b + 1) * 128, :], w=[xtb])
        if has_prev:
            mtt, mtb = mt[b % 2]
            k.dma("sync", mtt[:], mixT[:, b * 128:(b + 1) * 128].rearrange("(k p) t -> p k t", p=128), w=[mtb])
            for h in range(2):
                pyt, pyb = py[h]
                for kc in range(8):
                    k.T(lambda E, pyt=pyt, mtt=mtt, kc=kc, h=h: E.matmul(pyt[:], mtt[:, kc, :], wo[:, kc, h * 512:(h + 1) * 512], start=(kc == 0), stop=(kc == 7)),
                        [mtb], [pyb])
                k.V(lambda E, pyt=pyt, tt=tt, h=h, v=v: E.tensor_tensor(out=tt[:, h * 512:(h + 1) * 512], in0=pyt[:], in1=gp[:, v, h * 512:(h + 1) * 512], op=ALU.mult),
                    [pyb, gp_b], [tb])
            k.G(lambda E, xtt=xtt, tt=tt: E.tensor_tensor(out=xtt[:], in0=tt[:], in1=xtt[:], op=ALU.add), [tb, xtb], [xtb])
            k.dma("sync", x_out[b * 128:(b + 1) * 128, :], xtt[:], r=[xtb])
        k.A(lambda E, xtt=xtt, sst=sst: E.activation(out=junk[:], in_=xtt[:], func=AF.Square, accum_out=sst[:, 0:1]), [xtb], [junk_b, ssb])
        k.A(lambda E, sst=sst: E.activation(out=sst[:, 1:2], in_=sst[:, 0:1], func=AF.Sqrt, scale=1.0 / D, bias=eps_t[:]), [ssb, eps_b], [ssb])
        k.V(lambda E, sst=sst: E.reciprocal(out=sst[:, 2:3], in_=sst[:, 1:2]), [ssb], [ssb])
        if final:
            k.V(lambda E, xtt=xtt, tt=tt, sst=sst: E.scalar_tensor_tensor(out=tt[:], in0=xtt[:], scalar=sst[:, 2:3], in1=fg[:], op0=ALU.mult, op1=ALU.mult),
                [xtb, ssb, fg_b], [tb])
            k.dma("sync", y_out[b * 128:(b + 1) * 128, :], tt[:], r=[tb])
            continue
        xmt, xmb = xm[b % 2]
        k.V(lambda E, xtt=xtt, tt=tt, sst=sst, v=v: E.scalar_tensor_tensor(out=tt[:], in0=xtt[:], scalar=sst[:, 2:3], in1=Gt[:, v, :], op0=ALU.mult, op1=ALU.mult),
            [xtb, ssb, G_b], [tb])
        k.G(lambda E, tt=tt, xmt=xmt, v=v: E.tensor_tensor(out=xmt[:], in0=tt[:], in1=mod[:, v, 0:D], op=ALU.add), [tb, mod_b], [xmb])
        ptt, ptb = ptr[b % 2]
        xTt, xTb = xmT[b % 2]
        for kc in range(8):
            k.T(lambda E, ptt=ptt, xmt=xmt, kc=kc: E.transpose(ptt[:, kc, :], xmt[:, kc * 128:(kc + 1) * 128], identb[:]), [xmb, identb_b], [ptb])
        k.A(lambda E, ptt=ptt, xTt=xTt: E.copy(out=xTt[:], in_=ptt[:]), [ptb], [xTb])
        for g in range(ngroups):
            c0 = g * 512
            c1 = min(in_w, c0 + 512)
            put, pub = pu[ucount % 3]
            utt, utb = ut[ucount % 4]
            for kc in range(8):
                k.T(lambda E, put=put, xTt=xTt, kc=kc, c0=c0, c1=c1: E.matmul(put[:, 0:c1 - c0], xTt[:, kc, :], wi[:, kc, c0:c1], start=(kc == 0), stop=(kc == 7)),
                    [xTb], [pub])
            if ucount % 2 == 0:
                k.V(lambda E, put=put, utt=utt, c0=c0, c1=c1: E.tensor_copy(out=utt[:, 0:c1 - c0], in_=put[:, 0:c1 - c0]), [pub], [utb])
            else:
                k.A(lambda E, put=put, utt=utt, c0=c0, c1=c1: E.copy(out=utt[:, 0:c1 - c0], in_=put[:, 0:c1 - c0]), [pub], [utb])
            k.dma("sync", u_out[b * 128:(b + 1) * 128, c0:c1], utt[:, 0:c1 - c0], r=[utb])
            ucount += 1
    k.done()
    return nc


def _sc(s):
    return (s[0], [s[1]]) if isinstance(s, tuple) else (s, [])


def h_tt(k, out, a, b, op, eng="vector"):
    oa, ob = out; aa, ab = a; ba, bb = b
    return k.P.op(eng, lambda E: E.tensor_tensor(out=oa, in0=aa, in1=ba, op=op), [ab, bb], [ob])


def h_stt(k, out, a, s, b, op0, op1, accum=None):
    oa, ob = out; aa, ab = a; ba, bb = b
    sv, sb_ = _sc(s)
    if accum is None:
        return k.P.op("vector", lambda E: E.scalar_tensor_tensor(out=oa, in0=aa, scalar=sv, in1=ba, op0=op0, op1=op1), [ab, bb] + sb_, [ob])
    ca, cb = accum
    return k.P.op("vector", lambda E: E.scalar_tensor_tensor(out=oa, in0=aa, scalar=sv, in1=ba, op0=op0, op1=op1, accum_out=ca), [ab, bb] + sb_, [ob, cb])


def h_ts(k, out, a, s1, s2, op0, op1=None, eng="vector"):
    oa, ob = out; aa, ab = a
    s1v, s1b = _sc(s1)
    s2v, s2b = _sc(s2) if s2 is not None else (None, [])
    if op1 is None:
        return k.P.op(eng, lambda E: E.tensor_scalar(out=oa, in0=aa, scalar1=s1v, scalar2=None, op0=op0), [ab] + s1b, [ob])
    return k.P.op(eng, lambda E: E.tensor_scalar(out=oa, in0=aa, scalar1=s1v, scalar2=s2v, op0=op0, op1=op1), [ab] + s1b + s2b, [ob])


def h_act(k, out, a, func, scale=1.0, bias=None, accum=None):
    oa, ob = out; aa, ab = a
    kw = {}
    rd = [ab]
    wr = [ob]
    if bias is not None:
        bv, bb = _sc(bias)
        kw["bias"] = bv
        rd += bb
    if isinstance(scale, tuple):
        rd.append(scale[1]); scale = scale[0]
    if accum is not None:
        kw["accum_out"] = accum[0]
        wr.append(accum[1])
    return k.P.op("scalar", lambda E: E.activation(out=oa, in_=aa, func=func, scale=scale, **kw), rd, wr)


def h_recip(k, out, a):
    oa, ob = out; aa, ab = a
    return k.P.op("vector", lambda E: E.reciprocal(out=oa, in_=aa), [ab], [ob])


def h_copy(k, out, a, eng="vector"):
    oa, ob = out; aa, ab = a
    if eng == "scalar":
        return k.P.op(eng, lambda E: E.copy(out=oa, in_=aa), [ab], [ob])
    return k.P.op(eng, lambda E: E.tensor_copy(out=oa, in_=aa), [ab], [ob])


def h_mm(k, out, lhsT, rhs, start=True, stop=True):
    oa, ob = out; la, lb = lhsT; ra, rb = rhs
    return k.P.op("tensor", lambda E: E.matmul(oa, la, ra, start=start, stop=stop), [lb, rb], [ob])


def h_memset(k, out, val, eng="vector"):
    oa, ob = out
    return k.P.op(eng, lambda E: E.memset(oa, val), [], [ob])


def h_sigmoid(k, out, a, tmp, scale=1.0, bias=None):
    h_act(k, tmp, a, AF.Exp, scale=-scale, bias=bias)
    h_ts(k, tmp, tmp, 1.0, None, ALU.add)
    h_recip(k, out, tmp)


def h_silu(k, out, a, tmp):
    h_sigmoid(k, tmp, a, tmp)
    h_tt(k, out, tmp, a, ALU.mult)


import numpy as np

NEG = -30000.0


def na_plan(nlb):
    rows_blocks = nlb
    plan = []
    variants = {}
    for m in range(nlb):
        r0 = min(max(2 * m - 4, 0), 2 * nlb - 8)
        r1 = min(max(2 * m + 1 - 4, 0), 2 * nlb - 8) + 7
        kb0, kb1 = r0 // 2, r1 // 2
        edge = (m < 2) or (m >= nlb - 2)
        lst = []
        for kb in range(kb0, kb1 + 1):
            key = (m if edge else "i", kb - m)
            if key not in variants:
                variants[key] = (len(variants), m, kb)
            lst.append((kb, variants[key][0]))
        plan.append(lst)
    return plan, variants


def na_bias_tables(rpb_h, nlb):
    plan, variants = na_plan(nlb)
    rows = 2 * nlb
    out = np.full((128, len(variants), 128), NEG, np.float32)
    for key, (vid, m, kb) in variants.items():
        q = m * 128 + np.arange(128)
        kk = kb * 128 + np.arange(128)
        qr, qc = q // 64, q % 64
        kr, kc = kk // 64, kk % 64
        row0 = np.clip(qr - 4, 0, rows - 8)
        col0 = np.clip(qc - 8, 0, 64 - 16)
        inwin = ((kr[:, None] >= row0[None]) & (kr[:, None] < row0[None] + 8) &
                 (kc[:, None] >= col0[None]) & (kc[:, None] < col0[None] + 16))
        rr = np.clip(kr[:, None] - qr[None] + 7, 0, 14)
        cc = np.clip(kc[:, None] - qc[None] + 15, 0, 30)
        out[:, vid, :] = np.where(inwin, rpb_h[rr, cc], NEG)
    return out


def build_attn(nlb=128):
    NTK = (nlb + 2) * 128
    NKB = nlb + 2
    plan, variants = na_plan(nlb)
    nvar = len(variants)
    nc = bass.Bass("TRN2", target_bir_lowering=False)
    k = KB(nc)
    P = k.P
    qa_d = k.din("qaT", [64, NTK], BF16)
    ka_d = k.din("kaT", [64, NTK], BF16)
    va_d = k.din("va", [128, NKB, 65], BF16)
    ga_d = k.din("gaT", [64, NTK], BF16)
    qb_d = k.din("qbT", [64, NTK], BF16)
    kb_d = k.din("kbT", [64, NTK], BF16)
    vb_d = k.din("vb", [128, NKB, 65], BF16)
    gb_d = k.din("gbT", [64, NTK], BF16)
    bias_d = k.din("biasT", [128, nvar, 128], F32)
    cs_d = k.din("ropeC", [64, NTK], F32)
    sn_d = k.din("ropeS", [64, NTK], F32)
    gains_d = k.din("gains", [64, 2], F32)
    rmat_d = k.din("rmatT", [64, 64], F32)
    ident_d = k.din("ident", [128, 128], F32)
    sel_d = k.din("sel", [65, 64], F32)
    out_d = k.dout("mixT", [128, NTK], BF16)

    qT, qT_b = k.sb("qT", [64, NTK], BF16)
    kT, kT_b = k.sb("kT", [64, NTK], BF16)
    vE, vE_b = k.sb("vE", [128, NKB, 65], BF16)
    biasf, biasf_b = k.sb("biasf", [128, nvar, 128], F32)
    biasb, biasb_b = k.sb("biasb", [128, nvar, 128], BF16)
    identf, identf_b = k.sb("identf", [128, 128], F32)
    identb, identb_b = k.sb("identb", [128, 128], BF16)
    sel, sel_b = k.sb("sel_sb", [65, 64], F32)
    gains, gains_b = k.sb("gains_sb", [64, 2], F32)
    rmf, rmf_b = k.sb("rmf", [64, 64], F32)
    rmb, rmb_b = k.sb("rmb", [64, 64], BF16)
    ones64, ones64_b = k.sb("ones64", [64, 64], F32)
    eps_t, eps_b = k.sb("eps_t", [64, 1], F32)
    k.dma("sync", biasf[:], bias_d, w=[biasf_b])
    k.dma("sync", identf[:], ident_d, w=[identf_b])
    k.dma("sync", sel[:], sel_d, w=[sel_b])
    k.dma("sync", gains[:], gains_d, w=[gains_b])
    k.dma("sync", rmf[:], rmat_d, w=[rmf_b])
    k.V(lambda E: E.tensor_copy(out=biasb[:], in_=biasf[:]), [biasf_b], [biasb_b])
    k.V(lambda E: E.tensor_copy(out=identb[:], in_=identf[:]), [identf_b], [identb_b])
    k.V(lambda E: E.tensor_copy(out=rmb[:], in_=rmf[:]), [rmf_b], [rmb_b])
    k.V(lambda E: E.memset(ones64[:], 1.0 / 64), [], [ones64_b])
    k.V(lambda E: E.memset(eps_t[:], EPS), [], [eps_b])
    k.V(lambda E: E.tensor_scalar(out=gains[:, 0:1], in0=gains[:, 0:1], scalar1=0.125, scalar2=None, op0=ALU.mult), [gains_b], [gains_b])

    pS = [k.ps("pS%d" % i, [128, 512], F32) for i in range(4)]
    pacc = [k.ps("pacc%d" % i, [65, 512], F32) for i in range(2)]
    pmisc = [k.ps("pm%d" % i, [64, 512], F32) for i in range(2)]
    PT = [k.sb("PT%d" % i, [128, 512], BF16) for i in range(4)]
    accs = [k.sb("accs%d" % i, [65, 512], F32) for i in range(2)]
    gt = [k.sb("gt%d" % i, [64, 512], BF16) for i in range(2)]
    w1 = [k.sb("w1_%d" % i, [64, 512], F32) for i in range(2)]
    w2 = [k.sb("w2_%d" % i, [64, 512], F32) for i in range(2)]
    ot = [k.sb("ot%d" % i, [64, 512], BF16) for i in range(2)]
    cin = [k.sb("cin%d" % i, [64, 512], BF16) for i in range(2)]
    ctab = [k.sb("ctab%d" % i, [64, 512], F32) for i in range(2)]
    stab = [k.sb("stab%d" % i, [64, 512], F32) for i in range(2)]
    qnb = [k.sb("qnb%d" % i, [64, 512], BF16) for i in range(2)]

    state = {"fin": 0, "S": 0, "pre": 0}

    def finalize(pa, pab, g_d, row0, c0, n):
        i = state["fin"] % 2
        state["fin"] += 1
        at, ab = accs[i]
        gtt, gtb = gt[i]
        a1, a1b = w1[i]
        a2, a2b = w2[i]
        o, ob = ot[i]
        pm, pmb = pmisc[i]
        k.dma("sync", gtt[:, 0:n], g_d[:, c0:c0 + n], w=[gtb])
        k.A(lambda E: E.copy(out=at[:, 0:n], in_=pa[:, 0:n]), [pab], [ab])
        k.T(lambda E: E.matmul(pm[:, 0:n], sel[:], at[:, 0:n], start=True, stop=True), [sel_b, ab], [pmb])
        k.V(lambda E: E.reciprocal(out=a1[:, 0:n], in_=pm[:, 0:n]), [pmb], [a1b])
        k.V(lambda E: E.tensor_tensor(out=a1[:, 0:n], in0=a1[:, 0:n], in1=at[0:64, 0:n], op=ALU.mult), [a1b, ab], [a1b])
        k.A(lambda E: E.activation(out=a2[:, 0:n], in_=gtt[:, 0:n], func=AF.Exp, scale=-1.0), [gtb], [a2b])
        k.V(lambda E: E.tensor_scalar(out=a2[:, 0:n], in0=a2[:, 0:n], scalar1=1.0, scalar2=None, op0=ALU.add), [a2b], [a2b])
        k.V(lambda E: E.reciprocal(out=a2[:, 0:n], in_=a2[:, 0:n]), [a2b], [a2b])
        k.V(lambda E: E.tensor_tensor(out=a2[:, 0:n], in0=a2[:, 0:n], in1=gtt[:, 0:n], op=ALU.mult), [a2b, gtb], [a2b])
        k.V(lambda E: E.tensor_tensor(out=o[:, 0:n], in0=a1[:, 0:n], in1=a2[:, 0:n], op=ALU.mult), [a1b, a2b], [ob])
        k.dma("sync", out_d[row0:row0 + 64, c0:c0 + n], o[:, 0:n], r=[ob])

    def attend(qc0, n, kbs, pa, pab, first=True, last=True):
        pend = []
        nk = len(kbs)
        for idx, kb in enumerate(kbs):
            s = state["S"]
            state["S"] += 1
            pst, psb = pS[s % 4]
            ptt, ptb = PT[s % 4]
            k.T(lambda E, pst=pst, kb=kb: E.matmul(pst[:, 0:n], kT[:, kb * 128:(kb + 1) * 128], qT[:, qc0:qc0 + n], start=True, stop=True),
                [kT_b, qT_b], [psb])
            k.A(lambda E, pst=pst, ptt=ptt: E.activation(out=ptt[:, 0:n], in_=pst[:, 0:n], func=AF.Exp), [psb], [ptb])
            pend.append((idx, kb, ptt, ptb))
            if len(pend) > 2:
                j, kbj, pj, pjb = pend.pop(0)
                k.T(lambda E, kbj=kbj, pj=pj, j=j: E.matmul(pa[:, 0:n], vE[:, kbj, :], pj[:, 0:n], start=(first and j == 0), stop=(last and j == nk - 1)),
                    [vE_b, pjb], [pab])
        for (j, kbj, pj, pjb) in pend:
            k.T(lambda E, kbj=kbj, pj=pj, j=j: E.matmul(pa[:, 0:n], vE[:, kbj, :], pj[:, 0:n], start=(first and j == 0), stop=(last and j == nk - 1)),
                [vE_b, pjb], [pab])

    k.dma("sync", kT[:], ka_d, w=[kT_b])
    k.dma("sync", vE[:], va_d, w=[vE_b])
    k.dma("sync", qT[:], qa_d, w=[qT_b])
    k.V(lambda E: E.tensor_scalar(out=qT[:], in0=qT[:], scalar1=0.125, scalar2=None, op0=ALU.mult), [qT_b], [qT_b])
    ctxk = [nlb, nlb + 1]
    acc_i = 0
    for g0 in range(0, nlb, 4):
        pa, pab = pacc[acc_i % 2]
        acc_i += 1
        nq = min(4, nlb - g0)
        for mi in range(nq):
            m = g0 + mi
            lst = plan[m]
            nreg = len(lst) + 2
            s = state["S"]
            state["S"] += 2
            pA, pAb = pS[s % 4]
            pB, pBb = pS[(s + 1) % 4]
            tA, tAb = PT[s % 4]
            tB, tBb = PT[(s + 1) % 4]
            regs = []
            for j, (kb, var) in enumerate(lst + [(ctxk[0], None), (ctxk[1], None)]):
                pt_, pb_, tt_, tb_ = (pA, pAb, tA, tAb) if j < 4 else (pB, pBb, tB, tBb)
                jj = j % 4
                k.T(lambda E, pt_=pt_, kb=kb, jj=jj, m=m, var=var: E.matmul(pt_[:, jj * 128:(jj + 1) * 128], kT[:, kb * 128:(kb + 1) * 128], qT[:, m * 128:(m + 1) * 128], start=True, stop=(var is None)),
                    [kT_b, qT_b], [pb_])
                if var is not None:
                    k.T(lambda E, pt_=pt_, jj=jj, var=var: E.matmul(pt_[:, jj * 128:(jj + 1) * 128], identb[:], biasb[:, var, :], start=False, stop=True),
                        [identb_b, biasb_b], [pb_])
                regs.append((kb, tt_, tb_, jj))
            nA = min(4, nreg)
            k.A(lambda E, pA=pA, tA=tA, nA=nA: E.activation(out=tA[:, 0:nA * 128], in_=pA[:, 0:nA * 128], func=AF.Exp), [pAb], [tAb])
            if nreg > 4:
                nB = nreg - 4
                k.A(lambda E, pB=pB, tB=tB, nB=nB: E.activation(out=tB[:, 0:nB * 128], in_=pB[:, 0:nB * 128], func=AF.Exp), [pBb], [tBb])
            for j, (kb, tt_, tb_, jj) in enumerate(regs):
                k.T(lambda E, pa=pa, kb=kb, tt_=tt_, jj=jj, mi=mi, j=j, nreg=nreg: E.matmul(pa[:, mi * 128:(mi + 1) * 128], vE[:, kb, :], tt_[:, jj * 128:(jj + 1) * 128], start=(j == 0), stop=(j == nreg - 1)),
                    [vE_b, tb_], [pab])
        finalize(pa, pab, ga_d, 0, g0 * 128, nq * 128)
    pa, pab = pacc[acc_i % 2]
    acc_i += 1
    attend(nlb * 128, 256, ctxk, pa, pab)
    finalize(pa, pab, ga_d, 0, nlb * 128, 256)

    k.dma("sync", vE[:], vb_d, w=[vE_b])

    def prepass(src_d, dst, dst_b, gcol):
        for c0_ in range(0, NTK, 512):
            pre_chunk(src_d, dst, dst_b, gcol, c0_)

    def pre_chunk(src_d, dst, dst_b, gcol, c0):
        if True:
            n = min(512, NTK - c0)
            i = state["pre"] % 2
            state["pre"] += 1
            ci, cib = cin[i]
            ct, ctb = ctab[i]
            st_, stb = stab[i]
            qn, qnb_ = qnb[i]
            a1, a1b = w1[i]
            a2, a2b = w2[i]
            pm, pmb = pmisc[i]
            k.dma("sync", ci[:, 0:n], src_d[:, c0:c0 + n], w=[cib])
            k.dma("sync", ct[:, 0:n], cs_d[:, c0:c0 + n], w=[ctb])
            k.dma("sync", st_[:, 0:n], sn_d[:, c0:c0 + n], w=[stb])
            k.V(lambda E: E.tensor_tensor(out=a1[:, 0:n], in0=ci[:, 0:n], in1=ci[:, 0:n], op=ALU.mult), [cib], [a1b])
            k.T(lambda E: E.matmul(pm[:, 0:n], ones64[:], a1[:, 0:n], start=True, stop=True), [ones64_b, a1b], [pmb])
            k.A(lambda E: E.activation(out=a2[:, 0:n], in_=pm[:, 0:n], func=AF.Sqrt, bias=eps_t[:]), [pmb, eps_b], [a2b])
            k.V(lambda E: E.reciprocal(out=a2[:, 0:n], in_=a2[:, 0:n]), [a2b], [a2b])
            k.V(lambda E: E.scalar_tensor_tensor(out=qn[:, 0:n], in0=ci[:, 0:n], scalar=gains[:, gcol:gcol + 1], in1=a2[:, 0:n], op0=ALU.mult, op1=ALU.mult),
                [cib, gains_b, a2b], [qnb_])
            k.T(lambda E: E.matmul(pm[:, 0:n], rmb[:], qn[:, 0:n], start=True, stop=True), [rmb_b, qnb_], [pmb])
            k.V(lambda E: E.tensor_tensor(out=a1[:, 0:n], in0=qn[:, 0:n], in1=ct[:, 0:n], op=ALU.mult), [qnb_, ctb], [a1b])
            k.V(lambda E: E.tensor_tensor(out=a2[:, 0:n], in0=pm[:, 0:n], in1=st_[:, 0:n], op=ALU.mult), [pmb, stb], [a2b])
            k.G(lambda E: E.tensor_tensor(out=dst[:, c0:c0 + n], in0=a1[:, 0:n], in1=a2[:, 0:n], op=ALU.add), [a1b, a2b], [dst_b])

    prepass(kb_d, kT, kT_b, 1)
    prepass(qb_d, qT, qT_b, 0)
    allk = list(range(NKB))
    for c0 in range(0, nlb * 128, 512):
        n = min(512, nlb * 128 - c0)
        pa, pab = pacc[acc_i % 2]
        acc_i += 1
        attend(c0, n, allk, pa, pab)
        finalize(pa, pab, gb_d, 64, c0, n)
    pa, pab = pacc[acc_i % 2]
    acc_i += 1
    attend(nlb * 128, 256, ctxk, pa, pab)
    finalize(pa, pab, gb_d, 64, nlb * 128, 256)
    k.done()
    return nc


def rope_tables(nlb):
    n = nlb * 128
    t = np.arange(n)
    pos = np.stack([t // 64, t % 64], -1).astype(np.float32)
    inv = (10000.0 ** (-np.arange(16, dtype=np.float32) / 16)).astype(np.float32)
    ang = pos[:, :, None] * inv
    cos, sin = np.cos(ang), np.sin(ang)
    C = np.ones((64, n + 256), np.float32)
    S = np.zeros((64, n + 256), np.float32)
    for a in range(2):
        for hf in range(2):
            C[a * 32 + hf * 16:a * 32 + hf * 16 + 16, :n] = cos[:, a, :].T
            S[a * 32 + hf * 16:a * 32 + hf * 16 + 16, :n] = sin[:, a, :].T
    R = np.zeros((64, 64), np.float32)
    for a in range(2):
        for f in range(16):
            R[a * 32 + f, a * 32 + 16 + f] = -1.0
            R[a * 32 + 16 + f, a * 32 + f] = 1.0
    return C, S, np.ascontiguousarray(R.T)


import numpy as np

PC = {n: i for i, n in enumerate(
    ["mu_r", "mu_k", "mu_v", "k_k", "k_a", "w0_f", "w0_b", "a0_f", "a0_b", "r_k",
     "t0_v", "t1_v", "t2_v", "t0_x1", "t1_x1", "t2_x1", "t0_x2", "t1_x2", "t2_x2",
     "gn_w", "gn_b", "skip0", "skip1"])}
NPC = len(PC)
FO = {n: i for i, n in enumerate(
    ["w_f", "w_b", "kkn", "nb_f", "nb_b", "k_f", "k_b", "r", "v", "bonus", "sg_rw", "hv", "hx1", "hx2", "sg_hy"])}
NFO = len(FO)
FI = {n: i for i, n in enumerate(["r", "k", "v", "g_rw", "hv", "hx1", "hx2", "g_hy"])}
NFI = len(FI)


def seq_chunks(n_lat):
    ch = [(0, 0, 256)]
    for c0 in range(0, n_lat, 512):
        n = min(512, n_lat - c0)
        ch.append((258 + c0, 256 + c0, n))
    return ch


def build_rfeat(n_lat):
    T = 256 + n_lat
    TP = T + 4
    nc = bass.Bass("TRN2", target_bir_lowering=False)
    k = KB(nc)
    fin_d = k.din("fin", [NFI, 64, TP], F32)
    lora_d = k.din("lora", [128, TP], F32)
    pp_d = k.din("pp", [64, NPC], F32)
    mul_d = k.din("mu_lora", [128, 1], F32)
    wx_d = k.din("wx", [128, 4, 64], F32)
    fo_d = k.dout("fo", [NFO, 64, T], F32)

    pp = k.sb("pp_sb", [64, NPC], F32)
    npp = k.sb("npp_sb", [64, NPC], F32)
    mul = k.sb("mul_sb", [128, 1], F32)
    wx = k.sb("wx_sb", [128, 4, 64], F32)
    ones = k.sb("ones_sb", [64, 64], F32)
    tiny = k.sb("tiny_sb", [64, 1], F32)
    k.dma("sync", pp[0][:], pp_d, w=[pp[1]])
    k.dma("sync", mul[0][:], mul_d, w=[mul[1]])
    k.dma("sync", wx[0][:], wx_d, w=[wx[1]])
    h_memset(k, (ones[0][:], ones[1]), 1.0)
    h_memset(k, (tiny[0][:], tiny[1]), 1e-12)
    h_ts(k, (npp[0][:], npp[1]), (pp[0][:], pp[1]), -1.0, None, ALU.mult)
    h_ts(k, (npp[0][:, PC["r_k"]:PC["r_k"] + 1], npp[1]), (pp[0][:, PC["r_k"]:PC["r_k"] + 1], pp[1]), 0.5, None, ALU.mult)

    def pcol(name):
        return (pp[0][:, PC[name]:PC[name] + 1], pp[1])

    def ncol(name):
        return (npp[0][:, PC[name]:PC[name] + 1], npp[1])

    fin = [[k.sb("fin%d_%d" % (i, j), [64, 514], F32) for j in range(2)] for i in range(NFI)]
    lor = [k.sb("lor%d" % j, [128, 514], F32) for j in range(2)]
    fo = [[k.sb("fo%d_%d" % (i, j), [64, 512], F32) for j in range(2)] for i in range(NFO)]
    tm = [k.sb("tm%d" % i, [64, 512], F32) for i in range(8)]
    lt = [k.sb("lt%d" % i, [128, 512], F32) for i in range(3)]
    pz = [k.ps("pz%d" % i, [64, 512], F32) for i in range(6)]

    def chunk(ci, pc0, oc0, n):
        j = ci % 2

        def I(name, lo=1):
            t, b = fin[FI[name]][j]
            return (t[:, lo:lo + n], b)

        def O(name):
            t, b = fo[FO[name]][j]
            return (t[:, 0:n], b)

        def Tm(i):
            return (tm[i][0][:, 0:n], tm[i][1])

        def Lt(i):
            return (lt[i][0][:, 0:n], lt[i][1])

        def Pz(i):
            return (pz[i][0][:, 0:n], pz[i][1])

        for name, i in FI.items():
            t, b = fin[i][j]
            k.dma("sync", t[:, 0:n + 2], fin_d[i, :, pc0:pc0 + n + 2], w=[b])
        lt_, lb_ = lor[j]
        k.dma("sync", lt_[:, 0:n + 2], lora_d[:, pc0:pc0 + n + 2], w=[lb_])

        def shift(out, src_lo, src_mid, src_hi, mu, t):
            h_tt(k, t, src_lo, src_hi, ALU.add)
            h_stt(k, t, t, 0.5, src_mid, ALU.mult, ALU.subtract)
            h_stt(k, out, t, mu, src_mid, ALU.mult, ALU.add)

        rs, ks, vs = O("r"), Tm(0), O("v")
        shift(rs, I("r", 0), I("r", 1), I("r", 2), pcol("mu_r"), Tm(7))
        shift(ks, I("k", 0), I("k", 1), I("k", 2), pcol("mu_k"), Tm(7))
        shift(vs, I("v", 0), I("v", 1), I("v", 2), pcol("mu_v"), Tm(7))
        ls = Lt(0)
        shift(ls, (lt_[:, 0:n], lb_), (lt_[:, 1:n + 1], lb_), (lt_[:, 2:n + 2], lb_), (mul[0][:], mul[1]), Lt(2))
        lth = Lt(1)
        h_act(k, lth, ls, AF.Tanh)
        h_mm(k, Pz(0), (wx[0][:, 0, :], wx[1]), lth)
        h_mm(k, Pz(1), (wx[0][:, 1, :], wx[1]), lth)
        h_mm(k, Pz(2), (wx[0][:, 2, :], wx[1]), ls)
        h_mm(k, Pz(3), (wx[0][:, 3, :], wx[1]), ls)
        kk = Tm(1)
        h_ts(k, kk, ks, pcol("k_k"), None, ALU.mult)
        h_tt(k, Tm(2), kk, kk, ALU.mult)
        h_mm(k, Pz(4), (ones[0][:], ones[1]), Tm(2))
        h_act(k, Tm(2), Pz(4), AF.Sqrt, bias=(tiny[0][:], tiny[1]))
        h_recip(k, Tm(2), Tm(2))
        h_tt(k, O("kkn"), kk, Tm(2), ALU.mult)
        for d, sfx in enumerate(("f", "b")):
            h_sigmoid(k, Tm(3), Pz(d), Tm(3), bias=ncol("w0_" + sfx))
            h_act(k, O("w_" + sfx), Tm(3), AF.Exp, scale=-float(np.exp(-0.5)))
            a = Tm(4)
            h_sigmoid(k, a, Pz(2 + d), a, bias=ncol("a0_" + sfx))
            h_ts(k, Tm(5), a, -1.0, pcol("k_a"), ALU.add, ALU.mult)
            h_stt(k, O("k_" + sfx), Tm(5), 1.0, ks, ALU.add, ALU.mult)
            h_stt(k, O("nb_" + sfx), O("kkn"), -1.0, a, ALU.mult, ALU.mult)
        h_tt(k, Tm(5), O("k_f"), O("k_b"), ALU.add)
        h_stt(k, Tm(5), rs, ncol("r_k"), Tm(5), ALU.mult, ALU.mult)
        h_mm(k, Pz(5), (ones[0][:], ones[1]), Tm(5))
        h_tt(k, O("bonus"), Pz(5), vs, ALU.mult)
        h_silu(k, O("sg_rw"), I("g_rw", 1), Tm(6))
        h_silu(k, O("sg_hy"), I("g_hy", 1), Tm(6))
        for nm in ("v", "x1", "x2"):
            src = "h" + nm
            h_ts(k, Tm(6), I(src, 0), pcol("t0_" + nm), None, ALU.mult)
            h_stt(k, Tm(6), I(src, 1), pcol("t1_" + nm), Tm(6), ALU.mult, ALU.add)
            h_stt(k, O(src), I(src, 2), pcol("t2_" + nm), Tm(6), ALU.mult, ALU.add)
        for name, i in FO.items():
            t, b = fo[i][j]
            k.dma("sync", fo_d[i, :, oc0:oc0 + n], t[:, 0:n], r=[b])

    for ci, (pc0, oc0, n) in enumerate(seq_chunks(n_lat)):
        chunk(ci, pc0, oc0, n)
    k.done()
    return nc


import numpy as np

TC = 16


def build_scan(T):
    nc = bass.Bass("TRN2", target_bir_lowering=False)
    k = KB(nc)
    P = k.P
    bc_d = k.din("bc", [2, T, 320], F32)
    v_d = k.din("vT2", [128, T], F32)
    y_d = k.dout("yT2", [128, T], F32)
    S, S_b = k.sb("S", [128, 64], F32)
    tmp, _ = k.sb("tmp", [128, 64], F32)
    sa, _ = k.sb("sa", [128, 2], F32)
    NBUF = 4
    bt = [k.sb("bt%d" % i, [128, TC, 320], F32) for i in range(NBUF)]
    vt = [k.sb("vt%d" % i, [128, 512], F32) for i in range(2)]
    yt = [k.sb("yt%d" % i, [128, 512], F32) for i in range(2)]
    h_memset(k, (S[:], S_b), 0.0)
    nch = (T + TC - 1) // TC
    for c in range(nch):
        s0 = c * TC
        nst = min(TC, T - s0)
        btt, btb = bt[c % NBUF]
        q = "sync" if c % 2 == 0 else "scalar"
        k.dma(q, btt[0:64, 0:nst, :], bc_d[0, s0:s0 + nst, :].partition_broadcast(64), w=[btb])
        k.dma(q, btt[64:128, 0:nst, :], bc_d[1, s0:s0 + nst, :].partition_broadcast(64), w=[btb])
        if s0 % 512 == 0:
            vi = (s0 // 512) % 2
            vtt, vtb = vt[vi]
            ytt, ytb = yt[vi]
            nv = min(512, T - s0)
            k.dma("gpsimd", vtt[:, 0:nv], v_d[:, s0:s0 + nv], w=[vtb])
        for i in range(nst):
            s = s0 + i
            sl = s % 512
            W = btt[:, i, 0:64]; KK = btt[:, i, 64:128]; NB_ = btt[:, i, 128:192]; K_ = btt[:, i, 192:256]; R_ = btt[:, i, 256:320]
            sac = sa[:, s % 2:s % 2 + 1]
            P.op("vector", lambda E, KK=KK, sac=sac: E.scalar_tensor_tensor(out=tmp[:], in0=S[:], scalar=1.0, in1=KK, op0=ALU.mult, op1=ALU.mult, accum_out=sac), [S_b, btb], [S_b])
            P.op("vector", lambda E, W=W: E.tensor_tensor(out=S[:], in0=S[:], in1=W, op=ALU.mult), [S_b], [S_b])
            P.op("vector", lambda E, NB_=NB_, sac=sac: E.scalar_tensor_tensor(out=S[:], in0=NB_, scalar=sac, in1=S[:], op0=ALU.mult, op1=ALU.add), [S_b], [S_b])
            P.op("vector", lambda E, K_=K_, vtt=vtt, sl=sl: E.scalar_tensor_tensor(out=S[:], in0=K_, scalar=vtt[:, sl:sl + 1], in1=S[:], op0=ALU.mult, op1=ALU.add), [S_b, vtb], [S_b])
            P.op("vector", lambda E, R_=R_, ytt=ytt, sl=sl: E.scalar_tensor_tensor(out=tmp[:], in0=S[:], scalar=1.0, in1=R_, op0=ALU.mult, op1=ALU.mult, accum_out=ytt[:, sl:sl + 1]), [S_b, btb], [S_b, ytb])
            if sl == 511 or s == T - 1:
                c0 = s - sl
                k.dma("gpsimd", y_d[:, c0:s + 1], ytt[:, 0:sl + 1], r=[ytb])
    k.done()
    return nc


import numpy as np

TWO_PI = float(2 * np.pi)


def build_filt(L):
    nc = bass.Bass("TRN2", target_bir_lowering=False)
    k = KB(nc)
    feats_d = k.din("featsT", [33, L], F32)
    tv_d = k.din("tvals", [1, L], F32)
    w1_d = k.din("w1", [33, 64], F32)
    w2_d = k.din("w2", [64, 64], F32)
    w3_d = k.din("w3c", [64, 256], F32)
    bb_d = k.din("b12", [64, 2], F32)
    b3_d = k.din("b3c", [128, 2], F32)
    nd_d = k.din("negdelta", [128, 1], F32)
    tf_d = k.dout("tapsF", [128, L], BF16)
    tb_d = k.dout("tapsB", [128, L], BF16)
    ssq_d = k.dout("ssq", [128, 1], F32)
    w1 = k.sb("w1s", [33, 64], F32); w2 = k.sb("w2s", [64, 64], F32); w3 = k.sb("w3s", [64, 256], F32)
    bb = k.sb("bbs", [64, 2], F32); b3 = k.sb("b3s", [128, 2], F32); nd = k.sb("nds", [128, 1], F32)
    for t, d in ((w1, w1_d), (w2, w2_d), (w3, w3_d), (bb, bb_d), (b3, b3_d), (nd, nd_d)):
        k.dma("sync", t[0][:], d, w=[t[1]])
    nch = (L + 511) // 512
    part = k.sb("part", [128, 2 * nch], F32)
    h_memset(k, (part[0][:], part[1]), 0.0)
    ft = [k.sb("ft%d" % i, [33, 512], F32) for i in range(2)]
    tv = [k.sb("tv%d" % i, [128, 512], F32) for i in range(2)]
    hx = [k.sb("hx%d" % i, [64, 512], F32) for i in range(2)]
    hi = k.sb("hi", [64, 512], I32)
    hf = k.sb("hf", [64, 512], F32)
    win = k.sb("win", [128, 512], F32)
    tF = [k.sb("tF%d" % i, [128, 512], F32) for i in range(2)]
    tB = [k.sb("tB%d" % i, [128, 512], F32) for i in range(2)]
    oF = [k.sb("oF%d" % i, [128, 512], BF16) for i in range(2)]
    oB = [k.sb("oB%d" % i, [128, 512], BF16) for i in range(2)]
    junk = k.sb("junkf", [128, 512], F32)
    ph = [k.ps("ph%d" % i, [64, 512], F32) for i in range(2)]
    pt = [k.ps("pt%d" % i, [128, 512], F32) for i in range(2)]

    def sin_layer(out, psum, bias, n):
        x = (hf[0][:, 0:n], hf[1])
        ki = (hi[0][:, 0:n], hi[1])
        h_ts(k, out, psum, bias, None, ALU.add)
        h_ts(k, ki, out, 1.0 / TWO_PI, None, ALU.mult)
        h_copy(k, x, ki)
        h_stt(k, out, x, -TWO_PI, out, ALU.mult, ALU.add)
        h_ts(k, out, out, -float(np.pi), float(np.pi), ALU.max, ALU.min)
        h_act(k, out, out, AF.Sin)

    for c in range(nch):
        c0 = c * 512
        n = min(512, L - c0)
        j = c % 2
        k.dma("sync", ft[j][0][:, 0:n], feats_d[:, c0:c0 + n], w=[ft[j][1]])
        k.dma("sync", tv[j][0][:, 0:n], tv_d[:, c0:c0 + n].partition_broadcast(128), w=[tv[j][1]])
        h_mm(k, (ph[0][0][:, 0:n], ph[0][1]), (w1[0][:], w1[1]), (ft[j][0][:, 0:n], ft[j][1]))
        h1 = (hx[0][0][:, 0:n], hx[0][1])
        sin_layer(h1, (ph[0][0][:, 0:n], ph[0][1]), (bb[0][:, 0:1], bb[1]), n)
        h_mm(k, (ph[1][0][:, 0:n], ph[1][1]), (w2[0][:], w2[1]), h1)
        h2 = (hx[1][0][:, 0:n], hx[1][1])
        sin_layer(h2, (ph[1][0][:, 0:n], ph[1][1]), (bb[0][:, 1:2], bb[1]), n)
        h_mm(k, (pt[0][0][:, 0:n], pt[0][1]), (w3[0][:, 0:128], w3[1]), h2)
        h_mm(k, (pt[1][0][:, 0:n], pt[1][1]), (w3[0][:, 128:256], w3[1]), h2)
        w_ = (win[0][:, 0:n], win[1])
        h_act(k, w_, (tv[j][0][:, 0:n], tv[j][1]), AF.Exp, scale=(nd[0][:], nd[1]))
        F_ = (tF[j][0][:, 0:n], tF[j][1])
        B_ = (tB[j][0][:, 0:n], tB[j][1])
        h_stt(k, F_, (pt[0][0][:, 0:n], pt[0][1]), (b3[0][:, 0:1], b3[1]), w_, ALU.add, ALU.mult)
        h_stt(k, B_, (pt[1][0][:, 0:n], pt[1][1]), (b3[0][:, 1:2], b3[1]), w_, ALU.add, ALU.mult)
        lo = 0
        if c == 0:
            h_tt(k, (tF[j][0][:, 0:1], tF[j][1]), (tF[j][0][:, 0:1], tF[j][1]), (tB[j][0][:, 0:1], tB[j][1]), ALU.add)
            lo = 1
        h_act(k, (junk[0][:, 0:n], junk[1]), F_, AF.Square, accum=(part[0][:, 2 * c:2 * c + 1], part[1]))
        h_act(k, (junk[0][:, lo:n], junk[1]), (tB[j][0][:, lo:n], tB[j][1]), AF.Square, accum=(part[0][:, 2 * c + 1:2 * c + 2], part[1]))
        h_copy(k, (oF[j][0][:, 0:n], oF[j][1]), F_)
        h_copy(k, (oB[j][0][:, 0:n], oB[j][1]), B_, eng="gpsimd")
        k.dma("sync", tf_d[:, c0:c0 + n], oF[j][0][:, 0:n], r=[oF[j][1]])
        k.dma("sync", tb_d[:, c0:c0 + n], oB[j][0][:, 0:n], r=[oB[j][1]])
    tot = k.sb("tot", [128, 1], F32)
    k.V(lambda E: E.tensor_reduce(out=tot[0][:], in_=part[0][:], axis=mybir.AxisListType.X, op=ALU.add), [part[1]], [tot[1]])
    k.dma("sync", ssq_d, tot[0][:], r=[tot[1]])
    k.done()
    return nc


def filt_consts(L):
    t_idx = np.arange(L, dtype=np.float32)
    t = (t_idx / np.float32(max(L - 1, 1))).astype(np.float32)
    bands = np.linspace(1e-4, 15, 16, dtype=np.float32)
    ang = (np.float32(2.0 * np.pi) * bands[None, :] * t_idx[:, None] / np.float32(L)).astype(np.float32)
    feats = np.concatenate([t[:, None], np.cos(ang), -np.sin(ang)], -1).astype(np.float32)
    deltas = np.linspace(np.log(1e-2) / 0.3, np.log(1e-2) / 1.5, 512, dtype=np.float32)
    return np.ascontiguousarray(feats.T), t.reshape(1, L).copy(), np.abs(deltas)


def filt_inputs(L, h, prm):
    featsT, tv, adel = filt_consts(L)
    sl = slice(64 * h, 64 * h + 64)
    w3 = prm["hy_w3"]; b3 = prm["hy_b3"]
    cols = []
    for side in range(2):
        for o in range(2):
            cols.append(np.arange(side * 1024 + o * 512 + 64 * h, side * 1024 + o * 512 + 64 * h + 64))
    cols = np.concatenate(cols)
    nd = -np.concatenate([adel[sl], adel[sl]]).reshape(128, 1).astype(np.float32)
    return {"featsT": featsT, "tvals": tv, "w1": prm["hy_w1"], "w2": prm["hy_w2"], "w3c": np.ascontiguousarray(w3[:, cols]),
            "b12": np.stack([prm["hy_b1"], prm["hy_b2"]], 1).astype(np.float32),
            "b3c": np.stack([b3[cols[:128]], b3[cols[128:]]], 1).astype(np.float32), "negdelta": nd}


def toeplitz_src(tapsF, tapsB):
    L = tapsF.shape[1]
    KL = np.zeros((128, 2 * L), tapsF.dtype)
    KL[:, 0:L - 1] = tapsB[:, :0:-1]
    KL[:, L - 1:2 * L - 1] = tapsF
    return KL


def build_hy(nb, T_read=0):
    L = 128 * nb
    W = 128 * (2 * nb - 1)
    nc = bass.Bass("TRN2", target_bir_lowering=False)
    k = KB(nc)
    kl_h = nc.dram_tensor("KL", [128, 2 * L], BF16, kind="ExternalInput")
    ssq_d = k.din("ssqT", [1, 128], F32)
    skip_d = k.din("skipT", [1, 128], F32)
    z_d = k.din("z1", [64, 128, nb], F32)
    x1_d = k.din("x1g", [64, 128, nb], F32)
    x2_d = k.din("x2g", [64, 128, nb], F32)
    sg_d = k.din("sgh", [64, 128, nb], F32)
    J_d = k.din("Jmat", [128, 128], F32)
    y_d = k.dout("yhy", [64, 128, nb], BF16)
    Jf = k.sb("Jf", [128, 128], F32); Jb = k.sb("Jb", [128, 128], BF16)
    nrm = k.sb("nrm", [128, 128], F32); skp = k.sb("skp", [128, 128], F32)
    k.dma("sync", Jf[0][:], J_d, w=[Jf[1]])
    h_copy(k, (Jb[0][:], Jb[1]), (Jf[0][:], Jf[1]))
    k.dma("sync", nrm[0][:], ssq_d.partition_broadcast(128), w=[nrm[1]])
    k.dma("sync", skp[0][:], skip_d.partition_broadcast(128), w=[skp[1]])
    h_act(k, (nrm[0][:], nrm[1]), (nrm[0][:], nrm[1]), AF.Sqrt)
    h_recip(k, (nrm[0][:], nrm[1]), (nrm[0][:], nrm[1]))

    if T_read:
        pp_d = k.din("pp", [64, NPC], F32)
        yf_d = k.din("yf", [64, T_read], F32)
        yb_d = k.din("yb", [64, T_read], F32)
        bo_d = k.din("bonus", [64, T_read], F32)
        sgr_d = k.din("sgr", [64, T_read], F32)
        mr_d = k.dout("mixrw", [64, T_read], BF16)
        pp = k.sb("pp_sb", [64, NPC], F32)
        k.dma("sync", pp[0][:], pp_d, w=[pp[1]])
        o64 = k.sb("o64", [64, 64], F32)
        h_memset(k, (o64[0][:], o64[1]), 1.0 / 64)
        geps = k.sb("geps", [64, 1], F32)
        h_memset(k, (geps[0][:], geps[1]), 64e-5)
        ra = [k.sb("ra%d" % i, [64, 512], F32) for i in range(2)]
        rb = [k.sb("rb%d" % i, [64, 512], F32) for i in range(2)]
        rc = [k.sb("rc%d" % i, [64, 512], F32) for i in range(2)]
        rd = [k.sb("rd%d" % i, [64, 512], F32) for i in range(2)]
        r1 = k.sb("r1", [64, 512], F32); r2 = k.sb("r2", [64, 512], F32)
        rob = [k.sb("rob%d" % i, [64, 512], BF16) for i in range(2)]
        pr = [k.ps("pr%d" % i, [64, 512], F32) for i in range(2)]
        for ci, c0 in enumerate(range(0, T_read, 512)):
            n = min(512, T_read - c0)
            j = ci % 2
            A = (ra[j][0][:, 0:n], ra[j][1]); B = (rb[j][0][:, 0:n], rb[j][1])
            C = (rc[j][0][:, 0:n], rc[j][1]); D = (rd[j][0][:, 0:n], rd[j][1])
            R1 = (r1[0][:, 0:n], r1[1]); R2 = (r2[0][:, 0:n], r2[1])
            P0 = (pr[0][0][:, 0:n], pr[0][1]); P1 = (pr[1][0][:, 0:n], pr[1][1])
            k.dma("scalar", A[0], yf_d[:, c0:c0 + n], w=[A[1]])
            k.dma("scalar", B[0], yb_d[:, c0:c0 + n], w=[B[1]])
            k.dma("scalar", C[0], bo_d[:, c0:c0 + n], w=[C[1]])
            k.dma("scalar", D[0], sgr_d[:, c0:c0 + n], w=[D[1]])
            h_tt(k, A, A, B, ALU.add)
            h_mm(k, P0, (o64[0][:], o64[1]), A)
            h_tt(k, R1, A, P0, ALU.subtract)
            h_tt(k, R2, R1, R1, ALU.mult)
            h_mm(k, P1, (o64[0][:], o64[1]), R2)
            h_act(k, R2, P1, AF.Sqrt, bias=(geps[0][:], geps[1]))
            h_recip(k, R2, R2)
            h_tt(k, R1, R1, R2, ALU.mult)
            h_ts(k, R1, R1, (pp[0][:, PC["gn_w"]:PC["gn_w"] + 1], pp[1]), (pp[0][:, PC["gn_b"]:PC["gn_b"] + 1], pp[1]), ALU.mult, ALU.add)
            h_tt(k, R1, R1, C, ALU.add)
            OB = (rob[j][0][:, 0:n], rob[j][1])
            h_tt(k, OB, R1, D, ALU.mult)
            k.dma("scalar", mr_d[:, c0:c0 + n], OB[0], r=[OB[1]])

    ksh = [k.sb("ksh%d" % i, [128, W], BF16) for i in range(2)]
    zt = [k.sb("zt%d" % i, [128, nb], F32) for i in range(2)]
    x1t = [k.sb("x1t%d" % i, [128, nb], F32) for i in range(2)]
    x2t = [k.sb("x2t%d" % i, [128, nb], F32) for i in range(2)]
    sgt = [k.sb("sgt%d" % i, [128, nb], F32) for i in range(2)]
    zb = k.sb("zb", [128, nb], BF16)
    zf = k.sb("zf", [128, nb], BF16)
    z2 = k.sb("z2", [128, nb], F32)
    t1 = k.sb("t1", [128, nb], F32)
    ot = [k.sb("oth%d" % i, [128, nb], BF16) for i in range(2)]
    pf = k.ps("pf", [128, nb], F32)
    py = [k.ps("pyc%d" % i, [128, nb], F32) for i in range(2)]
    order = [0] + [d for d in range(-(nb - 1), nb) if d != 0]
    cnt = 0
    for c in range(64):
        j = c % 2
        Z = (zt[j][0][:], zt[j][1]); X1 = (x1t[j][0][:], x1t[j][1]); X2 = (x2t[j][0][:], x2t[j][1]); SG = (sgt[j][0][:], sgt[j][1])
        k.dma("gpsimd", Z[0], z_d[c], w=[Z[1]])
        k.dma("gpsimd", X1[0], x1_d[c], w=[X1[1]])
        k.dma("gpsimd", X2[0], x2_d[c], w=[X2[1]])
        k.dma("gpsimd", SG[0], sg_d[c], w=[SG[1]])
        zin = Z
        for o in range(2):
            row = o * 64 + c
            kt, kb_ = ksh[cnt % 2]
            q = "sync" if cnt % 2 == 0 else "scalar"
            cnt += 1
            src = bass.AP(kl_h, row * 2 * L, [[1, 128], [1, W]])
            k.dma(q, kt[:], src, w=[kb_])
            h_copy(k, (zb[0][:], zb[1]), zin, eng="gpsimd")
            h_mm(k, (pf[0][:], pf[1]), (Jb[0][:], Jb[1]), (zb[0][:], zb[1]))
            h_copy(k, (zf[0][:], zf[1]), (pf[0][:], pf[1]), eng="scalar")
            pyt, pyb = py[o]
            for ii, Dd in enumerate(order):
                e = Dd + nb - 1
                S0, S1 = max(0, -Dd), min(nb, nb - Dd)
                k.T(lambda E, pyt=pyt, kt=kt, e=e, S0=S0, S1=S1, Dd=Dd, ii=ii: E.matmul(pyt[:, S0 + Dd:S1 + Dd], kt[:, 128 * e:128 * e + 128], zf[0][:, S0:S1], start=(ii == 0), stop=(ii == len(order) - 1)),
                    [kb_, zf[1]], [pyb])
            ncol = (nrm[0][:, row:row + 1], nrm[1])
            scol = (skp[0][:, row:row + 1], skp[1])
            h_ts(k, (t1[0][:], t1[1]), (pyt[:], pyb), ncol, None, ALU.mult)
            h_stt(k, (t1[0][:], t1[1]), zin, scol, (t1[0][:], t1[1]), ALU.mult, ALU.add)
            if o == 0:
                h_tt(k, (z2[0][:], z2[1]), (t1[0][:], t1[1]), X1, ALU.mult)
                zin = (z2[0][:], z2[1])
            else:
                O_ = (ot[j][0][:], ot[j][1])
                h_tt(k, (t1[0][:], t1[1]), (t1[0][:], t1[1]), X2, ALU.mult)
                h_tt(k, O_, (t1[0][:], t1[1]), SG, ALU.mult, eng="gpsimd")
                k.dma("gpsimd", y_d[c], O_[0], r=[O_[1]])
    k.done()
    return nc


import numpy as np


def attn_inputs(uall, h, rpb, qg, kg, nlb):
    nkb = nlb + 2
    C, S, RT = rope_tables(nlb)

    def col(c0):
        return uall[:, c0:c0 + 64]

    def fm(a):
        return np.ascontiguousarray(a.T)

    def vext(a):
        v = np.concatenate([a, np.ones((a.shape[0], 1), a.dtype)], 1)
        return np.ascontiguousarray(v.reshape(nkb, 128, 65).transpose(1, 0, 2))
    kvh = h // 4
    sel = np.zeros((65, 64), np.float32)
    sel[64] = 1.0
    return {
        "qaT": fm(col(64 * h)), "kaT": fm(col(512 + 64 * h)), "va": vext(col(1024 + 64 * h)), "gaT": fm(col(1536 + 64 * h)),
        "qbT": fm(col(2048 + 64 * h)), "kbT": fm(col(2560 + 64 * kvh)), "vb": vext(col(2688 + 64 * kvh)), "gbT": fm(col(2816 + 64 * h)),
        "biasT": na_bias_tables(rpb[h], nlb), "ropeC": C, "ropeS": S,
        "gains": np.stack([qg, kg], 1).astype(np.float32), "rmatT": RT,
        "ident": np.eye(128, dtype=np.float32), "sel": sel,
    }


import numpy as np


def rfeat_inputs(u_ctx, u_lat, h, prm):
    n = u_lat.shape[0]
    def fm_pad(c0, w=64):
        a = np.zeros((w, 256 + n + 4), np.float32)
        a[:, 1:257] = u_ctx[:, c0:c0 + w].T
        a[:, 259:259 + n] = u_lat[:, c0:c0 + w].T
        return a
    cols = {"r": 64 * h, "k": 512 + 64 * h, "v": 1024 + 64 * h, "g_rw": 1664 + 64 * h,
            "hv": 2176 + 64 * h, "hx1": 2688 + 64 * h, "hx2": 3200 + 64 * h, "g_hy": 3712 + 64 * h}
    fin = np.stack([fm_pad(cols[nm]) for nm in FI], 0)
    lora = fm_pad(1536, 128)
    mu = prm["rwkv_mu"]
    sl = slice(64 * h, 64 * h + 64)
    pp = np.zeros((64, NPC), np.float32)
    pp[:, PC["mu_r"]] = mu[sl]
    pp[:, PC["mu_k"]] = mu[512 + 64 * h:512 + 64 * h + 64]
    pp[:, PC["mu_v"]] = mu[1024 + 64 * h:1024 + 64 * h + 64]
    pp[:, PC["k_k"]] = prm["rwkv_k_k"][sl]
    pp[:, PC["k_a"]] = prm["rwkv_k_a"][sl]
    pp[:, PC["w0_f"]] = prm["rwkv_w0"][0][sl]
    pp[:, PC["w0_b"]] = prm["rwkv_w0"][1][sl]
    pp[:, PC["a0_f"]] = prm["rwkv_a0"][0][sl]
    pp[:, PC["a0_b"]] = prm["rwkv_a0"][1][sl]
    pp[:, PC["r_k"]] = prm["rwkv_r_k"][h]
    for ai, nm in enumerate(("v", "x1", "x2")):
        for t in range(3):
            pp[:, PC["t%d_%s" % (t, nm)]] = prm["hy_short"][t][512 * ai + 64 * h:512 * ai + 64 * h + 64]
    pp[:, PC["gn_w"]] = prm["rwkv_gn_w"][sl]
    pp[:, PC["gn_b"]] = prm["rwkv_gn_b"][sl]
    pp[:, PC["skip0"]] = prm["hy_skip"][0][sl]
    pp[:, PC["skip1"]] = prm["hy_skip"][1][sl]
    wx = np.zeros((128, 4, 64), np.float32)
    wx[0:32, 0] = prm["rwkv_w_up"][0][:, sl]
    wx[32:64, 1] = prm["rwkv_w_up"][1][:, sl]
    wx[64:96, 2] = prm["rwkv_a_up"][0][:, sl]
    wx[96:128, 3] = prm["rwkv_a_up"][1][:, sl]
    return {"fin": fin, "lora": lora, "pp": pp, "mu_lora": mu[1536:1664].reshape(128, 1).copy(), "wx": wx}


import ml_dtypes
_BF = ml_dtypes.bfloat16
_NC_CACHE = {}


def _get(key, fn):
    if key not in _NC_CACHE:
        _NC_CACHE[key] = fn()
    return _NC_CACHE[key]


def _run(nc, maps):
    res = run_bass_kernel_spmd(nc, maps, core_ids=list(range(8)))
    return res.results


def _c_cols(c, c_ctx):
    return np.ascontiguousarray(np.concatenate([c.reshape(8, 128).T, c_ctx.reshape(8, 128).T], axis=1).astype(np.float32))


def kernel(**inp):
    inp = {k_: np.asarray(v) for k_, v in inp.items()}
    x = inp["x"][0].astype(np.float32)
    ctx = inp["ctx"][0].astype(np.float32)
    n = x.shape[0]
    nlb = n // 128
    nlc = nlb // 8
    NBc = nlc + 2
    tpc = nlc * 128
    T = 256 + n
    ident = np.eye(128, dtype=np.float32)
    ccols = _c_cols(inp["c"][0], inp["c_ctx"])
    xs = [np.ascontiguousarray(np.concatenate([x[i * tpc:(i + 1) * tpc], ctx], 0)) for i in range(8)]
    gates = None
    mixT = None
    for layer in range(4):
        i = layer // 2
        attn = layer % 2 == 0
        pre = "attn" if attn else "rec"
        in_w = 3328 if attn else 4224
        u_dt = BF16 if attn else F32
        has_prev = layer > 0
        nc = _get(("tok", in_w, has_prev, nlc), lambda: build_tok(in_w, has_prev, False, u_dt, NB=NBc, NLAT=nlc))
        maps = []
        for cidx in range(8):
            m = {"x_in": xs[cidx], "ident": ident, "c_cols": ccols,
                 "norm_g": inp[pre + "_norm"][i].reshape(1, -1), "ada_w": inp[pre + "_ada_w"][i],
                 "ada_b": inp[pre + "_ada_b"][i].reshape(1, -1), "w_in": inp[pre + "_w_in"][i]}
            if has_prev:
                m["mixT"] = np.ascontiguousarray(np.concatenate([mixT[:, cidx * tpc:(cidx + 1) * tpc], mixT[:, n:]], 1))
                m["w_out"] = inp[("rec" if attn else "attn") + "_w_out"][(layer - 1) // 2]
                m["gates_in"] = gates
            maps.append(m)
        res = _run(nc, maps)
        if has_prev:
            xs = [res[cidx]["x_out"] for cidx in range(8)]
        gates = res[0]["gates_out"]
        u_lat = np.concatenate([res[cidx]["u_out"][:tpc] for cidx in range(8)], 0)
        u_ctx = res[0]["u_out"][tpc:]
        if attn:
            uall = np.concatenate([u_lat, u_ctx], 0)
            nca = _get(("attn", nlb), lambda: build_attn(nlb))
            res = _run(nca, [attn_inputs(uall, h, inp["na_rpb"][i], inp["gqa_q_gain"][i], inp["gqa_k_gain"][i], nlb) for h in range(8)])
            mixT = np.concatenate([res[h]["mixT"][:64] for h in range(8)] + [res[h]["mixT"][64:] for h in range(8)], 0)
        else:
            ctx_out = layer < 3
            prm = {k_: inp[k_][i] for k_ in inp if k_.startswith("rwkv") or k_.startswith("hy")}
            ncf = _get(("rfeat", n), lambda: build_rfeat(n))
            fos = [r_["fo"] for r_ in _run(ncf, [rfeat_inputs(u_ctx, u_lat, h, prm) for h in range(8)])]
            ordf = np.arange(T)
            ordb = np.concatenate([np.arange(255, -1, -1), 256 + np.arange(n - 1, -1, -1)])
            maps = []
            for h in range(8):
                fo = fos[h]
                bc = np.empty((2, T, 320), np.float32)
                for d, (sfx, od) in enumerate((("f", ordf), ("b", ordb))):
                    for a, nm in enumerate(("w_" + sfx, "kkn", "nb_" + sfx, "k_" + sfx, "r")):
                        bc[d, :, 64 * a:64 * a + 64] = fo[FO[nm]][:, od].T
                vT2 = np.ascontiguousarray(np.concatenate([fo[FO["v"]][:, ordf], fo[FO["v"]][:, ordb]], 0))
                maps.append({"bc": bc, "vT2": vT2})
            ncs = _get(("scan", T), lambda: build_scan(T))
            ys = [r_["yT2"] for r_ in _run(ncs, maps)]
            inv_b = np.argsort(ordb)
            ncfl = _get(("filt", n), lambda: build_filt(n))
            fl = _run(ncfl, [filt_inputs(n, h, prm) for h in range(8)])
            nb = n // 128
            J = np.eye(128, dtype=np.float32)[::-1].copy()

            def jS(a, nb_):
                return np.ascontiguousarray(a.reshape(64, nb_, 128).transpose(0, 2, 1))

            def hy_map(h, fo, flr, lo, hi_, nb_):
                sl = slice(64 * h, 64 * h + 64)
                return {"KL": toeplitz_src(flr["tapsF"], flr["tapsB"]), "ssqT": flr["ssq"].reshape(1, 128).copy(),
                        "skipT": np.concatenate([prm["hy_skip"][0][sl], prm["hy_skip"][1][sl]]).reshape(1, 128).astype(np.float32),
                        "z1": jS(fo[FO["hv"]][:, lo:hi_], nb_), "x1g": jS(fo[FO["hx1"]][:, lo:hi_], nb_),
                        "x2g": jS(fo[FO["hx2"]][:, lo:hi_], nb_), "sgh": jS(fo[FO["sg_hy"]][:, lo:hi_], nb_), "Jmat": J}
            maps = []
            for h in range(8):
                m = hy_map(h, fos[h], fl[h], 256, T, nb)
                m["pp"] = rfeat_inputs(u_ctx[:1], u_lat[:1], h, prm)["pp"]
                m["yf"] = np.ascontiguousarray(ys[h][:64])
                m["yb"] = np.ascontiguousarray(ys[h][64:][:, inv_b])
                m["bonus"] = np.ascontiguousarray(fos[h][FO["bonus"]])
                m["sgr"] = np.ascontiguousarray(fos[h][FO["sg_rw"]])
                maps.append(m)
            nch = _get(("hy", nb, T), lambda: build_hy(nb, T))
            hr = _run(nch, maps)
            mix = np.zeros((1024, n + 256), _BF)
            for h in range(8):
                mr = hr[h]["mixrw"]
                mix[64 * h:64 * h + 64, :n] = mr[:, 256:]
                mix[64 * h:64 * h + 64, n:] = mr[:, :256]
                mix[512 + 64 * h:512 + 64 * h + 64, :n] = hr[h]["yhy"].transpose(0, 2, 1).reshape(64, n)
            if ctx_out:
                ncfc = _get(("filt", 256), lambda: build_filt(256))
                flc = _run(ncfc, [filt_inputs(256, h, prm) for h in range(8)])
                nchc = _get(("hy", 2, 0), lambda: build_hy(2, 0))
                hc = _run(nchc, [hy_map(h, fos[h], flc[h], 0, 256, 2) for h in range(8)])
                for h in range(8):
                    mix[512 + 64 * h:512 + 64 * h + 64, n:] = hc[h]["yhy"].transpose(0, 2, 1).reshape(64, 256)
            mixT = mix
    ncz = _get(("fin", nlc), lambda: build_tok(0, True, True, F32, NB=NBc, NLAT=nlc))
    maps = []
    for cidx in range(8):
        maps.append({"x_in": xs[cidx], "ident": ident,
                     "mixT": np.ascontiguousarray(np.concatenate([mixT[:, cidx * tpc:(cidx + 1) * tpc], mixT[:, n:]], 1)),
                     "w_out": inp["rec_w_out"][1], "gates_in": gates, "final_norm": inp["final_norm"].reshape(1, -1)})
    res = _run(ncz, maps)
    out = np.concatenate([res[cidx]["y_out"][:tpc] for cidx in range(8)], 0)
    return out[None].astype(np.float32)
```

```python
import numpy as np
import concourse.bass as bass
import concourse.mybir as mybir
from concourse.bass_utils import run_bass_kernel_spmd

F32 = mybir.dt.float32
BF16 = mybir.dt.bfloat16
I32 = mybir.dt.int32
ALU = mybir.AluOpType
AF = mybir.ActivationFunctionType


class Buf:
    __slots__ = ("w", "r", "name")

    def __init__(self, name=""):
        self.w = None
        self.r = {}
        self.name = name


class Prog:
    ENGS = ("tensor", "vector", "scalar", "gpsimd", "sync")
    NDSEM = 6

    def __init__(self, nc):
        self.nc = nc
        self.lists = {e: [] for e in self.ENGS}
        self.count = {e: 0 for e in self.ENGS}
        self.waited = {e: {} for e in self.ENGS}
        self.sems = {}
        self._ctx = []
        for e in self.ENGS:
            cm = nc.semaphore("s_" + e)
            self.sems[e] = cm.__enter__()
            self._ctx.append(cm)
        self.dsem = {}
        self.dcount = {}
        self.dnext = {}
        for q in ("sync", "scalar", "gpsimd"):
            self.dsem[q] = []
            for i in range(self.NDSEM):
                cm = nc.semaphore("d_%s%d" % (q, i))
                self.dsem[q].append(cm.__enter__())
                self._ctx.append(cm)
                self.sems["d_%s%d" % (q, i)] = self.dsem[q][-1]
            self.dcount[q] = [0] * self.NDSEM
            self.dnext[q] = 0

    def _need(self, eng, evs):
        best = {}
        for ev in evs:
            if ev is None:
                continue
            k, v = ev
            if best.get(k, 0) < v:
                best[k] = v
        for k, v in best.items():
            if self.waited[eng].get(k, 0) >= v:
                continue
            self.waited[eng][k] = v
            sem = self.sems[k]
            self.lists[eng].append(lambda E, sem=sem, v=v: E.wait_ge(sem, v))

    def _deps(self, reads, writes):
        evs = []
        for b in reads:
            evs.append(b.w)
        for b in writes:
            evs.append(b.w)
            evs.extend(b.r.items())
        return evs

    def op(self, eng, fn, reads=(), writes=()):
        self._need(eng, self._deps(reads, writes))
        self.count[eng] += 1
        c = self.count[eng]
        sem = self.sems[eng]
        self.lists[eng].append(lambda E, fn=fn, sem=sem: fn(E).then_inc(sem, 1))
        ev = (eng, c)
        for b in reads:
            b.r[eng] = c
        for b in writes:
            b.w = ev
            b.r = {}
        return ev

    def dma(self, q, out, in_, reads=(), writes=(), **kw):
        i = self.dnext[q]
        self.dnext[q] = (i + 1) % self.NDSEM
        key = "d_%s%d" % (q, i)
        evs = self._deps(reads, writes)
        if self.dcount[q][i] > 0:
            evs.append((key, self.dcount[q][i]))
        self._need(q, evs)
        self.dcount[q][i] += 16
        v = self.dcount[q][i]
        sem = self.sems[key]
        self.lists[q].append(lambda E, out=out, in_=in_, sem=sem, kw=kw: E.dma_start(out=out, in_=in_, **kw).then_inc(sem, 16))
        ev = (key, v)
        for b in reads:
            b.r[key] = v
        for b in writes:
            b.w = ev
            b.r = {}
        return ev

    def coll(self, kind, op, ins_ap, outs_ap, reads=(), writes=()):
        if "cc" not in self.sems:
            cm = self.nc.semaphore("s_cc")
            self.sems["cc"] = cm.__enter__()
            self._ctx.append(cm)
            self.cccount = 0
        evs = self._deps(reads, writes)
        if self.cccount:
            evs.append(("cc", self.cccount))
        self._need("gpsimd", evs)
        self.cccount += 1
        v = self.cccount
        sem = self.sems["cc"]
        self.lists["gpsimd"].append(lambda E: E.collective_compute(kind, op, replica_groups=[list(range(8))], ins=[ins_ap.opt()], outs=[outs_ap.opt()]).then_inc(sem))
        ev = ("cc", v)
        for b in reads:
            b.r["cc"] = v
        for b in writes:
            b.w = ev
            b.r = {}
        return ev

    def barrier(self):
        evs = []
        for q in self.dsem:
            for i in range(self.NDSEM):
                if self.dcount[q][i]:
                    evs.append(("d_%s%d" % (q, i), self.dcount[q][i]))
        for e in self.ENGS:
            if self.count[e]:
                evs.append((e, self.count[e]))
        if "cc" in self.sems and self.cccount:
            evs.append(("cc", self.cccount))
        for e in self.ENGS:
            self._need(e, evs)

    def finish(self):
        evs = []
        for q in self.dsem:
            for i in range(self.NDSEM):
                if self.dcount[q][i]:
                    evs.append(("d_%s%d" % (q, i), self.dcount[q][i]))
        for e in self.ENGS:
            if e != "sync" and self.count[e]:
                evs.append((e, self.count[e]))
        if "cc" in self.sems:
            evs.append(("cc", self.cccount))
        self._need("sync", evs)
        nc = self.nc
        lists = self.lists
        with nc.Block() as block:
            @block.sync
            def _(E):
                for f in lists["sync"]:
                    f(E)

            @block.tensor
            def _(E):
                for f in lists["tensor"]:
                    f(E)

            @block.vector
            def _(E):
                for f in lists["vector"]:
                    f(E)

            @block.scalar
            def _(E):
                for f in lists["scalar"]:
                    f(E)

            @block.gpsimd
            def _(E):
                for f in lists["gpsimd"]:
                    f(E)
        for cm in reversed(self._ctx):
            cm.__exit__(None, None, None)


from contextlib import ExitStack

NB = 18
NT = NB * 128
D = 1024
EPS = 1e-6


class KB:
    def __init__(self, nc):
        self.nc = nc
        self.P = Prog(nc)
        self.es = ExitStack()

    def _uname(self, name):
        self._uid = getattr(self, "_uid", 0) + 1
        return "%s_%d" % (name, self._uid)

    def sb(self, name, shape, dt):
        return self.es.enter_context(self.nc.sbuf_tensor(self._uname(name), shape, dt)), Buf(name)

    def ps(self, name, shape, dt):
        return self.es.enter_context(self.nc.psum_tensor(self._uname(name), shape, dt)), Buf(name)

    def din(self, name, shape, dt):
        return self.nc.dram_tensor(name, list(shape), dt, kind="ExternalInput").ap()

    def dout(self, name, shape, dt):
        return self.nc.dram_tensor(name, list(shape), dt, kind="ExternalOutput").ap()

    def V(self, fn, r=(), w=()):
        return self.P.op("vector", fn, r, w)

    def A(self, fn, r=(), w=()):
        return self.P.op("scalar", fn, r, w)

    def G(self, fn, r=(), w=()):
        return self.P.op("gpsimd", fn, r, w)

    def T(self, fn, r=(), w=()):
        return self.P.op("tensor", fn, r, w)

    def dma(self, q, out, in_, r=(), w=(), **kw):
        return self.P.dma(q, out, in_, r, w, **kw)

    def begin_phase(self):
        self._saved_es = self.es
        self.es = ExitStack()

    def end_phase(self):
        self.P.barrier()
        self.es.close()
        self.es = self._saved_es

    def done(self):
        self.P.finish()
        self.es.close()


def load_w_bf16(k, w_dram, wt, wb, ncols, rows=8):
    first = True
    for kc in range(rows):
        c0 = 0
        while c0 < ncols:
            c1 = min(ncols, c0 + 2048)
            k.dma("gpsimd", wt[:, kc, c0:c1], w_dram[kc * 128:(kc + 1) * 128, c0:c1], w=[] if not first else [wb])
            first = False
            c0 = c1
    return


def build_tok(in_w, has_prev, final, u_dt, NB=18, NLAT=16):
    NT = NB * 128
    nc = bass.Bass("TRN2", target_bir_lowering=False)
    k = KB(nc)
    P = k.P
    x_in = k.din("x_in", [NT, D], F32)
    ident_d = k.din("ident", [128, 128], F32)
    if has_prev:
        mixT = k.din("mixT", [D, NT], BF16)
        w_out = k.din("w_out", [D, D], F32)
        gates_in = k.din("gates_in", [2, 128, D], F32)
        x_out = k.dout("x_out", [NT, D], F32)
    if final:
        fn_g = k.din("final_norm", [1, D], F32)
        y_out = k.dout("y_out", [NT, D], F32)
    else:
        c_cols = k.din("c_cols", [128, 16], F32)
        norm_g = k.din("norm_g", [1, D], F32)
        ada_w = k.din("ada_w", [D, 3 * D], F32)
        ada_b = k.din("ada_b", [1, 3 * D], F32)
        w_in = k.din("w_in", [D, in_w], F32)
        u_out = k.dout("u_out", [NT, in_w], u_dt)
        gates_out = k.dout("gates_out", [2, 128, D], F32)

    ident, ident_b = k.sb("ident_sb", [128, 128], F32)
    identb, identb_b = k.sb("identb_sb", [128, 128], BF16)
    k.dma("sync", ident[:], ident_d, w=[ident_b])
    k.V(lambda E: E.tensor_copy(out=identb[:], in_=ident[:]), [ident_b], [identb_b])
    eps_t, eps_b = k.sb("eps_t", [128, 1], F32)
    k.V(lambda E: E.memset(eps_t[:], EPS), [], [eps_b])
    if has_prev:
        wo, wo_b = k.sb("wo", [128, 8, D], BF16)
        wo_evs = []
        for kc in range(8):
            wo_evs.append(k.dma("gpsimd", wo[:, kc, :], w_out[kc * 128:(kc + 1) * 128, :]))
        gp, gp_b = k.sb("gp", [128, 2, D], F32)
        k.dma("sync", gp[:], gates_in.rearrange("v p d -> p v d"), w=[gp_b])
    if final:
        fg, fg_b = k.sb("fg", [128, D], F32)
        k.dma("sync", fg[:], fn_g.partition_broadcast(128), w=[fg_b])
    else:
        wi, wi_b = k.sb("wi", [128, 8, in_w], BF16)
        wi_evs = []
        for kc in range(8):
            c0 = 0
            while c0 < in_w:
                c1 = min(in_w, c0 + 2048)
                wi_evs.append(k.dma("gpsimd", wi[:, kc, c0:c1], w_in[kc * 128:(kc + 1) * 128, c0:c1]))
                c0 = c1
        mod, mod_b = k.sb("mod", [128, 2, 3 * D], F32)
        Gt, G_b = k.sb("Gt", [128, 2, D], F32)
        with ExitStack() as es1:
            cc, cc_b = es1.enter_context(nc.sbuf_tensor("cc", [128, 16], F32)), Buf()
            rep, rep_b = es1.enter_context(nc.sbuf_tensor("rep", [128, 16, 128], F32)), Buf()
            ones, ones_b = es1.enter_context(nc.sbuf_tensor("ones", [128, 128], F32)), Buf()
            ab, ab_b = es1.enter_context(nc.sbuf_tensor("ab", [128, 3 * D], F32)), Buf()
            ng, ng_b = es1.enter_context(nc.sbuf_tensor("ng", [128, D], F32)), Buf()
            aw = [(es1.enter_context(nc.sbuf_tensor("aw%d" % i, [128, 8, 512], F32)), Buf()) for i in range(2)]
            pa = [(es1.enter_context(nc.psum_tensor("pa%d" % i, [128, 512], F32)), Buf()) for i in range(2)]
            k.dma("sync", cc[:], c_cols, w=[cc_b])
            k.dma("sync", ab[:], ada_b.partition_broadcast(128), w=[ab_b])
            k.dma("sync", ng[:], norm_g.partition_broadcast(128), w=[ng_b])
            k.A(lambda E: E.activation(out=cc[:], in_=cc[:], func=AF.Silu), [cc_b], [cc_b])
            k.V(lambda E: E.memset(ones[:], 1.0), [], [ones_b])
            for j in range(16):
                k.V(lambda E, j=j: E.tensor_scalar(out=rep[:, j, :], in0=ones[:], scalar1=cc[:, j:j + 1], scalar2=None, op0=ALU.mult),
                    [ones_b, cc_b], [rep_b])
            for g in range(6):
                awt, awb = aw[g % 2]
                k.dma("sync", awt[:], ada_w[:, g * 512:(g + 1) * 512].rearrange("(k p) n -> p k n", p=128), w=[awb])
                for v in range(2):
                    pt, pb = pa[v]
                    for kc in range(8):
                        k.T(lambda E, pt=pt, v=v, kc=kc, awt=awt: E.matmul(pt[:], rep[:, v * 8 + kc, :], awt[:, kc, :], start=(kc == 0), stop=(kc == 7)),
                            [rep_b, awb], [pb])
                    k.V(lambda E, pt=pt, v=v, g=g: E.tensor_tensor(out=mod[:, v, g * 512:(g + 1) * 512], in0=pt[:], in1=ab[:, g * 512:(g + 1) * 512], op=ALU.add),
                        [pb, ab_b], [mod_b])
            for v in range(2):
                k.V(lambda E, v=v: E.scalar_tensor_tensor(out=Gt[:, v, :], in0=mod[:, v, D:2 * D], scalar=1.0, in1=ng[:], op0=ALU.add, op1=ALU.mult),
                    [mod_b, ng_b], [G_b])
            k.dma("sync", gates_out.rearrange("v p d -> p v d"), mod[:, :, 2 * D:3 * D], r=[mod_b])
            scope_bufs = [cc_b, rep_b, ones_b, ab_b, ng_b, aw[0][1], aw[1][1], pa[0][1], pa[1][1]]

    xt = [k.sb("xt%d" % i, [128, D], F32) for i in range(2)]
    tmp = [k.sb("tmp%d" % i, [128, D], F32) for i in range(2)]
    ss = [k.sb("ss%d" % i, [128, 4], F32) for i in range(2)]
    junk, junk_b = k.sb("junk", [128, D], BF16)
    if has_prev:
        mt = [k.sb("mt%d" % i, [128, 8, 128], BF16) for i in range(2)]
        py = [k.ps("py%d" % i, [128, 512], F32) for i in range(2)]
    if not final:
        xm = [k.sb("xm%d" % i, [128, D], BF16) for i in range(2)]
        xmT = [k.sb("xmT%d" % i, [128, 8, 128], BF16) for i in range(2)]
        ptr = [k.ps("ptr%d" % i, [128, 8, 128], BF16) for i in range(2)]
        pu = [k.ps("pu%d" % i, [128, 512], F32) for i in range(3)]
        ut = [k.sb("ut%d" % i, [128, 512], u_dt) for i in range(4)]
    if not final:
        fence = Buf("fence")
        for b in scope_bufs:
            if b.w:
                fence.r[b.w[0]] = max(fence.r.get(b.w[0], 0), b.w[1])
            for kk, vv in b.r.items():
                fence.r[kk] = max(fence.r.get(kk, 0), vv)
    else:
        fence = Buf("fence")
    fence_evs = list(fence.r.items())
    for e in ("vector", "scalar", "gpsimd", "tensor", "sync"):
        P._need(e, fence_evs)
    if has_prev:
        for e in ("tensor",):
            P._need(e, wo_evs)
    if not final:
        P._need("tensor", wi_evs)

    ngroups = (in_w + 511) // 512 if not final else 0
    ucount = 0
    for b in range(NB):
        v = 0 if b < NLAT else 1
        xtt, xtb = xt[b % 2]
        tt, tb = tmp[b % 2]
        sst, ssb = ss[b % 2]
        k.dma("sync", xtt[:], x_in[b * 128:(b + 1) * 128, :], w=[xtb])
        if has_prev:
            mtt, mtb = mt[b % 2]
            k.dma("sync", mtt[:], mixT[:, b * 128:(b + 1) * 128].rearrange("(k p) t -> p k t", p=128), w=[mtb])
            for h in range(2):
                pyt, pyb = py[h]
                for kc in range(8):
                    k.T(lambda E, pyt=pyt, mtt=mtt, kc=kc, h=h: E.matmul(pyt[:], mtt[:, kc, :], wo[:, kc, h * 512:(h + 1) * 512], start=(kc == 0), stop=(kc == 7)),
                        [mtb], [pyb])
                k.V(lambda E, pyt=pyt, tt=tt, h=h, v=v: E.tensor_tensor(out=tt[:, h * 512:(h + 1) * 512], in0=pyt[:], in1=gp[:, v, h * 512:(h + 1) * 512], op=ALU.mult),
                    [pyb, gp_b], [tb])
            k.G(lambda E, xtt=xtt, tt=tt: E.tensor_tensor(out=xtt[:], in0=tt[:], in1=xtt[:], op=ALU.add), [tb, xtb], [xtb])
            k.dma("sync", x_out[b * 128:(b + 1) * 128, :], xtt[:], r=[xtb])
        k.A(lambda E, xtt=xtt, sst=sst: E.activation(out=junk[:], in_=xtt[:], func=AF.Square, accum_out=sst[:, 0:1]), [xtb], [junk_b, ssb])
        k.A(lambda E, sst=sst: E.activation(out=sst[:, 1:2], in_=sst[:, 0:1], func=AF.Sqrt, scale=1.0 / D, bias=eps_t[:]), [ssb, eps_b], [ssb])
        k.V(lambda E, sst=sst: E.reciprocal(out=sst[:, 2:3], in_=sst[:, 1:2]), [ssb], [ssb])
        if final:
            k.V(lambda E, xtt=xtt, tt=tt, sst=sst: E.scalar_tensor_tensor(out=tt[:], in0=xtt[:], scalar=sst[:, 2:3], in1=fg[:], op0=ALU.mult, op1=ALU.mult),
                [xtb, ssb, fg_b], [tb])
            k.dma("sync", y_out[b * 128:(b + 1) * 128, :], tt[:], r=[tb])
            continue
        xmt, xmb = xm[b % 2]
        k.V(lambda E, xtt=xtt, tt=tt, sst=sst, v=v: E.scalar_tensor_tensor(out=tt[:], in0=xtt[:], scalar=sst[:, 2:3], in1=Gt[:, v, :], op0=ALU.mult, op1=ALU.mult),
            [xtb, ssb, G_b], [tb])
        k.G(lambda E, tt=tt, xmt=xmt, v=v: E.tensor_tensor(out=xmt[:], in0=tt[:], in1=mod[:, v, 0:D], op=ALU.add), [tb, mod_b], [xmb])
        ptt, ptb = ptr[b % 2]
        xTt, xTb = xmT[b % 2]
        for kc in range(8):
            k.T(lambda E, ptt=ptt, xmt=xmt, kc=kc: E.transpose(ptt[:, kc, :], xmt[:, kc * 128:(kc + 1) * 128], identb[:]), [xmb, identb_b], [ptb])
        k.A(lambda E, ptt=ptt, xTt=xTt: E.copy(out=xTt[:], in_=ptt[:]), [ptb], [xTb])
        for g in range(ngroups):
            c0 = g * 512
            c1 = min(in_w, c0 + 512)
            put, pub = pu[ucount % 3]
            utt, utb = ut[ucount % 4]
            for kc in range(8):
                k.T(lambda E, put=put, xTt=xTt, kc=kc, c0=c0, c1=c1: E.matmul(put[:, 0:c1 - c0], xTt[:, kc, :], wi[:, kc, c0:c1], start=(kc == 0), stop=(kc == 7)),
                    [xTb], [pub])
            if ucount % 2 == 0:
                k.V(lambda E, put=put, utt=utt, c0=c0, c1=c1: E.tensor_copy(out=utt[:, 0:c1 - c0], in_=put[:, 0:c1 - c0]), [pub], [utb])
            else:
                k.A(lambda E, put=put, utt=utt, c0=c0, c1=c1: E.copy(out=utt[:, 0:c1 - c0], in_=put[:, 0:c1 - c0]), [pub], [utb])
            k.dma("sync", u_out[b * 128:(b + 1) * 128, c0:c1], utt[:, 0:c1 - c0], r=[utb])
            ucount += 1
    k.done()
    return nc


def _sc(s):
    return (s[0], [s[1]]) if isinstance(s, tuple) else (s, [])


def h_tt(k, out, a, b, op, eng="vector"):
    oa, ob = out; aa, ab = a; ba, bb = b
    return k.P.op(eng, lambda E: E.tensor_tensor(out=oa, in0=aa, in1=ba, op=op), [ab, bb], [ob])


def h_stt(k, out, a, s, b, op0, op1, accum=None):
    oa, ob = out; aa, ab = a; ba, bb = b
    sv, sb_ = _sc(s)
    if accum is None:
        return k.P.op("vector", lambda E: E.scalar_tensor_tensor(out=oa, in0=aa, scalar=sv, in1=ba, op0=op0, op1=op1), [ab, bb] + sb_, [ob])
    ca, cb = accum
    return k.P.op("vector", lambda E: E.scalar_tensor_tensor(out=oa, in0=aa, scalar=sv, in1=ba, op0=op0, op1=op1, accum_out=ca), [ab, bb] + sb_, [ob, cb])


def h_ts(k, out, a, s1, s2, op0, op1=None, eng="vector"):
    oa, ob = out; aa, ab = a
    s1v, s1b = _sc(s1)
    s2v, s2b = _sc(s2) if s2 is not None else (None, [])
    if op1 is None:
        return k.P.op(eng, lambda E: E.tensor_scalar(out=oa, in0=aa, scalar1=s1v, scalar2=None, op0=op0), [ab] + s1b, [ob])
    return k.P.op(eng, lambda E: E.tensor_scalar(out=oa, in0=aa, scalar1=s1v, scalar2=s2v, op0=op0, op1=op1), [ab] + s1b + s2b, [ob])


def h_act(k, out, a, func, scale=1.0, bias=None, accum=None):
    oa, ob = out; aa, ab = a
    kw = {}
    rd = [ab]
    wr = [ob]
    if bias is not None:
        bv, bb = _sc(bias)
        kw["bias"] = bv
        rd += bb
    if isinstance(scale, tuple):
        rd.append(scale[1]); scale = scale[0]
    if accum is not None:
        kw["accum_out"] = accum[0]
        wr.append(accum[1])
    return k.P.op("scalar", lambda E: E.activation(out=oa, in_=aa, func=func, scale=scale, **kw), rd, wr)


def h_recip(k, out, a):
    oa, ob = out; aa, ab = a
    return k.P.op("vector", lambda E: E.reciprocal(out=oa, in_=aa), [ab], [ob])


def h_copy(k, out, a, eng="vector"):
    oa, ob = out; aa, ab = a
    if eng == "scalar":
        return k.P.op(eng, lambda E: E.copy(out=oa, in_=aa), [ab], [ob])
    return k.P.op(eng, lambda E: E.tensor_copy(out=oa, in_=aa), [ab], [ob])


def h_mm(k, out, lhsT, rhs, start=True, stop=True):
    oa, ob = out; la, lb = lhsT; ra, rb = rhs
    return k.P.op("tensor", lambda E: E.matmul(oa, la, ra, start=start, stop=stop), [lb, rb], [ob])


def h_memset(k, out, val, eng="vector"):
    oa, ob = out
    return k.P.op(eng, lambda E: E.memset(oa, val), [], [ob])


def h_sigmoid(k, out, a, tmp, scale=1.0, bias=None):
    h_act(k, tmp, a, AF.Exp, scale=-scale, bias=bias)
    h_ts(k, tmp, tmp, 1.0, None, ALU.add)
    h_recip(k, out, tmp)


def h_silu(k, out, a, tmp):
    h_sigmoid(k, tmp, a, tmp)
    h_tt(k, out, tmp, a, ALU.mult)


import numpy as np

NEG = -30000.0


def na_plan(nlb):
    rows_blocks = nlb
    plan = []
    variants = {}
    for m in range(nlb):
        r0 = min(max(2 * m - 4, 0), 2 * nlb - 8)
        r1 = min(max(2 * m + 1 - 4, 0), 2 * nlb - 8) + 7
        kb0, kb1 = r0 // 2, r1 // 2
        edge = (m < 2) or (m >= nlb - 2)
        lst = []
        for kb in range(kb0, kb1 + 1):
            key = (m if edge else "i", kb - m)
            if key not in variants:
                variants[key] = (len(variants), m, kb)
            lst.append((kb, variants[key][0]))
        plan.append(lst)
    return plan, variants


def na_bias_tables(rpb_h, nlb):
    plan, variants = na_plan(nlb)
    rows = 2 * nlb
    out = np.full((128, len(variants), 128), NEG, np.float32)
    for key, (vid, m, kb) in variants.items():
        q = m * 128 + np.arange(128)
        kk = kb * 128 + np.arange(128)
        qr, qc = q // 64, q % 64
        kr, kc = kk // 64, kk % 64
        row0 = np.clip(qr - 4, 0, rows - 8)
        col0 = np.clip(qc - 8, 0, 64 - 16)
        inwin = ((kr[:, None] >= row0[None]) & (kr[:, None] < row0[None] + 8) &
                 (kc[:, None] >= col0[None]) & (kc[:, None] < col0[None] + 16))
        rr = np.clip(kr[:, None] - qr[None] + 7, 0, 14)
        cc = np.clip(kc[:, None] - qc[None] + 15, 0, 30)
        out[:, vid, :] = np.where(inwin, rpb_h[rr, cc], NEG)
    return out


def build_attn(nlb=128):
    NTK = (nlb + 2) * 128
    NKB = nlb + 2
    plan, variants = na_plan(nlb)
    nvar = len(variants)
    nc = bass.Bass("TRN2", target_bir_lowering=False)
    k = KB(nc)
    P = k.P
    qa_d = k.din("qaT", [64, NTK], BF16)
    ka_d = k.din("kaT", [64, NTK], BF16)
    va_d = k.din("va", [128, NKB, 65], BF16)
    ga_d = k.din("gaT", [64, NTK], BF16)
    qb_d = k.din("qbT", [64, NTK], BF16)
    kb_d = k.din("kbT", [64, NTK], BF16)
    vb_d = k.din("vb", [128, NKB, 65], BF16)
    gb_d = k.din("gbT", [64, NTK], BF16)
    bias_d = k.din("biasT", [128, nvar, 128], F32)
    cs_d = k.din("ropeC", [64, NTK], F32)
    sn_d = k.din("ropeS", [64, NTK], F32)
    gains_d = k.din("gains", [64, 2], F32)
    rmat_d = k.din("rmatT", [64, 64], F32)
    ident_d = k.din("ident", [128, 128], F32)
    sel_d = k.din("sel", [65, 64], F32)
    out_d = k.dout("mixT", [128, NTK], BF16)

    qT, qT_b = k.sb("qT", [64, NTK], BF16)
    kT, kT_b = k.sb("kT", [64, NTK], BF16)
    vE, vE_b = k.sb("vE", [128, NKB, 65], BF16)
    biasf, biasf_b = k.sb("biasf", [128, nvar, 128], F32)
    biasb, biasb_b = k.sb("biasb", [128, nvar, 128], BF16)
    identf, identf_b = k.sb("identf", [128, 128], F32)
    identb, identb_b = k.sb("identb", [128, 128], BF16)
    sel, sel_b = k.sb("sel_sb", [65, 64], F32)
    gains, gains_b = k.sb("gains_sb", [64, 2], F32)
    rmf, rmf_b = k.sb("rmf", [64, 64], F32)
    rmb, rmb_b = k.sb("rmb", [64, 64], BF16)
    ones64, ones64_b = k.sb("ones64", [64, 64], F32)
    eps_t, eps_b = k.sb("eps_t", [64, 1], F32)
    k.dma("sync", biasf[:], bias_d, w=[biasf_b])
    k.dma("sync", identf[:], ident_d, w=[identf_b])
    k.dma("sync", sel[:], sel_d, w=[sel_b])
    k.dma("sync", gains[:], gains_d, w=[gains_b])
    k.dma("sync", rmf[:], rmat_d, w=[rmf_b])
    k.V(lambda E: E.tensor_copy(out=biasb[:], in_=biasf[:]), [biasf_b], [biasb_b])
    k.V(lambda E: E.tensor_copy(out=identb[:], in_=identf[:]), [identf_b], [identb_b])
    k.V(lambda E: E.tensor_copy(out=rmb[:], in_=rmf[:]), [rmf_b], [rmb_b])
    k.V(lambda E: E.memset(ones64[:], 1.0 / 64), [], [ones64_b])
    k.V(lambda E: E.memset(eps_t[:], EPS), [], [eps_b])
    k.V(lambda E: E.tensor_scalar(out=gains[:, 0:1], in0=gains[:, 0:1], scalar1=0.125, scalar2=None, op0=ALU.mult), [gains_b], [gains_b])

    pS = [k.ps("pS%d" % i, [128, 512], F32) for i in range(4)]
    pacc = [k.ps("pacc%d" % i, [65, 512], F32) for i in range(2)]
    pmisc = [k.ps("pm%d" % i, [64, 512], F32) for i in range(2)]
    PT = [k.sb("PT%d" % i, [128, 512], BF16) for i in range(4)]
    accs = [k.sb("accs%d" % i, [65, 512], F32) for i in range(2)]
    gt = [k.sb("gt%d" % i, [64, 512], BF16) for i in range(2)]
    w1 = [k.sb("w1_%d" % i, [64, 512], F32) for i in range(2)]
    w2 = [k.sb("w2_%d" % i, [64, 512], F32) for i in range(2)]
    ot = [k.sb("ot%d" % i, [64, 512], BF16) for i in range(2)]
    cin = [k.sb("cin%d" % i, [64, 512], BF16) for i in range(2)]
    ctab = [k.sb("ctab%d" % i, [64, 512], F32) for i in range(2)]
    stab = [k.sb("stab%d" % i, [64, 512], F32) for i in range(2)]
    qnb = [k.sb("qnb%d" % i, [64, 512], BF16) for i in range(2)]

    state = {"fin": 0, "S": 0, "pre": 0}

    def finalize(pa, pab, g_d, row0, c0, n):
        i = state["fin"] % 2
        state["fin"] += 1
        at, ab = accs[i]
        gtt, gtb = gt[i]
        a1, a1b = w1[i]
        a2, a2b = w2[i]
        o, ob = ot[i]
        pm, pmb = pmisc[i]
        k.dma("sync", gtt[:, 0:n], g_d[:, c0:c0 + n], w=[gtb])
        k.A(lambda E: E.copy(out=at[:, 0:n], in_=pa[:, 0:n]), [pab], [ab])
        k.T(lambda E: E.matmul(pm[:, 0:n], sel[:], at[:, 0:n], start=True, stop=True), [sel_b, ab], [pmb])
        k.V(lambda E: E.reciprocal(out=a1[:, 0:n], in_=pm[:, 0:n]), [pmb], [a1b])
        k.V(lambda E: E.tensor_tensor(out=a1[:, 0:n], in0=a1[:, 0:n], in1=at[0:64, 0:n], op=ALU.mult), [a1b, ab], [a1b])
        k.A(lambda E: E.activation(out=a2[:, 0:n], in_=gtt[:, 0:n], func=AF.Exp, scale=-1.0), [gtb], [a2b])
        k.V(lambda E: E.tensor_scalar(out=a2[:, 0:n], in0=a2[:, 0:n], scalar1=1.0, scalar2=None, op0=ALU.add), [a2b], [a2b])
        k.V(lambda E: E.reciprocal(out=a2[:, 0:n], in_=a2[:, 0:n]), [a2b], [a2b])
        k.V(lambda E: E.tensor_tensor(out=a2[:, 0:n], in0=a2[:, 0:n], in1=gtt[:, 0:n], op=ALU.mult), [a2b, gtb], [a2b])
        k.V(lambda E: E.tensor_tensor(out=o[:, 0:n], in0=a1[:, 0:n], in1=a2[:, 0:n], op=ALU.mult), [a1b, a2b], [ob])
        k.dma("sync", out_d[row0:row0 + 64, c0:c0 + n], o[:, 0:n], r=[ob])

    def attend(qc0, n, kbs, pa, pab, first=True, last=True):
        pend = []
        nk = len(kbs)
        for idx, kb in enumerate(kbs):
            s = state["S"]
            state["S"] += 1
            pst, psb = pS[s % 4]
            ptt, ptb = PT[s % 4]
            k.T(lambda E, pst=pst, kb=kb: E.matmul(pst[:, 0:n], kT[:, kb * 128:(kb + 1) * 128], qT[:, qc0:qc0 + n], start=True, stop=True),
                [kT_b, qT_b], [psb])
            k.A(lambda E, pst=pst, ptt=ptt: E.activation(out=ptt[:, 0:n], in_=pst[:, 0:n], func=AF.Exp), [psb], [ptb])
            pend.append((idx, kb, ptt, ptb))
            if len(pend) > 2:
                j, kbj, pj, pjb = pend.pop(0)
                k.T(lambda E, kbj=kbj, pj=pj, j=j: E.matmul(pa[:, 0:n], vE[:, kbj, :], pj[:, 0:n], start=(first and j == 0), stop=(last and j == nk - 1)),
                    [vE_b, pjb], [pab])
        for (j, kbj, pj, pjb) in pend:
            k.T(lambda E, kbj=kbj, pj=pj, j=j: E.matmul(pa[:, 0:n], vE[:, kbj, :], pj[:, 0:n], start=(first and j == 0), stop=(last and j == nk - 1)),
                [vE_b, pjb], [pab])

    k.dma("sync", kT[:], ka_d, w=[kT_b])
    k.dma("sync", vE[:], va_d, w=[vE_b])
    k.dma("sync", qT[:], qa_d, w=[qT_b])
    k.V(lambda E: E.tensor_scalar(out=qT[:], in0=qT[:], scalar1=0.125, scalar2=None, op0=ALU.mult), [qT_b], [qT_b])
    ctxk = [nlb, nlb + 1]
    acc_i = 0
    for g0 in range(0, nlb, 4):
        pa, pab = pacc[acc_i % 2]
        acc_i += 1
        nq = min(4, nlb - g0)
        for mi in range(nq):
            m = g0 + mi
            lst = plan[m]
            nreg = len(lst) + 2
            s = state["S"]
            state["S"] += 2
            pA, pAb = pS[s % 4]
            pB, pBb = pS[(s + 1) % 4]
            tA, tAb = PT[s % 4]
            tB, tBb = PT[(s + 1) % 4]
            regs = []
            for j, (kb, var) in enumerate(lst + [(ctxk[0], None), (ctxk[1], None)]):
                pt_, pb_, tt_, tb_ = (pA, pAb, tA, tAb) if j < 4 else (pB, pBb, tB, tBb)
                jj = j % 4
                k.T(lambda E, pt_=pt_, kb=kb, jj=jj, m=m, var=var: E.matmul(pt_[:, jj * 128:(jj + 1) * 128], kT[:, kb * 128:(kb + 1) * 128], qT[:, m * 128:(m + 1) * 128], start=True, stop=(var is None)),
                    [kT_b, qT_b], [pb_])
                if var is not None:
                    k.T(lambda E, pt_=pt_, jj=jj, var=var: E.matmul(pt_[:, jj * 128:(jj + 1) * 128], identb[:], biasb[:, var, :], start=False, stop=True),
                        [identb_b, biasb_b], [pb_])
                regs.append((kb, tt_, tb_, jj))
            nA = min(4, nreg)
            k.A(lambda E, pA=pA, tA=tA, nA=nA: E.activation(out=tA[:, 0:nA * 128], in_=pA[:, 0:nA * 128], func=AF.Exp), [pAb], [tAb])
            if nreg > 4:
                nB = nreg - 4
                k.A(lambda E, pB=pB, tB=tB, nB=nB: E.activation(out=tB[:, 0:nB * 128], in_=pB[:, 0:nB * 128], func=AF.Exp), [pBb], [tBb])
            for j, (kb, tt_, tb_, jj) in enumerate(regs):
                k.T(lambda E, pa=pa, kb=kb, tt_=tt_, jj=jj, mi=mi, j=j, nreg=nreg: E.matmul(pa[:, mi * 128:(mi + 1) * 128], vE[:, kb, :], tt_[:, jj * 128:(jj + 1) * 128], start=(j == 0), stop=(j == nreg - 1)),
                    [vE_b, tb_], [pab])
        finalize(pa, pab, ga_d, 0, g0 * 128, nq * 128)
    pa, pab = pacc[acc_i % 2]
    acc_i += 1
    attend(nlb * 128, 256, ctxk, pa, pab)
    finalize(pa, pab, ga_d, 0, nlb * 128, 256)

    k.dma("sync", vE[:], vb_d, w=[vE_b])

    def prepass(src_d, dst, dst_b, gcol):
        for c0_ in range(0, NTK, 512):
            pre_chunk(src_d, dst, dst_b, gcol, c0_)

    def pre_chunk(src_d, dst, dst_b, gcol, c0):
        if True:
            n = min(512, NTK - c0)
            i = state["pre"] % 2
            state["pre"] += 1
            ci, cib = cin[i]
            ct, ctb = ctab[i]
            st_, stb = stab[i]
            qn, qnb_ = qnb[i]
            a1, a1b = w1[i]
            a2, a2b = w2[i]
            pm, pmb = pmisc[i]
            k.dma("sync", ci[:, 0:n], src_d[:, c0:c0 + n], w=[cib])
            k.dma("sync", ct[:, 0:n], cs_d[:, c0:c0 + n], w=[ctb])
            k.dma("sync", st_[:, 0:n], sn_d[:, c0:c0 + n], w=[stb])
            k.V(lambda E: E.tensor_tensor(out=a1[:, 0:n], in0=ci[:, 0:n], in1=ci[:, 0:n], op=ALU.mult), [cib], [a1b])
            k.T(lambda E: E.matmul(pm[:, 0:n], ones64[:], a1[:, 0:n], start=True, stop=True), [ones64_b, a1b], [pmb])
            k.A(lambda E: E.activation(out=a2[:, 0:n], in_=pm[:, 0:n], func=AF.Sqrt, bias=eps_t[:]), [pmb, eps_b], [a2b])
            k.V(lambda E: E.reciprocal(out=a2[:, 0:n], in_=a2[:, 0:n]), [a2b], [a2b])
            k.V(lambda E: E.scalar_tensor_tensor(out=qn[:, 0:n], in0=ci[:, 0:n], scalar=gains[:, gcol:gcol + 1], in1=a2[:, 0:n], op0=ALU.mult, op1=ALU.mult),
                [cib, gains_b, a2b], [qnb_])
            k.T(lambda E: E.matmul(pm[:, 0:n], rmb[:], qn[:, 0:n], start=True, stop=True), [rmb_b, qnb_], [pmb])
            k.V(lambda E: E.tensor_tensor(out=a1[:, 0:n], in0=qn[:, 0:n], in1=ct[:, 0:n], op=ALU.mult), [qnb_, ctb], [a1b])
            k.V(lambda E: E.tensor_tensor(out=a2[:, 0:n], in0=pm[:, 0:n], in1=st_[:, 0:n], op=ALU.mult), [pmb, stb], [a2b])
            k.G(lambda E: E.tensor_tensor(out=dst[:, c0:c0 + n], in0=a1[:, 0:n], in1=a2[:, 0:n], op=ALU.add), [a1b, a2b], [dst_b])

    prepass(kb_d, kT, kT_b, 1)
    prepass(qb_d, qT, qT_b, 0)
    allk = list(range(NKB))
    for c0 in range(0, nlb * 128, 512):
        n = min(512, nlb * 128 - c0)
        pa, pab = pacc[acc_i % 2]
        acc_i += 1
        attend(c0, n, allk, pa, pab)
        finalize(pa, pab, gb_d, 64, c0, n)
    pa, pab = pacc[acc_i % 2]
    acc_i += 1
    attend(nlb * 128, 256, ctxk, pa, pab)
    finalize(pa, pab, gb_d, 64, nlb * 128, 256)
    k.done()
    return nc


def rope_tables(nlb):
    n = nlb * 128
    t = np.arange(n)
    pos = np.stack([t // 64, t % 64], -1).astype(np.float32)
    inv = (10000.0 ** (-np.arange(16, dtype=np.float32) / 16)).astype(np.float32)
    ang = pos[:, :, None] * inv
    cos, sin = np.cos(ang), np.sin(ang)
    C = np.ones((64, n + 256), np.float32)
    S = np.zeros((64, n + 256), np.float32)
    for a in range(2):
        for hf in range(2):
            C[a * 32 + hf * 16:a * 32 + hf * 16 + 16, :n] = cos[:, a, :].T
            S[a * 32 + hf * 16:a * 32 + hf * 16 + 16, :n] = sin[:, a, :].T
    R = np.zeros((64, 64), np.float32)
    for a in range(2):
        for f in range(16):
            R[a * 32 + f, a * 32 + 16 + f] = -1.0
            R[a * 32 + 16 + f, a * 32 + f] = 1.0
    return C, S, np.ascontiguousarray(R.T)


import numpy as np

PC = {n: i for i, n in enumerate(
    ["mu_r", "mu_k", "mu_v", "k_k", "k_a", "w0_f", "w0_b", "a0_f", "a0_b", "r_k",
     "t0_v", "t1_v", "t2_v", "t0_x1", "t1_x1", "t2_x1", "t0_x2", "t1_x2", "t2_x2",
     "gn_w", "gn_b", "skip0", "skip1"])}
NPC = len(PC)
FO = {n: i for i, n in enumerate(
    ["w_f", "w_b", "kkn", "nb_f", "nb_b", "k_f", "k_b", "r", "v", "bonus", "sg_rw", "hv", "hx1", "hx2", "sg_hy"])}
NFO = len(FO)
FI = {n: i for i, n in enumerate(["r", "k", "v", "g_rw", "hv", "hx1", "hx2", "g_hy"])}
NFI = len(FI)


def seq_chunks(n_lat):
    ch = [(0, 0, 256)]
    for c0 in range(0, n_lat, 512):
        n = min(512, n_lat - c0)
        ch.append((258 + c0, 256 + c0, n))
    return ch


def build_rfeat(n_lat):
    T = 256 + n_lat
    TP = T + 4
    nc = bass.Bass("TRN2", target_bir_lowering=False)
    k = KB(nc)
    fin_d = k.din("fin", [NFI, 64, TP], F32)
    lora_d = k.din("lora", [128, TP], F32)
    pp_d = k.din("pp", [64, NPC], F32)
    mul_d = k.din("mu_lora", [128, 1], F32)
    wx_d = k.din("wx", [128, 4, 64], F32)
    fo_d = k.dout("fo", [NFO, 64, T], F32)

    pp = k.sb("pp_sb", [64, NPC], F32)
    npp = k.sb("npp_sb", [64, NPC], F32)
    mul = k.sb("mul_sb", [128, 1], F32)
    wx = k.sb("wx_sb", [128, 4, 64], F32)
    ones = k.sb("ones_sb", [64, 64], F32)
    tiny = k.sb("tiny_sb", [64, 1], F32)
    k.dma("sync", pp[0][:], pp_d, w=[pp[1]])
    k.dma("sync", mul[0][:], mul_d, w=[mul[1]])
    k.dma("sync", wx[0][:], wx_d, w=[wx[1]])
    h_memset(k, (ones[0][:], ones[1]), 1.0)
    h_memset(k, (tiny[0][:], tiny[1]), 1e-12)
    h_ts(k, (npp[0][:], npp[1]), (pp[0][:], pp[1]), -1.0, None, ALU.mult)
    h_ts(k, (npp[0][:, PC["r_k"]:PC["r_k"] + 1], npp[1]), (pp[0][:, PC["r_k"]:PC["r_k"] + 1], pp[1]), 0.5, None, ALU.mult)

    def pcol(name):
        return (pp[0][:, PC[name]:PC[name] + 1], pp[1])

    def ncol(name):
        return (npp[0][:, PC[name]:PC[name] + 1], npp[1])

    fin = [[k.sb("fin%d_%d" % (i, j), [64, 514], F32) for j in range(2)] for i in range(NFI)]
    lor = [k.sb("lor%d" % j, [128, 514], F32) for j in range(2)]
    fo = [[k.sb("fo%d_%d" % (i, j), [64, 512], F32) for j in range(2)] for i in range(NFO)]
    tm = [k.sb("tm%d" % i, [64, 512], F32) for i in range(8)]
    lt = [k.sb("lt%d" % i, [128, 512], F32) for i in range(3)]
    pz = [k.ps("pz%d" % i, [64, 512], F32) for i in range(6)]

    def chunk(ci, pc0, oc0, n):
        j = ci % 2

        def I(name, lo=1):
            t, b = fin[FI[name]][j]
            return (t[:, lo:lo + n], b)

        def O(name):
            t, b = fo[FO[name]][j]
            return (t[:, 0:n], b)

        def Tm(i):
            return (tm[i][0][:, 0:n], tm[i][1])

        def Lt(i):
            return (lt[i][0][:, 0:n], lt[i][1])

        def Pz(i):
            return (pz[i][0][:, 0:n], pz[i][1])

        for name, i in FI.items():
            t, b = fin[i][j]
            k.dma("sync", t[:, 0:n + 2], fin_d[i, :, pc0:pc0 + n + 2], w=[b])
        lt_, lb_ = lor[j]
        k.dma("sync", lt_[:, 0:n + 2], lora_d[:, pc0:pc0 + n + 2], w=[lb_])

        def shift(out, src_lo, src_mid, src_hi, mu, t):
            h_tt(k, t, src_lo, src_hi, ALU.add)
            h_stt(k, t, t, 0.5, src_mid, ALU.mult, ALU.subtract)
            h_stt(k, out, t, mu, src_mid, ALU.mult, ALU.add)

        rs, ks, vs = O("r"), Tm(0), O("v")
        shift(rs, I("r", 0), I("r", 1), I("r", 2), pcol("mu_r"), Tm(7))
        shift(ks, I("k", 0), I("k", 1), I("k", 2), pcol("mu_k"), Tm(7))
        shift(vs, I("v", 0), I("v", 1), I("v", 2), pcol("mu_v"), Tm(7))
        ls = Lt(0)
        shift(ls, (lt_[:, 0:n], lb_), (lt_[:, 1:n + 1], lb_), (lt_[:, 2:n + 2], lb_), (mul[0][:], mul[1]), Lt(2))
        lth = Lt(1)
        h_act(k, lth, ls, AF.Tanh)
        h_mm(k, Pz(0), (wx[0][:, 0, :], wx[1]), lth)
        h_mm(k, Pz(1), (wx[0][:, 1, :], wx[1]), lth)
        h_mm(k, Pz(2), (wx[0][:, 2, :], wx[1]), ls)
        h_mm(k, Pz(3), (wx[0][:, 3, :], wx[1]), ls)
        kk = Tm(1)
        h_ts(k, kk, ks, pcol("k_k"), None, ALU.mult)
        h_tt(k, Tm(2), kk, kk, ALU.mult)
        h_mm(k, Pz(4), (ones[0][:], ones[1]), Tm(2))
        h_act(k, Tm(2), Pz(4), AF.Sqrt, bias=(tiny[0][:], tiny[1]))
        h_recip(k, Tm(2), Tm(2))
        h_tt(k, O("kkn"), kk, Tm(2), ALU.mult)
        for d, sfx in enumerate(("f", "b")):
            h_sigmoid(k, Tm(3), Pz(d), Tm(3), bias=ncol("w0_" + sfx))
            h_act(k, O("w_" + sfx), Tm(3), AF.Exp, scale=-float(np.exp(-0.5)))
            a = Tm(4)
            h_sigmoid(k, a, Pz(2 + d), a, bias=ncol("a0_" + sfx))
            h_ts(k, Tm(5), a, -1.0, pcol("k_a"), ALU.add, ALU.mult)
            h_stt(k, O("k_" + sfx), Tm(5), 1.0, ks, ALU.add, ALU.mult)
            h_stt(k, O("nb_" + sfx), O("kkn"), -1.0, a, ALU.mult, ALU.mult)
        h_tt(k, Tm(5), O("k_f"), O("k_b"), ALU.add)
        h_stt(k, Tm(5), rs, ncol("r_k"), Tm(5), ALU.mult, ALU.mult)
        h_mm(k, Pz(5), (ones[0][:], ones[1]), Tm(5))
        h_tt(k, O("bonus"), Pz(5), vs, ALU.mult)
        h_silu(k, O("sg_rw"), I("g_rw", 1), Tm(6))
        h_silu(k, O("sg_hy"), I("g_hy", 1), Tm(6))
        for nm in ("v", "x1", "x2"):
            src = "h" + nm
            h_ts(k, Tm(6), I(src, 0), pcol("t0_" + nm), None, ALU.mult)
            h_stt(k, Tm(6), I(src, 1), pcol("t1_" + nm), Tm(6), ALU.mult, ALU.add)
            h_stt(k, O(src), I(src, 2), pcol("t2_" + nm), Tm(6), ALU.mult, ALU.add)
        for name, i in FO.items():
            t, b = fo[i][j]
            k.dma("sync", fo_d[i, :, oc0:oc0 + n], t[:, 0:n], r=[b])

    for ci, (pc0, oc0, n) in enumerate(seq_chunks(n_lat)):
        chunk(ci, pc0, oc0, n)
    k.done()
    return nc


import numpy as np

TWO_PI = float(2 * np.pi)


def build_filt(L):
    nc = bass.Bass("TRN2", target_bir_lowering=False)
    k = KB(nc)
    feats_d = k.din("featsT", [33, L], F32)
    tv_d = k.din("tvals", [1, L], F32)
    w1_d = k.din("w1", [33, 64], F32)
    w2_d = k.din("w2", [64, 64], F32)
    w3_d = k.din("w3c", [64, 256], F32)
    bb_d = k.din("b12", [64, 2], F32)
    b3_d = k.din("b3c", [128, 2], F32)
    nd_d = k.din("negdelta", [128, 1], F32)
    tf_d = k.dout("tapsF", [128, L], BF16)
    tb_d = k.dout("tapsB", [128, L], BF16)
    ssq_d = k.dout("ssq", [128, 1], F32)
    w1 = k.sb("w1s", [33, 64], F32); w2 = k.sb("w2s", [64, 64], F32); w3 = k.sb("w3s", [64, 256], F32)
    bb = k.sb("bbs", [64, 2], F32); b3 = k.sb("b3s", [128, 2], F32); nd = k.sb("nds", [128, 1], F32)
    for t, d in ((w1, w1_d), (w2, w2_d), (w3, w3_d), (bb, bb_d), (b3, b3_d), (nd, nd_d)):
        k.dma("sync", t[0][:], d, w=[t[1]])
    nch = (L + 511) // 512
    part = k.sb("part", [128, 2 * nch], F32)
    h_memset(k, (part[0][:], part[1]), 0.0)
    ft = [k.sb("ft%d" % i, [33, 512], F32) for i in range(2)]
    tv = [k.sb("tv%d" % i, [128, 512], F32) for i in range(2)]
    hx = [k.sb("hx%d" % i, [64, 512], F32) for i in range(2)]
    hi = k.sb("hi", [64, 512], I32)
    hf = k.sb("hf", [64, 512], F32)
    win = k.sb("win", [128, 512], F32)
    tF = [k.sb("tF%d" % i, [128, 512], F32) for i in range(2)]
    tB = [k.sb("tB%d" % i, [128, 512], F32) for i in range(2)]
    oF = [k.sb("oF%d" % i, [128, 512], BF16) for i in range(2)]
    oB = [k.sb("oB%d" % i, [128, 512], BF16) for i in range(2)]
    junk = k.sb("junkf", [128, 512], F32)
    ph = [k.ps("ph%d" % i, [64, 512], F32) for i in range(2)]
    pt = [k.ps("pt%d" % i, [128, 512], F32) for i in range(2)]

    def sin_layer(out, psum, bias, n):
        x = (hf[0][:, 0:n], hf[1])
        ki = (hi[0][:, 0:n], hi[1])
        h_ts(k, out, psum, bias, None, ALU.add)
        h_ts(k, ki, out, 1.0 / TWO_PI, None, ALU.mult)
        h_copy(k, x, ki)
        h_stt(k, out, x, -TWO_PI, out, ALU.mult, ALU.add)
        h_ts(k, out, out, -float(np.pi), float(np.pi), ALU.max, ALU.min)
        h_act(k, out, out, AF.Sin)

    for c in range(nch):
        c0 = c * 512
        n = min(512, L - c0)
        j = c % 2
        k.dma("sync", ft[j][0][:, 0:n], feats_d[:, c0:c0 + n], w=[ft[j][1]])
        k.dma("sync", tv[j][0][:, 0:n], tv_d[:, c0:c0 + n].partition_broadcast(128), w=[tv[j][1]])
        h_mm(k, (ph[0][0][:, 0:n], ph[0][1]), (w1[0][:], w1[1]), (ft[j][0][:, 0:n], ft[j][1]))
        h1 = (hx[0][0][:, 0:n], hx[0][1])
        sin_layer(h1, (ph[0][0][:, 0:n], ph[0][1]), (bb[0][:, 0:1], bb[1]), n)
        h_mm(k, (ph[1][0][:, 0:n], ph[1][1]), (w2[0][:], w2[1]), h1)
        h2 = (hx[1][0][:, 0:n], hx[1][1])
        sin_layer(h2, (ph[1][0][:, 0:n], ph[1][1]), (bb[0][:, 1:2], bb[1]), n)
        h_mm(k, (pt[0][0][:, 0:n], pt[0][1]), (w3[0][:, 0:128], w3[1]), h2)
        h_mm(k, (pt[1][0][:, 0:n], pt[1][1]), (w3[0][:, 128:256], w3[1]), h2)
        w_ = (win[0][:, 0:n], win[1])
        h_act(k, w_, (tv[j][0][:, 0:n], tv[j][1]), AF.Exp, scale=(nd[0][:], nd[1]))
        F_ = (tF[j][0][:, 0:n], tF[j][1])
        B_ = (tB[j][0][:, 0:n], tB[j][1])
        h_stt(k, F_, (pt[0][0][:, 0:n], pt[0][1]), (b3[0][:, 0:1], b3[1]), w_, ALU.add, ALU.mult)
        h_stt(k, B_, (pt[1][0][:, 0:n], pt[1][1]), (b3[0][:, 1:2], b3[1]), w_, ALU.add, ALU.mult)
        lo = 0
        if c == 0:
            h_tt(k, (tF[j][0][:, 0:1], tF[j][1]), (tF[j][0][:, 0:1], tF[j][1]), (tB[j][0][:, 0:1], tB[j][1]), ALU.add)
            lo = 1
        h_act(k, (junk[0][:, 0:n], junk[1]), F_, AF.Square, accum=(part[0][:, 2 * c:2 * c + 1], part[1]))
        h_act(k, (junk[0][:, lo:n], junk[1]), (tB[j][0][:, lo:n], tB[j][1]), AF.Square, accum=(part[0][:, 2 * c + 1:2 * c + 2], part[1]))
        h_copy(k, (oF[j][0][:, 0:n], oF[j][1]), F_)
        h_copy(k, (oB[j][0][:, 0:n], oB[j][1]), B_, eng="gpsimd")
        k.dma("sync", tf_d[:, c0:c0 + n], oF[j][0][:, 0:n], r=[oF[j][1]])
        k.dma("sync", tb_d[:, c0:c0 + n], oB[j][0][:, 0:n], r=[oB[j][1]])
    tot = k.sb("tot", [128, 1], F32)
    k.V(lambda E: E.tensor_reduce(out=tot[0][:], in_=part[0][:], axis=mybir.AxisListType.X, op=ALU.add), [part[1]], [tot[1]])
    k.dma("sync", ssq_d, tot[0][:], r=[tot[1]])
    k.done()
    return nc


def filt_consts(L):
    t_idx = np.arange(L, dtype=np.float32)
    t = (t_idx / np.float32(max(L - 1, 1))).astype(np.float32)
    bands = np.linspace(1e-4, 15, 16, dtype=np.float32)
    ang = (np.float32(2.0 * np.pi) * bands[None, :] * t_idx[:, None] / np.float32(L)).astype(np.float32)
    feats = np.concatenate([t[:, None], np.cos(ang), -np.sin(ang)], -1).astype(np.float32)
    deltas = np.linspace(np.log(1e-2) / 0.3, np.log(1e-2) / 1.5, 512, dtype=np.float32)
    return np.ascontiguousarray(feats.T), t.reshape(1, L).copy(), np.abs(deltas)


def filt_inputs(L, h, prm):
    featsT, tv, adel = filt_consts(L)
    sl = slice(64 * h, 64 * h + 64)
    w3 = prm["hy_w3"]; b3 = prm["hy_b3"]
    cols = []
    for side in range(2):
        for o in range(2):
            cols.append(np.arange(side * 1024 + o * 512 + 64 * h, side * 1024 + o * 512 + 64 * h + 64))
    cols = np.concatenate(cols)
    nd = -np.concatenate([adel[sl], adel[sl]]).reshape(128, 1).astype(np.float32)
    return {"featsT": featsT, "tvals": tv, "w1": prm["hy_w1"], "w2": prm["hy_w2"], "w3c": np.ascontiguousarray(w3[:, cols]),
            "b12": np.stack([prm["hy_b1"], prm["hy_b2"]], 1).astype(np.float32),
            "b3c": np.stack([b3[cols[:128]], b3[cols[128:]]], 1).astype(np.float32), "negdelta": nd}


def toeplitz_src(tapsF, tapsB):
    L = tapsF.shape[1]
    KL = np.zeros((128, 2 * L), tapsF.dtype)
    KL[:, 0:L - 1] = tapsB[:, :0:-1]
    KL[:, L - 1:2 * L - 1] = tapsF
    return KL


def build_hy(nb, T_read=0):
    L = 128 * nb
    W = 128 * (2 * nb - 1)
    nc = bass.Bass("TRN2", target_bir_lowering=False)
    k = KB(nc)
    kl_h = nc.dram_tensor("KL", [128, 2 * L], BF16, kind="ExternalInput")
    ssq_d = k.din("ssqT", [1, 128], F32)
    skip_d = k.din("skipT", [1, 128], F32)
    z_d = k.din("z1", [64, 128, nb], F32)
    x1_d = k.din("x1g", [64, 128, nb], F32)
    x2_d = k.din("x2g", [64, 128, nb], F32)
    sg_d = k.din("sgh", [64, 128, nb], F32)
    J_d = k.din("Jmat", [128, 128], F32)
    y_d = k.dout("yhy", [64, 128, nb], BF16)
    Jf = k.sb("Jf", [128, 128], F32); Jb = k.sb("Jb", [128, 128], BF16)
    nrm = k.sb("nrm", [128, 128], F32); skp = k.sb("skp", [128, 128], F32)
    k.dma("sync", Jf[0][:], J_d, w=[Jf[1]])
    h_copy(k, (Jb[0][:], Jb[1]), (Jf[0][:], Jf[1]))
    k.dma("sync", nrm[0][:], ssq_d.partition_broadcast(128), w=[nrm[1]])
    k.dma("sync", skp[0][:], skip_d.partition_broadcast(128), w=[skp[1]])
    h_act(k, (nrm[0][:], nrm[1]), (nrm[0][:], nrm[1]), AF.Sqrt)
    h_recip(k, (nrm[0][:], nrm[1]), (nrm[0][:], nrm[1]))

    if T_read:
        pp_d = k.din("pp", [64, NPC], F32)
        yf_d = k.din("yf", [64, T_read], F32)
        yb_d = k.din("yb", [64, T_read], F32)
        bo_d = k.din("bonus", [64, T_read], F32)
        sgr_d = k.din("sgr", [64, T_read], F32)
        mr_d = k.dout("mixrw", [64, T_read], BF16)
        pp = k.sb("pp_sb", [64, NPC], F32)
        k.dma("sync", pp[0][:], pp_d, w=[pp[1]])
        o64 = k.sb("o64", [64, 64], F32)
        h_memset(k, (o64[0][:], o64[1]), 1.0 / 64)
        geps = k.sb("geps", [64, 1], F32)
        h_memset(k, (geps[0][:], geps[1]), 64e-5)
        ra = [k.sb("ra%d" % i, [64, 512], F32) for i in range(2)]
        rb = [k.sb("rb%d" % i, [64, 512], F32) for i in range(2)]
        rc = [k.sb("rc%d" % i, [64, 512], F32) for i in range(2)]
        rd = [k.sb("rd%d" % i, [64, 512], F32) for i in range(2)]
        r1 = k.sb("r1", [64, 512], F32); r2 = k.sb("r2", [64, 512], F32)
        rob = [k.sb("rob%d" % i, [64, 512], BF16) for i in range(2)]
        pr = [k.ps("pr%d" % i, [64, 512], F32) for i in range(2)]
        for ci, c0 in enumerate(range(0, T_read, 512)):
            n = min(512, T_read - c0)
            j = ci % 2
            A = (ra[j][0][:, 0:n], ra[j][1]); B = (rb[j][0][:, 0:n], rb[j][1])
            C = (rc[j][0][:, 0:n], rc[j][1]); D = (rd[j][0][:, 0:n], rd[j][1])
            R1 = (r1[0][:, 0:n], r1[1]); R2 = (r2[0][:, 0:n], r2[1])
            P0 = (pr[0][0][:, 0:n], pr[0][1]); P1 = (pr[1][0][:, 0:n], pr[1][1])
            k.dma("scalar", A[0], yf_d[:, c0:c0 + n], w=[A[1]])
            k.dma("scalar", B[0], yb_d[:, c0:c0 + n], w=[B[1]])
            k.dma("scalar", C[0], bo_d[:, c0:c0 + n], w=[C[1]])
            k.dma("scalar", D[0], sgr_d[:, c0:c0 + n], w=[D[1]])
            h_tt(k, A, A, B, ALU.add)
            h_mm(k, P0, (o64[0][:], o64[1]), A)
            h_tt(k, R1, A, P0, ALU.subtract)
            h_tt(k, R2, R1, R1, ALU.mult)
            h_mm(k, P1, (o64[0][:], o64[1]), R2)
            h_act(k, R2, P1, AF.Sqrt, bias=(geps[0][:], geps[1]))
            h_recip(k, R2, R2)
            h_tt(k, R1, R1, R2, ALU.mult)
            h_ts(k, R1, R1, (pp[0][:, PC["gn_w"]:PC["gn_w"] + 1], pp[1]), (pp[0][:, PC["gn_b"]:PC["gn_b"] + 1], pp[1]), ALU.mult, ALU.add)
            h_tt(k, R1, R1, C, ALU.add)
            OB = (rob[j][0][:, 0:n], rob[j][1])
            h_tt(k, OB, R1, D, ALU.mult)
            k.dma("scalar", mr_d[:, c0:c0 + n], OB[0], r=[OB[1]])

    ksh = [k.sb("ksh%d" % i, [128, W], BF16) for i in range(2)]
    zt = [k.sb("zt%d" % i, [128, nb], F32) for i in range(2)]
    x1t = [k.sb("x1t%d" % i, [128, nb], F32) for i in range(2)]
    x2t = [k.sb("x2t%d" % i, [128, nb], F32) for i in range(2)]
    sgt = [k.sb("sgt%d" % i, [128, nb], F32) for i in range(2)]
    zb = k.sb("zb", [128, nb], BF16)
    zf = k.sb("zf", [128, nb], BF16)
    z2 = k.sb("z2", [128, nb], F32)
    t1 = k.sb("t1", [128, nb], F32)
    ot = [k.sb("oth%d" % i, [128, nb], BF16) for i in range(2)]
    pf = k.ps("pf", [128, nb], F32)
    py = [k.ps("pyc%d" % i, [128, nb], F32) for i in range(2)]
    order = [0] + [d for d in range(-(nb - 1), nb) if d != 0]
    cnt = 0
    for c in range(64):
        j = c % 2
        Z = (zt[j][0][:], zt[j][1]); X1 = (x1t[j][0][:], x1t[j][1]); X2 = (x2t[j][0][:], x2t[j][1]); SG = (sgt[j][0][:], sgt[j][1])
        k.dma("gpsimd", Z[0], z_d[c], w=[Z[1]])
        k.dma("gpsimd", X1[0], x1_d[c], w=[X1[1]])
        k.dma("gpsimd", X2[0], x2_d[c], w=[X2[1]])
        k.dma("gpsimd", SG[0], sg_d[c], w=[SG[1]])
        zin = Z
        for o in range(2):
            row = o * 64 + c
            kt, kb_ = ksh[cnt % 2]
            q = "sync" if cnt % 2 == 0 else "scalar"
            cnt += 1
            src = bass.AP(kl_h, row * 2 * L, [[1, 128], [1, W]])
            k.dma(q, kt[:], src, w=[kb_])
            h_copy(k, (zb[0][:], zb[1]), zin, eng="gpsimd")
            h_mm(k, (pf[0][:], pf[1]), (Jb[0][:], Jb[1]), (zb[0][:], zb[1]))
            h_copy(k, (zf[0][:], zf[1]), (pf[0][:], pf[1]), eng="scalar")
            pyt, pyb = py[o]
            for ii, Dd in enumerate(order):
                e = Dd + nb - 1
                S0, S1 = max(0, -Dd), min(nb, nb - Dd)
                k.T(lambda E, pyt=pyt, kt=kt, e=e, S0=S0, S1=S1, Dd=Dd, ii=ii: E.matmul(pyt[:, S0 + Dd:S1 + Dd], kt[:, 128 * e:128 * e + 128], zf[0][:, S0:S1], start=(ii == 0), stop=(ii == len(order) - 1)),
                    [kb_, zf[1]], [pyb])
            ncol = (nrm[0][:, row:row + 1], nrm[1])
            scol = (skp[0][:, row:row + 1], skp[1])
            h_ts(k, (t1[0][:], t1[1]), (pyt[:], pyb), ncol, None, ALU.mult)
            h_stt(k, (t1[0][:], t1[1]), zin, scol, (t1[0][:], t1[1]), ALU.mult, ALU.add)
            if o == 0:
                h_tt(k, (z2[0][:], z2[1]), (t1[0][:], t1[1]), X1, ALU.mult)
                zin = (z2[0][:], z2[1])
            else:
                O_ = (ot[j][0][:], ot[j][1])
                h_tt(k, (t1[0][:], t1[1]), (t1[0][:], t1[1]), X2, ALU.mult)
                h_tt(k, O_, (t1[0][:], t1[1]), SG, ALU.mult, eng="gpsimd")
                k.dma("gpsimd", y_d[c], O_[0], r=[O_[1]])
    k.done()
    return nc


import numpy as np


def rfeat_inputs(u_ctx, u_lat, h, prm):
    n = u_lat.shape[0]
    def fm_pad(c0, w=64):
        a = np.zeros((w, 256 + n + 4), np.float32)
        a[:, 1:257] = u_ctx[:, c0:c0 + w].T
        a[:, 259:259 + n] = u_lat[:, c0:c0 + w].T
        return a
    cols = {"r": 64 * h, "k": 512 + 64 * h, "v": 1024 + 64 * h, "g_rw": 1664 + 64 * h,
            "hv": 2176 + 64 * h, "hx1": 2688 + 64 * h, "hx2": 3200 + 64 * h, "g_hy": 3712 + 64 * h}
    fin = np.stack([fm_pad(cols[nm]) for nm in FI], 0)
    lora = fm_pad(1536, 128)
    mu = prm["rwkv_mu"]
    sl = slice(64 * h, 64 * h + 64)
    pp = np.zeros((64, NPC), np.float32)
    pp[:, PC["mu_r"]] = mu[sl]
    pp[:, PC["mu_k"]] = mu[512 + 64 * h:512 + 64 * h + 64]
    pp[:, PC["mu_v"]] = mu[1024 + 64 * h:1024 + 64 * h + 64]
    pp[:, PC["k_k"]] = prm["rwkv_k_k"][sl]
    pp[:, PC["k_a"]] = prm["rwkv_k_a"][sl]
    pp[:, PC["w0_f"]] = prm["rwkv_w0"][0][sl]
    pp[:, PC["w0_b"]] = prm["rwkv_w0"][1][sl]
    pp[:, PC["a0_f"]] = prm["rwkv_a0"][0][sl]
    pp[:, PC["a0_b"]] = prm["rwkv_a0"][1][sl]
    pp[:, PC["r_k"]] = prm["rwkv_r_k"][h]
    for ai, nm in enumerate(("v", "x1", "x2")):
        for t in range(3):
            pp[:, PC["t%d_%s" % (t, nm)]] = prm["hy_short"][t][512 * ai + 64 * h:512 * ai + 64 * h + 64]
    pp[:, PC["gn_w"]] = prm["rwkv_gn_w"][sl]
    pp[:, PC["gn_b"]] = prm["rwkv_gn_b"][sl]
    pp[:, PC["skip0"]] = prm["hy_skip"][0][sl]
    pp[:, PC["skip1"]] = prm["hy_skip"][1][sl]
    wx = np.zeros((128, 4, 64), np.float32)
    wx[0:32, 0] = prm["rwkv_w_up"][0][:, sl]
    wx[32:64, 1] = prm["rwkv_w_up"][1][:, sl]
    wx[64:96, 2] = prm["rwkv_a_up"][0][:, sl]
    wx[96:128, 3] = prm["rwkv_a_up"][1][:, sl]
    return {"fin": fin, "lora": lora, "pp": pp, "mu_lora": mu[1536:1664].reshape(128, 1).copy(), "wx": wx}


import numpy as np

TC = 16


def h_tr(k, out, a, ident):
    oa, ob = out; aa, ab = a; ia, ib = ident
    return k.P.op("tensor", lambda E: E.transpose(oa, aa, ia), [ab, ib], [ob])


def build_fused(n):
    nlb = n // 128
    NKB = nlb + 2
    NTK = NKB * 128
    T = 256 + n
    TP = T + 4
    nc = bass.Bass("TRN2", target_bir_lowering=False)
    k = KB(nc)
    P = k.P
    plan, variants = na_plan(nlb)
    nvar = len(variants)

    def scratch(name, shape, dt):
        return nc.dram_tensor(name, list(shape), dt).ap()

    x_d = k.din("x", [n, D], F32)
    ctx_d = k.din("ctx", [256, D], F32)
    ccols_d = k.din("c_cols", [128, 16], F32)
    ident_d = k.din("ident", [128, 128], F32)
    J_d = k.din("Jmat", [128, 128], F32)
    fng_d = k.din("final_norm", [1, D], F32)
    L_in = []
    for l in range(4):
        attn = l % 2 == 0
        d = {"norm_g": k.din("norm_g%d" % l, [1, D], F32), "ada_w": k.din("ada_w%d" % l, [D, 3 * D], F32),
             "ada_b": k.din("ada_b%d" % l, [1, 3 * D], F32), "w_in": k.din("w_in%d" % l, [D, 512 if attn else 640], F32),
             "w_out": k.din("w_out%d" % l, [D, D], F32)}
        if attn:
            d["biasT"] = k.din("biasT%d" % l, [128, nvar, 128], F32)
            d["gains"] = k.din("gains%d" % l, [64, 2], F32)
        else:
            d["pp"] = k.din("pp%d" % l, [64, NPC], F32)
            d["mu_lora"] = k.din("mu_lora%d" % l, [128, 1], F32)
            d["wx"] = k.din("wx%d" % l, [128, 4, 64], F32)
            d["hw1"] = k.din("hw1_%d" % l, [33, 64], F32)
            d["hw2"] = k.din("hw2_%d" % l, [64, 64], F32)
            d["hw3"] = k.din("hw3_%d" % l, [64, 256], F32)
            d["hb12"] = k.din("hb12_%d" % l, [64, 2], F32)
            d["hb3"] = k.din("hb3_%d" % l, [128, 2], F32)
            d["skipT"] = k.din("skipT%d" % l, [1, 128], F32)
        L_in.append(d)
    ropeC_d = k.din("ropeC", [64, NTK], F32)
    ropeS_d = k.din("ropeS", [64, NTK], F32)
    rmat_d = k.din("rmatT", [64, 64], F32)
    sel_d = k.din("sel", [65, 64], F32)
    fconst = {}
    for Lf in (n, 256):
        fconst[Lf] = {"featsT": k.din("featsT%d" % Lf, [33, Lf], F32), "featsTr": k.din("featsTr%d" % Lf, [33, Lf], F32),
                      "tv": k.din("tv%d" % Lf, [1, Lf], F32), "tvr": k.din("tvr%d" % Lf, [1, Lf], F32)}
    nd_d = k.din("negdelta", [128, 1], F32)
    sel2_d = k.din("sel2", [2, 128], F32)
    y_out = k.dout("y_out", [n, D], F32)

    xres = scratch("xres", [NTK, D], F32)
    gates_s = scratch("gates_s", [128, 2, D], F32)
    mix_send = scratch("mix_send", [128, NTK], BF16)
    mix_all = scratch("mix_all", [1024, NTK], BF16)
    A_s = {nm: scratch("a_" + nm, [64, NTK], BF16) for nm in ("qaT", "kaT", "gaT", "qbT", "kbT", "gbT")}
    va_s = scratch("va_s", [128, NKB, 65], BF16)
    vb_s = scratch("vb_s", [128, NKB, 65], BF16)
    fin_s = scratch("fin_s", [NFI, 64, TP], F32)
    lora_s = scratch("lora_s", [128, TP], F32)
    fo_s = scratch("fo_s", [NFO, 64, T], F32)
    bc_s = scratch("bc_s", [2, T, 322], F32)
    vT2_s = scratch("vT2_s", [128, T], F32)
    yT2_s = scratch("yT2_s", [128, T], F32)
    kl_h = {Lf: nc.dram_tensor("KL%d" % Lf, [128, 2 * Lf], BF16) for Lf in (n, 256)}
    ssq_s = {Lf: scratch("ssq%d" % Lf, [1, 128], F32) for Lf in (n, 256)}

    identf = k.sb("identf", [128, 128], F32)
    identb = k.sb("identb", [128, 128], BF16)
    Jf = k.sb("Jf", [128, 128], F32)
    Jb = k.sb("Jb", [128, 128], BF16)
    zer = k.sb("zer", [128, 8], F32)
    k.dma("sync", identf[0][:], ident_d, w=[identf[1]])
    k.dma("sync", Jf[0][:], J_d, w=[Jf[1]])
    h_copy(k, (identb[0][:], identb[1]), (identf[0][:], identf[1]))
    h_copy(k, (Jb[0][:], Jb[1]), (Jf[0][:], Jf[1]))
    h_memset(k, (zer[0][:], zer[1]), 0.0)
    ID = (identf[0][:], identf[1])
    IDB = (identb[0][:], identb[1])
    for col in (0, 257, 258, TP - 1):
        for i in range(NFI):
            k.dma("sync", fin_s[i, :, col:col + 1], zer[0][0:64, 0:1], r=[zer[1]], allow_slow_non_contiguous=True)
        k.dma("sync", lora_s[:, col:col + 1], zer[0][:, 0:1], r=[zer[1]], allow_slow_non_contiguous=True)

    def phase_tok(l, final=False):
        has_prev = l > 0
        attn = (l % 2 == 0) and not final
        k.begin_phase()
        eps_t = k.sb("eps_t", [128, 1], F32)
        h_memset(k, (eps_t[0][:], eps_t[1]), EPS)
        if has_prev:
            wo = k.sb("wo", [128, 8, D], BF16)
            for kc in range(8):
                k.dma("gpsimd", wo[0][:, kc, :], L_in[l - 1]["w_out"][kc * 128:(kc + 1) * 128, :], w=[wo[1]])
            gp = k.sb("gp", [128, 2, D], F32)
            k.dma("sync", gp[0][:], gates_s, w=[gp[1]])
        if final:
            fg = k.sb("fg", [128, D], F32)
            k.dma("sync", fg[0][:], fng_d.partition_broadcast(128), w=[fg[1]])
        else:
            Li = L_in[l]
            ncol = 512 if attn else 640
            wi = k.sb("wi", [128, 8, ncol], BF16)
            for kc in range(8):
                k.dma("gpsimd", wi[0][:, kc, :], Li["w_in"][kc * 128:(kc + 1) * 128, :], w=[wi[1]])
            mod = k.sb("mod", [128, 2, 3 * D], F32)
            Gt = k.sb("Gt", [128, 2, D], F32)
            k.begin_phase()
            cc = k.sb("cc", [128, 16], F32)
            rep = k.sb("rep", [128, 16, 128], F32)
            ones = k.sb("ones", [128, 128], F32)
            ab = k.sb("ab", [128, 3 * D], F32)
            ng = k.sb("ng", [128, D], F32)
            aw = [k.sb("aw%d" % i, [128, 8, 512], F32) for i in range(2)]
            pa = [k.ps("pa%d" % i, [128, 512], F32) for i in range(2)]
            k.dma("sync", cc[0][:], ccols_d, w=[cc[1]])
            k.dma("sync", ab[0][:], Li["ada_b"].partition_broadcast(128), w=[ab[1]])
            k.dma("sync", ng[0][:], Li["norm_g"].partition_broadcast(128), w=[ng[1]])
            h_act(k, (cc[0][:], cc[1]), (cc[0][:], cc[1]), AF.Silu)
            h_memset(k, (ones[0][:], ones[1]), 1.0)
            for j in range(16):
                h_ts(k, (rep[0][:, j, :], rep[1]), (ones[0][:], ones[1]), (cc[0][:, j:j + 1], cc[1]), None, ALU.mult)
            for g in range(6):
                awt, awb = aw[g % 2]
                k.dma("sync", awt[:], Li["ada_w"][:, g * 512:(g + 1) * 512].rearrange("(k p) n -> p k n", p=128), w=[awb])
                for v in range(2):
                    for kc in range(8):
                        h_mm(k, (pa[v][0][:], pa[v][1]), (rep[0][:, v * 8 + kc, :], rep[1]), (awt[:, kc, :], awb), start=(kc == 0), stop=(kc == 7))
                    h_tt(k, (mod[0][:, v, g * 512:(g + 1) * 512], mod[1]), (pa[v][0][:], pa[v][1]), (ab[0][:, g * 512:(g + 1) * 512], ab[1]), ALU.add)
            for v in range(2):
                h_stt(k, (Gt[0][:, v, :], Gt[1]), (mod[0][:, v, D:2 * D], mod[1]), 1.0, (ng[0][:], ng[1]), ALU.add, ALU.mult)
            k.end_phase()
        xt = [k.sb("xt%d" % i, [128, D], F32) for i in range(2)]
        tmp = [k.sb("tmp%d" % i, [128, D], F32) for i in range(2)]
        ss = [k.sb("ss%d" % i, [128, 4], F32) for i in range(2)]
        junk = k.sb("junk", [128, D], BF16)
        if has_prev:
            mt = [k.sb("mt%d" % i, [128, 8, 128], BF16) for i in range(2)]
            py = [k.ps("py%d" % i, [128, 512], F32) for i in range(2)]
        if not final:
            xm = [k.sb("xm%d" % i, [128, D], BF16) for i in range(2)]
            xmT = [k.sb("xmT%d" % i, [128, 8, 512], BF16) for i in range(2)]
            ptr = [k.ps("ptr%d" % i, [128, 8, 128], BF16) for i in range(2)]
            pu = [k.ps("pu%d" % i, [128, 512], F32) for i in range(2)]
            odt = BF16 if attn else F32
            ut = [k.sb("ut%d" % i, [128, 512], odt) for i in range(3)]
            if attn:
                vt = [k.sb("vt%d" % i, [128, 2, 65], BF16) for i in range(2)]
                for i in range(2):
                    h_memset(k, (vt[i][0][:], vt[i][1]), 1.0)
        bcount = 0
        ucount = 0
        nsb = (NTK + 511) // 512
        for sbi in range(nsb):
            t0 = sbi * 512
            ntok = min(512, NTK - t0)
            nblk = ntok // 128
            is_ctx = t0 >= n
            v = 1 if is_ctx else 0
            if not final:
                xTt, xTb = xmT[sbi % 2]
            for blk in range(nblk):
                b = t0 // 128 + blk
                X = (xt[bcount % 2][0][:], xt[bcount % 2][1])
                TT = (tmp[bcount % 2][0][:], tmp[bcount % 2][1])
                sst, ssb = ss[bcount % 2]
                if l <= 1:
                    src = ctx_d[(b - nlb) * 128:(b - nlb + 1) * 128, :] if is_ctx else x_d[b * 128:(b + 1) * 128, :]
                else:
                    src = xres[b * 128:(b + 1) * 128, :]
                k.dma("sync", X[0], src, w=[X[1]])
                if has_prev:
                    mtt, mtb = mt[bcount % 2]
                    k.dma("sync", mtt[:], mix_all[:, b * 128:(b + 1) * 128].rearrange("(k p) t -> p k t", p=128), w=[mtb])
                    for hh in range(2):
                        for kc in range(8):
                            h_mm(k, (py[hh][0][:], py[hh][1]), (mtt[:, kc, :], mtb), (wo[0][:, kc, hh * 512:(hh + 1) * 512], wo[1]), start=(kc == 0), stop=(kc == 7))
                        h_tt(k, (tmp[bcount % 2][0][:, hh * 512:(hh + 1) * 512], TT[1]), (py[hh][0][:], py[hh][1]), (gp[0][:, v, hh * 512:(hh + 1) * 512], gp[1]), ALU.mult)
                    h_tt(k, X, TT, X, ALU.add, eng="gpsimd")
                    if not final:
                        k.dma("sync", xres[b * 128:(b + 1) * 128, :], X[0], r=[X[1]])
                h_act(k, (junk[0][:], junk[1]), X, AF.Square, accum=(sst[:, 0:1], ssb))
                h_act(k, (sst[:, 1:2], ssb), (sst[:, 0:1], ssb), AF.Sqrt, scale=1.0 / D, bias=(eps_t[0][:], eps_t[1]))
                h_recip(k, (sst[:, 2:3], ssb), (sst[:, 1:2], ssb))
                if final:
                    if not is_ctx:
                        h_stt(k, TT, X, (sst[:, 2:3], ssb), (fg[0][:], fg[1]), ALU.mult, ALU.mult)
                        k.dma("sync", y_out[b * 128:(b + 1) * 128, :], TT[0], r=[TT[1]])
                    bcount += 1
                    continue
                XM = (xm[bcount % 2][0][:], xm[bcount % 2][1])
                h_stt(k, TT, X, (sst[:, 2:3], ssb), (Gt[0][:, v, :], Gt[1]), ALU.mult, ALU.mult)
                h_tt(k, XM, TT, (mod[0][:, v, 0:D], mod[1]), ALU.add, eng="gpsimd")
                ptt, ptb = ptr[bcount % 2]
                for kc in range(8):
                    h_tr(k, (ptt[:, kc, :], ptb), (xm[bcount % 2][0][:, kc * 128:(kc + 1) * 128], XM[1]), IDB)
                h_copy(k, (xTt[:, :, blk * 128:(blk + 1) * 128], xTb), (ptt[:], ptb), eng="scalar")
                if attn:
                    put, pub = pu[ucount % 2]
                    vtt, vtb = vt[ucount % 2]
                    ucount += 1
                    for kc in range(8):
                        h_mm(k, (put[:, 0:128], pub), (xTt[:, kc, blk * 128:(blk + 1) * 128], xTb), (wi[0][:, kc, 384:512], wi[1]), start=(kc == 0), stop=(kc == 7))
                    h_copy(k, (vtt[:, :, 0:64], vtb), (put[:, 0:128].rearrange("p (a c) -> p a c", a=2), pub))
                    k.dma("sync", va_s[:, b, :], vtt[:, 0, :], r=[vtb])
                    k.dma("sync", vb_s[:, b, :], vtt[:, 1, :], r=[vtb])
                bcount += 1
            if final:
                continue
            ncg = 3 if attn else 5
            for cg in range(ncg):
                put, pub = pu[ucount % 2]
                utt, utb = ut[ucount % 3]
                ucount += 1
                for kc in range(8):
                    h_mm(k, (put[:, 0:ntok], pub), (wi[0][:, kc, cg * 128:(cg + 1) * 128], wi[1]), (xTt[:, kc, 0:ntok], xTb), start=(kc == 0), stop=(kc == 7))
                h_copy(k, (utt[:, 0:ntok], utb), (put[:, 0:ntok], pub), eng=("vector" if cg % 2 == 0 else "scalar"))
                if attn:
                    names = [("qaT", "kaT"), ("gaT", "qbT"), ("kbT", "gbT")][cg]
                    k.dma("sync", A_s[names[0]][:, t0:t0 + ntok], utt[0:64, 0:ntok], r=[utb])
                    k.dma("sync", A_s[names[1]][:, t0:t0 + ntok], utt[64:128, 0:ntok], r=[utb])
                else:
                    pc = (1 + (t0 - n)) if is_ctx else (259 + t0)
                    if cg < 4:
                        k.dma("sync", fin_s[2 * cg, :, pc:pc + ntok], utt[0:64, 0:ntok], r=[utb])
                        k.dma("sync", fin_s[2 * cg + 1, :, pc:pc + ntok], utt[64:128, 0:ntok], r=[utb])
                    else:
                        k.dma("sync", lora_s[:, pc:pc + ntok], utt[:, 0:ntok], r=[utb])
        if not final:
            k.dma("sync", gates_s, mod[0][:, :, 2 * D:3 * D], r=[mod[1]])
        k.end_phase()

    def phase_attn(l):
        Li = L_in[l]
        k.begin_phase()
        qT = k.sb("qT", [64, NTK], BF16); kT = k.sb("kT", [64, NTK], BF16)
        vE = k.sb("vE", [128, NKB, 65], BF16)
        biasf = k.sb("biasf", [128, nvar, 128], F32); biasb = k.sb("biasb", [128, nvar, 128], BF16)
        sel = k.sb("sel_sb", [65, 64], F32); gains = k.sb("gains_sb", [64, 2], F32)
        rmf = k.sb("rmf", [64, 64], F32); rmb = k.sb("rmb", [64, 64], BF16)
        ones64 = k.sb("ones64", [64, 64], F32); eps_t = k.sb("eps_a", [64, 1], F32)
        k.dma("sync", biasf[0][:], Li["biasT"], w=[biasf[1]])
        k.dma("sync", sel[0][:], sel_d, w=[sel[1]])
        k.dma("sync", gains[0][:], Li["gains"], w=[gains[1]])
        k.dma("sync", rmf[0][:], rmat_d, w=[rmf[1]])
        h_copy(k, (biasb[0][:], biasb[1]), (biasf[0][:], biasf[1]))
        h_copy(k, (rmb[0][:], rmb[1]), (rmf[0][:], rmf[1]))
        h_memset(k, (ones64[0][:], ones64[1]), 1.0 / 64)
        h_memset(k, (eps_t[0][:], eps_t[1]), EPS)
        h_ts(k, (gains[0][:, 0:1], gains[1]), (gains[0][:, 0:1], gains[1]), 0.125, None, ALU.mult)
        pS = [k.ps("pS%d" % i, [128, 512], F32) for i in range(4)]
        pacc = [k.ps("pacc%d" % i, [65, 512], F32) for i in range(2)]
        pmisc = [k.ps("pm%d" % i, [64, 512], F32) for i in range(2)]
        PT = [k.sb("PT%d" % i, [128, 512], BF16) for i in range(4)]
        accs = [k.sb("accs%d" % i, [65, 512], F32) for i in range(2)]
        gt = [k.sb("gt%d" % i, [64, 512], BF16) for i in range(2)]
        w1 = [k.sb("w1_%d" % i, [64, 512], F32) for i in range(2)]
        w2 = [k.sb("w2_%d" % i, [64, 512], F32) for i in range(2)]
        ot = [k.sb("ot%d" % i, [64, 512], BF16) for i in range(2)]
        cin = [k.sb("cin%d" % i, [64, 512], BF16) for i in range(2)]
        ctab = [k.sb("ctab%d" % i, [64, 512], F32) for i in range(2)]
        stab = [k.sb("stab%d" % i, [64, 512], F32) for i in range(2)]
        qnb = [k.sb("qnb%d" % i, [64, 512], BF16) for i in range(2)]
        st = {"fin": 0, "S": 0, "pre": 0}

        def finalize(pa, g_d, row0, c0, nn):
            i = st["fin"] % 2
            st["fin"] += 1
            at = (accs[i][0][:, 0:nn], accs[i][1]); G = (gt[i][0][:, 0:nn], gt[i][1])
            a1 = (w1[i][0][:, 0:nn], w1[i][1]); a2 = (w2[i][0][:, 0:nn], w2[i][1])
            o = (ot[i][0][:, 0:nn], ot[i][1]); pm = (pmisc[i][0][:, 0:nn], pmisc[i][1])
            k.dma("sync", G[0], g_d[:, c0:c0 + nn], w=[G[1]])
            h_copy(k, at, (pa[0][:, 0:nn], pa[1]), eng="scalar")
            h_mm(k, pm, (sel[0][:], sel[1]), at)
            h_recip(k, a1, pm)
            h_tt(k, a1, a1, (accs[i][0][0:64, 0:nn], accs[i][1]), ALU.mult)
            h_silu(k, a2, G, a2)
            h_tt(k, o, a1, a2, ALU.mult)
            k.dma("sync", mix_send[row0:row0 + 64, c0:c0 + nn], o[0], r=[o[1]])

        def attend(qc0, nn, kbs, pa):
            pend = []
            nk = len(kbs)

            def pv(j, kbj, pj):
                h_mm(k, (pa[0][:, 0:nn], pa[1]), (vE[0][:, kbj, :], vE[1]), pj, start=(j == 0), stop=(j == nk - 1))
            for idx, kb in enumerate(kbs):
                s = st["S"]
                st["S"] += 1
                psx = (pS[s % 4][0][:, 0:nn], pS[s % 4][1])
                ptx = (PT[s % 4][0][:, 0:nn], PT[s % 4][1])
                h_mm(k, psx, (kT[0][:, kb * 128:(kb + 1) * 128], kT[1]), (qT[0][:, qc0:qc0 + nn], qT[1]))
                h_act(k, ptx, psx, AF.Exp)
                pend.append((idx, kb, ptx))
                if len(pend) > 2:
                    pv(*pend.pop(0))
            for it in pend:
                pv(*it)

        k.dma("sync", kT[0][:], A_s["kaT"], w=[kT[1]])
        k.dma("sync", vE[0][:], va_s, w=[vE[1]])
        k.dma("sync", qT[0][:], A_s["qaT"], w=[qT[1]])
        h_ts(k, (qT[0][:], qT[1]), (qT[0][:], qT[1]), 0.125, None, ALU.mult)
        ctxk = [nlb, nlb + 1]
        acc_i = 0
        for g0 in range(0, nlb, 4):
            pa = pacc[acc_i % 2]
            acc_i += 1
            nq = min(4, nlb - g0)
            for mi in range(nq):
                m = g0 + mi
                lst = plan[m]
                nreg = len(lst) + 2
                s = st["S"]
                st["S"] += 2
                banks = [(pS[s % 4], PT[s % 4]), (pS[(s + 1) % 4], PT[(s + 1) % 4])]
                regs = []
                for j, (kb, var) in enumerate(lst + [(ctxk[0], None), (ctxk[1], None)]):
                    (pt_, pb_), (tt_, tb_) = banks[j // 4]
                    jj = j % 4
                    h_mm(k, (pt_[:, jj * 128:(jj + 1) * 128], pb_), (kT[0][:, kb * 128:(kb + 1) * 128], kT[1]), (qT[0][:, m * 128:(m + 1) * 128], qT[1]), start=True, stop=(var is None))
                    if var is not None:
                        h_mm(k, (pt_[:, jj * 128:(jj + 1) * 128], pb_), IDB, (biasb[0][:, var, :], biasb[1]), start=False, stop=True)
                    regs.append((kb, tt_, tb_, jj))
                nA = min(4, nreg)
                h_act(k, (banks[0][1][0][:, 0:nA * 128], banks[0][1][1]), (banks[0][0][0][:, 0:nA * 128], banks[0][0][1]), AF.Exp)
                if nreg > 4:
                    nB = nreg - 4
                    h_act(k, (banks[1][1][0][:, 0:nB * 128], banks[1][1][1]), (banks[1][0][0][:, 0:nB * 128], banks[1][0][1]), AF.Exp)
                for j, (kb, tt_, tb_, jj) in enumerate(regs):
                    h_mm(k, (pa[0][:, mi * 128:(mi + 1) * 128], pa[1]), (vE[0][:, kb, :], vE[1]), (tt_[:, jj * 128:(jj + 1) * 128], tb_), start=(j == 0), stop=(j == nreg - 1))
            finalize(pa, A_s["gaT"], 0, g0 * 128, nq * 128)
        pa = pacc[acc_i % 2]
        acc_i += 1
        attend(nlb * 128, 256, ctxk, pa)
        finalize(pa, A_s["gaT"], 0, nlb * 128, 256)
        k.dma("sync", vE[0][:], vb_s, w=[vE[1]])

        def pre_chunk(src_d, dst, gcol, c0):
            nn = min(512, NTK - c0)
            i = st["pre"] % 2
            st["pre"] += 1
            ci = (cin[i][0][:, 0:nn], cin[i][1]); ct = (ctab[i][0][:, 0:nn], ctab[i][1]); sn = (stab[i][0][:, 0:nn], stab[i][1])
            qn = (qnb[i][0][:, 0:nn], qnb[i][1]); a1 = (w1[i][0][:, 0:nn], w1[i][1]); a2 = (w2[i][0][:, 0:nn], w2[i][1])
            pm = (pmisc[i][0][:, 0:nn], pmisc[i][1])
            k.dma("sync", ci[0], src_d[:, c0:c0 + nn], w=[ci[1]])
            k.dma("sync", ct[0], ropeC_d[:, c0:c0 + nn], w=[ct[1]])
            k.dma("sync", sn[0], ropeS_d[:, c0:c0 + nn], w=[sn[1]])
            h_tt(k, a1, ci, ci, ALU.mult)
            h_mm(k, pm, (ones64[0][:], ones64[1]), a1)
            h_act(k, a2, pm, AF.Sqrt, bias=(eps_t[0][:], eps_t[1]))
            h_recip(k, a2, a2)
            h_stt(k, qn, ci, (gains[0][:, gcol:gcol + 1], gains[1]), a2, ALU.mult, ALU.mult)
            h_mm(k, pm, (rmb[0][:], rmb[1]), qn)
            h_tt(k, a1, qn, ct, ALU.mult)
            h_tt(k, a2, pm, sn, ALU.mult)
            h_tt(k, (dst[0][:, c0:c0 + nn], dst[1]), a1, a2, ALU.add, eng="gpsimd")
        for c0 in range(0, NTK, 512):
            pre_chunk(A_s["kbT"], kT, 1, c0)
        for c0 in range(0, NTK, 512):
            pre_chunk(A_s["qbT"], qT, 0, c0)
        allk = list(range(NKB))
        for c0 in range(0, nlb * 128, 512):
            nn = min(512, nlb * 128 - c0)
            pa = pacc[acc_i % 2]
            acc_i += 1
            attend(c0, nn, allk, pa)
            finalize(pa, A_s["gbT"], 64, c0, nn)
        pa = pacc[acc_i % 2]
        acc_i += 1
        attend(nlb * 128, 256, ctxk, pa)
        finalize(pa, A_s["gbT"], 64, nlb * 128, 256)
        k.end_phase()

    def phase_rfeat(l):
        Li = L_in[l]
        k.begin_phase()
        pp = k.sb("pp_sb", [64, NPC], F32); npp = k.sb("npp_sb", [64, NPC], F32)
        mul = k.sb("mul_sb", [128, 1], F32); wx = k.sb("wx_sb", [128, 4, 64], F32)
        ones = k.sb("ones_sb", [64, 64], F32); tiny = k.sb("tiny_sb", [64, 1], F32)
        k.dma("sync", pp[0][:], Li["pp"], w=[pp[1]])
        k.dma("sync", mul[0][:], Li["mu_lora"], w=[mul[1]])
        k.dma("sync", wx[0][:], Li["wx"], w=[wx[1]])
        h_memset(k, (ones[0][:], ones[1]), 1.0)
        h_memset(k, (tiny[0][:], tiny[1]), 1e-12)
        h_ts(k, (npp[0][:], npp[1]), (pp[0][:], pp[1]), -1.0, None, ALU.mult)
        h_ts(k, (npp[0][:, PC["r_k"]:PC["r_k"] + 1], npp[1]), (pp[0][:, PC["r_k"]:PC["r_k"] + 1], pp[1]), 0.5, None, ALU.mult)

        def pcol(name):
            return (pp[0][:, PC[name]:PC[name] + 1], pp[1])

        def ncol(name):
            return (npp[0][:, PC[name]:PC[name] + 1], npp[1])
        fin = [[k.sb("fin%d_%d" % (i, j), [64, 514], F32) for j in range(2)] for i in range(NFI)]
        lor = [k.sb("lor%d" % j, [128, 514], F32) for j in range(2)]
        fo = [[k.sb("fo%d_%d" % (i, j), [64, 512], F32) for j in range(2)] for i in range(NFO)]
        tm = [k.sb("tm%d" % i, [64, 512], F32) for i in range(8)]
        lt = [k.sb("lt%d" % i, [128, 512], F32) for i in range(3)]
        pz = [k.ps("pz%d" % i, [64, 512], F32) for i in range(4)]
        ptk = [k.ps("ptk%d" % i, [128, 16, 64], F32) for i in range(1)]
        pfl = k.ps("pfl", [128, 512], F32)
        tokx = [k.sb("tokx%d" % i, [128, 708], F32) for i in range(2)]
        tokb = [k.sb("tokb%d" % i, [128, 322], F32) for i in range(2)]
        vrev = [k.sb("vrev%d" % i, [64, 128], F32) for i in range(2)]
        wrt = [[k.sb("wrt%d_%d" % (d, i), [64, 512], F32) for i in range(2)] for d in range(2)]
        djunk = k.sb("djunk", [128, 64], F32)
        tord = ["w_f", "nb_f", "k_f", "wr_f", "kkn", "w_b", "nb_b", "k_b", "wr_b", "r", "v"]
        blkc = [0]

        def chunk(ci, pc0, oc0, nn):
            j = ci % 2

            def I(name, lo=1):
                t, b = fin[FI[name]][j]
                return (t[:, lo:lo + nn], b)

            def O(name):
                t, b = fo[FO[name]][j]
                return (t[:, 0:nn], b)

            def Tm(i):
                return (tm[i][0][:, 0:nn], tm[i][1])

            def Lt(i):
                return (lt[i][0][:, 0:nn], lt[i][1])

            def Pz(i):
                return (pz[i][0][:, 0:nn], pz[i][1])
            for name, i in FI.items():
                t, b = fin[i][j]
                k.dma("sync", t[:, 0:nn + 2], fin_s[i, :, pc0:pc0 + nn + 2], w=[b])
            lt_, lb_ = lor[j]
            k.dma("sync", lt_[:, 0:nn + 2], lora_s[:, pc0:pc0 + nn + 2], w=[lb_])

            def shift(out, lo, mid, hi_, mu, t):
                h_tt(k, t, lo, hi_, ALU.add)
                h_stt(k, t, t, 0.5, mid, ALU.mult, ALU.subtract)
                h_stt(k, out, t, mu, mid, ALU.mult, ALU.add)
            rs, ks, vs = O("r"), Tm(0), O("v")
            shift(rs, I("r", 0), I("r", 1), I("r", 2), pcol("mu_r"), Tm(7))
            shift(ks, I("k", 0), I("k", 1), I("k", 2), pcol("mu_k"), Tm(7))
            shift(vs, I("v", 0), I("v", 1), I("v", 2), pcol("mu_v"), Tm(7))
            ls = Lt(0)
            shift(ls, (lt_[:, 0:nn], lb_), (lt_[:, 1:nn + 1], lb_), (lt_[:, 2:nn + 2], lb_), (mul[0][:], mul[1]), Lt(2))
            lth = Lt(1)
            h_act(k, lth, ls, AF.Tanh)
            kk = Tm(1)
            h_ts(k, kk, ks, pcol("k_k"), None, ALU.mult)
            h_tt(k, Tm(2), kk, kk, ALU.mult)
            h_mm(k, Pz(0), (ones[0][:], ones[1]), Tm(2))
            h_act(k, Tm(2), Pz(0), AF.Sqrt, bias=(tiny[0][:], tiny[1]))
            h_recip(k, Tm(2), Tm(2))
            h_tt(k, O("kkn"), kk, Tm(2), ALU.mult)
            for d, sfx in enumerate(("f", "b")):
                h_mm(k, Pz(1), (wx[0][:, d, :], wx[1]), lth)
                h_mm(k, Pz(2), (wx[0][:, 2 + d, :], wx[1]), ls)
                h_sigmoid(k, Tm(3), Pz(1), Tm(3), bias=ncol("w0_" + sfx))
                h_act(k, O("w_" + sfx), Tm(3), AF.Exp, scale=-float(np.exp(-0.5)))
                a = Tm(4)
                h_sigmoid(k, a, Pz(2), a, bias=ncol("a0_" + sfx))
                h_ts(k, Tm(5), a, -1.0, pcol("k_a"), ALU.add, ALU.mult)
                h_stt(k, O("k_" + sfx), Tm(5), 1.0, ks, ALU.add, ALU.mult)
                h_stt(k, O("nb_" + sfx), O("kkn"), -1.0, a, ALU.mult, ALU.mult)
            h_tt(k, Tm(5), O("k_f"), O("k_b"), ALU.add)
            h_stt(k, Tm(5), rs, ncol("r_k"), Tm(5), ALU.mult, ALU.mult)
            h_mm(k, Pz(3), (ones[0][:], ones[1]), Tm(5))
            h_tt(k, O("bonus"), Pz(3), vs, ALU.mult)
            h_silu(k, O("sg_rw"), I("g_rw", 1), Tm(6))
            h_silu(k, O("sg_hy"), I("g_hy", 1), Tm(6))
            for nm in ("v", "x1", "x2"):
                src = "h" + nm
                h_ts(k, Tm(6), I(src, 0), pcol("t0_" + nm), None, ALU.mult)
                h_stt(k, Tm(6), I(src, 1), pcol("t1_" + nm), Tm(6), ALU.mult, ALU.add)
                h_stt(k, O(src), I(src, 2), pcol("t2_" + nm), Tm(6), ALU.mult, ALU.add)
            for name in ("bonus", "sg_rw", "hv", "hx1", "hx2", "sg_hy"):
                t, b = fo[FO[name]][j]
                k.dma("sync", fo_s[FO[name], :, oc0:oc0 + nn], t[:, 0:nn], r=[b])
            k.dma("sync", vT2_s[0:64, oc0:oc0 + nn], fo[FO["v"]][j][0][:, 0:nn], r=[fo[FO["v"]][j][1]])
            h_tt(k, (wrt[0][j][0][:, 0:nn], wrt[0][j][1]), O("w_f"), rs, ALU.mult)
            h_tt(k, (wrt[1][j][0][:, 0:nn], wrt[1][j][1]), O("w_b"), rs, ALU.mult)
            for bi in range(nn // 128):
                tau0 = oc0 + bi * 128
                s_lo = (128 - tau0) if tau0 < 256 else (256 + n - 128 - (tau0 - 256))
                q = blkc[0] % 2
                blkc[0] += 1
                pk, pkb = ptk[0]
                for ai, nm in enumerate(tord):
                    if nm.startswith("wr_"):
                        t, b = wrt[0 if nm == "wr_f" else 1][j]
                    else:
                        t, b = fo[FO[nm]][j]
                    h_tr(k, (pk[:, ai, :], pkb), (t[:, bi * 128:(bi + 1) * 128], b), (identf[0][0:64, 0:64], identf[1]))
                tx, txb = tokx[q]
                h_copy(k, (tx[:, 0:320].rearrange("p (a c) -> p a c", a=5), txb), (pk[:, 0:5, :], pkb), eng="scalar")
                h_copy(k, (tx[:, 322:514].rearrange("p (a c) -> p a c", a=3), txb), (pk[:, 5:8, :], pkb))
                h_copy(k, (tx[:, 514:578], txb), (pk[:, 8, :], pkb), eng="scalar")
                h_copy(k, (tx[:, 580:708].rearrange("p (a c) -> p a c", a=2), txb), (pk[:, 9:11, :], pkb))
                R_ = (tx[:, 580:644], txb)
                dj = (djunk[0][:], djunk[1])
                h_stt(k, dj, (tx[:, 64:128], txb), 1.0, R_, ALU.mult, ALU.mult, accum=(tx[:, 320:321], txb))
                h_stt(k, dj, (tx[:, 128:192], txb), 1.0, R_, ALU.mult, ALU.mult, accum=(tx[:, 321:322], txb))
                h_stt(k, dj, (tx[:, 386:450], txb), 1.0, R_, ALU.mult, ALU.mult, accum=(tx[:, 578:579], txb))
                h_stt(k, dj, (tx[:, 450:514], txb), 1.0, R_, ALU.mult, ALU.mult, accum=(tx[:, 579:580], txb))
                k.dma("gpsimd", bc_s[0, tau0:tau0 + 128, :], tx[:, 0:322], r=[txb])
                h_mm(k, (pfl[0][:, 0:256], pfl[1]), (Jf[0][:], Jf[1]), (tx[:, 322:578], txb))
                h_mm(k, (pfl[0][:, 256:320], pfl[1]), (Jf[0][:], Jf[1]), (tx[:, 256:320], txb))
                h_mm(k, (pfl[0][:, 320:322], pfl[1]), (Jf[0][:], Jf[1]), (tx[:, 578:580], txb))
                h_copy(k, (tokb[q][0][:], tokb[q][1]), (pfl[0][:, 0:322], pfl[1]), eng="scalar")
                k.dma("gpsimd", bc_s[1, s_lo:s_lo + 128, :], tokb[q][0][:], r=[tokb[q][1]])
                h_mm(k, (pfl[0][0:64, 384:512], pfl[1]), (tx[:, 644:708], txb), (Jf[0][:], Jf[1]))
                h_copy(k, (vrev[q][0][:], vrev[q][1]), (pfl[0][0:64, 384:512], pfl[1]))
                k.dma("gpsimd", vT2_s[64:128, s_lo:s_lo + 128], vrev[q][0][:], r=[vrev[q][1]])
        for ci, (pc0, oc0, nn) in enumerate(seq_chunks(n)):
            chunk(ci, pc0, oc0, nn)
        k.end_phase()

    def phase_scan():
        k.begin_phase()
        S, S_b = k.sb("S", [128, 64], F32)
        prod, _ = k.sb("sprod", [128, 2, 64], F32)
        sel2 = k.sb("sel2", [2, 128], F32)
        k.dma("sync", sel2[0][:], sel2_d, w=[sel2[1]])
        NBUF = 4
        NPB = 6
        stg = [k.sb("stg%d" % i, [2, TC, 322], F32) for i in range(NBUF)]
        red = [k.sb("red%d" % i, [128, TC, 2], F32) for i in range(NBUF)]
        abt = [k.sb("abt%d" % i, [128, TC, 2], F32) for i in range(NBUF)]
        p16 = [k.sb("p16_%d" % i, [128, TC], F32) for i in range(2)]
        vt = [k.sb("svt%d" % i, [128, 512], F32) for i in range(2)]
        yt = [k.sb("syt%d" % i, [128, 512], F32) for i in range(2)]
        pb = [k.ps("pb%d" % i, [128, 322], F32) for i in range(NPB)]
        pab = [k.ps("pab%d" % i, [128, TC, 2], F32) for i in range(2)]
        h_memset(k, (S[:], S_b), 0.0)
        S_bc = S[:].unsqueeze(1).broadcast_to([128, 2, 64])
        nch = (T + TC - 1) // TC
        LOOK = NPB - 1

        def emit_bcast(s):
            c = s // TC
            i = s % TC
            st_, stb = stg[c % NBUF]
            pt_, ptb = pb[s % NPB]
            h_mm(k, (pt_[:], ptb), (sel2[0][:], sel2[1]), (st_[:, i, :], stb))

        def load_chunk(c):
            s0 = c * TC
            nst = min(TC, T - s0)
            st_, stb = stg[c % NBUF]
            k.dma("sync", st_[:, 0:nst, :], bc_s[:, s0:s0 + nst, :], w=[stb])
        for c in range(min(2, nch)):
            load_chunk(c)
        for s in range(min(LOOK, T)):
            emit_bcast(s)
        for c in range(nch):
            s0 = c * TC
            nst = min(TC, T - s0)
            if c + 2 < nch:
                load_chunk(c + 2)
            st_, stb = stg[c % NBUF]
            rd, rdb = red[c % NBUF]
            at_, atb = abt[c % NBUF]
            pa_, pab_b = pab[c % 2]
            h_mm(k, (pa_[:, 0:nst, :], pab_b), (sel2[0][:], sel2[1]), (st_[:, 0:nst, 320:322], stb))
            h_copy(k, (at_[:, 0:nst, :], atb), (pa_[:, 0:nst, :], pab_b), eng="scalar")
            if s0 % 512 == 0:
                vi = (s0 // 512) % 2
                vtt, vtb = vt[vi]
                ytt, ytb = yt[vi]
                nv = min(512, T - s0)
                k.dma("gpsimd", vtt[:, 0:nv], vT2_s[:, s0:s0 + nv], w=[vtb])
            for i in range(nst):
                s = s0 + i
                sl = s % 512
                if s + LOOK < T:
                    emit_bcast(s + LOOK)
                pt_, ptb = pb[s % NPB]
                W = pt_[:, 0:64]; NB_ = pt_[:, 64:128]; K_ = pt_[:, 128:192]
                M2 = pt_[:, 192:320].rearrange("p (a c) -> p a c", a=2)
                sac = rd[:, i, 1:2]
                P.op("vector", lambda E, M2=M2: E.tensor_tensor(out=prod[:], in0=S_bc, in1=M2, op=ALU.mult), [S_b, ptb], [S_b])
                P.op("vector", lambda E, rd=rd, i=i: E.tensor_reduce(out=rd[:, i, :], in_=prod[:], axis=mybir.AxisListType.X, op=ALU.add), [S_b], [S_b, rdb])
                P.op("vector", lambda E, W=W: E.tensor_tensor(out=S[:], in0=S[:], in1=W, op=ALU.mult), [S_b, ptb], [S_b])
                P.op("vector", lambda E, NB_=NB_, sac=sac: E.scalar_tensor_tensor(out=S[:], in0=NB_, scalar=sac, in1=S[:], op0=ALU.mult, op1=ALU.add), [S_b, ptb], [S_b])
                P.op("vector", lambda E, K_=K_, vtt=vtt, sl=sl: E.scalar_tensor_tensor(out=S[:], in0=K_, scalar=vtt[:, sl:sl + 1], in1=S[:], op0=ALU.mult, op1=ALU.add), [S_b, ptb, vtb], [S_b, ptb])
            sl0 = s0 % 512
            pA = (p16[0][0][:, 0:nst], p16[0][1]); pB = (p16[1][0][:, 0:nst], p16[1][1])
            h_tt(k, pA, (rd[:, 0:nst, 1], rdb), (at_[:, 0:nst, 0], atb), ALU.mult, eng="gpsimd")
            h_tt(k, pA, pA, (rd[:, 0:nst, 0], rdb), ALU.add, eng="gpsimd")
            h_tt(k, pB, (vtt[:, sl0:sl0 + nst], vtb), (at_[:, 0:nst, 1], atb), ALU.mult, eng="gpsimd")
            h_tt(k, (ytt[:, sl0:sl0 + nst], ytb), pA, pB, ALU.add, eng="gpsimd")
            s_last = s0 + nst - 1
            if s_last % 512 == 511 or s_last == T - 1:
                c0 = s_last - (s_last % 512)
                k.dma("gpsimd", yT2_s[:, c0:s_last + 1], ytt[:, 0:s_last % 512 + 1], r=[ytb])
        k.end_phase()

    def phase_filt(l, Lf):
        Li = L_in[l]
        fc = fconst[Lf]
        k.begin_phase()
        w1 = k.sb("w1s", [33, 64], F32); w2 = k.sb("w2s", [64, 64], F32); w3 = k.sb("w3s", [64, 256], F32)
        bb = k.sb("bbs", [64, 2], F32); b3 = k.sb("b3s", [128, 2], F32); nd = k.sb("nds", [128, 1], F32)
        for t, d in ((w1, Li["hw1"]), (w2, Li["hw2"]), (w3, Li["hw3"]), (bb, Li["hb12"]), (b3, Li["hb3"]), (nd, nd_d)):
            k.dma("sync", t[0][:], d, w=[t[1]])
        nch = (Lf + 511) // 512
        part = k.sb("part", [128, 2 * nch], F32)
        h_memset(k, (part[0][:], part[1]), 0.0)
        b0 = k.sb("b0", [128, 1], F32)
        ft = [k.sb("ft%d" % i, [33, 512], F32) for i in range(2)]
        tv = [k.sb("tv%d" % i, [128, 512], F32) for i in range(2)]
        hx = [k.sb("hx%d" % i, [64, 512], F32) for i in range(2)]
        hi = k.sb("hi", [64, 512], I32); hf = k.sb("hf", [64, 512], F32)
        win = k.sb("win", [128, 512], F32)
        tF = [k.sb("tF%d" % i, [128, 512], F32) for i in range(2)]
        oF = [k.sb("oF%d" % i, [128, 512], BF16) for i in range(2)]
        junk = k.sb("junkf", [128, 512], F32)
        ph = [k.ps("ph%d" % i, [64, 512], F32) for i in range(2)]
        pt = [k.ps("pt%d" % i, [128, 512], F32) for i in range(2)]
        kl = kl_h[Lf].ap()

        def sin_layer(out, psum, bias, nn):
            x = (hf[0][:, 0:nn], hf[1]); ki = (hi[0][:, 0:nn], hi[1])
            h_ts(k, out, psum, bias, None, ALU.add)
            h_ts(k, ki, out, 1.0 / TWO_PI, None, ALU.mult)
            h_copy(k, x, ki)
            h_stt(k, out, x, -TWO_PI, out, ALU.mult, ALU.add)
            h_ts(k, out, out, -float(np.pi), float(np.pi), ALU.max, ALU.min)
            h_act(k, out, out, AF.Sin)
        cnt = 0
        for side in (1, 0):
            for c in range(nch):
                c0 = c * 512
                nn = min(512, Lf - c0)
                j = cnt % 2
                cnt += 1
                k.dma("sync", ft[j][0][:, 0:nn], (fc["featsTr"] if side else fc["featsT"])[:, c0:c0 + nn], w=[ft[j][1]])
                k.dma("sync", tv[j][0][:, 0:nn], (fc["tvr"] if side else fc["tv"])[:, c0:c0 + nn].partition_broadcast(128), w=[tv[j][1]])
                p0 = (ph[0][0][:, 0:nn], ph[0][1]); p1 = (ph[1][0][:, 0:nn], ph[1][1])
                h_mm(k, p0, (w1[0][:], w1[1]), (ft[j][0][:, 0:nn], ft[j][1]))
                h1 = (hx[0][0][:, 0:nn], hx[0][1])
                sin_layer(h1, p0, (bb[0][:, 0:1], bb[1]), nn)
                h_mm(k, p1, (w2[0][:], w2[1]), h1)
                h2 = (hx[1][0][:, 0:nn], hx[1][1])
                sin_layer(h2, p1, (bb[0][:, 1:2], bb[1]), nn)
                ptx = (pt[j][0][:, 0:nn], pt[j][1])
                h_mm(k, ptx, (w3[0][:, side * 128:(side + 1) * 128], w3[1]), h2)
                w_ = (win[0][:, 0:nn], win[1])
                h_act(k, w_, (tv[j][0][:, 0:nn], tv[j][1]), AF.Exp, scale=(nd[0][:], nd[1]))
                F_ = (tF[j][0][:, 0:nn], tF[j][1])
                h_stt(k, F_, ptx, (b3[0][:, side:side + 1], b3[1]), w_, ALU.add, ALU.mult)
                hi_ = nn
                if side == 1 and c == nch - 1:
                    h_copy(k, (b0[0][:], b0[1]), (tF[j][0][:, nn - 1:nn], tF[j][1]))
                    hi_ = nn - 1
                if side == 0 and c == 0:
                    h_tt(k, (tF[j][0][:, 0:1], tF[j][1]), (tF[j][0][:, 0:1], tF[j][1]), (b0[0][:], b0[1]), ALU.add)
                pcol_ = 2 * c + side
                if hi_ > 0:
                    h_act(k, (junk[0][:, 0:hi_], junk[1]), (tF[j][0][:, 0:hi_], tF[j][1]), AF.Square, accum=(part[0][:, pcol_:pcol_ + 1], part[1]))
                    h_copy(k, (oF[j][0][:, 0:hi_], oF[j][1]), (tF[j][0][:, 0:hi_], tF[j][1]), eng="gpsimd")
                    base = c0 if side == 1 else (Lf - 1 + c0)
                    k.dma("sync", kl[:, base:base + hi_], oF[j][0][:, 0:hi_], r=[oF[j][1]])
        tot = k.sb("tot", [128, 1], F32)
        k.V(lambda E: E.tensor_reduce(out=tot[0][:], in_=part[0][:], axis=mybir.AxisListType.X, op=ALU.add), [part[1]], [tot[1]])
        k.dma("sync", ssq_s[Lf].rearrange("o p -> p o"), tot[0][:], r=[tot[1]], allow_slow_non_contiguous=True)
        k.end_phase()

    def phase_hy(l, nb, col_fo, col_mix, readout):
        Li = L_in[l]
        Lf = 128 * nb
        W = 128 * (2 * nb - 1)
        k.begin_phase()
        nrm = k.sb("nrm", [128, 128], F32); skp = k.sb("skp", [128, 128], F32)
        k.dma("sync", nrm[0][:], ssq_s[Lf].partition_broadcast(128), w=[nrm[1]])
        k.dma("sync", skp[0][:], Li["skipT"].partition_broadcast(128), w=[skp[1]])
        h_act(k, (nrm[0][:], nrm[1]), (nrm[0][:], nrm[1]), AF.Sqrt)
        h_recip(k, (nrm[0][:], nrm[1]), (nrm[0][:], nrm[1]))
        if readout:
            k.begin_phase()
            pp = k.sb("pp_sb", [64, NPC], F32)
            k.dma("sync", pp[0][:], Li["pp"], w=[pp[1]])
            o64 = k.sb("o64", [64, 64], F32)
            h_memset(k, (o64[0][:], o64[1]), 1.0 / 64)
            geps = k.sb("geps", [64, 1], F32)
            h_memset(k, (geps[0][:], geps[1]), 64e-5)
            ra = [k.sb("ra%d" % i, [64, 512], F32) for i in range(2)]
            rbk = [k.sb("rbk%d" % i, [64, 128], F32) for i in range(2)]
            rbt = [k.sb("rbt%d" % i, [128, 64], F32) for i in range(2)]
            rc = [k.sb("rc%d" % i, [64, 512], F32) for i in range(2)]
            rd = [k.sb("rd%d" % i, [64, 512], F32) for i in range(2)]
            r1 = k.sb("r1", [64, 512], F32); r2 = k.sb("r2", [64, 512], F32)
            rob = [k.sb("rob%d" % i, [64, 512], BF16) for i in range(2)]
            pr = [k.ps("pr%d" % i, [64, 512], F32) for i in range(2)]
            prt = k.ps("prt", [128, 64], F32)
            prb = k.ps("prb", [64, 512], F32)
            bq = 0
            chunks = [(0, 256)] + [(256 + u0, min(512, n - u0)) for u0 in range(0, n, 512)]
            for ci, (c0, nn) in enumerate(chunks):
                j = ci % 2
                A = (ra[j][0][:, 0:nn], ra[j][1]); C = (rc[j][0][:, 0:nn], rc[j][1]); Dg = (rd[j][0][:, 0:nn], rd[j][1])
                R1 = (r1[0][:, 0:nn], r1[1]); R2 = (r2[0][:, 0:nn], r2[1])
                P0 = (pr[0][0][:, 0:nn], pr[0][1]); P1 = (pr[1][0][:, 0:nn], pr[1][1])
                k.dma("scalar", A[0], yT2_s[0:64, c0:c0 + nn], w=[A[1]])
                k.dma("scalar", C[0], fo_s[FO["bonus"], :, c0:c0 + nn], w=[C[1]])
                k.dma("scalar", Dg[0], fo_s[FO["sg_rw"], :, c0:c0 + nn], w=[Dg[1]])
                for bi in range(nn // 128):
                    tau0 = c0 + bi * 128
                    s_lo = (128 - tau0) if tau0 < 256 else (256 + n - 128 - (tau0 - 256))
                    q = bq % 2
                    bq += 1
                    k.dma("scalar", rbk[q][0][:], yT2_s[64:128, s_lo:s_lo + 128], w=[rbk[q][1]])
                    h_tr(k, (prt[0][:], prt[1]), (rbk[q][0][:], rbk[q][1]), (identf[0][0:64, 0:64], identf[1]))
                    h_copy(k, (rbt[q][0][:], rbt[q][1]), (prt[0][:], prt[1]), eng="scalar")
                    h_mm(k, (prb[0][:, bi * 128:(bi + 1) * 128], prb[1]), (rbt[q][0][:], rbt[q][1]), (Jf[0][:], Jf[1]))
                h_tt(k, A, A, (prb[0][:, 0:nn], prb[1]), ALU.add)
                h_mm(k, P0, (o64[0][:], o64[1]), A)
                h_tt(k, R1, A, P0, ALU.subtract)
                h_tt(k, R2, R1, R1, ALU.mult)
                h_mm(k, P1, (o64[0][:], o64[1]), R2)
                h_act(k, R2, P1, AF.Sqrt, bias=(geps[0][:], geps[1]))
                h_recip(k, R2, R2)
                h_tt(k, R1, R1, R2, ALU.mult)
                h_ts(k, R1, R1, (pp[0][:, PC["gn_w"]:PC["gn_w"] + 1], pp[1]), (pp[0][:, PC["gn_b"]:PC["gn_b"] + 1], pp[1]), ALU.mult, ALU.add)
                h_tt(k, R1, R1, C, ALU.add)
                OB = (rob[j][0][:, 0:nn], rob[j][1])
                h_tt(k, OB, R1, Dg, ALU.mult)
                mc = (n + c0) if c0 < 256 else (c0 - 256)
                k.dma("scalar", mix_send[0:64, mc:mc + nn], OB[0], r=[OB[1]])
            k.end_phase()
        ksh = [k.sb("ksh%d" % i, [128, W], BF16) for i in range(2)]
        ld = [[k.sb("ld%d_%d" % (a, i), [nb, 128], F32) for i in range(2)] for a in range(4)]
        zt = [k.sb("zt%d" % i, [128, nb], F32) for i in range(2)]
        x1t = [k.sb("x1t%d" % i, [128, nb], F32) for i in range(2)]
        x2t = [k.sb("x2t%d" % i, [128, nb], F32) for i in range(2)]
        sgt = [k.sb("sgt%d" % i, [128, nb], F32) for i in range(2)]
        zb = k.sb("zb", [128, nb], BF16); zf = k.sb("zf", [128, nb], BF16)
        z2 = k.sb("z2", [128, nb], F32); t1 = k.sb("t1", [128, nb], F32)
        ot = [k.sb("oth%d" % i, [128, nb], BF16) for i in range(2)]
        otT = [k.sb("otT%d" % i, [nb, 128], BF16) for i in range(2)]
        pin = [k.ps("pin%d" % i, [128, nb], F32) for i in range(2)]
        pf = k.ps("pf", [128, nb], F32)
        py = [k.ps("pyc%d" % i, [128, nb], F32) for i in range(2)]
        pot = k.ps("pot", [nb, 128], BF16)
        order = [0] + [d for d in range(-(nb - 1), nb) if d != 0]
        cnt = 0
        names = ["hv", "hx1", "hx2", "sg_hy"]
        for c in range(64):
            j = c % 2
            dst = [zt[j], x1t[j], x2t[j], sgt[j]]
            for a in range(4):
                lt_, lb_ = ld[a][j]
                k.dma("gpsimd", lt_[:], fo_s[FO[names[a]], c, col_fo:col_fo + Lf].rearrange("(s j) -> s j", j=128), w=[lb_])
                h_tr(k, (pin[a % 2][0][:], pin[a % 2][1]), (lt_[:], lb_), (identf[0][0:nb, 0:nb], identf[1]))
                h_copy(k, (dst[a][0][:], dst[a][1]), (pin[a % 2][0][:], pin[a % 2][1]), eng=("scalar" if a % 2 else "vector"))
            Z = (zt[j][0][:], zt[j][1]); X1 = (x1t[j][0][:], x1t[j][1]); X2 = (x2t[j][0][:], x2t[j][1]); SG = (sgt[j][0][:], sgt[j][1])
            zin = Z
            for o in range(2):
                row = o * 64 + c
                kt, kb_ = ksh[cnt % 2]
                q = "sync" if cnt % 2 == 0 else "scalar"
                cnt += 1
                src = bass.AP(kl_h[Lf], row * 2 * Lf, [[1, 128], [1, W]])
                k.dma(q, kt[:], src, w=[kb_])
                h_copy(k, (zb[0][:], zb[1]), zin, eng="gpsimd")
                h_mm(k, (pf[0][:], pf[1]), (Jb[0][:], Jb[1]), (zb[0][:], zb[1]))
                h_copy(k, (zf[0][:], zf[1]), (pf[0][:], pf[1]), eng="scalar")
                pyt, pyb = py[o]
                for ii, Dd in enumerate(order):
                    e = Dd + nb - 1
                    S0, S1 = max(0, -Dd), min(nb, nb - Dd)
                    h_mm(k, (pyt[:, S0 + Dd:S1 + Dd], pyb), (kt[:, 128 * e:128 * e + 128], kb_), (zf[0][:, S0:S1], zf[1]), start=(ii == 0), stop=(ii == len(order) - 1))
                ncol_ = (nrm[0][:, row:row + 1], nrm[1])
                scol = (skp[0][:, row:row + 1], skp[1])
                h_ts(k, (t1[0][:], t1[1]), (pyt[:], pyb), ncol_, None, ALU.mult)
                h_stt(k, (t1[0][:], t1[1]), zin, scol, (t1[0][:], t1[1]), ALU.mult, ALU.add)
                if o == 0:
                    h_tt(k, (z2[0][:], z2[1]), (t1[0][:], t1[1]), X1, ALU.mult)
                    zin = (z2[0][:], z2[1])
                else:
                    O_ = (ot[j][0][:], ot[j][1])
                    h_tt(k, (t1[0][:], t1[1]), (t1[0][:], t1[1]), X2, ALU.mult)
                    h_tt(k, O_, (t1[0][:], t1[1]), SG, ALU.mult, eng="gpsimd")
                    h_tr(k, (pot[0][:], pot[1]), O_, IDB)
                    h_copy(k, (otT[j][0][:], otT[j][1]), (pot[0][:], pot[1]), eng="scalar")
                    k.dma("gpsimd", mix_send[64 + c, col_mix:col_mix + Lf].rearrange("(s j) -> s j", j=128), otT[j][0][:], r=[otT[j][1]])
        k.end_phase()

    for l in range(4):
        phase_tok(l)
        if l % 2 == 0:
            phase_attn(l)
        else:
            phase_rfeat(l)
            phase_scan()
            phase_filt(l, n)
            phase_hy(l, nlb, 256, 0, True)
            if l < 3:
                phase_filt(l, 256)
                phase_hy(l, 2, 0, n, False)
        P.coll("AllGather", mybir.AluOpType.bypass, mix_send, mix_all)
        P.barrier()
    phase_tok(4, final=True)
    k.done()
    return nc


_FUSED_CACHE = {}


def _c_cols(c, c_ctx):
    return np.ascontiguousarray(np.concatenate([c.reshape(8, 128).T, c_ctx.reshape(8, 128).T], axis=1).astype(np.float32))


def fused_inputs(inp, n):
    nlb = n // 128
    x = np.ascontiguousarray(inp["x"][0], dtype=np.float32)
    ctx = np.ascontiguousarray(inp["ctx"][0], dtype=np.float32)
    C, S, RT = rope_tables(nlb)
    sel = np.zeros((65, 64), np.float32)
    sel[64] = 1.0
    perm = np.concatenate([np.concatenate([np.arange(64 * r, 64 * r + 64), np.arange(512 + 64 * r, 512 + 64 * r + 64)]) for r in range(8)])
    common = {"x": x, "ctx": ctx, "c_cols": _c_cols(inp["c"][0], inp["c_ctx"]), "ident": np.eye(128, dtype=np.float32),
              "Jmat": np.eye(128, dtype=np.float32)[::-1].copy(), "final_norm": inp["final_norm"].reshape(1, -1).astype(np.float32),
              "ropeC": C, "ropeS": S, "rmatT": RT, "sel": sel,
              "sel2": np.concatenate([np.repeat(np.array([[1.0], [0.0]], np.float32), 64, 1), np.repeat(np.array([[0.0], [1.0]], np.float32), 64, 1)], 1)}
    for Lf in (n, 256):
        fT, tv, adel = filt_consts(Lf)
        common["featsT%d" % Lf] = fT
        common["featsTr%d" % Lf] = np.ascontiguousarray(fT[:, ::-1])
        common["tv%d" % Lf] = tv
        common["tvr%d" % Lf] = np.ascontiguousarray(tv[:, ::-1])
    maps = []
    for h in range(8):
        m = dict(common)
        kvh = h // 4
        sl = slice(64 * h, 64 * h + 64)
        m["negdelta"] = -np.concatenate([adel[sl], adel[sl]]).reshape(128, 1).astype(np.float32)
        for l in range(4):
            i = l // 2
            attn = l % 2 == 0
            pre = "attn" if attn else "rec"
            m["norm_g%d" % l] = inp[pre + "_norm"][i].reshape(1, -1)
            m["ada_w%d" % l] = inp[pre + "_ada_w"][i]
            m["ada_b%d" % l] = inp[pre + "_ada_b"][i].reshape(1, -1)
            m["w_out%d" % l] = np.ascontiguousarray(inp[pre + "_w_out"][i][perm])
            w_in = inp[pre + "_w_in"][i]
            if attn:
                starts = [64 * h, 512 + 64 * h, 1536 + 64 * h, 2048 + 64 * h, 2560 + 64 * kvh, 2816 + 64 * h, 1024 + 64 * h, 2688 + 64 * kvh]
                cols = np.concatenate([np.arange(s0, s0 + 64) for s0 in starts])
                m["w_in%d" % l] = np.ascontiguousarray(w_in[:, cols])
                m["biasT%d" % l] = na_bias_tables(inp["na_rpb"][i][h], nlb)
                m["gains%d" % l] = np.stack([inp["gqa_q_gain"][i], inp["gqa_k_gain"][i]], 1).astype(np.float32)
            else:
                starts = [64 * h, 512 + 64 * h, 1024 + 64 * h, 1664 + 64 * h, 2176 + 64 * h, 2688 + 64 * h, 3200 + 64 * h, 3712 + 64 * h]
                cols = np.concatenate([np.arange(s0, s0 + 64) for s0 in starts] + [np.arange(1536, 1664)])
                m["w_in%d" % l] = np.ascontiguousarray(w_in[:, cols])
                prm = {k_: inp[k_][i] for k_ in inp if k_.startswith("rwkv") or k_.startswith("hy")}
                ri = rfeat_inputs(np.zeros((1, 4224), np.float32), np.zeros((1, 4224), np.float32), h, prm)
                m["pp%d" % l] = ri["pp"]
                m["mu_lora%d" % l] = ri["mu_lora"]
                m["wx%d" % l] = ri["wx"]
                fi = filt_inputs(256, h, prm)
                m["hw1_%d" % l] = fi["w1"]
                m["hw2_%d" % l] = fi["w2"]
                m["hw3_%d" % l] = fi["w3c"]
                m["hb12_%d" % l] = fi["b12"]
                m["hb3_%d" % l] = fi["b3c"]
                m["skipT%d" % l] = np.concatenate([prm["hy_skip"][0][sl], prm["hy_skip"][1][sl]]).reshape(1, 128).astype(np.float32)
        maps.append(m)
    return maps


def kernel(**inp):
    inp = {k_: np.asarray(v) for k_, v in inp.items()}
    n = inp["x"].shape[1]
    if n not in _FUSED_CACHE:
        _FUSED_CACHE[n] = build_fused(n)
    nc = _FUSED_CACHE[n]
    res = run_bass_kernel_spmd(nc, fused_inputs(inp, n), core_ids=list(range(8)))
    tpc = n // 8
    out = np.concatenate([res.results[i]["y_out"][i * tpc:(i + 1) * tpc] for i in range(8)], 0)
    return out[None].astype(np.float32)
```

```python
import numpy as np
import concourse.bass as bass
import concourse.mybir as mybir
from concourse.bass_utils import run_bass_kernel_spmd

F32 = mybir.dt.float32
BF16 = mybir.dt.bfloat16
I32 = mybir.dt.int32
ALU = mybir.AluOpType
AF = mybir.ActivationFunctionType


class Buf:
    __slots__ = ("w", "r", "name")

    def __init__(self, name=""):
        self.w = None
        self.r = {}
        self.name = name


class Prog:
    ENGS = ("tensor", "vector", "scalar", "gpsimd", "sync")
    NDSEM = 6

    def __init__(self, nc):
        self.nc = nc
        self.lists = {e: [] for e in self.ENGS}
        self.count = {e: 0 for e in self.ENGS}
        self.waited = {e: {} for e in self.ENGS}
        self.sems = {}
        self._ctx = []
        for e in self.ENGS:
            cm = nc.semaphore("s_" + e)
            self.sems[e] = cm.__enter__()
            self._ctx.append(cm)
        self.dsem = {}
        self.dcount = {}
        self.dnext = {}
        for q in ("sync", "scalar", "gpsimd"):
            self.dsem[q] = []
            for i in range(self.NDSEM):
                cm = nc.semaphore("d_%s%d" % (q, i))
                self.dsem[q].append(cm.__enter__())
                self._ctx.append(cm)
                self.sems["d_%s%d" % (q, i)] = self.dsem[q][-1]
            self.dcount[q] = [0] * self.NDSEM
            self.dnext[q] = 0

    def _need(self, eng, evs):
        best = {}
        for ev in evs:
            if ev is None:
                continue
            k, v = ev
            if best.get(k, 0) < v:
                best[k] = v
        for k, v in best.items():
            if self.waited[eng].get(k, 0) >= v:
                continue
            self.waited[eng][k] = v
            sem = self.sems[k]
            self.lists[eng].append(lambda E, sem=sem, v=v: E.wait_ge(sem, v))

    def _deps(self, reads, writes):
        evs = []
        for b in reads:
            evs.append(b.w)
        for b in writes:
            evs.append(b.w)
            evs.extend(b.r.items())
        return evs

    def op(self, eng, fn, reads=(), writes=()):
        self._need(eng, self._deps(reads, writes))
        self.count[eng] += 1
        c = self.count[eng]
        sem = self.sems[eng]
        self.lists[eng].append(lambda E, fn=fn, sem=sem: fn(E).then_inc(sem, 1))
        ev = (eng, c)
        for b in reads:
            b.r[eng] = c
        for b in writes:
            b.w = ev
            b.r = {}
        return ev

    def dma(self, q, out, in_, reads=(), writes=(), **kw):
        i = self.dnext[q]
        self.dnext[q] = (i + 1) % self.NDSEM
        key = "d_%s%d" % (q, i)
        evs = self._deps(reads, writes)
        if self.dcount[q][i] > 0:
            evs.append((key, self.dcount[q][i]))
        self._need(q, evs)
        self.dcount[q][i] += 16
        v = self.dcount[q][i]
        sem = self.sems[key]
        self.lists[q].append(lambda E, out=out, in_=in_, sem=sem, kw=kw: E.dma_start(out=out, in_=in_, **kw).then_inc(sem, 16))
        ev = (key, v)
        for b in reads:
            b.r[key] = v
        for b in writes:
            b.w = ev
            b.r = {}
        return ev

    def coll(self, kind, op, ins_ap, outs_ap, reads=(), writes=()):
        if "cc" not in self.sems:
            cm = self.nc.semaphore("s_cc")
            self.sems["cc"] = cm.__enter__()
            self._ctx.append(cm)
            self.cccount = 0
        evs = self._deps(reads, writes)
        if self.cccount:
            evs.append(("cc", self.cccount))
        self._need("gpsimd", evs)
        self.cccount += 1
        v = self.cccount
        sem = self.sems["cc"]
        self.lists["gpsimd"].append(lambda E: E.collective_compute(kind, op, replica_groups=[list(range(8))], ins=[ins_ap.opt()], outs=[outs_ap.opt()]).then_inc(sem))
        ev = ("cc", v)
        for b in reads:
            b.r["cc"] = v
        for b in writes:
            b.w = ev
            b.r = {}
        return ev

    def barrier(self):
        evs = []
        for q in self.dsem:
            for i in range(self.NDSEM):
                if self.dcount[q][i]:
                    evs.append(("d_%s%d" % (q, i), self.dcount[q][i]))
        for e in self.ENGS:
            if self.count[e]:
                evs.append((e, self.count[e]))
        if "cc" in self.sems and self.cccount:
            evs.append(("cc", self.cccount))
        for e in self.ENGS:
            self._need(e, evs)

    def finish(self):
        evs = []
        for q in self.dsem:
            for i in range(self.NDSEM):
                if self.dcount[q][i]:
                    evs.append(("d_%s%d" % (q, i), self.dcount[q][i]))
        for e in self.ENGS:
            if e != "sync" and self.count[e]:
                evs.append((e, self.count[e]))
        if "cc" in self.sems:
            evs.append(("cc", self.cccount))
        self._need("sync", evs)
        nc = self.nc
        lists = self.lists
        with nc.Block() as block:
            @block.sync
            def _(E):
                for f in lists["sync"]:
                    f(E)

            @block.tensor
            def _(E):
                for f in lists["tensor"]:
                    f(E)

            @block.vector
            def _(E):
                for f in lists["vector"]:
                    f(E)

            @block.scalar
            def _(E):
                for f in lists["scalar"]:
                    f(E)

            @block.gpsimd
            def _(E):
                for f in lists["gpsimd"]:
                    f(E)
        for cm in reversed(self._ctx):
            cm.__exit__(None, None, None)


from contextlib import ExitStack

NB = 18
NT = NB * 128
D = 1024
EPS = 1e-6


class KB:
    def __init__(self, nc):
        self.nc = nc
        self.P = Prog(nc)
        self.es = ExitStack()

    def _uname(self, name):
        self._uid = getattr(self, "_uid", 0) + 1
        return "%s_%d" % (name, self._uid)

    def sb(self, name, shape, dt):
        return self.es.enter_context(self.nc.sbuf_tensor(self._uname(name), shape, dt)), Buf(name)

    def ps(self, name, shape, dt):
        return self.es.enter_context(self.nc.psum_tensor(self._uname(name), shape, dt)), Buf(name)

    def din(self, name, shape, dt):
        return self.nc.dram_tensor(name, list(shape), dt, kind="ExternalInput").ap()

    def dout(self, name, shape, dt):
        return self.nc.dram_tensor(name, list(shape), dt, kind="ExternalOutput").ap()

    def V(self, fn, r=(), w=()):
        return self.P.op("vector", fn, r, w)

    def A(self, fn, r=(), w=()):
        return self.P.op("scalar", fn, r, w)

    def G(self, fn, r=(), w=()):
        return self.P.op("gpsimd", fn, r, w)

    def T(self, fn, r=(), w=()):
        return self.P.op("tensor", fn, r, w)

    def dma(self, q, out, in_, r=(), w=(), **kw):
        return self.P.dma(q, out, in_, r, w, **kw)

    def begin_phase(self):
        self._saved_es = self.es
        self.es = ExitStack()

    def end_phase(self):
        self.P.barrier()
        self.es.close()
        self.es = self._saved_es

    def done(self):
        self.P.finish()
        self.es.close()


def load_w_bf16(k, w_dram, wt, wb, ncols, rows=8):
    first = True
    for kc in range(rows):
        c0 = 0
        while c0 < ncols:
            c1 = min(ncols, c0 + 2048)
            k.dma("gpsimd", wt[:, kc, c0:c1], w_dram[kc * 128:(kc + 1) * 128, c0:c1], w=[] if not first else [wb])
            first = False
            c0 = c1
    return


def build_tok(in_w, has_prev, final, u_dt, NB=18, NLAT=16):
    NT = NB * 128
    nc = bass.Bass("TRN2", target_bir_lowering=False)
    k = KB(nc)
    P = k.P
    x_in = k.din("x_in", [NT, D], F32)
    ident_d = k.din("ident", [128, 128], F32)
    if has_prev:
        mixT = k.din("mixT", [D, NT], BF16)
        w_out = k.din("w_out", [D, D], F32)
        gates_in = k.din("gates_in", [2, 128, D], F32)
        x_out = k.dout("x_out", [NT, D], F32)
    if final:
        fn_g = k.din("final_norm", [1, D], F32)
        y_out = k.dout("y_out", [NT, D], F32)
    else:
        c_cols = k.din("c_cols", [128, 16], F32)
        norm_g = k.din("norm_g", [1, D], F32)
        ada_w = k.din("ada_w", [D, 3 * D], F32)
        ada_b = k.din("ada_b", [1, 3 * D], F32)
        w_in = k.din("w_in", [D, in_w], F32)
        u_out = k.dout("u_out", [NT, in_w], u_dt)
        gates_out = k.dout("gates_out", [2, 128, D], F32)

    ident, ident_b = k.sb("ident_sb", [128, 128], F32)
    identb, identb_b = k.sb("identb_sb", [128, 128], BF16)
    k.dma("sync", ident[:], ident_d, w=[ident_b])
    k.V(lambda E: E.tensor_copy(out=identb[:], in_=ident[:]), [ident_b], [identb_b])
    eps_t, eps_b = k.sb("eps_t", [128, 1], F32)
    k.V(lambda E: E.memset(eps_t[:], EPS), [], [eps_b])
    if has_prev:
        wo, wo_b = k.sb("wo", [128, 8, D], BF16)
        wo_evs = []
        for kc in range(8):
            wo_evs.append(k.dma("gpsimd", wo[:, kc, :], w_out[kc * 128:(kc + 1) * 128, :]))
        gp, gp_b = k.sb("gp", [128, 2, D], F32)
        k.dma("sync", gp[:], gates_in.rearrange("v p d -> p v d"), w=[gp_b])
    if final:
        fg, fg_b = k.sb("fg", [128, D], F32)
        k.dma("sync", fg[:], fn_g.partition_broadcast(128), w=[fg_b])
    else:
        wi, wi_b = k.sb("wi", [128, 8, in_w], BF16)
        wi_evs = []
        for kc in range(8):
            c0 = 0
            while c0 < in_w:
                c1 = min(in_w, c0 + 2048)
                wi_evs.append(k.dma("gpsimd", wi[:, kc, c0:c1], w_in[kc * 128:(kc + 1) * 128, c0:c1]))
                c0 = c1
        mod, mod_b = k.sb("mod", [128, 2, 3 * D], F32)
        Gt, G_b = k.sb("Gt", [128, 2, D], F32)
        with ExitStack() as es1:
            cc, cc_b = es1.enter_context(nc.sbuf_tensor("cc", [128, 16], F32)), Buf()
            rep, rep_b = es1.enter_context(nc.sbuf_tensor("rep", [128, 16, 128], F32)), Buf()
            ones, ones_b = es1.enter_context(nc.sbuf_tensor("ones", [128, 128], F32)), Buf()
            ab, ab_b = es1.enter_context(nc.sbuf_tensor("ab", [128, 3 * D], F32)), Buf()
            ng, ng_b = es1.enter_context(nc.sbuf_tensor("ng", [128, D], F32)), Buf()
            aw = [(es1.enter_context(nc.sbuf_tensor("aw%d" % i, [128, 8, 512], F32)), Buf()) for i in range(2)]
            pa = [(es1.enter_context(nc.psum_tensor("pa%d" % i, [128, 512], F32)), Buf()) for i in range(2)]
            k.dma("sync", cc[:], c_cols, w=[cc_b])
            k.dma("sync", ab[:], ada_b.partition_broadcast(128), w=[ab_b])
            k.dma("sync", ng[:], norm_g.partition_broadcast(128), w=[ng_b])
            k.A(lambda E: E.activation(out=cc[:], in_=cc[:], func=AF.Silu), [cc_b], [cc_b])
            k.V(lambda E: E.memset(ones[:], 1.0), [], [ones_b])
            for j in range(16):
                k.V(lambda E, j=j: E.tensor_scalar(out=rep[:, j, :], in0=ones[:], scalar1=cc[:, j:j + 1], scalar2=None, op0=ALU.mult),
                    [ones_b, cc_b], [rep_b])
            for g in range(6):
                awt, awb = aw[g % 2]
                k.dma("sync", awt[:], ada_w[:, g * 512:(g + 1) * 512].rearrange("(k p) n -> p k n", p=128), w=[awb])
                for v in range(2):
                    pt, pb = pa[v]
                    for kc in range(8):
                        k.T(lambda E, pt=pt, v=v, kc=kc, awt=awt: E.matmul(pt[:], rep[:, v * 8 + kc, :], awt[:, kc, :], start=(kc == 0), stop=(kc == 7)),
                            [rep_b, awb], [pb])
                    k.V(lambda E, pt=pt, v=v, g=g: E.tensor_tensor(out=mod[:, v, g * 512:(g + 1) * 512], in0=pt[:], in1=ab[:, g * 512:(g + 1) * 512], op=ALU.add),
                        [pb, ab_b], [mod_b])
            for v in range(2):
                k.V(lambda E, v=v: E.scalar_tensor_tensor(out=Gt[:, v, :], in0=mod[:, v, D:2 * D], scalar=1.0, in1=ng[:], op0=ALU.add, op1=ALU.mult),
                    [mod_b, ng_b], [G_b])
            k.dma("sync", gates_out.rearrange("v p d -> p v d"), mod[:, :, 2 * D:3 * D], r=[mod_b])
            scope_bufs = [cc_b, rep_b, ones_b, ab_b, ng_b, aw[0][1], aw[1][1], pa[0][1], pa[1][1]]

    xt = [k.sb("xt%d" % i, [128, D], F32) for i in range(2)]
    tmp = [k.sb("tmp%d" % i, [128, D], F32) for i in range(2)]
    ss = [k.sb("ss%d" % i, [128, 4], F32) for i in range(2)]
    junk, junk_b = k.sb("junk", [128, D], BF16)
    if has_prev:
        mt = [k.sb("mt%d" % i, [128, 8, 128], BF16) for i in range(2)]
        py = [k.ps("py%d" % i, [128, 512], F32) for i in range(2)]
    if not final:
        xm = [k.sb("xm%d" % i, [128, D], BF16) for i in range(2)]
        xmT = [k.sb("xmT%d" % i, [128, 8, 128], BF16) for i in range(2)]
        ptr = [k.ps("ptr%d" % i, [128, 8, 128], BF16) for i in range(2)]
        pu = [k.ps("pu%d" % i, [128, 512], F32) for i in range(3)]
        ut = [k.sb("ut%d" % i, [128, 512], u_dt) for i in range(4)]
    if not final:
        fence = Buf("fence")
        for b in scope_bufs:
            if b.w:
                fence.r[b.w[0]] = max(fence.r.get(b.w[0], 0), b.w[1])
            for kk, vv in b.r.items():
                fence.r[kk] = max(fence.r.get(kk, 0), vv)
    else:
        fence = Buf("fence")
    fence_evs = list(fence.r.items())
    for e in ("vector", "scalar", "gpsimd", "tensor", "sync"):
        P._need(e, fence_evs)
    if has_prev:
        for e in ("tensor",):
            P._need(e, wo_evs)
    if not final:
        P._need("tensor", wi_evs)

    ngroups = (in_w + 511) // 512 if not final else 0
    ucount = 0
    for b in range(NB):
        v = 0 if b < NLAT else 1
        xtt, xtb = xt[b % 2]
        tt, tb = tmp[b % 2]
        sst, ssb = ss[b % 2]
        k.dma("sync", xtt[:], x_in[b * 128:(b + 1) * 128, :], w=[xtb])
        if has_prev:
            mtt, mtb = mt[b % 2]
            k.dma("sync", mtt[:], mixT[:, b * 128:(b + 1) * 128].rearrange("(k p) t -> p k t", p=128), w=[mtb])
            for h in range(2):
                pyt, pyb = py[h]
                for kc in range(8):
                    k.T(lambda E, pyt=pyt, mtt=mtt, kc=kc, h=h: E.matmul(pyt[:], mtt[:, kc, :], wo[:, kc, h * 512:(h + 1) * 512], start=(kc == 0), stop=(kc == 7)),
                        [mtb], [pyb])
                k.V(lambda E, pyt=pyt, tt=tt, h=h, v=v: E.tensor_tensor(out=tt[:, h * 512:(h + 1) * 512], in0=pyt[:], in1=gp[:, v, h * 512:(h + 1) * 512], op=ALU.mult),
                    [pyb, gp_b], [tb])
            k.G(lambda E, xtt=xtt, tt=tt: E.tensor_tensor(out=xtt[:], in0=tt[:], in1=xtt[:], op=ALU.add), [tb, xtb], [xtb])
            k.dma("sync", x_out[b * 128:(b + 1) * 128, :], xtt[:], r=[xtb])
        k.A(lambda E, xtt=xtt, sst=sst: E.activation(out=junk[:], in_=xtt[:], func=AF.Square, accum_out=sst[:, 0:1]), [xtb], [junk_b, ssb])
        k.A(lambda E, sst=sst: E.activation(out=sst[:, 1:2], in_=sst[:, 0:1], func=AF.Sqrt, scale=1.0 / D, bias=eps_t[:]), [ssb, eps_b], [ssb])
        k.V(lambda E, sst=sst: E.reciprocal(out=sst[:, 2:3], in_=sst[:, 1:2]), [ssb], [ssb])
        if final:
            k.V(lambda E, xtt=xtt, tt=tt, sst=sst: E.scalar_tensor_tensor(out=tt[:], in0=xtt[:], scalar=sst[:, 2:3], in1=fg[:], op0=ALU.mult, op1=ALU.mult),
                [xtb, ssb, fg_b], [tb])
            k.dma("sync", y_out[b * 128:(b + 1) * 128, :], tt[:], r=[tb])
            continue
        xmt, xmb = xm[b % 2]
        k.V(lambda E, xtt=xtt, tt=tt, sst=sst, v=v: E.scalar_tensor_tensor(out=tt[:], in0=xtt[:], scalar=sst[:, 2:3], in1=Gt[:, v, :], op0=ALU.mult, op1=ALU.mult),
            [xtb, ssb, G_b], [tb])
        k.G(lambda E, tt=tt, xmt=xmt, v=v: E.tensor_tensor(out=xmt[:], in0=tt[:], in1=mod[:, v, 0:D], op=ALU.add), [tb, mod_b], [xmb])
        ptt, ptb = ptr[b % 2]
        xTt, xTb = xmT[b % 2]
        for kc in range(8):
            k.T(lambda E, ptt=ptt, xmt=xmt, kc=kc: E.transpose(ptt[:, kc, :], xmt[:, kc * 128:(kc + 1) * 128], identb[:]), [xmb, identb_b], [ptb])
        k.A(lambda E, ptt=ptt, xTt=xTt: E.copy(out=xTt[:], in_=ptt[:]), [ptb], [xTb])
        for g in range(ngroups):
            c0 = g * 512
            c1 = min(in_w, c0 + 512)
            put, pub = pu[ucount % 3]
            utt, utb = ut[ucount % 4]
            for kc in range(8):
                k.T(lambda E, put=put, xTt=xTt, kc=kc, c0=c0, c1=c1: E.matmul(put[:, 0:c1 - c0], xTt[:, kc, :], wi[:, kc, c0:c1], start=(kc == 0), stop=(kc == 7)),
                    [xTb], [pub])
            if ucount % 2 == 0:
                k.V(lambda E, put=put, utt=utt, c0=c0, c1=c1: E.tensor_copy(out=utt[:, 0:c1 - c0], in_=put[:, 0:c1 - c0]), [pub], [utb])
            else:
                k.A(lambda E, put=put, utt=utt, c0=c0, c1=c1: E.copy(out=utt[:, 0:c1 - c0], in_=put[:, 0:c1 - c0]), [pub], [utb])
            k.dma("sync", u_out[b * 128:(b + 1) * 128, c0:c1], utt[:, 0:c1 - c0], r=[utb])
            ucount += 1
    k.done()
    return nc


def _sc(s):
    return (s[0], [s[1]]) if isinstance(s, tuple) else (s, [])


def h_tt(k, out, a, b, op, eng="vector"):
    oa, ob = out; aa, ab = a; ba, bb = b
    return k.P.op(eng, lambda E: E.tensor_tensor(out=oa, in0=aa, in1=ba, op=op), [ab, bb], [ob])


def h_stt(k, out, a, s, b, op0, op1, accum=None):
    oa, ob = out; aa, ab = a; ba, bb = b
    sv, sb_ = _sc(s)
    if accum is None:
        return k.P.op("vector", lambda E: E.scalar_tensor_tensor(out=oa, in0=aa, scalar=sv, in1=ba, op0=op0, op1=op1), [ab, bb] + sb_, [ob])
    ca, cb = accum
    return k.P.op("vector", lambda E: E.scalar_tensor_tensor(out=oa, in0=aa, scalar=sv, in1=ba, op0=op0, op1=op1, accum_out=ca), [ab, bb] + sb_, [ob, cb])


def h_ts(k, out, a, s1, s2, op0, op1=None, eng="vector"):
    oa, ob = out; aa, ab = a
    s1v, s1b = _sc(s1)
    s2v, s2b = _sc(s2) if s2 is not None else (None, [])
    if op1 is None:
        return k.P.op(eng, lambda E: E.tensor_scalar(out=oa, in0=aa, scalar1=s1v, scalar2=None, op0=op0), [ab] + s1b, [ob])
    return k.P.op(eng, lambda E: E.tensor_scalar(out=oa, in0=aa, scalar1=s1v, scalar2=s2v, op0=op0, op1=op1), [ab] + s1b + s2b, [ob])


def h_act(k, out, a, func, scale=1.0, bias=None, accum=None):
    oa, ob = out; aa, ab = a
    kw = {}
    rd = [ab]
    wr = [ob]
    if bias is not None:
        bv, bb = _sc(bias)
        kw["bias"] = bv
        rd += bb
    if isinstance(scale, tuple):
        rd.append(scale[1]); scale = scale[0]
    if accum is not None:
        kw["accum_out"] = accum[0]
        wr.append(accum[1])
    return k.P.op("scalar", lambda E: E.activation(out=oa, in_=aa, func=func, scale=scale, **kw), rd, wr)


def h_recip(k, out, a):
    oa, ob = out; aa, ab = a
    return k.P.op("vector", lambda E: E.reciprocal(out=oa, in_=aa), [ab], [ob])


def h_copy(k, out, a, eng="vector"):
    oa, ob = out; aa, ab = a
    if eng == "scalar":
        return k.P.op(eng, lambda E: E.copy(out=oa, in_=aa), [ab], [ob])
    return k.P.op(eng, lambda E: E.tensor_copy(out=oa, in_=aa), [ab], [ob])


def h_mm(k, out, lhsT, rhs, start=True, stop=True):
    oa, ob = out; la, lb = lhsT; ra, rb = rhs
    return k.P.op("tensor", lambda E: E.matmul(oa, la, ra, start=start, stop=stop), [lb, rb], [ob])


def h_memset(k, out, val, eng="vector"):
    oa, ob = out
    return k.P.op(eng, lambda E: E.memset(oa, val), [], [ob])


def h_sigmoid(k, out, a, tmp, scale=1.0, bias=None):
    h_act(k, tmp, a, AF.Exp, scale=-scale, bias=bias)
    h_ts(k, tmp, tmp, 1.0, None, ALU.add)
    h_recip(k, out, tmp)


def h_silu(k, out, a, tmp):
    h_sigmoid(k, tmp, a, tmp)
    h_tt(k, out, tmp, a, ALU.mult)


import numpy as np

NEG = -30000.0


def na_plan(nlb):
    rows_blocks = nlb
    plan = []
    variants = {}
    for m in range(nlb):
        r0 = min(max(2 * m - 4, 0), 2 * nlb - 8)
        r1 = min(max(2 * m + 1 - 4, 0), 2 * nlb - 8) + 7
        kb0, kb1 = r0 // 2, r1 // 2
        edge = (m < 2) or (m >= nlb - 2)
        lst = []
        for kb in range(kb0, kb1 + 1):
            key = (m if edge else "i", kb - m)
            if key not in variants:
                variants[key] = (len(variants), m, kb)
            lst.append((kb, variants[key][0]))
        plan.append(lst)
    return plan, variants


def na_bias_tables(rpb_h, nlb):
    plan, variants = na_plan(nlb)
    rows = 2 * nlb
    out = np.full((128, len(variants), 128), NEG, np.float32)
    for key, (vid, m, kb) in variants.items():
        q = m * 128 + np.arange(128)
        kk = kb * 128 + np.arange(128)
        qr, qc = q // 64, q % 64
        kr, kc = kk // 64, kk % 64
        row0 = np.clip(qr - 4, 0, rows - 8)
        col0 = np.clip(qc - 8, 0, 64 - 16)
        inwin = ((kr[:, None] >= row0[None]) & (kr[:, None] < row0[None] + 8) &
                 (kc[:, None] >= col0[None]) & (kc[:, None] < col0[None] + 16))
        rr = np.clip(kr[:, None] - qr[None] + 7, 0, 14)
        cc = np.clip(kc[:, None] - qc[None] + 15, 0, 30)
        out[:, vid, :] = np.where(inwin, rpb_h[rr, cc], NEG)
    return out


def build_attn(nlb=128):
    NTK = (nlb + 2) * 128
    NKB = nlb + 2
    plan, variants = na_plan(nlb)
    nvar = len(variants)
    nc = bass.Bass("TRN2", target_bir_lowering=False)
    k = KB(nc)
    P = k.P
    qa_d = k.din("qaT", [64, NTK], BF16)
    ka_d = k.din("kaT", [64, NTK], BF16)
    va_d = k.din("va", [128, NKB, 65], BF16)
    ga_d = k.din("gaT", [64, NTK], BF16)
    qb_d = k.din("qbT", [64, NTK], BF16)
    kb_d = k.din("kbT", [64, NTK], BF16)
    vb_d = k.din("vb", [128, NKB, 65], BF16)
    gb_d = k.din("gbT", [64, NTK], BF16)
    bias_d = k.din("biasT", [128, nvar, 128], F32)
    cs_d = k.din("ropeC", [64, NTK], F32)
    sn_d = k.din("ropeS", [64, NTK], F32)
    gains_d = k.din("gains", [64, 2], F32)
    rmat_d = k.din("rmatT", [64, 64], F32)
    ident_d = k.din("ident", [128, 128], F32)
    sel_d = k.din("sel", [65, 64], F32)
    out_d = k.dout("mixT", [128, NTK], BF16)

    qT, qT_b = k.sb("qT", [64, NTK], BF16)
    kT, kT_b = k.sb("kT", [64, NTK], BF16)
    vE, vE_b = k.sb("vE", [128, NKB, 65], BF16)
    biasf, biasf_b = k.sb("biasf", [128, nvar, 128], F32)
    biasb, biasb_b = k.sb("biasb", [128, nvar, 128], BF16)
    identf, identf_b = k.sb("identf", [128, 128], F32)
    identb, identb_b = k.sb("identb", [128, 128], BF16)
    sel, sel_b = k.sb("sel_sb", [65, 64], F32)
    gains, gains_b = k.sb("gains_sb", [64, 2], F32)
    rmf, rmf_b = k.sb("rmf", [64, 64], F32)
    rmb, rmb_b = k.sb("rmb", [64, 64], BF16)
    ones64, ones64_b = k.sb("ones64", [64, 64], F32)
    eps_t, eps_b = k.sb("eps_t", [64, 1], F32)
    k.dma("sync", biasf[:], bias_d, w=[biasf_b])
    k.dma("sync", identf[:], ident_d, w=[identf_b])
    k.dma("sync", sel[:], sel_d, w=[sel_b])
    k.dma("sync", gains[:], gains_d, w=[gains_b])
    k.dma("sync", rmf[:], rmat_d, w=[rmf_b])
    k.V(lambda E: E.tensor_copy(out=biasb[:], in_=biasf[:]), [biasf_b], [biasb_b])
    k.V(lambda E: E.tensor_copy(out=identb[:], in_=identf[:]), [identf_b], [identb_b])
    k.V(lambda E: E.tensor_copy(out=rmb[:], in_=rmf[:]), [rmf_b], [rmb_b])
    k.V(lambda E: E.memset(ones64[:], 1.0 / 64), [], [ones64_b])
    k.V(lambda E: E.memset(eps_t[:], EPS), [], [eps_b])
    k.V(lambda E: E.tensor_scalar(out=gains[:, 0:1], in0=gains[:, 0:1], scalar1=0.125, scalar2=None, op0=ALU.mult), [gains_b], [gains_b])

    pS = [k.ps("pS%d" % i, [128, 512], F32) for i in range(4)]
    pacc = [k.ps("pacc%d" % i, [65, 512], F32) for i in range(2)]
    pmisc = [k.ps("pm%d" % i, [64, 512], F32) for i in range(2)]
    PT = [k.sb("PT%d" % i, [128, 512], BF16) for i in range(4)]
    accs = [k.sb("accs%d" % i, [65, 512], F32) for i in range(2)]
    gt = [k.sb("gt%d" % i, [64, 512], BF16) for i in range(2)]
    w1 = [k.sb("w1_%d" % i, [64, 512], F32) for i in range(2)]
    w2 = [k.sb("w2_%d" % i, [64, 512], F32) for i in range(2)]
    ot = [k.sb("ot%d" % i, [64, 512], BF16) for i in range(2)]
    cin = [k.sb("cin%d" % i, [64, 512], BF16) for i in range(2)]
    ctab = [k.sb("ctab%d" % i, [64, 512], F32) for i in range(2)]
    stab = [k.sb("stab%d" % i, [64, 512], F32) for i in range(2)]
    qnb = [k.sb("qnb%d" % i, [64, 512], BF16) for i in range(2)]

    state = {"fin": 0, "S": 0, "pre": 0}

    def finalize(pa, pab, g_d, row0, c0, n):
        i = state["fin"] % 2
        state["fin"] += 1
        at, ab = accs[i]
        gtt, gtb = gt[i]
        a1, a1b = w1[i]
        a2, a2b = w2[i]
        o, ob = ot[i]
        pm, pmb = pmisc[i]
        k.dma("sync", gtt[:, 0:n], g_d[:, c0:c0 + n], w=[gtb])
        k.A(lambda E: E.copy(out=at[:, 0:n], in_=pa[:, 0:n]), [pab], [ab])
        k.T(lambda E: E.matmul(pm[:, 0:n], sel[:], at[:, 0:n], start=True, stop=True), [sel_b, ab], [pmb])
        k.V(lambda E: E.reciprocal(out=a1[:, 0:n], in_=pm[:, 0:n]), [pmb], [a1b])
        k.V(lambda E: E.tensor_tensor(out=a1[:, 0:n], in0=a1[:, 0:n], in1=at[0:64, 0:n], op=ALU.mult), [a1b, ab], [a1b])
        k.A(lambda E: E.activation(out=a2[:, 0:n], in_=gtt[:, 0:n], func=AF.Exp, scale=-1.0), [gtb], [a2b])
        k.V(lambda E: E.tensor_scalar(out=a2[:, 0:n], in0=a2[:, 0:n], scalar1=1.0, scalar2=None, op0=ALU.add), [a2b], [a2b])
        k.V(lambda E: E.reciprocal(out=a2[:, 0:n], in_=a2[:, 0:n]), [a2b], [a2b])
        k.V(lambda E: E.tensor_tensor(out=a2[:, 0:n], in0=a2[:, 0:n], in1=gtt[:, 0:n], op=ALU.mult), [a2b, gtb], [a2b])
        k.V(lambda E: E.tensor_tensor(out=o[:, 0:n], in0=a1[:, 0:n], in1=a2[:, 0:n], op=ALU.mult), [a1b, a2b], [ob])
        k.dma("sync", out_d[row0:row0 + 64, c0:c0 + n], o[:, 0:n], r=[ob])

    def attend(qc0, n, kbs, pa, pab, first=True, last=True):
        pend = []
        nk = len(kbs)
        for idx, kb in enumerate(kbs):
            s = state["S"]
            state["S"] += 1
            pst, psb = pS[s % 4]
            ptt, ptb = PT[s % 4]
            k.T(lambda E, pst=pst, kb=kb: E.matmul(pst[:, 0:n], kT[:, kb * 128:(kb + 1) * 128], qT[:, qc0:qc0 + n], start=True, stop=True),
                [kT_b, qT_b], [psb])
            k.A(lambda E, pst=pst, ptt=ptt: E.activation(out=ptt[:, 0:n], in_=pst[:, 0:n], func=AF.Exp), [psb], [ptb])
            pend.append((idx, kb, ptt, ptb))
            if len(pend) > 2:
                j, kbj, pj, pjb = pend.pop(0)
                k.T(lambda E, kbj=kbj, pj=pj, j=j: E.matmul(pa[:, 0:n], vE[:, kbj, :], pj[:, 0:n], start=(first and j == 0), stop=(last and j == nk - 1)),
                    [vE_b, pjb], [pab])
        for (j, kbj, pj, pjb) in pend:
            k.T(lambda E, kbj=kbj, pj=pj, j=j: E.matmul(pa[:, 0:n], vE[:, kbj, :], pj[:, 0:n], start=(first and j == 0), stop=(last and j == nk - 1)),
                [vE_b, pjb], [pab])

    k.dma("sync", kT[:], ka_d, w=[kT_b])
    k.dma("sync", vE[:], va_d, w=[vE_b])
    k.dma("sync", qT[:], qa_d, w=[qT_b])
    k.V(lambda E: E.tensor_scalar(out=qT[:], in0=qT[:], scalar1=0.125, scalar2=None, op0=ALU.mult), [qT_b], [qT_b])
    ctxk = [nlb, nlb + 1]
    acc_i = 0
    for g0 in range(0, nlb, 4):
        pa, pab = pacc[acc_i % 2]
        acc_i += 1
        nq = min(4, nlb - g0)
        for mi in range(nq):
            m = g0 + mi
            lst = plan[m]
            nreg = len(lst) + 2
            s = state["S"]
            state["S"] += 2
            pA, pAb = pS[s % 4]
            pB, pBb = pS[(s + 1) % 4]
            tA, tAb = PT[s % 4]
            tB, tBb = PT[(s + 1) % 4]
            regs = []
            for j, (kb, var) in enumerate(lst + [(ctxk[0], None), (ctxk[1], None)]):
                pt_, pb_, tt_, tb_ = (pA, pAb, tA, tAb) if j < 4 else (pB, pBb, tB, tBb)
                jj = j % 4
                k.T(lambda E, pt_=pt_, kb=kb, jj=jj, m=m, var=var: E.matmul(pt_[:, jj * 128:(jj + 1) * 128], kT[:, kb * 128:(kb + 1) * 128], qT[:, m * 128:(m + 1) * 128], start=True, stop=(var is None)),
                    [kT_b, qT_b], [pb_])
                if var is not None:
                    k.T(lambda E, pt_=pt_, jj=jj, var=var: E.matmul(pt_[:, jj * 128:(jj + 1) * 128], identb[:], biasb[:, var, :], start=False, stop=True),
                        [identb_b, biasb_b], [pb_])
                regs.append((kb, tt_, tb_, jj))
            nA = min(4, nreg)
            k.A(lambda E, pA=pA, tA=tA, nA=nA: E.activation(out=tA[:, 0:nA * 128], in_=pA[:, 0:nA * 128], func=AF.Exp), [pAb], [tAb])
            if nreg > 4:
                nB = nreg - 4
                k.A(lambda E, pB=pB, tB=tB, nB=nB: E.activation(out=tB[:, 0:nB * 128], in_=pB[:, 0:nB * 128], func=AF.Exp), [pBb], [tBb])
            for j, (kb, tt_, tb_, jj) in enumerate(regs):
                k.T(lambda E, pa=pa, kb=kb, tt_=tt_, jj=jj, mi=mi, j=j, nreg=nreg: E.matmul(pa[:, mi * 128:(mi + 1) * 128], vE[:, kb, :], tt_[:, jj * 128:(jj + 1) * 128], start=(j == 0), stop=(j == nreg - 1)),
                    [vE_b, tb_], [pab])
        finalize(pa, pab, ga_d, 0, g0 * 128, nq * 128)
    pa, pab = pacc[acc_i % 2]
    acc_i += 1
    attend(nlb * 128, 256, ctxk, pa, pab)
    finalize(pa, pab, ga_d, 0, nlb * 128, 256)

    k.dma("sync", vE[:], vb_d, w=[vE_b])

    def prepass(src_d, dst, dst_b, gcol):
        for c0_ in range(0, NTK, 512):
            pre_chunk(src_d, dst, dst_b, gcol, c0_)

    def pre_chunk(src_d, dst, dst_b, gcol, c0):
        if True:
            n = min(512, NTK - c0)
            i = state["pre"] % 2
            state["pre"] += 1
            ci, cib = cin[i]
            ct, ctb = ctab[i]
            st_, stb = stab[i]
            qn, qnb_ = qnb[i]
            a1, a1b = w1[i]
            a2, a2b = w2[i]
            pm, pmb = pmisc[i]
            k.dma("sync", ci[:, 0:n], src_d[:, c0:c0 + n], w=[cib])
            k.dma("sync", ct[:, 0:n], cs_d[:, c0:c0 + n], w=[ctb])
            k.dma("sync", st_[:, 0:n], sn_d[:, c0:c0 + n], w=[stb])
            k.V(lambda E: E.tensor_tensor(out=a1[:, 0:n], in0=ci[:, 0:n], in1=ci[:, 0:n], op=ALU.mult), [cib], [a1b])
            k.T(lambda E: E.matmul(pm[:, 0:n], ones64[:], a1[:, 0:n], start=True, stop=True), [ones64_b, a1b], [pmb])
            k.A(lambda E: E.activation(out=a2[:, 0:n], in_=pm[:, 0:n], func=AF.Sqrt, bias=eps_t[:]), [pmb, eps_b], [a2b])
            k.V(lambda E: E.reciprocal(out=a2[:, 0:n], in_=a2[:, 0:n]), [a2b], [a2b])
            k.V(lambda E: E.scalar_tensor_tensor(out=qn[:, 0:n], in0=ci[:, 0:n], scalar=gains[:, gcol:gcol + 1], in1=a2[:, 0:n], op0=ALU.mult, op1=ALU.mult),
                [cib, gains_b, a2b], [qnb_])
            k.T(lambda E: E.matmul(pm[:, 0:n], rmb[:], qn[:, 0:n], start=True, stop=True), [rmb_b, qnb_], [pmb])
            k.V(lambda E: E.tensor_tensor(out=a1[:, 0:n], in0=qn[:, 0:n], in1=ct[:, 0:n], op=ALU.mult), [qnb_, ctb], [a1b])
            k.V(lambda E: E.tensor_tensor(out=a2[:, 0:n], in0=pm[:, 0:n], in1=st_[:, 0:n], op=ALU.mult), [pmb, stb], [a2b])
            k.G(lambda E: E.tensor_tensor(out=dst[:, c0:c0 + n], in0=a1[:, 0:n], in1=a2[:, 0:n], op=ALU.add), [a1b, a2b], [dst_b])

    prepass(kb_d, kT, kT_b, 1)
    prepass(qb_d, qT, qT_b, 0)
    allk = list(range(NKB))
    for c0 in range(0, nlb * 128, 512):
        n = min(512, nlb * 128 - c0)
        pa, pab = pacc[acc_i % 2]
        acc_i += 1
        attend(c0, n, allk, pa, pab)
        finalize(pa, pab, gb_d, 64, c0, n)
    pa, pab = pacc[acc_i % 2]
    acc_i += 1
    attend(nlb * 128, 256, ctxk, pa, pab)
    finalize(pa, pab, gb_d, 64, nlb * 128, 256)
    k.done()
    return nc


def rope_tables(nlb):
    n = nlb * 128
    t = np.arange(n)
    pos = np.stack([t // 64, t % 64], -1).astype(np.float32)
    inv = (10000.0 ** (-np.arange(16, dtype=np.float32) / 16)).astype(np.float32)
    ang = pos[:, :, None] * inv
    cos, sin = np.cos(ang), np.sin(ang)
    C = np.ones((64, n + 256), np.float32)
    S = np.zeros((64, n + 256), np.float32)
    for a in range(2):
        for hf in range(2):
            C[a * 32 + hf * 16:a * 32 + hf * 16 + 16, :n] = cos[:, a, :].T
            S[a * 32 + hf * 16:a * 32 + hf * 16 + 16, :n] = sin[:, a, :].T
    R = np.zeros((64, 64), np.float32)
    for a in range(2):
        for f in range(16):
            R[a * 32 + f, a * 32 + 16 + f] = -1.0
            R[a * 32 + 16 + f, a * 32 + f] = 1.0
    return C, S, np.ascontiguousarray(R.T)


import numpy as np

PC = {n: i for i, n in enumerate(
    ["mu_r", "mu_k", "mu_v", "k_k", "k_a", "w0_f", "w0_b", "a0_f", "a0_b", "r_k",
     "t0_v", "t1_v", "t2_v", "t0_x1", "t1_x1", "t2_x1", "t0_x2", "t1_x2", "t2_x2",
     "gn_w", "gn_b", "skip0", "skip1"])}
NPC = len(PC)
FO = {n: i for i, n in enumerate(
    ["w_f", "w_b", "kkn", "nb_f", "nb_b", "k_f", "k_b", "r", "v", "bonus", "sg_rw", "hv", "hx1", "hx2", "sg_hy"])}
NFO = len(FO)
FI = {n: i for i, n in enumerate(["r", "k", "v", "g_rw", "hv", "hx1", "hx2", "g_hy"])}
NFI = len(FI)


def seq_chunks(n_lat):
    ch = [(0, 0, 256)]
    for c0 in range(0, n_lat, 512):
        n = min(512, n_lat - c0)
        ch.append((258 + c0, 256 + c0, n))
    return ch


def build_rfeat(n_lat):
    T = 256 + n_lat
    TP = T + 4
    nc = bass.Bass("TRN2", target_bir_lowering=False)
    k = KB(nc)
    fin_d = k.din("fin", [NFI, 64, TP], F32)
    lora_d = k.din("lora", [128, TP], F32)
    pp_d = k.din("pp", [64, NPC], F32)
    mul_d = k.din("mu_lora", [128, 1], F32)
    wx_d = k.din("wx", [128, 4, 64], F32)
    fo_d = k.dout("fo", [NFO, 64, T], F32)

    pp = k.sb("pp_sb", [64, NPC], F32)
    npp = k.sb("npp_sb", [64, NPC], F32)
    mul = k.sb("mul_sb", [128, 1], F32)
    wx = k.sb("wx_sb", [128, 4, 64], F32)
    ones = k.sb("ones_sb", [64, 64], F32)
    tiny = k.sb("tiny_sb", [64, 1], F32)
    k.dma("sync", pp[0][:], pp_d, w=[pp[1]])
    k.dma("sync", mul[0][:], mul_d, w=[mul[1]])
    k.dma("sync", wx[0][:], wx_d, w=[wx[1]])
    h_memset(k, (ones[0][:], ones[1]), 1.0)
    h_memset(k, (tiny[0][:], tiny[1]), 1e-12)
    h_ts(k, (npp[0][:], npp[1]), (pp[0][:], pp[1]), -1.0, None, ALU.mult)
    h_ts(k, (npp[0][:, PC["r_k"]:PC["r_k"] + 1], npp[1]), (pp[0][:, PC["r_k"]:PC["r_k"] + 1], pp[1]), 0.5, None, ALU.mult)

    def pcol(name):
        return (pp[0][:, PC[name]:PC[name] + 1], pp[1])

    def ncol(name):
        return (npp[0][:, PC[name]:PC[name] + 1], npp[1])

    fin = [[k.sb("fin%d_%d" % (i, j), [64, 514], F32) for j in range(2)] for i in range(NFI)]
    lor = [k.sb("lor%d" % j, [128, 514], F32) for j in range(2)]
    fo = [[k.sb("fo%d_%d" % (i, j), [64, 512], F32) for j in range(2)] for i in range(NFO)]
    tm = [k.sb("tm%d" % i, [64, 512], F32) for i in range(8)]
    lt = [k.sb("lt%d" % i, [128, 512], F32) for i in range(3)]
    pz = [k.ps("pz%d" % i, [64, 512], F32) for i in range(6)]

    def chunk(ci, pc0, oc0, n):
        j = ci % 2

        def I(name, lo=1):
            t, b = fin[FI[name]][j]
            return (t[:, lo:lo + n], b)

        def O(name):
            t, b = fo[FO[name]][j]
            return (t[:, 0:n], b)

        def Tm(i):
            return (tm[i][0][:, 0:n], tm[i][1])

        def Lt(i):
            return (lt[i][0][:, 0:n], lt[i][1])

        def Pz(i):
            return (pz[i][0][:, 0:n], pz[i][1])

        for name, i in FI.items():
            t, b = fin[i][j]
            k.dma("sync", t[:, 0:n + 2], fin_d[i, :, pc0:pc0 + n + 2], w=[b])
        lt_, lb_ = lor[j]
        k.dma("sync", lt_[:, 0:n + 2], lora_d[:, pc0:pc0 + n + 2], w=[lb_])

        def shift(out, src_lo, src_mid, src_hi, mu, t):
            h_tt(k, t, src_lo, src_hi, ALU.add)
            h_stt(k, t, t, 0.5, src_mid, ALU.mult, ALU.subtract)
            h_stt(k, out, t, mu, src_mid, ALU.mult, ALU.add)

        rs, ks, vs = O("r"), Tm(0), O("v")
        shift(rs, I("r", 0), I("r", 1), I("r", 2), pcol("mu_r"), Tm(7))
        shift(ks, I("k", 0), I("k", 1), I("k", 2), pcol("mu_k"), Tm(7))
        shift(vs, I("v", 0), I("v", 1), I("v", 2), pcol("mu_v"), Tm(7))
        ls = Lt(0)
        shift(ls, (lt_[:, 0:n], lb_), (lt_[:, 1:n + 1], lb_), (lt_[:, 2:n + 2], lb_), (mul[0][:], mul[1]), Lt(2))
        lth = Lt(1)
        h_act(k, lth, ls, AF.Tanh)
        h_mm(k, Pz(0), (wx[0][:, 0, :], wx[1]), lth)
        h_mm(k, Pz(1), (wx[0][:, 1, :], wx[1]), lth)
        h_mm(k, Pz(2), (wx[0][:, 2, :], wx[1]), ls)
        h_mm(k, Pz(3), (wx[0][:, 3, :], wx[1]), ls)
        kk = Tm(1)
        h_ts(k, kk, ks, pcol("k_k"), None, ALU.mult)
        h_tt(k, Tm(2), kk, kk, ALU.mult)
        h_mm(k, Pz(4), (ones[0][:], ones[1]), Tm(2))
        h_act(k, Tm(2), Pz(4), AF.Sqrt, bias=(tiny[0][:], tiny[1]))
        h_recip(k, Tm(2), Tm(2))
        h_tt(k, O("kkn"), kk, Tm(2), ALU.mult)
        for d, sfx in enumerate(("f", "b")):
            h_sigmoid(k, Tm(3), Pz(d), Tm(3), bias=ncol("w0_" + sfx))
            h_act(k, O("w_" + sfx), Tm(3), AF.Exp, scale=-float(np.exp(-0.5)))
            a = Tm(4)
            h_sigmoid(k, a, Pz(2 + d), a, bias=ncol("a0_" + sfx))
            h_ts(k, Tm(5), a, -1.0, pcol("k_a"), ALU.add, ALU.mult)
            h_stt(k, O("k_" + sfx), Tm(5), 1.0, ks, ALU.add, ALU.mult)
            h_stt(k, O("nb_" + sfx), O("kkn"), -1.0, a, ALU.mult, ALU.mult)
        h_tt(k, Tm(5), O("k_f"), O("k_b"), ALU.add)
        h_stt(k, Tm(5), rs, ncol("r_k"), Tm(5), ALU.mult, ALU.mult)
        h_mm(k, Pz(5), (ones[0][:], ones[1]), Tm(5))
        h_tt(k, O("bonus"), Pz(5), vs, ALU.mult)
        h_silu(k, O("sg_rw"), I("g_rw", 1), Tm(6))
        h_silu(k, O("sg_hy"), I("g_hy", 1), Tm(6))
        for nm in ("v", "x1", "x2"):
            src = "h" + nm
            h_ts(k, Tm(6), I(src, 0), pcol("t0_" + nm), None, ALU.mult)
            h_stt(k, Tm(6), I(src, 1), pcol("t1_" + nm), Tm(6), ALU.mult, ALU.add)
            h_stt(k, O(src), I(src, 2), pcol("t2_" + nm), Tm(6), ALU.mult, ALU.add)
        for name, i in FO.items():
            t, b = fo[i][j]
            k.dma("sync", fo_d[i, :, oc0:oc0 + n], t[:, 0:n], r=[b])

    for ci, (pc0, oc0, n) in enumerate(seq_chunks(n_lat)):
        chunk(ci, pc0, oc0, n)
    k.done()
    return nc


import numpy as np

TWO_PI = float(2 * np.pi)


def build_filt(L):
    nc = bass.Bass("TRN2", target_bir_lowering=False)
    k = KB(nc)
    feats_d = k.din("featsT", [33, L], F32)
    tv_d = k.din("tvals", [1, L], F32)
    w1_d = k.din("w1", [33, 64], F32)
    w2_d = k.din("w2", [64, 64], F32)
    w3_d = k.din("w3c", [64, 256], F32)
    bb_d = k.din("b12", [64, 2], F32)
    b3_d = k.din("b3c", [128, 2], F32)
    nd_d = k.din("negdelta", [128, 1], F32)
    tf_d = k.dout("tapsF", [128, L], BF16)
    tb_d = k.dout("tapsB", [128, L], BF16)
    ssq_d = k.dout("ssq", [128, 1], F32)
    w1 = k.sb("w1s", [33, 64], F32); w2 = k.sb("w2s", [64, 64], F32); w3 = k.sb("w3s", [64, 256], F32)
    bb = k.sb("bbs", [64, 2], F32); b3 = k.sb("b3s", [128, 2], F32); nd = k.sb("nds", [128, 1], F32)
    for t, d in ((w1, w1_d), (w2, w2_d), (w3, w3_d), (bb, bb_d), (b3, b3_d), (nd, nd_d)):
        k.dma("sync", t[0][:], d, w=[t[1]])
    nch = (L + 511) // 512
    part = k.sb("part", [128, 2 * nch], F32)
    h_memset(k, (part[0][:], part[1]), 0.0)
    ft = [k.sb("ft%d" % i, [33, 512], F32) for i in range(2)]
    tv = [k.sb("tv%d" % i, [128, 512], F32) for i in range(2)]
    hx = [k.sb("hx%d" % i, [64, 512], F32) for i in range(2)]
    hi = k.sb("hi", [64, 512], I32)
    hf = k.sb("hf", [64, 512], F32)
    win = k.sb("win", [128, 512], F32)
    tF = [k.sb("tF%d" % i, [128, 512], F32) for i in range(2)]
    tB = [k.sb("tB%d" % i, [128, 512], F32) for i in range(2)]
    oF = [k.sb("oF%d" % i, [128, 512], BF16) for i in range(2)]
    oB = [k.sb("oB%d" % i, [128, 512], BF16) for i in range(2)]
    junk = k.sb("junkf", [128, 512], F32)
    ph = [k.ps("ph%d" % i, [64, 512], F32) for i in range(2)]
    pt = [k.ps("pt%d" % i, [128, 512], F32) for i in range(2)]

    def sin_layer(out, psum, bias, n):
        x = (hf[0][:, 0:n], hf[1])
        ki = (hi[0][:, 0:n], hi[1])
        h_ts(k, out, psum, bias, None, ALU.add)
        h_ts(k, ki, out, 1.0 / TWO_PI, None, ALU.mult)
        h_copy(k, x, ki)
        h_stt(k, out, x, -TWO_PI, out, ALU.mult, ALU.add)
        h_ts(k, out, out, -float(np.pi), float(np.pi), ALU.max, ALU.min)
        h_act(k, out, out, AF.Sin)

    for c in range(nch):
        c0 = c * 512
        n = min(512, L - c0)
        j = c % 2
        k.dma("sync", ft[j][0][:, 0:n], feats_d[:, c0:c0 + n], w=[ft[j][1]])
        k.dma("sync", tv[j][0][:, 0:n], tv_d[:, c0:c0 + n].partition_broadcast(128), w=[tv[j][1]])
        h_mm(k, (ph[0][0][:, 0:n], ph[0][1]), (w1[0][:], w1[1]), (ft[j][0][:, 0:n], ft[j][1]))
        h1 = (hx[0][0][:, 0:n], hx[0][1])
        sin_layer(h1, (ph[0][0][:, 0:n], ph[0][1]), (bb[0][:, 0:1], bb[1]), n)
        h_mm(k, (ph[1][0][:, 0:n], ph[1][1]), (w2[0][:], w2[1]), h1)
        h2 = (hx[1][0][:, 0:n], hx[1][1])
        sin_layer(h2, (ph[1][0][:, 0:n], ph[1][1]), (bb[0][:, 1:2], bb[1]), n)
        h_mm(k, (pt[0][0][:, 0:n], pt[0][1]), (w3[0][:, 0:128], w3[1]), h2)
        h_mm(k, (pt[1][0][:, 0:n], pt[1][1]), (w3[0][:, 128:256], w3[1]), h2)
        w_ = (win[0][:, 0:n], win[1])
        h_act(k, w_, (tv[j][0][:, 0:n], tv[j][1]), AF.Exp, scale=(nd[0][:], nd[1]))
        F_ = (tF[j][0][:, 0:n], tF[j][1])
        B_ = (tB[j][0][:, 0:n], tB[j][1])
        h_stt(k, F_, (pt[0][0][:, 0:n], pt[0][1]), (b3[0][:, 0:1], b3[1]), w_, ALU.add, ALU.mult)
        h_stt(k, B_, (pt[1][0][:, 0:n], pt[1][1]), (b3[0][:, 1:2], b3[1]), w_, ALU.add, ALU.mult)
        lo = 0
        if c == 0:
            h_tt(k, (tF[j][0][:, 0:1], tF[j][1]), (tF[j][0][:, 0:1], tF[j][1]), (tB[j][0][:, 0:1], tB[j][1]), ALU.add)
            lo = 1
        h_act(k, (junk[0][:, 0:n], junk[1]), F_, AF.Square, accum=(part[0][:, 2 * c:2 * c + 1], part[1]))
        h_act(k, (junk[0][:, lo:n], junk[1]), (tB[j][0][:, lo:n], tB[j][1]), AF.Square, accum=(part[0][:, 2 * c + 1:2 * c + 2], part[1]))
        h_copy(k, (oF[j][0][:, 0:n], oF[j][1]), F_)
        h_copy(k, (oB[j][0][:, 0:n], oB[j][1]), B_, eng="gpsimd")
        k.dma("sync", tf_d[:, c0:c0 + n], oF[j][0][:, 0:n], r=[oF[j][1]])
        k.dma("sync", tb_d[:, c0:c0 + n], oB[j][0][:, 0:n], r=[oB[j][1]])
    tot = k.sb("tot", [128, 1], F32)
    k.V(lambda E: E.tensor_reduce(out=tot[0][:], in_=part[0][:], axis=mybir.AxisListType.X, op=ALU.add), [part[1]], [tot[1]])
    k.dma("sync", ssq_d, tot[0][:], r=[tot[1]])
    k.done()
    return nc


def filt_consts(L):
    t_idx = np.arange(L, dtype=np.float32)
    t = (t_idx / np.float32(max(L - 1, 1))).astype(np.float32)
    bands = np.linspace(1e-4, 15, 16, dtype=np.float32)
    ang = (np.float32(2.0 * np.pi) * bands[None, :] * t_idx[:, None] / np.float32(L)).astype(np.float32)
    feats = np.concatenate([t[:, None], np.cos(ang), -np.sin(ang)], -1).astype(np.float32)
    deltas = np.linspace(np.log(1e-2) / 0.3, np.log(1e-2) / 1.5, 512, dtype=np.float32)
    return np.ascontiguousarray(feats.T), t.reshape(1, L).copy(), np.abs(deltas)


def filt_inputs(L, h, prm):
    featsT, tv, adel = filt_consts(L)
    sl = slice(64 * h, 64 * h + 64)
    w3 = prm["hy_w3"]; b3 = prm["hy_b3"]
    cols = []
    for side in range(2):
        for o in range(2):
            cols.append(np.arange(side * 1024 + o * 512 + 64 * h, side * 1024 + o * 512 + 64 * h + 64))
    cols = np.concatenate(cols)
    nd = -np.concatenate([adel[sl], adel[sl]]).reshape(128, 1).astype(np.float32)
    return {"featsT": featsT, "tvals": tv, "w1": prm["hy_w1"], "w2": prm["hy_w2"], "w3c": np.ascontiguousarray(w3[:, cols]),
            "b12": np.stack([prm["hy_b1"], prm["hy_b2"]], 1).astype(np.float32),
            "b3c": np.stack([b3[cols[:128]], b3[cols[128:]]], 1).astype(np.float32), "negdelta": nd}


def toeplitz_src(tapsF, tapsB):
    L = tapsF.shape[1]
    KL = np.zeros((128, 2 * L), tapsF.dtype)
    KL[:, 0:L - 1] = tapsB[:, :0:-1]
    KL[:, L - 1:2 * L - 1] = tapsF
    return KL


def build_hy(nb, T_read=0):
    L = 128 * nb
    W = 128 * (2 * nb - 1)
    nc = bass.Bass("TRN2", target_bir_lowering=False)
    k = KB(nc)
    kl_h = nc.dram_tensor("KL", [128, 2 * L], BF16, kind="ExternalInput")
    ssq_d = k.din("ssqT", [1, 128], F32)
    skip_d = k.din("skipT", [1, 128], F32)
    z_d = k.din("z1", [64, 128, nb], F32)
    x1_d = k.din("x1g", [64, 128, nb], F32)
    x2_d = k.din("x2g", [64, 128, nb], F32)
    sg_d = k.din("sgh", [64, 128, nb], F32)
    J_d = k.din("Jmat", [128, 128], F32)
    y_d = k.dout("yhy", [64, 128, nb], BF16)
    Jf = k.sb("Jf", [128, 128], F32); Jb = k.sb("Jb", [128, 128], BF16)
    nrm = k.sb("nrm", [128, 128], F32); skp = k.sb("skp", [128, 128], F32)
    k.dma("sync", Jf[0][:], J_d, w=[Jf[1]])
    h_copy(k, (Jb[0][:], Jb[1]), (Jf[0][:], Jf[1]))
    k.dma("sync", nrm[0][:], ssq_d.partition_broadcast(128), w=[nrm[1]])
    k.dma("sync", skp[0][:], skip_d.partition_broadcast(128), w=[skp[1]])
    h_act(k, (nrm[0][:], nrm[1]), (nrm[0][:], nrm[1]), AF.Sqrt)
    h_recip(k, (nrm[0][:], nrm[1]), (nrm[0][:], nrm[1]))

    if T_read:
        pp_d = k.din("pp", [64, NPC], F32)
        yf_d = k.din("yf", [64, T_read], F32)
        yb_d = k.din("yb", [64, T_read], F32)
        bo_d = k.din("bonus", [64, T_read], F32)
        sgr_d = k.din("sgr", [64, T_read], F32)
        mr_d = k.dout("mixrw", [64, T_read], BF16)
        pp = k.sb("pp_sb", [64, NPC], F32)
        k.dma("sync", pp[0][:], pp_d, w=[pp[1]])
        o64 = k.sb("o64", [64, 64], F32)
        h_memset(k, (o64[0][:], o64[1]), 1.0 / 64)
        geps = k.sb("geps", [64, 1], F32)
        h_memset(k, (geps[0][:], geps[1]), 64e-5)
        ra = [k.sb("ra%d" % i, [64, 512], F32) for i in range(2)]
        rb = [k.sb("rb%d" % i, [64, 512], F32) for i in range(2)]
        rc = [k.sb("rc%d" % i, [64, 512], F32) for i in range(2)]
        rd = [k.sb("rd%d" % i, [64, 512], F32) for i in range(2)]
        r1 = k.sb("r1", [64, 512], F32); r2 = k.sb("r2", [64, 512], F32)
        rob = [k.sb("rob%d" % i, [64, 512], BF16) for i in range(2)]
        pr = [k.ps("pr%d" % i, [64, 512], F32) for i in range(2)]
        for ci, c0 in enumerate(range(0, T_read, 512)):
            n = min(512, T_read - c0)
            j = ci % 2
            A = (ra[j][0][:, 0:n], ra[j][1]); B = (rb[j][0][:, 0:n], rb[j][1])
            C = (rc[j][0][:, 0:n], rc[j][1]); D = (rd[j][0][:, 0:n], rd[j][1])
            R1 = (r1[0][:, 0:n], r1[1]); R2 = (r2[0][:, 0:n], r2[1])
            P0 = (pr[0][0][:, 0:n], pr[0][1]); P1 = (pr[1][0][:, 0:n], pr[1][1])
            k.dma("scalar", A[0], yf_d[:, c0:c0 + n], w=[A[1]])
            k.dma("scalar", B[0], yb_d[:, c0:c0 + n], w=[B[1]])
            k.dma("scalar", C[0], bo_d[:, c0:c0 + n], w=[C[1]])
            k.dma("scalar", D[0], sgr_d[:, c0:c0 + n], w=[D[1]])
            h_tt(k, A, A, B, ALU.add)
            h_mm(k, P0, (o64[0][:], o64[1]), A)
            h_tt(k, R1, A, P0, ALU.subtract)
            h_tt(k, R2, R1, R1, ALU.mult)
            h_mm(k, P1, (o64[0][:], o64[1]), R2)
            h_act(k, R2, P1, AF.Sqrt, bias=(geps[0][:], geps[1]))
            h_recip(k, R2, R2)
            h_tt(k, R1, R1, R2, ALU.mult)
            h_ts(k, R1, R1, (pp[0][:, PC["gn_w"]:PC["gn_w"] + 1], pp[1]), (pp[0][:, PC["gn_b"]:PC["gn_b"] + 1], pp[1]), ALU.mult, ALU.add)
            h_tt(k, R1, R1, C, ALU.add)
            OB = (rob[j][0][:, 0:n], rob[j][1])
            h_tt(k, OB, R1, D, ALU.mult)
            k.dma("scalar", mr_d[:, c0:c0 + n], OB[0], r=[OB[1]])

    ksh = [k.sb("ksh%d" % i, [128, W], BF16) for i in range(2)]
    zt = [k.sb("zt%d" % i, [128, nb], F32) for i in range(2)]
    x1t = [k.sb("x1t%d" % i, [128, nb], F32) for i in range(2)]
    x2t = [k.sb("x2t%d" % i, [128, nb], F32) for i in range(2)]
    sgt = [k.sb("sgt%d" % i, [128, nb], F32) for i in range(2)]
    zb = k.sb("zb", [128, nb], BF16)
    zf = k.sb("zf", [128, nb], BF16)
    z2 = k.sb("z2", [128, nb], F32)
    t1 = k.sb("t1", [128, nb], F32)
    ot = [k.sb("oth%d" % i, [128, nb], BF16) for i in range(2)]
    pf = k.ps("pf", [128, nb], F32)
    py = [k.ps("pyc%d" % i, [128, nb], F32) for i in range(2)]
    order = [0] + [d for d in range(-(nb - 1), nb) if d != 0]
    cnt = 0
    for c in range(64):
        j = c % 2
        Z = (zt[j][0][:], zt[j][1]); X1 = (x1t[j][0][:], x1t[j][1]); X2 = (x2t[j][0][:], x2t[j][1]); SG = (sgt[j][0][:], sgt[j][1])
        k.dma("gpsimd", Z[0], z_d[c], w=[Z[1]])
        k.dma("gpsimd", X1[0], x1_d[c], w=[X1[1]])
        k.dma("gpsimd", X2[0], x2_d[c], w=[X2[1]])
        k.dma("gpsimd", SG[0], sg_d[c], w=[SG[1]])
        zin = Z
        for o in range(2):
            row = o * 64 + c
            kt, kb_ = ksh[cnt % 2]
            q = "sync" if cnt % 2 == 0 else "scalar"
            cnt += 1
            src = bass.AP(kl_h, row * 2 * L, [[1, 128], [1, W]])
            k.dma(q, kt[:], src, w=[kb_])
            h_copy(k, (zb[0][:], zb[1]), zin, eng="gpsimd")
            h_mm(k, (pf[0][:], pf[1]), (Jb[0][:], Jb[1]), (zb[0][:], zb[1]))
            h_copy(k, (zf[0][:], zf[1]), (pf[0][:], pf[1]), eng="scalar")
            pyt, pyb = py[o]
            for ii, Dd in enumerate(order):
                e = Dd + nb - 1
                S0, S1 = max(0, -Dd), min(nb, nb - Dd)
                k.T(lambda E, pyt=pyt, kt=kt, e=e, S0=S0, S1=S1, Dd=Dd, ii=ii: E.matmul(pyt[:, S0 + Dd:S1 + Dd], kt[:, 128 * e:128 * e + 128], zf[0][:, S0:S1], start=(ii == 0), stop=(ii == len(order) - 1)),
                    [kb_, zf[1]], [pyb])
            ncol = (nrm[0][:, row:row + 1], nrm[1])
            scol = (skp[0][:, row:row + 1], skp[1])
            h_ts(k, (t1[0][:], t1[1]), (pyt[:], pyb), ncol, None, ALU.mult)
            h_stt(k, (t1[0][:], t1[1]), zin, scol, (t1[0][:], t1[1]), ALU.mult, ALU.add)
            if o == 0:
                h_tt(k, (z2[0][:], z2[1]), (t1[0][:], t1[1]), X1, ALU.mult)
                zin = (z2[0][:], z2[1])
            else:
                O_ = (ot[j][0][:], ot[j][1])
                h_tt(k, (t1[0][:], t1[1]), (t1[0][:], t1[1]), X2, ALU.mult)
                h_tt(k, O_, (t1[0][:], t1[1]), SG, ALU.mult, eng="gpsimd")
                k.dma("gpsimd", y_d[c], O_[0], r=[O_[1]])
    k.done()
    return nc


import numpy as np


def rfeat_inputs(u_ctx, u_lat, h, prm):
    n = u_lat.shape[0]
    def fm_pad(c0, w=64):
        a = np.zeros((w, 256 + n + 4), np.float32)
        a[:, 1:257] = u_ctx[:, c0:c0 + w].T
        a[:, 259:259 + n] = u_lat[:, c0:c0 + w].T
        return a
    cols = {"r": 64 * h, "k": 512 + 64 * h, "v": 1024 + 64 * h, "g_rw": 1664 + 64 * h,
            "hv": 2176 + 64 * h, "hx1": 2688 + 64 * h, "hx2": 3200 + 64 * h, "g_hy": 3712 + 64 * h}
    fin = np.stack([fm_pad(cols[nm]) for nm in FI], 0)
    lora = fm_pad(1536, 128)
    mu = prm["rwkv_mu"]
    sl = slice(64 * h, 64 * h + 64)
    pp = np.zeros((64, NPC), np.float32)
    pp[:, PC["mu_r"]] = mu[sl]
    pp[:, PC["mu_k"]] = mu[512 + 64 * h:512 + 64 * h + 64]
    pp[:, PC["mu_v"]] = mu[1024 + 64 * h:1024 + 64 * h + 64]
    pp[:, PC["k_k"]] = prm["rwkv_k_k"][sl]
    pp[:, PC["k_a"]] = prm["rwkv_k_a"][sl]
    pp[:, PC["w0_f"]] = prm["rwkv_w0"][0][sl]
    pp[:, PC["w0_b"]] = prm["rwkv_w0"][1][sl]
    pp[:, PC["a0_f"]] = prm["rwkv_a0"][0][sl]
    pp[:, PC["a0_b"]] = prm["rwkv_a0"][1][sl]
    pp[:, PC["r_k"]] = prm["rwkv_r_k"][h]
    for ai, nm in enumerate(("v", "x1", "x2")):
        for t in range(3):
            pp[:, PC["t%d_%s" % (t, nm)]] = prm["hy_short"][t][512 * ai + 64 * h:512 * ai + 64 * h + 64]
    pp[:, PC["gn_w"]] = prm["rwkv_gn_w"][sl]
    pp[:, PC["gn_b"]] = prm["rwkv_gn_b"][sl]
    pp[:, PC["skip0"]] = prm["hy_skip"][0][sl]
    pp[:, PC["skip1"]] = prm["hy_skip"][1][sl]
    wx = np.zeros((128, 4, 64), np.float32)
    wx[0:32, 0] = prm["rwkv_w_up"][0][:, sl]
    wx[32:64, 1] = prm["rwkv_w_up"][1][:, sl]
    wx[64:96, 2] = prm["rwkv_a_up"][0][:, sl]
    wx[96:128, 3] = prm["rwkv_a_up"][1][:, sl]
    return {"fin": fin, "lora": lora, "pp": pp, "mu_lora": mu[1536:1664].reshape(128, 1).copy(), "wx": wx}


import numpy as np

TC = 16


def h_tr(k, out, a, ident):
    oa, ob = out; aa, ab = a; ia, ib = ident
    return k.P.op("tensor", lambda E: E.transpose(oa, aa, ia), [ab, ib], [ob])


def build_fused(n):
    nlb = n // 128
    NKB = nlb + 2
    NTK = NKB * 128
    T = 256 + n
    TP = T + 4
    nc = bass.Bass("TRN2", target_bir_lowering=False)
    k = KB(nc)
    P = k.P
    plan, variants = na_plan(nlb)
    nvar = len(variants)

    def scratch(name, shape, dt):
        return nc.dram_tensor(name, list(shape), dt).ap()

    x_d = k.din("x", [n, D], F32)
    ctx_d = k.din("ctx", [256, D], F32)
    ccols_d = k.din("c_cols", [128, 16], F32)
    ident_d = k.din("ident", [128, 128], F32)
    J_d = k.din("Jmat", [128, 128], F32)
    fng_d = k.din("final_norm", [1, D], F32)
    L_in = []
    for l in range(4):
        attn = l % 2 == 0
        d = {"norm_g": k.din("norm_g%d" % l, [1, D], F32), "ada_w": k.din("ada_w%d" % l, [D, 3 * D], F32),
             "ada_b": k.din("ada_b%d" % l, [1, 3 * D], F32), "w_in": k.din("w_in%d" % l, [D, 512 if attn else 640], F32),
             "w_out": k.din("w_out%d" % l, [D, D], F32)}
        if attn:
            d["biasT"] = k.din("biasT%d" % l, [128, nvar, 128], F32)
            d["gains"] = k.din("gains%d" % l, [64, 2], F32)
        else:
            d["pp"] = k.din("pp%d" % l, [64, NPC], F32)
            d["mu_lora"] = k.din("mu_lora%d" % l, [128, 1], F32)
            d["wx"] = k.din("wx%d" % l, [128, 4, 64], F32)
            d["hw1"] = k.din("hw1_%d" % l, [33, 64], F32)
            d["hw2"] = k.din("hw2_%d" % l, [64, 64], F32)
            d["hw3"] = k.din("hw3_%d" % l, [64, 256], F32)
            d["hb12"] = k.din("hb12_%d" % l, [64, 2], F32)
            d["hb3"] = k.din("hb3_%d" % l, [128, 2], F32)
            d["skipT"] = k.din("skipT%d" % l, [1, 128], F32)
        L_in.append(d)
    ropeC_d = k.din("ropeC", [64, NTK], F32)
    ropeS_d = k.din("ropeS", [64, NTK], F32)
    rmat_d = k.din("rmatT", [64, 64], F32)
    sel_d = k.din("sel", [65, 64], F32)
    fconst = {}
    for Lf in (n, 256):
        fconst[Lf] = {"featsT": k.din("featsT%d" % Lf, [33, Lf], F32), "featsTr": k.din("featsTr%d" % Lf, [33, Lf], F32),
                      "tv": k.din("tv%d" % Lf, [1, Lf], F32), "tvr": k.din("tvr%d" % Lf, [1, Lf], F32)}
    nd_d = k.din("negdelta", [128, 1], F32)
    sel2_d = k.din("sel2", [2, 128], F32)
    y_out = k.dout("y_out", [n, D], F32)

    xres = scratch("xres", [NTK, D], F32)
    gates_s = scratch("gates_s", [128, 2, D], F32)
    mix_send = scratch("mix_send", [128, NTK], BF16)
    mix_all = scratch("mix_all", [1024, NTK], BF16)
    A_s = {nm: scratch("a_" + nm, [64, NTK], BF16) for nm in ("qaT", "kaT", "gaT", "qbT", "kbT", "gbT")}
    va_s = scratch("va_s", [128, NKB, 65], BF16)
    vb_s = scratch("vb_s", [128, NKB, 65], BF16)
    fin_s = scratch("fin_s", [NFI, 64, TP], F32)
    lora_s = scratch("lora_s", [128, TP], F32)
    fo_s = scratch("fo_s", [NFO, 64, T], F32)
    bc32_s = scratch("bc32_s", [2, T, 66], F32)
    bc16_s = scratch("bc16_s", [2, T, 256], BF16)
    vT2_s = scratch("vT2_s", [128, T], F32)
    yT2_s = scratch("yT2_s", [128, T], F32)
    kl_h = {Lf: nc.dram_tensor("KL%d" % Lf, [128, 2 * Lf], BF16) for Lf in (n, 256)}
    ssq_s = {Lf: scratch("ssq%d" % Lf, [1, 128], F32) for Lf in (n, 256)}

    identf = k.sb("identf", [128, 128], F32)
    identb = k.sb("identb", [128, 128], BF16)
    Jf = k.sb("Jf", [128, 128], F32)
    Jb = k.sb("Jb", [128, 128], BF16)
    zer = k.sb("zer", [128, 8], F32)
    k.dma("sync", identf[0][:], ident_d, w=[identf[1]])
    k.dma("sync", Jf[0][:], J_d, w=[Jf[1]])
    h_copy(k, (identb[0][:], identb[1]), (identf[0][:], identf[1]))
    h_copy(k, (Jb[0][:], Jb[1]), (Jf[0][:], Jf[1]))
    h_memset(k, (zer[0][:], zer[1]), 0.0)
    ID = (identf[0][:], identf[1])
    IDB = (identb[0][:], identb[1])
    for col in (0, 257, 258, TP - 1):
        for i in range(NFI):
            k.dma("sync", fin_s[i, :, col:col + 1], zer[0][0:64, 0:1], r=[zer[1]], allow_slow_non_contiguous=True)
        k.dma("sync", lora_s[:, col:col + 1], zer[0][:, 0:1], r=[zer[1]], allow_slow_non_contiguous=True)

    def phase_tok(l, final=False):
        has_prev = l > 0
        attn = (l % 2 == 0) and not final
        k.begin_phase()
        eps_t = k.sb("eps_t", [128, 1], F32)
        h_memset(k, (eps_t[0][:], eps_t[1]), EPS)
        if has_prev:
            wo = k.sb("wo", [128, 8, D], BF16)
            for kc in range(8):
                k.dma("gpsimd", wo[0][:, kc, :], L_in[l - 1]["w_out"][kc * 128:(kc + 1) * 128, :], w=[wo[1]])
            gp = k.sb("gp", [128, 2, D], F32)
            k.dma("sync", gp[0][:], gates_s, w=[gp[1]])
        if final:
            fg = k.sb("fg", [128, D], F32)
            k.dma("sync", fg[0][:], fng_d.partition_broadcast(128), w=[fg[1]])
        else:
            Li = L_in[l]
            ncol = 512 if attn else 640
            wi = k.sb("wi", [128, 8, ncol], BF16)
            for kc in range(8):
                k.dma("gpsimd", wi[0][:, kc, :], Li["w_in"][kc * 128:(kc + 1) * 128, :], w=[wi[1]])
            mod = k.sb("mod", [128, 2, 3 * D], F32)
            Gt = k.sb("Gt", [128, 2, D], F32)
            k.begin_phase()
            cc = k.sb("cc", [128, 16], F32)
            rep = k.sb("rep", [128, 16, 128], F32)
            ones = k.sb("ones", [128, 128], F32)
            ab = k.sb("ab", [128, 3 * D], F32)
            ng = k.sb("ng", [128, D], F32)
            aw = [k.sb("aw%d" % i, [128, 8, 512], F32) for i in range(2)]
            pa = [k.ps("pa%d" % i, [128, 512], F32) for i in range(2)]
            k.dma("sync", cc[0][:], ccols_d, w=[cc[1]])
            k.dma("sync", ab[0][:], Li["ada_b"].partition_broadcast(128), w=[ab[1]])
            k.dma("sync", ng[0][:], Li["norm_g"].partition_broadcast(128), w=[ng[1]])
            h_act(k, (cc[0][:], cc[1]), (cc[0][:], cc[1]), AF.Silu)
            h_memset(k, (ones[0][:], ones[1]), 1.0)
            for j in range(16):
                h_ts(k, (rep[0][:, j, :], rep[1]), (ones[0][:], ones[1]), (cc[0][:, j:j + 1], cc[1]), None, ALU.mult)
            for g in range(6):
                awt, awb = aw[g % 2]
                k.dma("sync", awt[:], Li["ada_w"][:, g * 512:(g + 1) * 512].rearrange("(k p) n -> p k n", p=128), w=[awb])
                for v in range(2):
                    for kc in range(8):
                        h_mm(k, (pa[v][0][:], pa[v][1]), (rep[0][:, v * 8 + kc, :], rep[1]), (awt[:, kc, :], awb), start=(kc == 0), stop=(kc == 7))
                    h_tt(k, (mod[0][:, v, g * 512:(g + 1) * 512], mod[1]), (pa[v][0][:], pa[v][1]), (ab[0][:, g * 512:(g + 1) * 512], ab[1]), ALU.add)
            for v in range(2):
                h_stt(k, (Gt[0][:, v, :], Gt[1]), (mod[0][:, v, D:2 * D], mod[1]), 1.0, (ng[0][:], ng[1]), ALU.add, ALU.mult)
            k.end_phase()
        xt = [k.sb("xt%d" % i, [128, D], F32) for i in range(2)]
        tmp = [k.sb("tmp%d" % i, [128, D], F32) for i in range(2)]
        ss = [k.sb("ss%d" % i, [128, 4], F32) for i in range(2)]
        junk = k.sb("junk", [128, D], BF16)
        if has_prev:
            mt = [k.sb("mt%d" % i, [128, 8, 128], BF16) for i in range(2)]
            py = [k.ps("py%d" % i, [128, 512], F32) for i in range(2)]
        if not final:
            xm = [k.sb("xm%d" % i, [128, D], BF16) for i in range(2)]
            xmT = [k.sb("xmT%d" % i, [128, 8, 512], BF16) for i in range(2)]
            ptr = [k.ps("ptr%d" % i, [128, 8, 128], BF16) for i in range(2)]
            pu = [k.ps("pu%d" % i, [128, 512], F32) for i in range(2)]
            odt = BF16 if attn else F32
            ut = [k.sb("ut%d" % i, [128, 512], odt) for i in range(3)]
            if attn:
                vt = [k.sb("vt%d" % i, [128, 2, 65], BF16) for i in range(2)]
                for i in range(2):
                    h_memset(k, (vt[i][0][:], vt[i][1]), 1.0)
        bcount = 0
        ucount = 0
        nsb = (NTK + 511) // 512
        for sbi in range(nsb):
            t0 = sbi * 512
            ntok = min(512, NTK - t0)
            nblk = ntok // 128
            is_ctx = t0 >= n
            v = 1 if is_ctx else 0
            if not final:
                xTt, xTb = xmT[sbi % 2]
            for blk in range(nblk):
                b = t0 // 128 + blk
                X = (xt[bcount % 2][0][:], xt[bcount % 2][1])
                TT = (tmp[bcount % 2][0][:], tmp[bcount % 2][1])
                sst, ssb = ss[bcount % 2]
                if l <= 1:
                    src = ctx_d[(b - nlb) * 128:(b - nlb + 1) * 128, :] if is_ctx else x_d[b * 128:(b + 1) * 128, :]
                else:
                    src = xres[b * 128:(b + 1) * 128, :]
                k.dma("sync", X[0], src, w=[X[1]])
                if has_prev:
                    mtt, mtb = mt[bcount % 2]
                    k.dma("sync", mtt[:], mix_all[:, b * 128:(b + 1) * 128].rearrange("(k p) t -> p k t", p=128), w=[mtb])
                    for hh in range(2):
                        for kc in range(8):
                            h_mm(k, (py[hh][0][:], py[hh][1]), (mtt[:, kc, :], mtb), (wo[0][:, kc, hh * 512:(hh + 1) * 512], wo[1]), start=(kc == 0), stop=(kc == 7))
                        h_tt(k, (tmp[bcount % 2][0][:, hh * 512:(hh + 1) * 512], TT[1]), (py[hh][0][:], py[hh][1]), (gp[0][:, v, hh * 512:(hh + 1) * 512], gp[1]), ALU.mult)
                    h_tt(k, X, TT, X, ALU.add, eng="gpsimd")
                    if not final:
                        k.dma("sync", xres[b * 128:(b + 1) * 128, :], X[0], r=[X[1]])
                h_act(k, (junk[0][:], junk[1]), X, AF.Square, accum=(sst[:, 0:1], ssb))
                h_act(k, (sst[:, 1:2], ssb), (sst[:, 0:1], ssb), AF.Sqrt, scale=1.0 / D, bias=(eps_t[0][:], eps_t[1]))
                h_recip(k, (sst[:, 2:3], ssb), (sst[:, 1:2], ssb))
                if final:
                    if not is_ctx:
                        h_stt(k, TT, X, (sst[:, 2:3], ssb), (fg[0][:], fg[1]), ALU.mult, ALU.mult)
                        k.dma("sync", y_out[b * 128:(b + 1) * 128, :], TT[0], r=[TT[1]])
                    bcount += 1
                    continue
                XM = (xm[bcount % 2][0][:], xm[bcount % 2][1])
                h_stt(k, TT, X, (sst[:, 2:3], ssb), (Gt[0][:, v, :], Gt[1]), ALU.mult, ALU.mult)
                h_tt(k, XM, TT, (mod[0][:, v, 0:D], mod[1]), ALU.add, eng="gpsimd")
                ptt, ptb = ptr[bcount % 2]
                for kc in range(8):
                    h_tr(k, (ptt[:, kc, :], ptb), (xm[bcount % 2][0][:, kc * 128:(kc + 1) * 128], XM[1]), IDB)
                h_copy(k, (xTt[:, :, blk * 128:(blk + 1) * 128], xTb), (ptt[:], ptb), eng="scalar")
                if attn:
                    put, pub = pu[ucount % 2]
                    vtt, vtb = vt[ucount % 2]
                    ucount += 1
                    for kc in range(8):
                        h_mm(k, (put[:, 0:128], pub), (xTt[:, kc, blk * 128:(blk + 1) * 128], xTb), (wi[0][:, kc, 384:512], wi[1]), start=(kc == 0), stop=(kc == 7))
                    h_copy(k, (vtt[:, :, 0:64], vtb), (put[:, 0:128].rearrange("p (a c) -> p a c", a=2), pub))
                    k.dma("sync", va_s[:, b, :], vtt[:, 0, :], r=[vtb])
                    k.dma("sync", vb_s[:, b, :], vtt[:, 1, :], r=[vtb])
                bcount += 1
            if final:
                continue
            ncg = 3 if attn else 5
            for cg in range(ncg):
                put, pub = pu[ucount % 2]
                utt, utb = ut[ucount % 3]
                ucount += 1
                for kc in range(8):
                    h_mm(k, (put[:, 0:ntok], pub), (wi[0][:, kc, cg * 128:(cg + 1) * 128], wi[1]), (xTt[:, kc, 0:ntok], xTb), start=(kc == 0), stop=(kc == 7))
                h_copy(k, (utt[:, 0:ntok], utb), (put[:, 0:ntok], pub), eng=("vector" if cg % 2 == 0 else "scalar"))
                if attn:
                    names = [("qaT", "kaT"), ("gaT", "qbT"), ("kbT", "gbT")][cg]
                    k.dma("sync", A_s[names[0]][:, t0:t0 + ntok], utt[0:64, 0:ntok], r=[utb])
                    k.dma("sync", A_s[names[1]][:, t0:t0 + ntok], utt[64:128, 0:ntok], r=[utb])
                else:
                    pc = (1 + (t0 - n)) if is_ctx else (259 + t0)
                    if cg < 4:
                        k.dma("sync", fin_s[2 * cg, :, pc:pc + ntok], utt[0:64, 0:ntok], r=[utb])
                        k.dma("sync", fin_s[2 * cg + 1, :, pc:pc + ntok], utt[64:128, 0:ntok], r=[utb])
                    else:
                        k.dma("sync", lora_s[:, pc:pc + ntok], utt[:, 0:ntok], r=[utb])
        if not final:
            k.dma("sync", gates_s, mod[0][:, :, 2 * D:3 * D], r=[mod[1]])
        k.end_phase()

    def phase_attn(l):
        Li = L_in[l]
        k.begin_phase()
        qT = k.sb("qT", [64, NTK], BF16); kT = k.sb("kT", [64, NTK], BF16)
        vE = k.sb("vE", [128, NKB, 65], BF16)
        biasf = k.sb("biasf", [128, nvar, 128], F32); biasb = k.sb("biasb", [128, nvar, 128], BF16)
        sel = k.sb("sel_sb", [65, 64], F32); gains = k.sb("gains_sb", [64, 2], F32)
        rmf = k.sb("rmf", [64, 64], F32); rmb = k.sb("rmb", [64, 64], BF16)
        ones64 = k.sb("ones64", [64, 64], F32); eps_t = k.sb("eps_a", [64, 1], F32)
        k.dma("sync", biasf[0][:], Li["biasT"], w=[biasf[1]])
        k.dma("sync", sel[0][:], sel_d, w=[sel[1]])
        k.dma("sync", gains[0][:], Li["gains"], w=[gains[1]])
        k.dma("sync", rmf[0][:], rmat_d, w=[rmf[1]])
        h_copy(k, (biasb[0][:], biasb[1]), (biasf[0][:], biasf[1]))
        h_copy(k, (rmb[0][:], rmb[1]), (rmf[0][:], rmf[1]))
        h_memset(k, (ones64[0][:], ones64[1]), 1.0 / 64)
        h_memset(k, (eps_t[0][:], eps_t[1]), EPS)
        h_ts(k, (gains[0][:, 0:1], gains[1]), (gains[0][:, 0:1], gains[1]), 0.125, None, ALU.mult)
        pS = [k.ps("pS%d" % i, [128, 512], F32) for i in range(4)]
        pacc = [k.ps("pacc%d" % i, [65, 512], F32) for i in range(2)]
        pmisc = [k.ps("pm%d" % i, [64, 512], F32) for i in range(2)]
        PT = [k.sb("PT%d" % i, [128, 512], BF16) for i in range(4)]
        accs = [k.sb("accs%d" % i, [65, 512], F32) for i in range(2)]
        gt = [k.sb("gt%d" % i, [64, 512], BF16) for i in range(2)]
        w1 = [k.sb("w1_%d" % i, [64, 512], F32) for i in range(2)]
        w2 = [k.sb("w2_%d" % i, [64, 512], F32) for i in range(2)]
        ot = [k.sb("ot%d" % i, [64, 512], BF16) for i in range(2)]
        cin = [k.sb("cin%d" % i, [64, 512], BF16) for i in range(2)]
        ctab = [k.sb("ctab%d" % i, [64, 512], F32) for i in range(2)]
        stab = [k.sb("stab%d" % i, [64, 512], F32) for i in range(2)]
        qnb = [k.sb("qnb%d" % i, [64, 512], BF16) for i in range(2)]
        st = {"fin": 0, "S": 0, "pre": 0}

        def finalize(pa, g_d, row0, c0, nn):
            i = st["fin"] % 2
            st["fin"] += 1
            at = (accs[i][0][:, 0:nn], accs[i][1]); G = (gt[i][0][:, 0:nn], gt[i][1])
            a1 = (w1[i][0][:, 0:nn], w1[i][1]); a2 = (w2[i][0][:, 0:nn], w2[i][1])
            o = (ot[i][0][:, 0:nn], ot[i][1]); pm = (pmisc[i][0][:, 0:nn], pmisc[i][1])
            k.dma("sync", G[0], g_d[:, c0:c0 + nn], w=[G[1]])
            h_copy(k, at, (pa[0][:, 0:nn], pa[1]), eng="scalar")
            h_mm(k, pm, (sel[0][:], sel[1]), at)
            h_recip(k, a1, pm)
            h_tt(k, a1, a1, (accs[i][0][0:64, 0:nn], accs[i][1]), ALU.mult)
            h_silu(k, a2, G, a2)
            h_tt(k, o, a1, a2, ALU.mult)
            k.dma("sync", mix_send[row0:row0 + 64, c0:c0 + nn], o[0], r=[o[1]])

        def attend(qc0, nn, kbs, pa):
            pend = []
            nk = len(kbs)

            def pv(j, kbj, pj):
                h_mm(k, (pa[0][:, 0:nn], pa[1]), (vE[0][:, kbj, :], vE[1]), pj, start=(j == 0), stop=(j == nk - 1))
            for idx, kb in enumerate(kbs):
                s = st["S"]
                st["S"] += 1
                psx = (pS[s % 4][0][:, 0:nn], pS[s % 4][1])
                ptx = (PT[s % 4][0][:, 0:nn], PT[s % 4][1])
                h_mm(k, psx, (kT[0][:, kb * 128:(kb + 1) * 128], kT[1]), (qT[0][:, qc0:qc0 + nn], qT[1]))
                h_act(k, ptx, psx, AF.Exp)
                pend.append((idx, kb, ptx))
                if len(pend) > 2:
                    pv(*pend.pop(0))
            for it in pend:
                pv(*it)

        k.dma("sync", kT[0][:], A_s["kaT"], w=[kT[1]])
        k.dma("sync", vE[0][:], va_s, w=[vE[1]])
        k.dma("sync", qT[0][:], A_s["qaT"], w=[qT[1]])
        h_ts(k, (qT[0][:], qT[1]), (qT[0][:], qT[1]), 0.125, None, ALU.mult)
        ctxk = [nlb, nlb + 1]
        acc_i = 0
        for g0 in range(0, nlb, 4):
            pa = pacc[acc_i % 2]
            acc_i += 1
            nq = min(4, nlb - g0)
            for mi in range(nq):
                m = g0 + mi
                lst = plan[m]
                nreg = len(lst) + 2
                s = st["S"]
                st["S"] += 2
                banks = [(pS[s % 4], PT[s % 4]), (pS[(s + 1) % 4], PT[(s + 1) % 4])]
                regs = []
                for j, (kb, var) in enumerate(lst + [(ctxk[0], None), (ctxk[1], None)]):
                    (pt_, pb_), (tt_, tb_) = banks[j // 4]
                    jj = j % 4
                    h_mm(k, (pt_[:, jj * 128:(jj + 1) * 128], pb_), (kT[0][:, kb * 128:(kb + 1) * 128], kT[1]), (qT[0][:, m * 128:(m + 1) * 128], qT[1]), start=True, stop=(var is None))
                    if var is not None:
                        h_mm(k, (pt_[:, jj * 128:(jj + 1) * 128], pb_), IDB, (biasb[0][:, var, :], biasb[1]), start=False, stop=True)
                    regs.append((kb, tt_, tb_, jj))
                nA = min(4, nreg)
                h_act(k, (banks[0][1][0][:, 0:nA * 128], banks[0][1][1]), (banks[0][0][0][:, 0:nA * 128], banks[0][0][1]), AF.Exp)
                if nreg > 4:
                    nB = nreg - 4
                    h_act(k, (banks[1][1][0][:, 0:nB * 128], banks[1][1][1]), (banks[1][0][0][:, 0:nB * 128], banks[1][0][1]), AF.Exp)
                for j, (kb, tt_, tb_, jj) in enumerate(regs):
                    h_mm(k, (pa[0][:, mi * 128:(mi + 1) * 128], pa[1]), (vE[0][:, kb, :], vE[1]), (tt_[:, jj * 128:(jj + 1) * 128], tb_), start=(j == 0), stop=(j == nreg - 1))
            finalize(pa, A_s["gaT"], 0, g0 * 128, nq * 128)
        pa = pacc[acc_i % 2]
        acc_i += 1
        attend(nlb * 128, 256, ctxk, pa)
        finalize(pa, A_s["gaT"], 0, nlb * 128, 256)
        k.dma("sync", vE[0][:], vb_s, w=[vE[1]])

        def pre_chunk(src_d, dst, gcol, c0):
            nn = min(512, NTK - c0)
            i = st["pre"] % 2
            st["pre"] += 1
            ci = (cin[i][0][:, 0:nn], cin[i][1]); ct = (ctab[i][0][:, 0:nn], ctab[i][1]); sn = (stab[i][0][:, 0:nn], stab[i][1])
            qn = (qnb[i][0][:, 0:nn], qnb[i][1]); a1 = (w1[i][0][:, 0:nn], w1[i][1]); a2 = (w2[i][0][:, 0:nn], w2[i][1])
            pm = (pmisc[i][0][:, 0:nn], pmisc[i][1])
            k.dma("sync", ci[0], src_d[:, c0:c0 + nn], w=[ci[1]])
            k.dma("sync", ct[0], ropeC_d[:, c0:c0 + nn], w=[ct[1]])
            k.dma("sync", sn[0], ropeS_d[:, c0:c0 + nn], w=[sn[1]])
            h_tt(k, a1, ci, ci, ALU.mult)
            h_mm(k, pm, (ones64[0][:], ones64[1]), a1)
            h_act(k, a2, pm, AF.Sqrt, bias=(eps_t[0][:], eps_t[1]))
            h_recip(k, a2, a2)
            h_stt(k, qn, ci, (gains[0][:, gcol:gcol + 1], gains[1]), a2, ALU.mult, ALU.mult)
            h_mm(k, pm, (rmb[0][:], rmb[1]), qn)
            h_tt(k, a1, qn, ct, ALU.mult)
            h_tt(k, a2, pm, sn, ALU.mult)
            h_tt(k, (dst[0][:, c0:c0 + nn], dst[1]), a1, a2, ALU.add, eng="gpsimd")
        for c0 in range(0, NTK, 512):
            pre_chunk(A_s["kbT"], kT, 1, c0)
        for c0 in range(0, NTK, 512):
            pre_chunk(A_s["qbT"], qT, 0, c0)
        allk = list(range(NKB))
        for c0 in range(0, nlb * 128, 512):
            nn = min(512, nlb * 128 - c0)
            pa = pacc[acc_i % 2]
            acc_i += 1
            attend(c0, nn, allk, pa)
            finalize(pa, A_s["gbT"], 64, c0, nn)
        pa = pacc[acc_i % 2]
        acc_i += 1
        attend(nlb * 128, 256, ctxk, pa)
        finalize(pa, A_s["gbT"], 64, nlb * 128, 256)
        k.end_phase()

    def phase_rfeat(l):
        Li = L_in[l]
        k.begin_phase()
        pp = k.sb("pp_sb", [64, NPC], F32); npp = k.sb("npp_sb", [64, NPC], F32)
        mul = k.sb("mul_sb", [128, 1], F32); wx = k.sb("wx_sb", [128, 4, 64], F32)
        ones = k.sb("ones_sb", [64, 64], F32); tiny = k.sb("tiny_sb", [64, 1], F32)
        k.dma("sync", pp[0][:], Li["pp"], w=[pp[1]])
        k.dma("sync", mul[0][:], Li["mu_lora"], w=[mul[1]])
        k.dma("sync", wx[0][:], Li["wx"], w=[wx[1]])
        h_memset(k, (ones[0][:], ones[1]), 1.0)
        h_memset(k, (tiny[0][:], tiny[1]), 1e-12)
        h_ts(k, (npp[0][:], npp[1]), (pp[0][:], pp[1]), -1.0, None, ALU.mult)
        h_ts(k, (npp[0][:, PC["r_k"]:PC["r_k"] + 1], npp[1]), (pp[0][:, PC["r_k"]:PC["r_k"] + 1], pp[1]), 0.5, None, ALU.mult)

        def pcol(name):
            return (pp[0][:, PC[name]:PC[name] + 1], pp[1])

        def ncol(name):
            return (npp[0][:, PC[name]:PC[name] + 1], npp[1])
        fin = [[k.sb("fin%d_%d" % (i, j), [64, 514], F32) for j in range(2)] for i in range(NFI)]
        lor = [k.sb("lor%d" % j, [128, 514], F32) for j in range(2)]
        fo = [[k.sb("fo%d_%d" % (i, j), [64, 512], F32) for j in range(2)] for i in range(NFO)]
        tm = [k.sb("tm%d" % i, [64, 512], F32) for i in range(8)]
        lt = [k.sb("lt%d" % i, [128, 512], F32) for i in range(3)]
        pz = [k.ps("pz%d" % i, [64, 512], F32) for i in range(4)]
        ptk = [k.ps("ptk%d" % i, [128, 16, 64], F32) for i in range(1)]
        pfl = k.ps("pfl", [128, 512], F32)
        tokx = [k.sb("tokx%d" % i, [128, 708], F32) for i in range(2)]
        tokb = [k.sb("tokb%d" % i, [128, 322], F32) for i in range(2)]
        vrev = [k.sb("vrev%d" % i, [64, 128], F32) for i in range(2)]
        wrt = [[k.sb("wrt%d_%d" % (d, i), [64, 512], F32) for i in range(2)] for d in range(2)]
        djunk = k.sb("djunk", [128, 64], F32)
        t16 = [k.sb("t16_%d" % i, [128, 256], BF16) for i in range(2)]
        tb16 = [k.sb("tb16_%d" % i, [128, 256], BF16) for i in range(2)]
        t32 = [k.sb("t32_%d" % i, [128, 66], F32) for i in range(2)]
        tb32 = [k.sb("tb32_%d" % i, [128, 66], F32) for i in range(2)]
        tord = ["w_f", "nb_f", "k_f", "wr_f", "kkn", "w_b", "nb_b", "k_b", "wr_b", "r", "v"]
        blkc = [0]

        def chunk(ci, pc0, oc0, nn):
            j = ci % 2

            def I(name, lo=1):
                t, b = fin[FI[name]][j]
                return (t[:, lo:lo + nn], b)

            def O(name):
                t, b = fo[FO[name]][j]
                return (t[:, 0:nn], b)

            def Tm(i):
                return (tm[i][0][:, 0:nn], tm[i][1])

            def Lt(i):
                return (lt[i][0][:, 0:nn], lt[i][1])

            def Pz(i):
                return (pz[i][0][:, 0:nn], pz[i][1])
            for name, i in FI.items():
                t, b = fin[i][j]
                k.dma("sync", t[:, 0:nn + 2], fin_s[i, :, pc0:pc0 + nn + 2], w=[b])
            lt_, lb_ = lor[j]
            k.dma("sync", lt_[:, 0:nn + 2], lora_s[:, pc0:pc0 + nn + 2], w=[lb_])

            def shift(out, lo, mid, hi_, mu, t):
                h_tt(k, t, lo, hi_, ALU.add)
                h_stt(k, t, t, 0.5, mid, ALU.mult, ALU.subtract)
                h_stt(k, out, t, mu, mid, ALU.mult, ALU.add)
            rs, ks, vs = O("r"), Tm(0), O("v")
            shift(rs, I("r", 0), I("r", 1), I("r", 2), pcol("mu_r"), Tm(7))
            shift(ks, I("k", 0), I("k", 1), I("k", 2), pcol("mu_k"), Tm(7))
            shift(vs, I("v", 0), I("v", 1), I("v", 2), pcol("mu_v"), Tm(7))
            ls = Lt(0)
            shift(ls, (lt_[:, 0:nn], lb_), (lt_[:, 1:nn + 1], lb_), (lt_[:, 2:nn + 2], lb_), (mul[0][:], mul[1]), Lt(2))
            lth = Lt(1)
            h_act(k, lth, ls, AF.Tanh)
            kk = Tm(1)
            h_ts(k, kk, ks, pcol("k_k"), None, ALU.mult)
            h_tt(k, Tm(2), kk, kk, ALU.mult)
            h_mm(k, Pz(0), (ones[0][:], ones[1]), Tm(2))
            h_act(k, Tm(2), Pz(0), AF.Sqrt, bias=(tiny[0][:], tiny[1]))
            h_recip(k, Tm(2), Tm(2))
            h_tt(k, O("kkn"), kk, Tm(2), ALU.mult)
            for d, sfx in enumerate(("f", "b")):
                h_mm(k, Pz(1), (wx[0][:, d, :], wx[1]), lth)
                h_mm(k, Pz(2), (wx[0][:, 2 + d, :], wx[1]), ls)
                h_sigmoid(k, Tm(3), Pz(1), Tm(3), bias=ncol("w0_" + sfx))
                h_act(k, O("w_" + sfx), Tm(3), AF.Exp, scale=-float(np.exp(-0.5)))
                a = Tm(4)
                h_sigmoid(k, a, Pz(2), a, bias=ncol("a0_" + sfx))
                h_ts(k, Tm(5), a, -1.0, pcol("k_a"), ALU.add, ALU.mult)
                h_stt(k, O("k_" + sfx), Tm(5), 1.0, ks, ALU.add, ALU.mult)
                h_stt(k, O("nb_" + sfx), O("kkn"), -1.0, a, ALU.mult, ALU.mult)
            h_tt(k, Tm(5), O("k_f"), O("k_b"), ALU.add)
            h_stt(k, Tm(5), rs, ncol("r_k"), Tm(5), ALU.mult, ALU.mult)
            h_mm(k, Pz(3), (ones[0][:], ones[1]), Tm(5))
            h_tt(k, O("bonus"), Pz(3), vs, ALU.mult)
            h_silu(k, O("sg_rw"), I("g_rw", 1), Tm(6))
            h_silu(k, O("sg_hy"), I("g_hy", 1), Tm(6))
            for nm in ("v", "x1", "x2"):
                src = "h" + nm
                h_ts(k, Tm(6), I(src, 0), pcol("t0_" + nm), None, ALU.mult)
                h_stt(k, Tm(6), I(src, 1), pcol("t1_" + nm), Tm(6), ALU.mult, ALU.add)
                h_stt(k, O(src), I(src, 2), pcol("t2_" + nm), Tm(6), ALU.mult, ALU.add)
            for name in ("bonus", "sg_rw", "hv", "hx1", "hx2", "sg_hy"):
                t, b = fo[FO[name]][j]
                k.dma("sync", fo_s[FO[name], :, oc0:oc0 + nn], t[:, 0:nn], r=[b])
            k.dma("sync", vT2_s[0:64, oc0:oc0 + nn], fo[FO["v"]][j][0][:, 0:nn], r=[fo[FO["v"]][j][1]])
            h_tt(k, (wrt[0][j][0][:, 0:nn], wrt[0][j][1]), O("w_f"), rs, ALU.mult)
            h_tt(k, (wrt[1][j][0][:, 0:nn], wrt[1][j][1]), O("w_b"), rs, ALU.mult)
            for bi in range(nn // 128):
                tau0 = oc0 + bi * 128
                s_lo = (128 - tau0) if tau0 < 256 else (256 + n - 128 - (tau0 - 256))
                q = blkc[0] % 2
                blkc[0] += 1
                pk, pkb = ptk[0]
                for ai, nm in enumerate(tord):
                    if nm.startswith("wr_"):
                        t, b = wrt[0 if nm == "wr_f" else 1][j]
                    else:
                        t, b = fo[FO[nm]][j]
                    h_tr(k, (pk[:, ai, :], pkb), (t[:, bi * 128:(bi + 1) * 128], b), (identf[0][0:64, 0:64], identf[1]))
                tx, txb = tokx[q]
                h_copy(k, (tx[:, 0:320].rearrange("p (a c) -> p a c", a=5), txb), (pk[:, 0:5, :], pkb), eng="scalar")
                h_copy(k, (tx[:, 322:514].rearrange("p (a c) -> p a c", a=3), txb), (pk[:, 5:8, :], pkb))
                h_copy(k, (tx[:, 514:578], txb), (pk[:, 8, :], pkb), eng="scalar")
                h_copy(k, (tx[:, 580:708].rearrange("p (a c) -> p a c", a=2), txb), (pk[:, 9:11, :], pkb))
                R_ = (tx[:, 580:644], txb)
                dj = (djunk[0][:], djunk[1])
                h_stt(k, dj, (tx[:, 64:128], txb), 1.0, R_, ALU.mult, ALU.mult, accum=(tx[:, 320:321], txb))
                h_stt(k, dj, (tx[:, 128:192], txb), 1.0, R_, ALU.mult, ALU.mult, accum=(tx[:, 321:322], txb))
                h_stt(k, dj, (tx[:, 386:450], txb), 1.0, R_, ALU.mult, ALU.mult, accum=(tx[:, 578:579], txb))
                h_stt(k, dj, (tx[:, 450:514], txb), 1.0, R_, ALU.mult, ALU.mult, accum=(tx[:, 579:580], txb))
                h_copy(k, (t16[q][0][:], t16[q][1]), (tx[:, 64:320], txb), eng="gpsimd")
                h_copy(k, (t32[q][0][:, 0:64], t32[q][1]), (tx[:, 0:64], txb), eng="gpsimd")
                h_copy(k, (t32[q][0][:, 64:66], t32[q][1]), (tx[:, 320:322], txb), eng="gpsimd")
                k.dma("sync", bc32_s[0, tau0:tau0 + 128, :], t32[q][0][:], r=[t32[q][1]])
                k.dma("sync", bc16_s[0, tau0:tau0 + 128, :], t16[q][0][:], r=[t16[q][1]])
                h_mm(k, (pfl[0][:, 0:256], pfl[1]), (Jf[0][:], Jf[1]), (tx[:, 322:578], txb))
                h_mm(k, (pfl[0][:, 256:320], pfl[1]), (Jf[0][:], Jf[1]), (tx[:, 256:320], txb))
                h_mm(k, (pfl[0][:, 320:322], pfl[1]), (Jf[0][:], Jf[1]), (tx[:, 578:580], txb))
                h_copy(k, (tokb[q][0][:], tokb[q][1]), (pfl[0][:, 0:322], pfl[1]), eng="scalar")
                h_copy(k, (tb16[q][0][:], tb16[q][1]), (tokb[q][0][:, 64:320], tokb[q][1]), eng="gpsimd")
                h_copy(k, (tb32[q][0][:, 0:64], tb32[q][1]), (tokb[q][0][:, 0:64], tokb[q][1]), eng="gpsimd")
                h_copy(k, (tb32[q][0][:, 64:66], tb32[q][1]), (tokb[q][0][:, 320:322], tokb[q][1]), eng="gpsimd")
                k.dma("sync", bc32_s[1, s_lo:s_lo + 128, :], tb32[q][0][:], r=[tb32[q][1]])
                k.dma("sync", bc16_s[1, s_lo:s_lo + 128, :], tb16[q][0][:], r=[tb16[q][1]])
                h_mm(k, (pfl[0][0:64, 384:512], pfl[1]), (tx[:, 644:708], txb), (Jf[0][:], Jf[1]))
                h_copy(k, (vrev[q][0][:], vrev[q][1]), (pfl[0][0:64, 384:512], pfl[1]))
                k.dma("sync", vT2_s[64:128, s_lo:s_lo + 128], vrev[q][0][:], r=[vrev[q][1]])
        for ci, (pc0, oc0, nn) in enumerate(seq_chunks(n)):
            chunk(ci, pc0, oc0, nn)
        k.end_phase()

    def phase_scan():
        k.begin_phase()
        S, S_b = k.sb("S", [128, 64], F32)
        prod, _ = k.sb("sprod", [128, 2, 64], F32)
        NBUF = 4
        b32 = [k.sb("b32_%d" % i, [128, TC, 66], F32) for i in range(NBUF)]
        b16 = [k.sb("b16_%d" % i, [128, TC, 256], BF16) for i in range(NBUF)]
        red = [k.sb("red%d" % i, [128, TC, 2], F32) for i in range(NBUF)]
        p16 = [k.sb("p16_%d" % i, [128, TC], F32) for i in range(2)]
        vt = [k.sb("svt%d" % i, [128, 512], F32) for i in range(2)]
        yt = [k.sb("syt%d" % i, [128, 512], F32) for i in range(2)]
        b32x = [[Buf(), Buf()] for _ in range(NBUF)]
        b16x = [[Buf(), Buf()] for _ in range(NBUF)]
        h_memset(k, (S[:], S_b), 0.0)
        S_bc = S[:].unsqueeze(1).broadcast_to([128, 2, 64])
        nch = (T + TC - 1) // TC

        def load_chunk(c):
            s0 = c * TC
            nst = min(TC, T - s0)
            t32, t32b = b32[c % NBUF]
            t16_, t16b = b16[c % NBUF]
            q = "sync" if c % 2 == 0 else "scalar"
            for d in range(2):
                k.dma(q, t32[64 * d:64 * d + 64, 0:nst, :], bc32_s[d, s0:s0 + nst, :].partition_broadcast(64), w=[b32x[c % NBUF][d]])
                k.dma(q, t16_[64 * d:64 * d + 64, 0:nst, :], bc16_s[d, s0:s0 + nst, :].partition_broadcast(64), w=[b16x[c % NBUF][d]])
        for c in range(min(2, nch)):
            load_chunk(c)
        for c in range(nch):
            s0 = c * TC
            nst = min(TC, T - s0)
            if c + 2 < nch:
                load_chunk(c + 2)
            t32, t32b = b32[c % NBUF]
            t16_, t16b = b16[c % NBUF]
            rd, rdb = red[c % NBUF]
            x32 = b32x[c % NBUF]
            x16 = b16x[c % NBUF]
            if s0 % 512 == 0:
                vi = (s0 // 512) % 2
                vtt, vtb = vt[vi]
                ytt, ytb = yt[vi]
                nv = min(512, T - s0)
                k.dma("gpsimd", vtt[:, 0:nv], vT2_s[:, s0:s0 + nv], w=[vtb])
            for i in range(nst):
                s = s0 + i
                sl = s % 512
                W = t32[:, i, 0:64]; NB_ = t16_[:, i, 0:64]; K_ = t16_[:, i, 64:128]
                M2 = t16_[:, i, 128:256].rearrange("p (a c) -> p a c", a=2)
                sac = rd[:, i, 1:2]
                P.op("vector", lambda E, M2=M2: E.tensor_tensor(out=prod[:], in0=S_bc, in1=M2, op=ALU.mult), [S_b] + x16, [S_b])
                P.op("vector", lambda E, rd=rd, i=i: E.tensor_reduce(out=rd[:, i, :], in_=prod[:], axis=mybir.AxisListType.X, op=ALU.add), [S_b], [S_b, rdb])
                P.op("vector", lambda E, W=W: E.tensor_tensor(out=S[:], in0=S[:], in1=W, op=ALU.mult), [S_b] + x32, [S_b])
                P.op("vector", lambda E, NB_=NB_, sac=sac: E.scalar_tensor_tensor(out=S[:], in0=NB_, scalar=sac, in1=S[:], op0=ALU.mult, op1=ALU.add), [S_b] + x16, [S_b])
                P.op("vector", lambda E, K_=K_, vtt=vtt, sl=sl: E.scalar_tensor_tensor(out=S[:], in0=K_, scalar=vtt[:, sl:sl + 1], in1=S[:], op0=ALU.mult, op1=ALU.add), [S_b, vtb] + x16, [S_b])
            sl0 = s0 % 512
            pA = (p16[0][0][:, 0:nst], p16[0][1]); pB = (p16[1][0][:, 0:nst], p16[1][1])
            h_tt(k, pA, (rd[:, 0:nst, 1], rdb), (t32[:, 0:nst, 64], x32[0]), ALU.mult, eng="gpsimd")
            h_tt(k, pA, pA, (rd[:, 0:nst, 0], rdb), ALU.add, eng="gpsimd")
            h_tt(k, pB, (vtt[:, sl0:sl0 + nst], vtb), (t32[:, 0:nst, 65], x32[1]), ALU.mult, eng="gpsimd")
            h_tt(k, (ytt[:, sl0:sl0 + nst], ytb), pA, pB, ALU.add, eng="gpsimd")
            s_last = s0 + nst - 1
            if s_last % 512 == 511 or s_last == T - 1:
                c0 = s_last - (s_last % 512)
                k.dma("gpsimd", yT2_s[:, c0:s_last + 1], ytt[:, 0:s_last % 512 + 1], r=[ytb])
        k.end_phase()

    def phase_filt(l, Lf):
        Li = L_in[l]
        fc = fconst[Lf]
        k.begin_phase()
        w1 = k.sb("w1s", [33, 64], F32); w2 = k.sb("w2s", [64, 64], F32); w3 = k.sb("w3s", [64, 256], F32)
        bb = k.sb("bbs", [64, 2], F32); b3 = k.sb("b3s", [128, 2], F32); nd = k.sb("nds", [128, 1], F32)
        for t, d in ((w1, Li["hw1"]), (w2, Li["hw2"]), (w3, Li["hw3"]), (bb, Li["hb12"]), (b3, Li["hb3"]), (nd, nd_d)):
            k.dma("sync", t[0][:], d, w=[t[1]])
        nch = (Lf + 511) // 512
        part = k.sb("part", [128, 2 * nch], F32)
        h_memset(k, (part[0][:], part[1]), 0.0)
        b0 = k.sb("b0", [128, 1], F32)
        ft = [k.sb("ft%d" % i, [33, 512], F32) for i in range(2)]
        tv = [k.sb("tv%d" % i, [128, 512], F32) for i in range(2)]
        hx = [k.sb("hx%d" % i, [64, 512], F32) for i in range(2)]
        hi = k.sb("hi", [64, 512], I32); hf = k.sb("hf", [64, 512], F32)
        win = k.sb("win", [128, 512], F32)
        tF = [k.sb("tF%d" % i, [128, 512], F32) for i in range(2)]
        oF = [k.sb("oF%d" % i, [128, 512], BF16) for i in range(2)]
        junk = k.sb("junkf", [128, 512], F32)
        ph = [k.ps("ph%d" % i, [64, 512], F32) for i in range(2)]
        pt = [k.ps("pt%d" % i, [128, 512], F32) for i in range(2)]
        kl = kl_h[Lf].ap()

        def sin_layer(out, psum, bias, nn):
            x = (hf[0][:, 0:nn], hf[1]); ki = (hi[0][:, 0:nn], hi[1])
            h_ts(k, out, psum, bias, None, ALU.add)
            h_ts(k, ki, out, 1.0 / TWO_PI, None, ALU.mult)
            h_copy(k, x, ki)
            h_stt(k, out, x, -TWO_PI, out, ALU.mult, ALU.add)
            h_ts(k, out, out, -float(np.pi), float(np.pi), ALU.max, ALU.min)
            h_act(k, out, out, AF.Sin)
        cnt = 0
        for side in (1, 0):
            for c in range(nch):
                c0 = c * 512
                nn = min(512, Lf - c0)
                j = cnt % 2
                cnt += 1
                k.dma("sync", ft[j][0][:, 0:nn], (fc["featsTr"] if side else fc["featsT"])[:, c0:c0 + nn], w=[ft[j][1]])
                k.dma("sync", tv[j][0][:, 0:nn], (fc["tvr"] if side else fc["tv"])[:, c0:c0 + nn].partition_broadcast(128), w=[tv[j][1]])
                p0 = (ph[0][0][:, 0:nn], ph[0][1]); p1 = (ph[1][0][:, 0:nn], ph[1][1])
                h_mm(k, p0, (w1[0][:], w1[1]), (ft[j][0][:, 0:nn], ft[j][1]))
                h1 = (hx[0][0][:, 0:nn], hx[0][1])
                sin_layer(h1, p0, (bb[0][:, 0:1], bb[1]), nn)
                h_mm(k, p1, (w2[0][:], w2[1]), h1)
                h2 = (hx[1][0][:, 0:nn], hx[1][1])
                sin_layer(h2, p1, (bb[0][:, 1:2], bb[1]), nn)
                ptx = (pt[j][0][:, 0:nn], pt[j][1])
                h_mm(k, ptx, (w3[0][:, side * 128:(side + 1) * 128], w3[1]), h2)
                w_ = (win[0][:, 0:nn], win[1])
                h_act(k, w_, (tv[j][0][:, 0:nn], tv[j][1]), AF.Exp, scale=(nd[0][:], nd[1]))
                F_ = (tF[j][0][:, 0:nn], tF[j][1])
                h_stt(k, F_, ptx, (b3[0][:, side:side + 1], b3[1]), w_, ALU.add, ALU.mult)
                hi_ = nn
                if side == 1 and c == nch - 1:
                    h_copy(k, (b0[0][:], b0[1]), (tF[j][0][:, nn - 1:nn], tF[j][1]))
                    hi_ = nn - 1
                if side == 0 and c == 0:
                    h_tt(k, (tF[j][0][:, 0:1], tF[j][1]), (tF[j][0][:, 0:1], tF[j][1]), (b0[0][:], b0[1]), ALU.add)
                pcol_ = 2 * c + side
                if hi_ > 0:
                    h_act(k, (junk[0][:, 0:hi_], junk[1]), (tF[j][0][:, 0:hi_], tF[j][1]), AF.Square, accum=(part[0][:, pcol_:pcol_ + 1], part[1]))
                    h_copy(k, (oF[j][0][:, 0:hi_], oF[j][1]), (tF[j][0][:, 0:hi_], tF[j][1]), eng="gpsimd")
                    base = c0 if side == 1 else (Lf - 1 + c0)
                    k.dma("sync", kl[:, base:base + hi_], oF[j][0][:, 0:hi_], r=[oF[j][1]])
        tot = k.sb("tot", [128, 1], F32)
        k.V(lambda E: E.tensor_reduce(out=tot[0][:], in_=part[0][:], axis=mybir.AxisListType.X, op=ALU.add), [part[1]], [tot[1]])
        k.dma("sync", ssq_s[Lf].rearrange("o p -> p o"), tot[0][:], r=[tot[1]], allow_slow_non_contiguous=True)
        k.end_phase()

    def phase_hy(l, nb, col_fo, col_mix, readout):
        Li = L_in[l]
        Lf = 128 * nb
        W = 128 * (2 * nb - 1)
        k.begin_phase()
        nrm = k.sb("nrm", [128, 128], F32); skp = k.sb("skp", [128, 128], F32)
        k.dma("sync", nrm[0][:], ssq_s[Lf].partition_broadcast(128), w=[nrm[1]])
        k.dma("sync", skp[0][:], Li["skipT"].partition_broadcast(128), w=[skp[1]])
        h_act(k, (nrm[0][:], nrm[1]), (nrm[0][:], nrm[1]), AF.Sqrt)
        h_recip(k, (nrm[0][:], nrm[1]), (nrm[0][:], nrm[1]))
        if readout:
            k.begin_phase()
            pp = k.sb("pp_sb", [64, NPC], F32)
            k.dma("sync", pp[0][:], Li["pp"], w=[pp[1]])
            o64 = k.sb("o64", [64, 64], F32)
            h_memset(k, (o64[0][:], o64[1]), 1.0 / 64)
            geps = k.sb("geps", [64, 1], F32)
            h_memset(k, (geps[0][:], geps[1]), 64e-5)
            ra = [k.sb("ra%d" % i, [64, 512], F32) for i in range(2)]
            rbk = [k.sb("rbk%d" % i, [64, 128], F32) for i in range(2)]
            rbt = [k.sb("rbt%d" % i, [128, 64], F32) for i in range(2)]
            rc = [k.sb("rc%d" % i, [64, 512], F32) for i in range(2)]
            rd = [k.sb("rd%d" % i, [64, 512], F32) for i in range(2)]
            r1 = k.sb("r1", [64, 512], F32); r2 = k.sb("r2", [64, 512], F32)
            rob = [k.sb("rob%d" % i, [64, 512], BF16) for i in range(2)]
            pr = [k.ps("pr%d" % i, [64, 512], F32) for i in range(2)]
            prt = k.ps("prt", [128, 64], F32)
            prb = k.ps("prb", [64, 512], F32)
            bq = 0
            chunks = [(0, 256)] + [(256 + u0, min(512, n - u0)) for u0 in range(0, n, 512)]
            for ci, (c0, nn) in enumerate(chunks):
                j = ci % 2
                A = (ra[j][0][:, 0:nn], ra[j][1]); C = (rc[j][0][:, 0:nn], rc[j][1]); Dg = (rd[j][0][:, 0:nn], rd[j][1])
                R1 = (r1[0][:, 0:nn], r1[1]); R2 = (r2[0][:, 0:nn], r2[1])
                P0 = (pr[0][0][:, 0:nn], pr[0][1]); P1 = (pr[1][0][:, 0:nn], pr[1][1])
                k.dma("scalar", A[0], yT2_s[0:64, c0:c0 + nn], w=[A[1]])
                k.dma("scalar", C[0], fo_s[FO["bonus"], :, c0:c0 + nn], w=[C[1]])
                k.dma("scalar", Dg[0], fo_s[FO["sg_rw"], :, c0:c0 + nn], w=[Dg[1]])
                for bi in range(nn // 128):
                    tau0 = c0 + bi * 128
                    s_lo = (128 - tau0) if tau0 < 256 else (256 + n - 128 - (tau0 - 256))
                    q = bq % 2
                    bq += 1
                    k.dma("scalar", rbk[q][0][:], yT2_s[64:128, s_lo:s_lo + 128], w=[rbk[q][1]])
                    h_tr(k, (prt[0][:], prt[1]), (rbk[q][0][:], rbk[q][1]), (identf[0][0:64, 0:64], identf[1]))
                    h_copy(k, (rbt[q][0][:], rbt[q][1]), (prt[0][:], prt[1]), eng="scalar")
                    h_mm(k, (prb[0][:, bi * 128:(bi + 1) * 128], prb[1]), (rbt[q][0][:], rbt[q][1]), (Jf[0][:], Jf[1]))
                h_tt(k, A, A, (prb[0][:, 0:nn], prb[1]), ALU.add)
                h_mm(k, P0, (o64[0][:], o64[1]), A)
                h_tt(k, R1, A, P0, ALU.subtract)
                h_tt(k, R2, R1, R1, ALU.mult)
                h_mm(k, P1, (o64[0][:], o64[1]), R2)
                h_act(k, R2, P1, AF.Sqrt, bias=(geps[0][:], geps[1]))
                h_recip(k, R2, R2)
                h_tt(k, R1, R1, R2, ALU.mult)
                h_ts(k, R1, R1, (pp[0][:, PC["gn_w"]:PC["gn_w"] + 1], pp[1]), (pp[0][:, PC["gn_b"]:PC["gn_b"] + 1], pp[1]), ALU.mult, ALU.add)
                h_tt(k, R1, R1, C, ALU.add)
                OB = (rob[j][0][:, 0:nn], rob[j][1])
                h_tt(k, OB, R1, Dg, ALU.mult)
                mc = (n + c0) if c0 < 256 else (c0 - 256)
                k.dma("scalar", mix_send[0:64, mc:mc + nn], OB[0], r=[OB[1]])
            k.end_phase()
        ksh = [k.sb("ksh%d" % i, [128, W], BF16) for i in range(2)]
        ld = [[k.sb("ld%d_%d" % (a, i), [nb, 128], F32) for i in range(2)] for a in range(4)]
        zt = [k.sb("zt%d" % i, [128, nb], F32) for i in range(2)]
        x1t = [k.sb("x1t%d" % i, [128, nb], F32) for i in range(2)]
        x2t = [k.sb("x2t%d" % i, [128, nb], F32) for i in range(2)]
        sgt = [k.sb("sgt%d" % i, [128, nb], F32) for i in range(2)]
        zb = k.sb("zb", [128, nb], BF16); zf = k.sb("zf", [128, nb], BF16)
        z2 = k.sb("z2", [128, nb], F32); t1 = k.sb("t1", [128, nb], F32)
        ot = [k.sb("oth%d" % i, [128, nb], BF16) for i in range(2)]
        otT = [k.sb("otT%d" % i, [nb, 128], BF16) for i in range(2)]
        pin = [k.ps("pin%d" % i, [128, nb], F32) for i in range(2)]
        pf = k.ps("pf", [128, nb], F32)
        py = [k.ps("pyc%d" % i, [128, nb], F32) for i in range(2)]
        pot = k.ps("pot", [nb, 128], BF16)
        order = [0] + [d for d in range(-(nb - 1), nb) if d != 0]
        cnt = 0
        names = ["hv", "hx1", "hx2", "sg_hy"]
        for c in range(64):
            j = c % 2
            dst = [zt[j], x1t[j], x2t[j], sgt[j]]
            for a in range(4):
                lt_, lb_ = ld[a][j]
                k.dma("gpsimd", lt_[:], fo_s[FO[names[a]], c, col_fo:col_fo + Lf].rearrange("(s j) -> s j", j=128), w=[lb_])
                h_tr(k, (pin[a % 2][0][:], pin[a % 2][1]), (lt_[:], lb_), (identf[0][0:nb, 0:nb], identf[1]))
                h_copy(k, (dst[a][0][:], dst[a][1]), (pin[a % 2][0][:], pin[a % 2][1]), eng=("scalar" if a % 2 else "vector"))
            Z = (zt[j][0][:], zt[j][1]); X1 = (x1t[j][0][:], x1t[j][1]); X2 = (x2t[j][0][:], x2t[j][1]); SG = (sgt[j][0][:], sgt[j][1])
            zin = Z
            for o in range(2):
                row = o * 64 + c
                kt, kb_ = ksh[cnt % 2]
                q = "sync" if cnt % 2 == 0 else "scalar"
                cnt += 1
                src = bass.AP(kl_h[Lf], row * 2 * Lf, [[1, 128], [1, W]])
                k.dma(q, kt[:], src, w=[kb_])
                h_copy(k, (zb[0][:], zb[1]), zin, eng="gpsimd")
                h_mm(k, (pf[0][:], pf[1]), (Jb[0][:], Jb[1]), (zb[0][:], zb[1]))
                h_copy(k, (zf[0][:], zf[1]), (pf[0][:], pf[1]), eng="scalar")
                pyt, pyb = py[o]
                for ii, Dd in enumerate(order):
                    e = Dd + nb - 1
                    S0, S1 = max(0, -Dd), min(nb, nb - Dd)
                    h_mm(k, (pyt[:, S0 + Dd:S1 + Dd], pyb), (kt[:, 128 * e:128 * e + 128], kb_), (zf[0][:, S0:S1], zf[1]), start=(ii == 0), stop=(ii == len(order) - 1))
                ncol_ = (nrm[0][:, row:row + 1], nrm[1])
                scol = (skp[0][:, row:row + 1], skp[1])
                h_ts(k, (t1[0][:], t1[1]), (pyt[:], pyb), ncol_, None, ALU.mult)
                h_stt(k, (t1[0][:], t1[1]), zin, scol, (t1[0][:], t1[1]), ALU.mult, ALU.add)
                if o == 0:
                    h_tt(k, (z2[0][:], z2[1]), (t1[0][:], t1[1]), X1, ALU.mult)
                    zin = (z2[0][:], z2[1])
                else:
                    O_ = (ot[j][0][:], ot[j][1])
                    h_tt(k, (t1[0][:], t1[1]), (t1[0][:], t1[1]), X2, ALU.mult)
                    h_tt(k, O_, (t1[0][:], t1[1]), SG, ALU.mult, eng="gpsimd")
                    h_tr(k, (pot[0][:], pot[1]), O_, IDB)
                    h_copy(k, (otT[j][0][:], otT[j][1]), (pot[0][:], pot[1]), eng="scalar")
                    k.dma("gpsimd", mix_send[64 + c, col_mix:col_mix + Lf].rearrange("(s j) -> s j", j=128), otT[j][0][:], r=[otT[j][1]])
        k.end_phase()

    for l in range(4):
        phase_tok(l)
        if l % 2 == 0:
            phase_attn(l)
        else:
            phase_rfeat(l)
            phase_scan()
            phase_filt(l, n)
            phase_hy(l, nlb, 256, 0, True)
            if l < 3:
                phase_filt(l, 256)
                phase_hy(l, 2, 0, n, False)
        P.coll("AllGather", mybir.AluOpType.bypass, mix_send, mix_all)
        P.barrier()
    phase_tok(4, final=True)
    k.done()
    return nc


_FUSED_CACHE = {}


def _c_cols(c, c_ctx):
    return np.ascontiguousarray(np.concatenate([c.reshape(8, 128).T, c_ctx.reshape(8, 128).T], axis=1).astype(np.float32))


def fused_inputs(inp, n):
    nlb = n // 128
    x = np.ascontiguousarray(inp["x"][0], dtype=np.float32)
    ctx = np.ascontiguousarray(inp["ctx"][0], dtype=np.float32)
    C, S, RT = rope_tables(nlb)
    sel = np.zeros((65, 64), np.float32)
    sel[64] = 1.0
    perm = np.concatenate([np.concatenate([np.arange(64 * r, 64 * r + 64), np.arange(512 + 64 * r, 512 + 64 * r + 64)]) for r in range(8)])
    common = {"x": x, "ctx": ctx, "c_cols": _c_cols(inp["c"][0], inp["c_ctx"]), "ident": np.eye(128, dtype=np.float32),
              "Jmat": np.eye(128, dtype=np.float32)[::-1].copy(), "final_norm": inp["final_norm"].reshape(1, -1).astype(np.float32),
              "ropeC": C, "ropeS": S, "rmatT": RT, "sel": sel,
              "sel2": np.concatenate([np.repeat(np.array([[1.0], [0.0]], np.float32), 64, 1), np.repeat(np.array([[0.0], [1.0]], np.float32), 64, 1)], 1)}
    for Lf in (n, 256):
        fT, tv, adel = filt_consts(Lf)
        common["featsT%d" % Lf] = fT
        common["featsTr%d" % Lf] = np.ascontiguousarray(fT[:, ::-1])
        common["tv%d" % Lf] = tv
        common["tvr%d" % Lf] = np.ascontiguousarray(tv[:, ::-1])
    maps = []
    for h in range(8):
        m = dict(common)
        kvh = h // 4
        sl = slice(64 * h, 64 * h + 64)
        m["negdelta"] = -np.concatenate([adel[sl], adel[sl]]).reshape(128, 1).astype(np.float32)
        for l in range(4):
            i = l // 2
            attn = l % 2 == 0
            pre = "attn" if attn else "rec"
            m["norm_g%d" % l] = inp[pre + "_norm"][i].reshape(1, -1)
            m["ada_w%d" % l] = inp[pre + "_ada_w"][i]
            m["ada_b%d" % l] = inp[pre + "_ada_b"][i].reshape(1, -1)
            m["w_out%d" % l] = np.ascontiguousarray(inp[pre + "_w_out"][i][perm])
            w_in = inp[pre + "_w_in"][i]
            if attn:
                starts = [64 * h, 512 + 64 * h, 1536 + 64 * h, 2048 + 64 * h, 2560 + 64 * kvh, 2816 + 64 * h, 1024 + 64 * h, 2688 + 64 * kvh]
                cols = np.concatenate([np.arange(s0, s0 + 64) for s0 in starts])
                m["w_in%d" % l] = np.ascontiguousarray(w_in[:, cols])
                m["biasT%d" % l] = na_bias_tables(inp["na_rpb"][i][h], nlb)
                m["gains%d" % l] = np.stack([inp["gqa_q_gain"][i], inp["gqa_k_gain"][i]], 1).astype(np.float32)
            else:
                starts = [64 * h, 512 + 64 * h, 1024 + 64 * h, 1664 + 64 * h, 2176 + 64 * h, 2688 + 64 * h, 3200 + 64 * h, 3712 + 64 * h]
                cols = np.concatenate([np.arange(s0, s0 + 64) for s0 in starts] + [np.arange(1536, 1664)])
                m["w_in%d" % l] = np.ascontiguousarray(w_in[:, cols])
                prm = {k_: inp[k_][i] for k_ in inp if k_.startswith("rwkv") or k_.startswith("hy")}
                ri = rfeat_inputs(np.zeros((1, 4224), np.float32), np.zeros((1, 4224), np.float32), h, prm)
                m["pp%d" % l] = ri["pp"]
                m["mu_lora%d" % l] = ri["mu_lora"]
                m["wx%d" % l] = ri["wx"]
                fi = filt_inputs(256, h, prm)
                m["hw1_%d" % l] = fi["w1"]
                m["hw2_%d" % l] = fi["w2"]
                m["hw3_%d" % l] = fi["w3c"]
                m["hb12_%d" % l] = fi["b12"]
                m["hb3_%d" % l] = fi["b3c"]
                m["skipT%d" % l] = np.concatenate([prm["hy_skip"][0][sl], prm["hy_skip"][1][sl]]).reshape(1, 128).astype(np.float32)
        maps.append(m)
    return maps


def kernel(**inp):
    inp = {k_: np.asarray(v) for k_, v in inp.items()}
    n = inp["x"].shape[1]
    if n not in _FUSED_CACHE:
        _FUSED_CACHE[n] = build_fused(n)
    nc = _FUSED_CACHE[n]
    res = run_bass_kernel_spmd(nc, fused_inputs(inp, n), core_ids=list(range(8)))
    tpc = n // 8
    out = np.concatenate([res.results[i]["y_out"][i * tpc:(i + 1) * tpc] for i in range(8)], 0)
    return out[None].astype(np.float32)
```

```python
import numpy as np
import concourse.bass as bass
import concourse.mybir as mybir
from concourse.bass_utils import run_bass_kernel_spmd

F32 = mybir.dt.float32
BF16 = mybir.dt.bfloat16
I32 = mybir.dt.int32
ALU = mybir.AluOpType
AF = mybir.ActivationFunctionType


class Buf:
    __slots__ = ("w", "r", "name")

    def __init__(self, name=""):
        self.w = None
        self.r = {}
        self.name = name


class Prog:
    ENGS = ("tensor", "vector", "scalar", "gpsimd", "sync")
    NDSEM = 6

    def __init__(self, nc):
        self.nc = nc
        self.lists = {e: [] for e in self.ENGS}
        self.count = {e: 0 for e in self.ENGS}
        self.waited = {e: {} for e in self.ENGS}
        self.sems = {}
        self._ctx = []
        for e in self.ENGS:
            cm = nc.semaphore("s_" + e)
            self.sems[e] = cm.__enter__()
            self._ctx.append(cm)
        self.dsem = {}
        self.dcount = {}
        self.dnext = {}
        for q in ("sync", "scalar", "gpsimd"):
            self.dsem[q] = []
            for i in range(self.NDSEM):
                cm = nc.semaphore("d_%s%d" % (q, i))
                self.dsem[q].append(cm.__enter__())
                self._ctx.append(cm)
                self.sems["d_%s%d" % (q, i)] = self.dsem[q][-1]
            self.dcount[q] = [0] * self.NDSEM
            self.dnext[q] = 0

    def _need(self, eng, evs):
        best = {}
        for ev in evs:
            if ev is None:
                continue
            k, v = ev
            if best.get(k, 0) < v:
                best[k] = v
        for k, v in best.items():
            if self.waited[eng].get(k, 0) >= v:
                continue
            self.waited[eng][k] = v
            sem = self.sems[k]
            self.lists[eng].append(lambda E, sem=sem, v=v: E.wait_ge(sem, v))

    def _deps(self, reads, writes):
        evs = []
        for b in reads:
            evs.append(b.w)
        for b in writes:
            evs.append(b.w)
            evs.extend(b.r.items())
        return evs

    def op(self, eng, fn, reads=(), writes=()):
        self._need(eng, self._deps(reads, writes))
        self.count[eng] += 1
        c = self.count[eng]
        sem = self.sems[eng]
        self.lists[eng].append(lambda E, fn=fn, sem=sem: fn(E).then_inc(sem, 1))
        ev = (eng, c)
        for b in reads:
            b.r[eng] = c
        for b in writes:
            b.w = ev
            b.r = {}
        return ev

    def dma(self, q, out, in_, reads=(), writes=(), **kw):
        i = self.dnext[q]
        self.dnext[q] = (i + 1) % self.NDSEM
        key = "d_%s%d" % (q, i)
        evs = self._deps(reads, writes)
        if self.dcount[q][i] > 0:
            evs.append((key, self.dcount[q][i]))
        self._need(q, evs)
        self.dcount[q][i] += 16
        v = self.dcount[q][i]
        sem = self.sems[key]
        self.lists[q].append(lambda E, out=out, in_=in_, sem=sem, kw=kw: E.dma_start(out=out, in_=in_, **kw).then_inc(sem, 16))
        ev = (key, v)
        for b in reads:
            b.r[key] = v
        for b in writes:
            b.w = ev
            b.r = {}
        return ev

    def coll(self, kind, op, ins_ap, outs_ap, reads=(), writes=()):
        if "cc" not in self.sems:
            cm = self.nc.semaphore("s_cc")
            self.sems["cc"] = cm.__enter__()
            self._ctx.append(cm)
            self.cccount = 0
        evs = self._deps(reads, writes)
        if self.cccount:
            evs.append(("cc", self.cccount))
        self._need("gpsimd", evs)
        self.cccount += 1
        v = self.cccount
        sem = self.sems["cc"]
        self.lists["gpsimd"].append(lambda E: E.collective_compute(kind, op, replica_groups=[list(range(8))], ins=[ins_ap.opt()], outs=[outs_ap.opt()]).then_inc(sem))
        ev = ("cc", v)
        for b in reads:
            b.r["cc"] = v
        for b in writes:
            b.w = ev
            b.r = {}
        return ev

    def barrier(self):
        evs = []
        for q in self.dsem:
            for i in range(self.NDSEM):
                if self.dcount[q][i]:
                    evs.append(("d_%s%d" % (q, i), self.dcount[q][i]))
        for e in self.ENGS:
            if self.count[e]:
                evs.append((e, self.count[e]))
        if "cc" in self.sems and self.cccount:
            evs.append(("cc", self.cccount))
        for e in self.ENGS:
            self._need(e, evs)

    def finish(self):
        evs = []
        for q in self.dsem:
            for i in range(self.NDSEM):
                if self.dcount[q][i]:
                    evs.append(("d_%s%d" % (q, i), self.dcount[q][i]))
        for e in self.ENGS:
            if e != "sync" and self.count[e]:
                evs.append((e, self.count[e]))
        if "cc" in self.sems:
            evs.append(("cc", self.cccount))
        self._need("sync", evs)
        nc = self.nc
        lists = self.lists
        with nc.Block() as block:
            @block.sync
            def _(E):
                for f in lists["sync"]:
                    f(E)

            @block.tensor
            def _(E):
                for f in lists["tensor"]:
                    f(E)

            @block.vector
            def _(E):
                for f in lists["vector"]:
                    f(E)

            @block.scalar
            def _(E):
                for f in lists["scalar"]:
                    f(E)

            @block.gpsimd
            def _(E):
                for f in lists["gpsimd"]:
                    f(E)
        for cm in reversed(self._ctx):
            cm.__exit__(None, None, None)


from contextlib import ExitStack

NB = 18
NT = NB * 128
D = 1024
EPS = 1e-6


class KB:
    def __init__(self, nc):
        self.nc = nc
        self.P = Prog(nc)
        self.es = ExitStack()

    def _uname(self, name):
        self._uid = getattr(self, "_uid", 0) + 1
        return "%s_%d" % (name, self._uid)

    def sb(self, name, shape, dt):
        return self.es.enter_context(self.nc.sbuf_tensor(self._uname(name), shape, dt)), Buf(name)

    def ps(self, name, shape, dt):
        return self.es.enter_context(self.nc.psum_tensor(self._uname(name), shape, dt)), Buf(name)

    def din(self, name, shape, dt):
        return self.nc.dram_tensor(name, list(shape), dt, kind="ExternalInput").ap()

    def dout(self, name, shape, dt):
        return self.nc.dram_tensor(name, list(shape), dt, kind="ExternalOutput").ap()

    def V(self, fn, r=(), w=()):
        return self.P.op("vector", fn, r, w)

    def A(self, fn, r=(), w=()):
        return self.P.op("scalar", fn, r, w)

    def G(self, fn, r=(), w=()):
        return self.P.op("gpsimd", fn, r, w)

    def T(self, fn, r=(), w=()):
        return self.P.op("tensor", fn, r, w)

    def dma(self, q, out, in_, r=(), w=(), **kw):
        return self.P.dma(q, out, in_, r, w, **kw)

    def begin_phase(self):
        self._saved_es = self.es
        self.es = ExitStack()

    def end_phase(self):
        self.P.barrier()
        self.es.close()
        self.es = self._saved_es

    def done(self):
        self.P.finish()
        self.es.close()


def load_w_bf16(k, w_dram, wt, wb, ncols, rows=8):
    first = True
    for kc in range(rows):
        c0 = 0
        while c0 < ncols:
            c1 = min(ncols, c0 + 2048)
            k.dma("gpsimd", wt[:, kc, c0:c1], w_dram[kc * 128:(kc + 1) * 128, c0:c1], w=[] if not first else [wb])
            first = False
            c0 = c1
    return


def build_tok(in_w, has_prev, final, u_dt, NB=18, NLAT=16):
    NT = NB * 128
    nc = bass.Bass("TRN2", target_bir_lowering=False)
    k = KB(nc)
    P = k.P
    x_in = k.din("x_in", [NT, D], F32)
    ident_d = k.din("ident", [128, 128], F32)
    if has_prev:
        mixT = k.din("mixT", [D, NT], BF16)
        w_out = k.din("w_out", [D, D], F32)
        gates_in = k.din("gates_in", [2, 128, D], F32)
        x_out = k.dout("x_out", [NT, D], F32)
    if final:
        fn_g = k.din("final_norm", [1, D], F32)
        y_out = k.dout("y_out", [NT, D], F32)
    else:
        c_cols = k.din("c_cols", [128, 16], F32)
        norm_g = k.din("norm_g", [1, D], F32)
        ada_w = k.din("ada_w", [D, 3 * D], F32)
        ada_b = k.din("ada_b", [1, 3 * D], F32)
        w_in = k.din("w_in", [D, in_w], F32)
        u_out = k.dout("u_out", [NT, in_w], u_dt)
        gates_out = k.dout("gates_out", [2, 128, D], F32)

    ident, ident_b = k.sb("ident_sb", [128, 128], F32)
    identb, identb_b = k.sb("identb_sb", [128, 128], BF16)
    k.dma("sync", ident[:], ident_d, w=[ident_b])
    k.V(lambda E: E.tensor_copy(out=identb[:], in_=ident[:]), [ident_b], [identb_b])
    eps_t, eps_b = k.sb("eps_t", [128, 1], F32)
    k.V(lambda E: E.memset(eps_t[:], EPS), [], [eps_b])
    if has_prev:
        wo, wo_b = k.sb("wo", [128, 8, D], BF16)
        wo_evs = []
        for kc in range(8):
            wo_evs.append(k.dma("gpsimd", wo[:, kc, :], w_out[kc * 128:(kc + 1) * 128, :]))
        gp, gp_b = k.sb("gp", [128, 2, D], F32)
        k.dma("sync", gp[:], gates_in.rearrange("v p d -> p v d"), w=[gp_b])
    if final:
        fg, fg_b = k.sb("fg", [128, D], F32)
        k.dma("sync", fg[:], fn_g.partition_broadcast(128), w=[fg_b])
    else:
        wi, wi_b = k.sb("wi", [128, 8, in_w], BF16)
        wi_evs = []
        for kc in range(8):
            c0 = 0
            while c0 < in_w:
                c1 = min(in_w, c0 + 2048)
                wi_evs.append(k.dma("gpsimd", wi[:, kc, c0:c1], w_in[kc * 128:(kc + 1) * 128, c0:c1]))
                c0 = c1
        mod, mod_b = k.sb("mod", [128, 2, 3 * D], F32)
        Gt, G_b = k.sb("Gt", [128, 2, D], F32)
        with ExitStack() as es1:
            cc, cc_b = es1.enter_context(nc.sbuf_tensor("cc", [128, 16], F32)), Buf()
            rep, rep_b = es1.enter_context(nc.sbuf_tensor("rep", [128, 16, 128], F32)), Buf()
            ones, ones_b = es1.enter_context(nc.sbuf_tensor("ones", [128, 128], F32)), Buf()
            ab, ab_b = es1.enter_context(nc.sbuf_tensor("ab", [128, 3 * D], F32)), Buf()
            ng, ng_b = es1.enter_context(nc.sbuf_tensor("ng", [128, D], F32)), Buf()
            aw = [(es1.enter_context(nc.sbuf_tensor("aw%d" % i, [128, 8, 512], F32)), Buf()) for i in range(2)]
            pa = [(es1.enter_context(nc.psum_tensor("pa%d" % i, [128, 512], F32)), Buf()) for i in range(2)]
            k.dma("sync", cc[:], c_cols, w=[cc_b])
            k.dma("sync", ab[:], ada_b.partition_broadcast(128), w=[ab_b])
            k.dma("sync", ng[:], norm_g.partition_broadcast(128), w=[ng_b])
            k.A(lambda E: E.activation(out=cc[:], in_=cc[:], func=AF.Silu), [cc_b], [cc_b])
            k.V(lambda E: E.memset(ones[:], 1.0), [], [ones_b])
            for j in range(16):
                k.V(lambda E, j=j: E.tensor_scalar(out=rep[:, j, :], in0=ones[:], scalar1=cc[:, j:j + 1], scalar2=None, op0=ALU.mult),
                    [ones_b, cc_b], [rep_b])
            for g in range(6):
                awt, awb = aw[g % 2]
                k.dma("sync", awt[:], ada_w[:, g * 512:(g + 1) * 512].rearrange("(k p) n -> p k n", p=128), w=[awb])
                for v in range(2):
                    pt, pb = pa[v]
                    for kc in range(8):
                        k.T(lambda E, pt=pt, v=v, kc=kc, awt=awt: E.matmul(pt[:], rep[:, v * 8 + kc, :], awt[:, kc, :], start=(kc == 0), stop=(kc == 7)),
                            [rep_b, awb], [pb])
                    k.V(lambda E, pt=pt, v=v, g=g: E.tensor_tensor(out=mod[:, v, g * 512:(g + 1) * 512], in0=pt[:], in1=ab[:, g * 512:(g + 1) * 512], op=ALU.add),
                        [pb, ab_b], [mod_b])
            for v in range(2):
                k.V(lambda E, v=v: E.scalar_tensor_tensor(out=Gt[:, v, :], in0=mod[:, v, D:2 * D], scalar=1.0, in1=ng[:], op0=ALU.add, op1=ALU.mult),
                    [mod_b, ng_b], [G_b])
            k.dma("sync", gates_out.rearrange("v p d -> p v d"), mod[:, :, 2 * D:3 * D], r=[mod_b])
            scope_bufs = [cc_b, rep_b, ones_b, ab_b, ng_b, aw[0][1], aw[1][1], pa[0][1], pa[1][1]]

    xt = [k.sb("xt%d" % i, [128, D], F32) for i in range(2)]
    tmp = [k.sb("tmp%d" % i, [128, D], F32) for i in range(2)]
    ss = [k.sb("ss%d" % i, [128, 4], F32) for i in range(2)]
    junk, junk_b = k.sb("junk", [128, D], BF16)
    if has_prev:
        mt = [k.sb("mt%d" % i, [128, 8, 128], BF16) for i in range(2)]
        py = [k.ps("py%d" % i, [128, 512], F32) for i in range(2)]
    if not final:
        xm = [k.sb("xm%d" % i, [128, D], BF16) for i in range(2)]
        xmT = [k.sb("xmT%d" % i, [128, 8, 128], BF16) for i in range(2)]
        ptr = [k.ps("ptr%d" % i, [128, 8, 128], BF16) for i in range(2)]
        pu = [k.ps("pu%d" % i, [128, 512], F32) for i in range(3)]
        ut = [k.sb("ut%d" % i, [128, 512], u_dt) for i in range(4)]
    if not final:
        fence = Buf("fence")
        for b in scope_bufs:
            if b.w:
                fence.r[b.w[0]] = max(fence.r.get(b.w[0], 0), b.w[1])
            for kk, vv in b.r.items():
                fence.r[kk] = max(fence.r.get(kk, 0), vv)
    else:
        fence = Buf("fence")
    fence_evs = list(fence.r.items())
    for e in ("vector", "scalar", "gpsimd", "tensor", "sync"):
        P._need(e, fence_evs)
    if has_prev:
        for e in ("tensor",):
            P._need(e, wo_evs)
    if not final:
        P._need("tensor", wi_evs)

    ngroups = (in_w + 511) // 512 if not final else 0
    ucount = 0
    for b in range(NB):
        v = 0 if b < NLAT else 1
        xtt, xtb = xt[b % 2]
        tt, tb = tmp[b % 2]
        sst, ssb = ss[b % 2]
        k.dma("sync", xtt[:], x_in[b * 128:(b + 1) * 128, :], w=[xtb])
        if has_prev:
            mtt, mtb = mt[b % 2]
            k.dma("sync", mtt[:], mixT[:, b * 128:(b + 1) * 128].rearrange("(k p) t -> p k t", p=128), w=[mtb])
            for h in range(2):
                pyt, pyb = py[h]
                for kc in range(8):
                    k.T(lambda E, pyt=pyt, mtt=mtt, kc=kc, h=h: E.matmul(pyt[:], mtt[:, kc, :], wo[:, kc, h * 512:(h + 1) * 512], start=(kc == 0), stop=(kc == 7)),
                        [mtb], [pyb])
                k.V(lambda E, pyt=pyt, tt=tt, h=h, v=v: E.tensor_tensor(out=tt[:, h * 512:(h + 1) * 512], in0=pyt[:], in1=gp[:, v, h * 512:(h + 1) * 512], op=ALU.mult),
                    [pyb, gp_b], [tb])
            k.G(lambda E, xtt=xtt, tt=tt: E.tensor_tensor(out=xtt[:], in0=tt[:], in1=xtt[:], op=ALU.add), [tb, xtb], [xtb])
            k.dma("sync", x_out[b * 128:(b + 1) * 128, :], xtt[:], r=[xtb])
        k.A(lambda E, xtt=xtt, sst=sst: E.activation(out=junk[:], in_=xtt[:], func=AF.Square, accum_out=sst[:, 0:1]), [xtb], [junk_b, ssb])
        k.A(lambda E, sst=sst: E.activation(out=sst[:, 1:2], in_=sst[:, 0:1], func=AF.Sqrt, scale=1.0 / D, bias=eps_t[:]), [ssb, eps_b], [ssb])
        k.V(lambda E, sst=sst: E.reciprocal(out=sst[:, 2:3], in_=sst[:, 1:2]), [ssb], [ssb])
        if final:
            k.V(lambda E, xtt=xtt, tt=tt, sst=sst: E.scalar_tensor_tensor(out=tt[:], in0=xtt[:], scalar=sst[:, 2:3], in1=fg[:], op0=ALU.mult, op1=ALU.mult),
                [xtb, ssb, fg_b], [tb])
            k.dma("sync", y_out[b * 128:(b + 1) * 128, :], tt[:], r=[tb])
            continue
        xmt, xmb = xm[b % 2]
        k.V(lambda E, xtt=xtt, tt=tt, sst=sst, v=v: E.scalar_tensor_tensor(out=tt[:], in0=xtt[:], scalar=sst[:, 2:3], in1=Gt[:, v, :], op0=ALU.mult, op1=ALU.mult),
            [xtb, ssb, G_b], [tb])
        k.G(lambda E, tt=tt, xmt=xmt, v=v: E.tensor_tensor(out=xmt[:], in0=tt[:], in1=mod[:, v, 0:D], op=ALU.add), [tb, mod_b], [xmb])
        ptt, ptb = ptr[b % 2]
        xTt, xTb = xmT[b % 2]
        for kc in range(8):
            k.T(lambda E, ptt=ptt, xmt=xmt, kc=kc: E.transpose(ptt[:, kc, :], xmt[:, kc * 128:(kc + 1) * 128], identb[:]), [xmb, identb_b], [ptb])
        k.A(lambda E, ptt=ptt, xTt=xTt: E.copy(out=xTt[:], in_=ptt[:]), [ptb], [xTb])
        for g in range(ngroups):
            c0 = g * 512
            c1 = min(in_w, c0 + 512)
            put, pub = pu[ucount % 3]
            utt, utb = ut[ucount % 4]
            for kc in range(8):
                k.T(lambda E, put=put, xTt=xTt, kc=kc, c0=c0, c1=c1: E.matmul(put[:, 0:c1 - c0], xTt[:, kc, :], wi[:, kc, c0:c1], start=(kc == 0), stop=(kc == 7)),
                    [xTb], [pub])
            if ucount % 2 == 0:
                k.V(lambda E, put=put, utt=utt, c0=c0, c1=c1: E.tensor_copy(out=utt[:, 0:c1 - c0], in_=put[:, 0:c1 - c0]), [pub], [utb])
            else:
                k.A(lambda E, put=put, utt=utt, c0=c0, c1=c1: E.copy(out=utt[:, 0:c1 - c0], in_=put[:, 0:c1 - c0]), [pub], [utb])
            k.dma("sync", u_out[b * 128:(b + 1) * 128, c0:c1], utt[:, 0:c1 - c0], r=[utb])
            ucount += 1
    k.done()
    return nc


def _sc(s):
    return (s[0], [s[1]]) if isinstance(s, tuple) else (s, [])


def h_tt(k, out, a, b, op, eng="vector"):
    oa, ob = out; aa, ab = a; ba, bb = b
    return k.P.op(eng, lambda E: E.tensor_tensor(out=oa, in0=aa, in1=ba, op=op), [ab, bb], [ob])


def h_stt(k, out, a, s, b, op0, op1, accum=None):
    oa, ob = out; aa, ab = a; ba, bb = b
    sv, sb_ = _sc(s)
    if accum is None:
        return k.P.op("vector", lambda E: E.scalar_tensor_tensor(out=oa, in0=aa, scalar=sv, in1=ba, op0=op0, op1=op1), [ab, bb] + sb_, [ob])
    ca, cb = accum
    return k.P.op("vector", lambda E: E.scalar_tensor_tensor(out=oa, in0=aa, scalar=sv, in1=ba, op0=op0, op1=op1, accum_out=ca), [ab, bb] + sb_, [ob, cb])


def h_ts(k, out, a, s1, s2, op0, op1=None, eng="vector"):
    oa, ob = out; aa, ab = a
    s1v, s1b = _sc(s1)
    s2v, s2b = _sc(s2) if s2 is not None else (None, [])
    if op1 is None:
        return k.P.op(eng, lambda E: E.tensor_scalar(out=oa, in0=aa, scalar1=s1v, scalar2=None, op0=op0), [ab] + s1b, [ob])
    return k.P.op(eng, lambda E: E.tensor_scalar(out=oa, in0=aa, scalar1=s1v, scalar2=s2v, op0=op0, op1=op1), [ab] + s1b + s2b, [ob])


def h_act(k, out, a, func, scale=1.0, bias=None, accum=None):
    oa, ob = out; aa, ab = a
    kw = {}
    rd = [ab]
    wr = [ob]
    if bias is not None:
        bv, bb = _sc(bias)
        kw["bias"] = bv
        rd += bb
    if isinstance(scale, tuple):
        rd.append(scale[1]); scale = scale[0]
    if accum is not None:
        kw["accum_out"] = accum[0]
        wr.append(accum[1])
    return k.P.op("scalar", lambda E: E.activation(out=oa, in_=aa, func=func, scale=scale, **kw), rd, wr)


def h_recip(k, out, a):
    oa, ob = out; aa, ab = a
    return k.P.op("vector", lambda E: E.reciprocal(out=oa, in_=aa), [ab], [ob])


def h_copy(k, out, a, eng="vector"):
    oa, ob = out; aa, ab = a
    if eng == "scalar":
        return k.P.op(eng, lambda E: E.copy(out=oa, in_=aa), [ab], [ob])
    return k.P.op(eng, lambda E: E.tensor_copy(out=oa, in_=aa), [ab], [ob])


def h_mm(k, out, lhsT, rhs, start=True, stop=True):
    oa, ob = out; la, lb = lhsT; ra, rb = rhs
    return k.P.op("tensor", lambda E: E.matmul(oa, la, ra, start=start, stop=stop), [lb, rb], [ob])


def h_memset(k, out, val, eng="vector"):
    oa, ob = out
    return k.P.op(eng, lambda E: E.memset(oa, val), [], [ob])


def h_sigmoid(k, out, a, tmp, scale=1.0, bias=None):
    h_act(k, tmp, a, AF.Exp, scale=-scale, bias=bias)
    h_ts(k, tmp, tmp, 1.0, None, ALU.add)
    h_recip(k, out, tmp)


def h_silu(k, out, a, tmp):
    h_sigmoid(k, tmp, a, tmp)
    h_tt(k, out, tmp, a, ALU.mult)


import numpy as np

NEG = -30000.0


def na_plan(nlb):
    rows_blocks = nlb
    plan = []
    variants = {}
    for m in range(nlb):
        r0 = min(max(2 * m - 4, 0), 2 * nlb - 8)
        r1 = min(max(2 * m + 1 - 4, 0), 2 * nlb - 8) + 7
        kb0, kb1 = r0 // 2, r1 // 2
        edge = (m < 2) or (m >= nlb - 2)
        lst = []
        for kb in range(kb0, kb1 + 1):
            key = (m if edge else "i", kb - m)
            if key not in variants:
                variants[key] = (len(variants), m, kb)
            lst.append((kb, variants[key][0]))
        plan.append(lst)
    return plan, variants


def na_bias_tables(rpb_h, nlb):
    plan, variants = na_plan(nlb)
    rows = 2 * nlb
    out = np.full((128, len(variants), 128), NEG, np.float32)
    for key, (vid, m, kb) in variants.items():
        q = m * 128 + np.arange(128)
        kk = kb * 128 + np.arange(128)
        qr, qc = q // 64, q % 64
        kr, kc = kk // 64, kk % 64
        row0 = np.clip(qr - 4, 0, rows - 8)
        col0 = np.clip(qc - 8, 0, 64 - 16)
        inwin = ((kr[:, None] >= row0[None]) & (kr[:, None] < row0[None] + 8) &
                 (kc[:, None] >= col0[None]) & (kc[:, None] < col0[None] + 16))
        rr = np.clip(kr[:, None] - qr[None] + 7, 0, 14)
        cc = np.clip(kc[:, None] - qc[None] + 15, 0, 30)
        out[:, vid, :] = np.where(inwin, rpb_h[rr, cc], NEG)
    return out


def build_attn(nlb=128):
    NTK = (nlb + 2) * 128
    NKB = nlb + 2
    plan, variants = na_plan(nlb)
    nvar = len(variants)
    nc = bass.Bass("TRN2", target_bir_lowering=False)
    k = KB(nc)
    P = k.P
    qa_d = k.din("qaT", [64, NTK], BF16)
    ka_d = k.din("kaT", [64, NTK], BF16)
    va_d = k.din("va", [128, NKB, 65], BF16)
    ga_d = k.din("gaT", [64, NTK], BF16)
    qb_d = k.din("qbT", [64, NTK], BF16)
    kb_d = k.din("kbT", [64, NTK], BF16)
    vb_d = k.din("vb", [128, NKB, 65], BF16)
    gb_d = k.din("gbT", [64, NTK], BF16)
    bias_d = k.din("biasT", [128, nvar, 128], F32)
    cs_d = k.din("ropeC", [64, NTK], F32)
    sn_d = k.din("ropeS", [64, NTK], F32)
    gains_d = k.din("gains", [64, 2], F32)
    rmat_d = k.din("rmatT", [64, 64], F32)
    ident_d = k.din("ident", [128, 128], F32)
    sel_d = k.din("sel", [65, 64], F32)
    out_d = k.dout("mixT", [128, NTK], BF16)

    qT, qT_b = k.sb("qT", [64, NTK], BF16)
    kT, kT_b = k.sb("kT", [64, NTK], BF16)
    vE, vE_b = k.sb("vE", [128, NKB, 65], BF16)
    biasf, biasf_b = k.sb("biasf", [128, nvar, 128], F32)
    biasb, biasb_b = k.sb("biasb", [128, nvar, 128], BF16)
    identf, identf_b = k.sb("identf", [128, 128], F32)
    identb, identb_b = k.sb("identb", [128, 128], BF16)
    sel, sel_b = k.sb("sel_sb", [65, 64], F32)
    gains, gains_b = k.sb("gains_sb", [64, 2], F32)
    rmf, rmf_b = k.sb("rmf", [64, 64], F32)
    rmb, rmb_b = k.sb("rmb", [64, 64], BF16)
    ones64, ones64_b = k.sb("ones64", [64, 64], F32)
    eps_t, eps_b = k.sb("eps_t", [64, 1], F32)
    k.dma("sync", biasf[:], bias_d, w=[biasf_b])
    k.dma("sync", identf[:], ident_d, w=[identf_b])
    k.dma("sync", sel[:], sel_d, w=[sel_b])
    k.dma("sync", gains[:], gains_d, w=[gains_b])
    k.dma("sync", rmf[:], rmat_d, w=[rmf_b])
    k.V(lambda E: E.tensor_copy(out=biasb[:], in_=biasf[:]), [biasf_b], [biasb_b])
    k.V(lambda E: E.tensor_copy(out=identb[:], in_=identf[:]), [identf_b], [identb_b])
    k.V(lambda E: E.tensor_copy(out=rmb[:], in_=rmf[:]), [rmf_b], [rmb_b])
    k.V(lambda E: E.memset(ones64[:], 1.0 / 64), [], [ones64_b])
    k.V(lambda E: E.memset(eps_t[:], EPS), [], [eps_b])
    k.V(lambda E: E.tensor_scalar(out=gains[:, 0:1], in0=gains[:, 0:1], scalar1=0.125, scalar2=None, op0=ALU.mult), [gains_b], [gains_b])

    pS = [k.ps("pS%d" % i, [128, 512], F32) for i in range(4)]
    pacc = [k.ps("pacc%d" % i, [65, 512], F32) for i in range(2)]
    pmisc = [k.ps("pm%d" % i, [64, 512], F32) for i in range(2)]
    PT = [k.sb("PT%d" % i, [128, 512], BF16) for i in range(4)]
    accs = [k.sb("accs%d" % i, [65, 512], F32) for i in range(2)]
    gt = [k.sb("gt%d" % i, [64, 512], BF16) for i in range(2)]
    w1 = [k.sb("w1_%d" % i, [64, 512], F32) for i in range(2)]
    w2 = [k.sb("w2_%d" % i, [64, 512], F32) for i in range(2)]
    ot = [k.sb("ot%d" % i, [64, 512], BF16) for i in range(2)]
    cin = [k.sb("cin%d" % i, [64, 512], BF16) for i in range(2)]
    ctab = [k.sb("ctab%d" % i, [64, 512], F32) for i in range(2)]
    stab = [k.sb("stab%d" % i, [64, 512], F32) for i in range(2)]
    qnb = [k.sb("qnb%d" % i, [64, 512], BF16) for i in range(2)]

    state = {"fin": 0, "S": 0, "pre": 0}

    def finalize(pa, pab, g_d, row0, c0, n):
        i = state["fin"] % 2
        state["fin"] += 1
        at, ab = accs[i]
        gtt, gtb = gt[i]
        a1, a1b = w1[i]
        a2, a2b = w2[i]
        o, ob = ot[i]
        pm, pmb = pmisc[i]
        k.dma("sync", gtt[:, 0:n], g_d[:, c0:c0 + n], w=[gtb])
        k.A(lambda E: E.copy(out=at[:, 0:n], in_=pa[:, 0:n]), [pab], [ab])
        k.T(lambda E: E.matmul(pm[:, 0:n], sel[:], at[:, 0:n], start=True, stop=True), [sel_b, ab], [pmb])
        k.V(lambda E: E.reciprocal(out=a1[:, 0:n], in_=pm[:, 0:n]), [pmb], [a1b])
        k.V(lambda E: E.tensor_tensor(out=a1[:, 0:n], in0=a1[:, 0:n], in1=at[0:64, 0:n], op=ALU.mult), [a1b, ab], [a1b])
        k.A(lambda E: E.activation(out=a2[:, 0:n], in_=gtt[:, 0:n], func=AF.Exp, scale=-1.0), [gtb], [a2b])
        k.V(lambda E: E.tensor_scalar(out=a2[:, 0:n], in0=a2[:, 0:n], scalar1=1.0, scalar2=None, op0=ALU.add), [a2b], [a2b])
        k.V(lambda E: E.reciprocal(out=a2[:, 0:n], in_=a2[:, 0:n]), [a2b], [a2b])
        k.V(lambda E: E.tensor_tensor(out=a2[:, 0:n], in0=a2[:, 0:n], in1=gtt[:, 0:n], op=ALU.mult), [a2b, gtb], [a2b])
        k.V(lambda E: E.tensor_tensor(out=o[:, 0:n], in0=a1[:, 0:n], in1=a2[:, 0:n], op=ALU.mult), [a1b, a2b], [ob])
        k.dma("sync", out_d[row0:row0 + 64, c0:c0 + n], o[:, 0:n], r=[ob])

    def attend(qc0, n, kbs, pa, pab, first=True, last=True):
        pend = []
        nk = len(kbs)
        for idx, kb in enumerate(kbs):
            s = state["S"]
            state["S"] += 1
            pst, psb = pS[s % 4]
            ptt, ptb = PT[s % 4]
            k.T(lambda E, pst=pst, kb=kb: E.matmul(pst[:, 0:n], kT[:, kb * 128:(kb + 1) * 128], qT[:, qc0:qc0 + n], start=True, stop=True),
                [kT_b, qT_b], [psb])
            k.A(lambda E, pst=pst, ptt=ptt: E.activation(out=ptt[:, 0:n], in_=pst[:, 0:n], func=AF.Exp), [psb], [ptb])
            pend.append((idx, kb, ptt, ptb))
            if len(pend) > 2:
                j, kbj, pj, pjb = pend.pop(0)
                k.T(lambda E, kbj=kbj, pj=pj, j=j: E.matmul(pa[:, 0:n], vE[:, kbj, :], pj[:, 0:n], start=(first and j == 0), stop=(last and j == nk - 1)),
                    [vE_b, pjb], [pab])
        for (j, kbj, pj, pjb) in pend:
            k.T(lambda E, kbj=kbj, pj=pj, j=j: E.matmul(pa[:, 0:n], vE[:, kbj, :], pj[:, 0:n], start=(first and j == 0), stop=(last and j == nk - 1)),
                [vE_b, pjb], [pab])

    k.dma("sync", kT[:], ka_d, w=[kT_b])
    k.dma("sync", vE[:], va_d, w=[vE_b])
    k.dma("sync", qT[:], qa_d, w=[qT_b])
    k.V(lambda E: E.tensor_scalar(out=qT[:], in0=qT[:], scalar1=0.125, scalar2=None, op0=ALU.mult), [qT_b], [qT_b])
    ctxk = [nlb, nlb + 1]
    acc_i = 0
    for g0 in range(0, nlb, 4):
        pa, pab = pacc[acc_i % 2]
        acc_i += 1
        nq = min(4, nlb - g0)
        for mi in range(nq):
            m = g0 + mi
            lst = plan[m]
            nreg = len(lst) + 2
            s = state["S"]
            state["S"] += 2
            pA, pAb = pS[s % 4]
            pB, pBb = pS[(s + 1) % 4]
            tA, tAb = PT[s % 4]
            tB, tBb = PT[(s + 1) % 4]
            regs = []
            for j, (kb, var) in enumerate(lst + [(ctxk[0], None), (ctxk[1], None)]):
                pt_, pb_, tt_, tb_ = (pA, pAb, tA, tAb) if j < 4 else (pB, pBb, tB, tBb)
                jj = j % 4
                k.T(lambda E, pt_=pt_, kb=kb, jj=jj, m=m, var=var: E.matmul(pt_[:, jj * 128:(jj + 1) * 128], kT[:, kb * 128:(kb + 1) * 128], qT[:, m * 128:(m + 1) * 128], start=True, stop=(var is None)),
                    [kT_b, qT_b], [pb_])
                if var is not None:
                    k.T(lambda E, pt_=pt_, jj=jj, var=var: E.matmul(pt_[:, jj * 128:(jj + 1) * 128], identb[:], biasb[:, var, :], start=False, stop=True),
                        [identb_b, biasb_b], [pb_])
                regs.append((kb, tt_, tb_, jj))
            nA = min(4, nreg)
            k.A(lambda E, pA=pA, tA=tA, nA=nA: E.activation(out=tA[:, 0:nA * 128], in_=pA[:, 0:nA * 128], func=AF.Exp), [pAb], [tAb])
            if nreg > 4:
                nB = nreg - 4
                k.A(lambda E, pB=pB, tB=tB, nB=nB: E.activation(out=tB[:, 0:nB * 128], in_=pB[:, 0:nB * 128], func=AF.Exp), [pBb], [tBb])
            for j, (kb, tt_, tb_, jj) in enumerate(regs):
                k.T(lambda E, pa=pa, kb=kb, tt_=tt_, jj=jj, mi=mi, j=j, nreg=nreg: E.matmul(pa[:, mi * 128:(mi + 1) * 128], vE[:, kb, :], tt_[:, jj * 128:(jj + 1) * 128], start=(j == 0), stop=(j == nreg - 1)),
                    [vE_b, tb_], [pab])
        finalize(pa, pab, ga_d, 0, g0 * 128, nq * 128)
    pa, pab = pacc[acc_i % 2]
    acc_i += 1
    attend(nlb * 128, 256, ctxk, pa, pab)
    finalize(pa, pab, ga_d, 0, nlb * 128, 256)

    k.dma("sync", vE[:], vb_d, w=[vE_b])

    def prepass(src_d, dst, dst_b, gcol):
        for c0_ in range(0, NTK, 512):
            pre_chunk(src_d, dst, dst_b, gcol, c0_)

    def pre_chunk(src_d, dst, dst_b, gcol, c0):
        if True:
            n = min(512, NTK - c0)
            i = state["pre"] % 2
            state["pre"] += 1
            ci, cib = cin[i]
            ct, ctb = ctab[i]
            st_, stb = stab[i]
            qn, qnb_ = qnb[i]
            a1, a1b = w1[i]
            a2, a2b = w2[i]
            pm, pmb = pmisc[i]
            k.dma("sync", ci[:, 0:n], src_d[:, c0:c0 + n], w=[cib])
            k.dma("sync", ct[:, 0:n], cs_d[:, c0:c0 + n], w=[ctb])
            k.dma("sync", st_[:, 0:n], sn_d[:, c0:c0 + n], w=[stb])
            k.V(lambda E: E.tensor_tensor(out=a1[:, 0:n], in0=ci[:, 0:n], in1=ci[:, 0:n], op=ALU.mult), [cib], [a1b])
            k.T(lambda E: E.matmul(pm[:, 0:n], ones64[:], a1[:, 0:n], start=True, stop=True), [ones64_b, a1b], [pmb])
            k.A(lambda E: E.activation(out=a2[:, 0:n], in_=pm[:, 0:n], func=AF.Sqrt, bias=eps_t[:]), [pmb, eps_b], [a2b])
            k.V(lambda E: E.reciprocal(out=a2[:, 0:n], in_=a2[:, 0:n]), [a2b], [a2b])
            k.V(lambda E: E.scalar_tensor_tensor(out=qn[:, 0:n], in0=ci[:, 0:n], scalar=gains[:, gcol:gcol + 1], in1=a2[:, 0:n], op0=ALU.mult, op1=ALU.mult),
                [cib, gains_b, a2b], [qnb_])
            k.T(lambda E: E.matmul(pm[:, 0:n], rmb[:], qn[:, 0:n], start=True, stop=True), [rmb_b, qnb_], [pmb])
            k.V(lambda E: E.tensor_tensor(out=a1[:, 0:n], in0=qn[:, 0:n], in1=ct[:, 0:n], op=ALU.mult), [qnb_, ctb], [a1b])
            k.V(lambda E: E.tensor_tensor(out=a2[:, 0:n], in0=pm[:, 0:n], in1=st_[:, 0:n], op=ALU.mult), [pmb, stb], [a2b])
            k.G(lambda E: E.tensor_tensor(out=dst[:, c0:c0 + n], in0=a1[:, 0:n], in1=a2[:, 0:n], op=ALU.add), [a1b, a2b], [dst_b])

    prepass(kb_d, kT, kT_b, 1)
    prepass(qb_d, qT, qT_b, 0)
    allk = list(range(NKB))
    for c0 in range(0, nlb * 128, 512):
        n = min(512, nlb * 128 - c0)
        pa, pab = pacc[acc_i % 2]
        acc_i += 1
        attend(c0, n, allk, pa, pab)
        finalize(pa, pab, gb_d, 64, c0, n)
    pa, pab = pacc[acc_i % 2]
    acc_i += 1
    attend(nlb * 128, 256, ctxk, pa, pab)
    finalize(pa, pab, gb_d, 64, nlb * 128, 256)
    k.done()
    return nc


def rope_tables(nlb):
    n = nlb * 128
    t = np.arange(n)
    pos = np.stack([t // 64, t % 64], -1).astype(np.float32)
    inv = (10000.0 ** (-np.arange(16, dtype=np.float32) / 16)).astype(np.float32)
    ang = pos[:, :, None] * inv
    cos, sin = np.cos(ang), np.sin(ang)
    C = np.ones((64, n + 256), np.float32)
    S = np.zeros((64, n + 256), np.float32)
    for a in range(2):
        for hf in range(2):
            C[a * 32 + hf * 16:a * 32 + hf * 16 + 16, :n] = cos[:, a, :].T
            S[a * 32 + hf * 16:a * 32 + hf * 16 + 16, :n] = sin[:, a, :].T
    R = np.zeros((64, 64), np.float32)
    for a in range(2):
        for f in range(16):
            R[a * 32 + f, a * 32 + 16 + f] = -1.0
            R[a * 32 + 16 + f, a * 32 + f] = 1.0
    return C, S, np.ascontiguousarray(R.T)


import numpy as np

PC = {n: i for i, n in enumerate(
    ["mu_r", "mu_k", "mu_v", "k_k", "k_a", "w0_f", "w0_b", "a0_f", "a0_b", "r_k",
     "t0_v", "t1_v", "t2_v", "t0_x1", "t1_x1", "t2_x1", "t0_x2", "t1_x2", "t2_x2",
     "gn_w", "gn_b", "skip0", "skip1"])}
NPC = len(PC)
FO = {n: i for i, n in enumerate(
    ["w_f", "w_b", "kkn", "nb_f", "nb_b", "k_f", "k_b", "r", "v", "bonus", "sg_rw", "hv", "hx1", "hx2", "sg_hy"])}
NFO = len(FO)
FI = {n: i for i, n in enumerate(["r", "k", "v", "g_rw", "hv", "hx1", "hx2", "g_hy"])}
NFI = len(FI)


def seq_chunks(n_lat):
    ch = [(0, 0, 256)]
    for c0 in range(0, n_lat, 512):
        n = min(512, n_lat - c0)
        ch.append((258 + c0, 256 + c0, n))
    return ch


def build_rfeat(n_lat):
    T = 256 + n_lat
    TP = T + 4
    nc = bass.Bass("TRN2", target_bir_lowering=False)
    k = KB(nc)
    fin_d = k.din("fin", [NFI, 64, TP], F32)
    lora_d = k.din("lora", [128, TP], F32)
    pp_d = k.din("pp", [64, NPC], F32)
    mul_d = k.din("mu_lora", [128, 1], F32)
    wx_d = k.din("wx", [128, 4, 64], F32)
    fo_d = k.dout("fo", [NFO, 64, T], F32)

    pp = k.sb("pp_sb", [64, NPC], F32)
    npp = k.sb("npp_sb", [64, NPC], F32)
    mul = k.sb("mul_sb", [128, 1], F32)
    wx = k.sb("wx_sb", [128, 4, 64], F32)
    ones = k.sb("ones_sb", [64, 64], F32)
    tiny = k.sb("tiny_sb", [64, 1], F32)
    k.dma("sync", pp[0][:], pp_d, w=[pp[1]])
    k.dma("sync", mul[0][:], mul_d, w=[mul[1]])
    k.dma("sync", wx[0][:], wx_d, w=[wx[1]])
    h_memset(k, (ones[0][:], ones[1]), 1.0)
    h_memset(k, (tiny[0][:], tiny[1]), 1e-12)
    h_ts(k, (npp[0][:], npp[1]), (pp[0][:], pp[1]), -1.0, None, ALU.mult)
    h_ts(k, (npp[0][:, PC["r_k"]:PC["r_k"] + 1], npp[1]), (pp[0][:, PC["r_k"]:PC["r_k"] + 1], pp[1]), 0.5, None, ALU.mult)

    def pcol(name):
        return (pp[0][:, PC[name]:PC[name] + 1], pp[1])

    def ncol(name):
        return (npp[0][:, PC[name]:PC[name] + 1], npp[1])

    fin = [[k.sb("fin%d_%d" % (i, j), [64, 514], F32) for j in range(2)] for i in range(NFI)]
    lor = [k.sb("lor%d" % j, [128, 514], F32) for j in range(2)]
    fo = [[k.sb("fo%d_%d" % (i, j), [64, 512], F32) for j in range(2)] for i in range(NFO)]
    tm = [k.sb("tm%d" % i, [64, 512], F32) for i in range(8)]
    lt = [k.sb("lt%d" % i, [128, 512], F32) for i in range(3)]
    pz = [k.ps("pz%d" % i, [64, 512], F32) for i in range(6)]

    def chunk(ci, pc0, oc0, n):
        j = ci % 2

        def I(name, lo=1):
            t, b = fin[FI[name]][j]
            return (t[:, lo:lo + n], b)

        def O(name):
            t, b = fo[FO[name]][j]
            return (t[:, 0:n], b)

        def Tm(i):
            return (tm[i][0][:, 0:n], tm[i][1])

        def Lt(i):
            return (lt[i][0][:, 0:n], lt[i][1])

        def Pz(i):
            return (pz[i][0][:, 0:n], pz[i][1])

        for name, i in FI.items():
            t, b = fin[i][j]
            k.dma("sync", t[:, 0:n + 2], fin_d[i, :, pc0:pc0 + n + 2], w=[b])
        lt_, lb_ = lor[j]
        k.dma("sync", lt_[:, 0:n + 2], lora_d[:, pc0:pc0 + n + 2], w=[lb_])

        def shift(out, src_lo, src_mid, src_hi, mu, t):
            h_tt(k, t, src_lo, src_hi, ALU.add)
            h_stt(k, t, t, 0.5, src_mid, ALU.mult, ALU.subtract)
            h_stt(k, out, t, mu, src_mid, ALU.mult, ALU.add)

        rs, ks, vs = O("r"), Tm(0), O("v")
        shift(rs, I("r", 0), I("r", 1), I("r", 2), pcol("mu_r"), Tm(7))
        shift(ks, I("k", 0), I("k", 1), I("k", 2), pcol("mu_k"), Tm(7))
        shift(vs, I("v", 0), I("v", 1), I("v", 2), pcol("mu_v"), Tm(7))
        ls = Lt(0)
        shift(ls, (lt_[:, 0:n], lb_), (lt_[:, 1:n + 1], lb_), (lt_[:, 2:n + 2], lb_), (mul[0][:], mul[1]), Lt(2))
        lth = Lt(1)
        h_act(k, lth, ls, AF.Tanh)
        h_mm(k, Pz(0), (wx[0][:, 0, :], wx[1]), lth)
        h_mm(k, Pz(1), (wx[0][:, 1, :], wx[1]), lth)
        h_mm(k, Pz(2), (wx[0][:, 2, :], wx[1]), ls)
        h_mm(k, Pz(3), (wx[0][:, 3, :], wx[1]), ls)
        kk = Tm(1)
        h_ts(k, kk, ks, pcol("k_k"), None, ALU.mult)
        h_tt(k, Tm(2), kk, kk, ALU.mult)
        h_mm(k, Pz(4), (ones[0][:], ones[1]), Tm(2))
        h_act(k, Tm(2), Pz(4), AF.Sqrt, bias=(tiny[0][:], tiny[1]))
        h_recip(k, Tm(2), Tm(2))
        h_tt(k, O("kkn"), kk, Tm(2), ALU.mult)
        for d, sfx in enumerate(("f", "b")):
            h_sigmoid(k, Tm(3), Pz(d), Tm(3), bias=ncol("w0_" + sfx))
            h_act(k, O("w_" + sfx), Tm(3), AF.Exp, scale=-float(np.exp(-0.5)))
            a = Tm(4)
            h_sigmoid(k, a, Pz(2 + d), a, bias=ncol("a0_" + sfx))
            h_ts(k, Tm(5), a, -1.0, pcol("k_a"), ALU.add, ALU.mult)
            h_stt(k, O("k_" + sfx), Tm(5), 1.0, ks, ALU.add, ALU.mult)
            h_stt(k, O("nb_" + sfx), O("kkn"), -1.0, a, ALU.mult, ALU.mult)
        h_tt(k, Tm(5), O("k_f"), O("k_b"), ALU.add)
        h_stt(k, Tm(5), rs, ncol("r_k"), Tm(5), ALU.mult, ALU.mult)
        h_mm(k, Pz(5), (ones[0][:], ones[1]), Tm(5))
        h_tt(k, O("bonus"), Pz(5), vs, ALU.mult)
        h_silu(k, O("sg_rw"), I("g_rw", 1), Tm(6))
        h_silu(k, O("sg_hy"), I("g_hy", 1), Tm(6))
        for nm in ("v", "x1", "x2"):
            src = "h" + nm
            h_ts(k, Tm(6), I(src, 0), pcol("t0_" + nm), None, ALU.mult)
            h_stt(k, Tm(6), I(src, 1), pcol("t1_" + nm), Tm(6), ALU.mult, ALU.add)
            h_stt(k, O(src), I(src, 2), pcol("t2_" + nm), Tm(6), ALU.mult, ALU.add)
        for name, i in FO.items():
            t, b = fo[i][j]
            k.dma("sync", fo_d[i, :, oc0:oc0 + n], t[:, 0:n], r=[b])

    for ci, (pc0, oc0, n) in enumerate(seq_chunks(n_lat)):
        chunk(ci, pc0, oc0, n)
    k.done()
    return nc


import numpy as np

TWO_PI = float(2 * np.pi)


def build_filt(L):
    nc = bass.Bass("TRN2", target_bir_lowering=False)
    k = KB(nc)
    feats_d = k.din("featsT", [33, L], F32)
    tv_d = k.din("tvals", [1, L], F32)
    w1_d = k.din("w1", [33, 64], F32)
    w2_d = k.din("w2", [64, 64], F32)
    w3_d = k.din("w3c", [64, 256], F32)
    bb_d = k.din("b12", [64, 2], F32)
    b3_d = k.din("b3c", [128, 2], F32)
    nd_d = k.din("negdelta", [128, 1], F32)
    tf_d = k.dout("tapsF", [128, L], BF16)
    tb_d = k.dout("tapsB", [128, L], BF16)
    ssq_d = k.dout("ssq", [128, 1], F32)
    w1 = k.sb("w1s", [33, 64], F32); w2 = k.sb("w2s", [64, 64], F32); w3 = k.sb("w3s", [64, 256], F32)
    bb = k.sb("bbs", [64, 2], F32); b3 = k.sb("b3s", [128, 2], F32); nd = k.sb("nds", [128, 1], F32)
    for t, d in ((w1, w1_d), (w2, w2_d), (w3, w3_d), (bb, bb_d), (b3, b3_d), (nd, nd_d)):
        k.dma("sync", t[0][:], d, w=[t[1]])
    nch = (L + 511) // 512
    part = k.sb("part", [128, 2 * nch], F32)
    h_memset(k, (part[0][:], part[1]), 0.0)
    ft = [k.sb("ft%d" % i, [33, 512], F32) for i in range(2)]
    tv = [k.sb("tv%d" % i, [128, 512], F32) for i in range(2)]
    hx = [k.sb("hx%d" % i, [64, 512], F32) for i in range(2)]
    hi = k.sb("hi", [64, 512], I32)
    hf = k.sb("hf", [64, 512], F32)
    win = k.sb("win", [128, 512], F32)
    tF = [k.sb("tF%d" % i, [128, 512], F32) for i in range(2)]
    tB = [k.sb("tB%d" % i, [128, 512], F32) for i in range(2)]
    oF = [k.sb("oF%d" % i, [128, 512], BF16) for i in range(2)]
    oB = [k.sb("oB%d" % i, [128, 512], BF16) for i in range(2)]
    junk = k.sb("junkf", [128, 512], F32)
    ph = [k.ps("ph%d" % i, [64, 512], F32) for i in range(2)]
    pt = [k.ps("pt%d" % i, [128, 512], F32) for i in range(2)]

    def sin_layer(out, psum, bias, n):
        x = (hf[0][:, 0:n], hf[1])
        ki = (hi[0][:, 0:n], hi[1])
        h_ts(k, out, psum, bias, None, ALU.add)
        h_ts(k, ki, out, 1.0 / TWO_PI, None, ALU.mult)
        h_copy(k, x, ki)
        h_stt(k, out, x, -TWO_PI, out, ALU.mult, ALU.add)
        h_ts(k, out, out, -float(np.pi), float(np.pi), ALU.max, ALU.min)
        h_act(k, out, out, AF.Sin)

    for c in range(nch):
        c0 = c * 512
        n = min(512, L - c0)
        j = c % 2
        k.dma("sync", ft[j][0][:, 0:n], feats_d[:, c0:c0 + n], w=[ft[j][1]])
        k.dma("sync", tv[j][0][:, 0:n], tv_d[:, c0:c0 + n].partition_broadcast(128), w=[tv[j][1]])
        h_mm(k, (ph[0][0][:, 0:n], ph[0][1]), (w1[0][:], w1[1]), (ft[j][0][:, 0:n], ft[j][1]))
        h1 = (hx[0][0][:, 0:n], hx[0][1])
        sin_layer(h1, (ph[0][0][:, 0:n], ph[0][1]), (bb[0][:, 0:1], bb[1]), n)
        h_mm(k, (ph[1][0][:, 0:n], ph[1][1]), (w2[0][:], w2[1]), h1)
        h2 = (hx[1][0][:, 0:n], hx[1][1])
        sin_layer(h2, (ph[1][0][:, 0:n], ph[1][1]), (bb[0][:, 1:2], bb[1]), n)
        h_mm(k, (pt[0][0][:, 0:n], pt[0][1]), (w3[0][:, 0:128], w3[1]), h2)
        h_mm(k, (pt[1][0][:, 0:n], pt[1][1]), (w3[0][:, 128:256], w3[1]), h2)
        w_ = (win[0][:, 0:n], win[1])
        h_act(k, w_, (tv[j][0][:, 0:n], tv[j][1]), AF.Exp, scale=(nd[0][:], nd[1]))
        F_ = (tF[j][0][:, 0:n], tF[j][1])
        B_ = (tB[j][0][:, 0:n], tB[j][1])
        h_stt(k, F_, (pt[0][0][:, 0:n], pt[0][1]), (b3[0][:, 0:1], b3[1]), w_, ALU.add, ALU.mult)
        h_stt(k, B_, (pt[1][0][:, 0:n], pt[1][1]), (b3[0][:, 1:2], b3[1]), w_, ALU.add, ALU.mult)
        lo = 0
        if c == 0:
            h_tt(k, (tF[j][0][:, 0:1], tF[j][1]), (tF[j][0][:, 0:1], tF[j][1]), (tB[j][0][:, 0:1], tB[j][1]), ALU.add)
            lo = 1
        h_act(k, (junk[0][:, 0:n], junk[1]), F_, AF.Square, accum=(part[0][:, 2 * c:2 * c + 1], part[1]))
        h_act(k, (junk[0][:, lo:n], junk[1]), (tB[j][0][:, lo:n], tB[j][1]), AF.Square, accum=(part[0][:, 2 * c + 1:2 * c + 2], part[1]))
        h_copy(k, (oF[j][0][:, 0:n], oF[j][1]), F_)
        h_copy(k, (oB[j][0][:, 0:n], oB[j][1]), B_, eng="gpsimd")
        k.dma("sync", tf_d[:, c0:c0 + n], oF[j][0][:, 0:n], r=[oF[j][1]])
        k.dma("sync", tb_d[:, c0:c0 + n], oB[j][0][:, 0:n], r=[oB[j][1]])
    tot = k.sb("tot", [128, 1], F32)
    k.V(lambda E: E.tensor_reduce(out=tot[0][:], in_=part[0][:], axis=mybir.AxisListType.X, op=ALU.add), [part[1]], [tot[1]])
    k.dma("sync", ssq_d, tot[0][:], r=[tot[1]])
    k.done()
    return nc


def filt_consts(L):
    t_idx = np.arange(L, dtype=np.float32)
    t = (t_idx / np.float32(max(L - 1, 1))).astype(np.float32)
    bands = np.linspace(1e-4, 15, 16, dtype=np.float32)
    ang = (np.float32(2.0 * np.pi) * bands[None, :] * t_idx[:, None] / np.float32(L)).astype(np.float32)
    feats = np.concatenate([t[:, None], np.cos(ang), -np.sin(ang)], -1).astype(np.float32)
    deltas = np.linspace(np.log(1e-2) / 0.3, np.log(1e-2) / 1.5, 512, dtype=np.float32)
    return np.ascontiguousarray(feats.T), t.reshape(1, L).copy(), np.abs(deltas)


def filt_inputs(L, h, prm):
    featsT, tv, adel = filt_consts(L)
    sl = slice(64 * h, 64 * h + 64)
    w3 = prm["hy_w3"]; b3 = prm["hy_b3"]
    cols = []
    for side in range(2):
        for o in range(2):
            cols.append(np.arange(side * 1024 + o * 512 + 64 * h, side * 1024 + o * 512 + 64 * h + 64))
    cols = np.concatenate(cols)
    nd = -np.concatenate([adel[sl], adel[sl]]).reshape(128, 1).astype(np.float32)
    return {"featsT": featsT, "tvals": tv, "w1": prm["hy_w1"], "w2": prm["hy_w2"], "w3c": np.ascontiguousarray(w3[:, cols]),
            "b12": np.stack([prm["hy_b1"], prm["hy_b2"]], 1).astype(np.float32),
            "b3c": np.stack([b3[cols[:128]], b3[cols[128:]]], 1).astype(np.float32), "negdelta": nd}


def toeplitz_src(tapsF, tapsB):
    L = tapsF.shape[1]
    KL = np.zeros((128, 2 * L), tapsF.dtype)
    KL[:, 0:L - 1] = tapsB[:, :0:-1]
    KL[:, L - 1:2 * L - 1] = tapsF
    return KL


def build_hy(nb, T_read=0):
    L = 128 * nb
    W = 128 * (2 * nb - 1)
    nc = bass.Bass("TRN2", target_bir_lowering=False)
    k = KB(nc)
    kl_h = nc.dram_tensor("KL", [128, 2 * L], BF16, kind="ExternalInput")
    ssq_d = k.din("ssqT", [1, 128], F32)
    skip_d = k.din("skipT", [1, 128], F32)
    z_d = k.din("z1", [64, 128, nb], F32)
    x1_d = k.din("x1g", [64, 128, nb], F32)
    x2_d = k.din("x2g", [64, 128, nb], F32)
    sg_d = k.din("sgh", [64, 128, nb], F32)
    J_d = k.din("Jmat", [128, 128], F32)
    y_d = k.dout("yhy", [64, 128, nb], BF16)
    Jf = k.sb("Jf", [128, 128], F32); Jb = k.sb("Jb", [128, 128], BF16)
    nrm = k.sb("nrm", [128, 128], F32); skp = k.sb("skp", [128, 128], F32)
    k.dma("sync", Jf[0][:], J_d, w=[Jf[1]])
    h_copy(k, (Jb[0][:], Jb[1]), (Jf[0][:], Jf[1]))
    k.dma("sync", nrm[0][:], ssq_d.partition_broadcast(128), w=[nrm[1]])
    k.dma("sync", skp[0][:], skip_d.partition_broadcast(128), w=[skp[1]])
    h_act(k, (nrm[0][:], nrm[1]), (nrm[0][:], nrm[1]), AF.Sqrt)
    h_recip(k, (nrm[0][:], nrm[1]), (nrm[0][:], nrm[1]))

    if T_read:
        pp_d = k.din("pp", [64, NPC], F32)
        yf_d = k.din("yf", [64, T_read], F32)
        yb_d = k.din("yb", [64, T_read], F32)
        bo_d = k.din("bonus", [64, T_read], F32)
        sgr_d = k.din("sgr", [64, T_read], F32)
        mr_d = k.dout("mixrw", [64, T_read], BF16)
        pp = k.sb("pp_sb", [64, NPC], F32)
        k.dma("sync", pp[0][:], pp_d, w=[pp[1]])
        o64 = k.sb("o64", [64, 64], F32)
        h_memset(k, (o64[0][:], o64[1]), 1.0 / 64)
        geps = k.sb("geps", [64, 1], F32)
        h_memset(k, (geps[0][:], geps[1]), 64e-5)
        ra = [k.sb("ra%d" % i, [64, 512], F32) for i in range(2)]
        rb = [k.sb("rb%d" % i, [64, 512], F32) for i in range(2)]
        rc = [k.sb("rc%d" % i, [64, 512], F32) for i in range(2)]
        rd = [k.sb("rd%d" % i, [64, 512], F32) for i in range(2)]
        r1 = k.sb("r1", [64, 512], F32); r2 = k.sb("r2", [64, 512], F32)
        rob = [k.sb("rob%d" % i, [64, 512], BF16) for i in range(2)]
        pr = [k.ps("pr%d" % i, [64, 512], F32) for i in range(2)]
        for ci, c0 in enumerate(range(0, T_read, 512)):
            n = min(512, T_read - c0)
            j = ci % 2
            A = (ra[j][0][:, 0:n], ra[j][1]); B = (rb[j][0][:, 0:n], rb[j][1])
            C = (rc[j][0][:, 0:n], rc[j][1]); D = (rd[j][0][:, 0:n], rd[j][1])
            R1 = (r1[0][:, 0:n], r1[1]); R2 = (r2[0][:, 0:n], r2[1])
            P0 = (pr[0][0][:, 0:n], pr[0][1]); P1 = (pr[1][0][:, 0:n], pr[1][1])
            k.dma("scalar", A[0], yf_d[:, c0:c0 + n], w=[A[1]])
            k.dma("scalar", B[0], yb_d[:, c0:c0 + n], w=[B[1]])
            k.dma("scalar", C[0], bo_d[:, c0:c0 + n], w=[C[1]])
            k.dma("scalar", D[0], sgr_d[:, c0:c0 + n], w=[D[1]])
            h_tt(k, A, A, B, ALU.add)
            h_mm(k, P0, (o64[0][:], o64[1]), A)
            h_tt(k, R1, A, P0, ALU.subtract)
            h_tt(k, R2, R1, R1, ALU.mult)
            h_mm(k, P1, (o64[0][:], o64[1]), R2)
            h_act(k, R2, P1, AF.Sqrt, bias=(geps[0][:], geps[1]))
            h_recip(k, R2, R2)
            h_tt(k, R1, R1, R2, ALU.mult)
            h_ts(k, R1, R1, (pp[0][:, PC["gn_w"]:PC["gn_w"] + 1], pp[1]), (pp[0][:, PC["gn_b"]:PC["gn_b"] + 1], pp[1]), ALU.mult, ALU.add)
            h_tt(k, R1, R1, C, ALU.add)
            OB = (rob[j][0][:, 0:n], rob[j][1])
            h_tt(k, OB, R1, D, ALU.mult)
            k.dma("scalar", mr_d[:, c0:c0 + n], OB[0], r=[OB[1]])

    ksh = [k.sb("ksh%d" % i, [128, W], BF16) for i in range(2)]
    zt = [k.sb("zt%d" % i, [128, nb], F32) for i in range(2)]
    x1t = [k.sb("x1t%d" % i, [128, nb], F32) for i in range(2)]
    x2t = [k.sb("x2t%d" % i, [128, nb], F32) for i in range(2)]
    sgt = [k.sb("sgt%d" % i, [128, nb], F32) for i in range(2)]
    zb = k.sb("zb", [128, nb], BF16)
    zf = k.sb("zf", [128, nb], BF16)
    z2 = k.sb("z2", [128, nb], F32)
    t1 = k.sb("t1", [128, nb], F32)
    ot = [k.sb("oth%d" % i, [128, nb], BF16) for i in range(2)]
    pf = k.ps("pf", [128, nb], F32)
    py = [k.ps("pyc%d" % i, [128, nb], F32) for i in range(2)]
    order = [0] + [d for d in range(-(nb - 1), nb) if d != 0]
    cnt = 0
    for c in range(64):
        j = c % 2
        Z = (zt[j][0][:], zt[j][1]); X1 = (x1t[j][0][:], x1t[j][1]); X2 = (x2t[j][0][:], x2t[j][1]); SG = (sgt[j][0][:], sgt[j][1])
        k.dma("gpsimd", Z[0], z_d[c], w=[Z[1]])
        k.dma("gpsimd", X1[0], x1_d[c], w=[X1[1]])
        k.dma("gpsimd", X2[0], x2_d[c], w=[X2[1]])
        k.dma("gpsimd", SG[0], sg_d[c], w=[SG[1]])
        zin = Z
        for o in range(2):
            row = o * 64 + c
            kt, kb_ = ksh[cnt % 2]
            q = "sync" if cnt % 2 == 0 else "scalar"
            cnt += 1
            src = bass.AP(kl_h, row * 2 * L, [[1, 128], [1, W]])
            k.dma(q, kt[:], src, w=[kb_])
            h_copy(k, (zb[0][:], zb[1]), zin, eng="gpsimd")
            h_mm(k, (pf[0][:], pf[1]), (Jb[0][:], Jb[1]), (zb[0][:], zb[1]))
            h_copy(k, (zf[0][:], zf[1]), (pf[0][:], pf[1]), eng="scalar")
            pyt, pyb = py[o]
            for ii, Dd in enumerate(order):
                e = Dd + nb - 1
                S0, S1 = max(0, -Dd), min(nb, nb - Dd)
                k.T(lambda E, pyt=pyt, kt=kt, e=e, S0=S0, S1=S1, Dd=Dd, ii=ii: E.matmul(pyt[:, S0 + Dd:S1 + Dd], kt[:, 128 * e:128 * e + 128], zf[0][:, S0:S1], start=(ii == 0), stop=(ii == len(order) - 1)),
                    [kb_, zf[1]], [pyb])
            ncol = (nrm[0][:, row:row + 1], nrm[1])
            scol = (skp[0][:, row:row + 1], skp[1])
            h_ts(k, (t1[0][:], t1[1]), (pyt[:], pyb), ncol, None, ALU.mult)
            h_stt(k, (t1[0][:], t1[1]), zin, scol, (t1[0][:], t1[1]), ALU.mult, ALU.add)
            if o == 0:
                h_tt(k, (z2[0][:], z2[1]), (t1[0][:], t1[1]), X1, ALU.mult)
                zin = (z2[0][:], z2[1])
            else:
                O_ = (ot[j][0][:], ot[j][1])
                h_tt(k, (t1[0][:], t1[1]), (t1[0][:], t1[1]), X2, ALU.mult)
                h_tt(k, O_, (t1[0][:], t1[1]), SG, ALU.mult, eng="gpsimd")
                k.dma("gpsimd", y_d[c], O_[0], r=[O_[1]])
    k.done()
    return nc


import numpy as np


def rfeat_inputs(u_ctx, u_lat, h, prm):
    n = u_lat.shape[0]
    def fm_pad(c0, w=64):
        a = np.zeros((w, 256 + n + 4), np.float32)
        a[:, 1:257] = u_ctx[:, c0:c0 + w].T
        a[:, 259:259 + n] = u_lat[:, c0:c0 + w].T
        return a
    cols = {"r": 64 * h, "k": 512 + 64 * h, "v": 1024 + 64 * h, "g_rw": 1664 + 64 * h,
            "hv": 2176 + 64 * h, "hx1": 2688 + 64 * h, "hx2": 3200 + 64 * h, "g_hy": 3712 + 64 * h}
    fin = np.stack([fm_pad(cols[nm]) for nm in FI], 0)
    lora = fm_pad(1536, 128)
    mu = prm["rwkv_mu"]
    sl = slice(64 * h, 64 * h + 64)
    pp = np.zeros((64, NPC), np.float32)
    pp[:, PC["mu_r"]] = mu[sl]
    pp[:, PC["mu_k"]] = mu[512 + 64 * h:512 + 64 * h + 64]
    pp[:, PC["mu_v"]] = mu[1024 + 64 * h:1024 + 64 * h + 64]
    pp[:, PC["k_k"]] = prm["rwkv_k_k"][sl]
    pp[:, PC["k_a"]] = prm["rwkv_k_a"][sl]
    pp[:, PC["w0_f"]] = prm["rwkv_w0"][0][sl]
    pp[:, PC["w0_b"]] = prm["rwkv_w0"][1][sl]
    pp[:, PC["a0_f"]] = prm["rwkv_a0"][0][sl]
    pp[:, PC["a0_b"]] = prm["rwkv_a0"][1][sl]
    pp[:, PC["r_k"]] = prm["rwkv_r_k"][h]
    for ai, nm in enumerate(("v", "x1", "x2")):
        for t in range(3):
            pp[:, PC["t%d_%s" % (t, nm)]] = prm["hy_short"][t][512 * ai + 64 * h:512 * ai + 64 * h + 64]
    pp[:, PC["gn_w"]] = prm["rwkv_gn_w"][sl]
    pp[:, PC["gn_b"]] = prm["rwkv_gn_b"][sl]
    pp[:, PC["skip0"]] = prm["hy_skip"][0][sl]
    pp[:, PC["skip1"]] = prm["hy_skip"][1][sl]
    wx = np.zeros((128, 4, 64), np.float32)
    wx[0:32, 0] = prm["rwkv_w_up"][0][:, sl]
    wx[32:64, 1] = prm["rwkv_w_up"][1][:, sl]
    wx[64:96, 2] = prm["rwkv_a_up"][0][:, sl]
    wx[96:128, 3] = prm["rwkv_a_up"][1][:, sl]
    return {"fin": fin, "lora": lora, "pp": pp, "mu_lora": mu[1536:1664].reshape(128, 1).copy(), "wx": wx}


import numpy as np

TC = 16


def h_tr(k, out, a, ident):
    oa, ob = out; aa, ab = a; ia, ib = ident
    return k.P.op("tensor", lambda E: E.transpose(oa, aa, ia), [ab, ib], [ob])


def build_fused(n):
    nlb = n // 128
    NKB = nlb + 2
    NTK = NKB * 128
    T = 256 + n
    TP = T + 4
    nc = bass.Bass("TRN2", target_bir_lowering=False)
    k = KB(nc)
    P = k.P
    plan, variants = na_plan(nlb)
    nvar = len(variants)

    def scratch(name, shape, dt):
        return nc.dram_tensor(name, list(shape), dt).ap()

    x_d = k.din("x", [n, D], F32)
    ctx_d = k.din("ctx", [256, D], F32)
    ccols_d = k.din("c_cols", [128, 16], F32)
    ident_d = k.din("ident", [128, 128], F32)
    J_d = k.din("Jmat", [128, 128], F32)
    fng_d = k.din("final_norm", [1, D], F32)
    L_in = []
    for l in range(4):
        attn = l % 2 == 0
        d = {"norm_g": k.din("norm_g%d" % l, [1, D], F32), "ada_w": k.din("ada_w%d" % l, [D, 3 * D], F32),
             "ada_b": k.din("ada_b%d" % l, [1, 3 * D], F32), "w_in": k.din("w_in%d" % l, [D, 512 if attn else 640], F32),
             "w_out": k.din("w_out%d" % l, [D, D], F32)}
        if attn:
            d["biasT"] = k.din("biasT%d" % l, [128, nvar, 128], F32)
            d["gains"] = k.din("gains%d" % l, [64, 2], F32)
        else:
            d["pp"] = k.din("pp%d" % l, [64, NPC], F32)
            d["mu_lora"] = k.din("mu_lora%d" % l, [128, 1], F32)
            d["wx"] = k.din("wx%d" % l, [128, 4, 64], F32)
            d["hw1"] = k.din("hw1_%d" % l, [33, 64], F32)
            d["hw2"] = k.din("hw2_%d" % l, [64, 64], F32)
            d["hw3"] = k.din("hw3_%d" % l, [64, 256], F32)
            d["hb12"] = k.din("hb12_%d" % l, [64, 2], F32)
            d["hb3"] = k.din("hb3_%d" % l, [128, 2], F32)
            d["skipT"] = k.din("skipT%d" % l, [1, 128], F32)
        L_in.append(d)
    ropeC_d = k.din("ropeC", [64, NTK], F32)
    ropeS_d = k.din("ropeS", [64, NTK], F32)
    rmat_d = k.din("rmatT", [64, 64], F32)
    sel_d = k.din("sel", [65, 64], F32)
    fconst = {}
    for Lf in (n, 256):
        fconst[Lf] = {"featsT": k.din("featsT%d" % Lf, [33, Lf], F32), "featsTr": k.din("featsTr%d" % Lf, [33, Lf], F32),
                      "tv": k.din("tv%d" % Lf, [1, Lf], F32), "tvr": k.din("tvr%d" % Lf, [1, Lf], F32)}
    nd_d = k.din("negdelta", [128, 1], F32)
    y_out = k.dout("y_out", [n, D], F32)

    xres = scratch("xres", [NTK, D], F32)
    gates_s = scratch("gates_s", [128, 2, D], F32)
    mix_send = scratch("mix_send", [128, NTK], BF16)
    mix_all = scratch("mix_all", [1024, NTK], BF16)
    A_s = {nm: scratch("a_" + nm, [64, NTK], BF16) for nm in ("qaT", "kaT", "gaT", "qbT", "kbT", "gbT")}
    va_s = scratch("va_s", [128, NKB, 65], BF16)
    vb_s = scratch("vb_s", [128, NKB, 65], BF16)
    fin_s = scratch("fin_s", [NFI, 64, TP], F32)
    lora_s = scratch("lora_s", [128, TP], F32)
    fo_s = scratch("fo_s", [NFO, 64, T], F32)
    bc_s = scratch("bc_s", [2, T, 320], F32)
    vT2_s = scratch("vT2_s", [128, T], F32)
    yT2_s = scratch("yT2_s", [128, T], F32)
    kl_h = {Lf: nc.dram_tensor("KL%d" % Lf, [128, 2 * Lf], BF16) for Lf in (n, 256)}
    ssq_s = {Lf: scratch("ssq%d" % Lf, [1, 128], F32) for Lf in (n, 256)}

    identf = k.sb("identf", [128, 128], F32)
    identb = k.sb("identb", [128, 128], BF16)
    Jf = k.sb("Jf", [128, 128], F32)
    Jb = k.sb("Jb", [128, 128], BF16)
    zer = k.sb("zer", [128, 8], F32)
    k.dma("sync", identf[0][:], ident_d, w=[identf[1]])
    k.dma("sync", Jf[0][:], J_d, w=[Jf[1]])
    h_copy(k, (identb[0][:], identb[1]), (identf[0][:], identf[1]))
    h_copy(k, (Jb[0][:], Jb[1]), (Jf[0][:], Jf[1]))
    h_memset(k, (zer[0][:], zer[1]), 0.0)
    ID = (identf[0][:], identf[1])
    IDB = (identb[0][:], identb[1])
    for col in (0, 257, 258, TP - 1):
        for i in range(NFI):
            k.dma("sync", fin_s[i, :, col:col + 1], zer[0][0:64, 0:1], r=[zer[1]], allow_slow_non_contiguous=True)
        k.dma("sync", lora_s[:, col:col + 1], zer[0][:, 0:1], r=[zer[1]], allow_slow_non_contiguous=True)

    def phase_tok(l, final=False):
        has_prev = l > 0
        attn = (l % 2 == 0) and not final
        k.begin_phase()
        eps_t = k.sb("eps_t", [128, 1], F32)
        h_memset(k, (eps_t[0][:], eps_t[1]), EPS)
        if has_prev:
            wo = k.sb("wo", [128, 8, D], BF16)
            for kc in range(8):
                k.dma("gpsimd", wo[0][:, kc, :], L_in[l - 1]["w_out"][kc * 128:(kc + 1) * 128, :], w=[wo[1]])
            gp = k.sb("gp", [128, 2, D], F32)
            k.dma("sync", gp[0][:], gates_s, w=[gp[1]])
        if final:
            fg = k.sb("fg", [128, D], F32)
            k.dma("sync", fg[0][:], fng_d.partition_broadcast(128), w=[fg[1]])
        else:
            Li = L_in[l]
            ncol = 512 if attn else 640
            wi = k.sb("wi", [128, 8, ncol], BF16)
            for kc in range(8):
                k.dma("gpsimd", wi[0][:, kc, :], Li["w_in"][kc * 128:(kc + 1) * 128, :], w=[wi[1]])
            mod = k.sb("mod", [128, 2, 3 * D], F32)
            Gt = k.sb("Gt", [128, 2, D], F32)
            k.begin_phase()
            cc = k.sb("cc", [128, 16], F32)
            rep = k.sb("rep", [128, 16, 128], F32)
            ones = k.sb("ones", [128, 128], F32)
            ab = k.sb("ab", [128, 3 * D], F32)
            ng = k.sb("ng", [128, D], F32)
            aw = [k.sb("aw%d" % i, [128, 8, 512], F32) for i in range(2)]
            pa = [k.ps("pa%d" % i, [128, 512], F32) for i in range(2)]
            k.dma("sync", cc[0][:], ccols_d, w=[cc[1]])
            k.dma("sync", ab[0][:], Li["ada_b"].partition_broadcast(128), w=[ab[1]])
            k.dma("sync", ng[0][:], Li["norm_g"].partition_broadcast(128), w=[ng[1]])
            h_act(k, (cc[0][:], cc[1]), (cc[0][:], cc[1]), AF.Silu)
            h_memset(k, (ones[0][:], ones[1]), 1.0)
            for j in range(16):
                h_ts(k, (rep[0][:, j, :], rep[1]), (ones[0][:], ones[1]), (cc[0][:, j:j + 1], cc[1]), None, ALU.mult)
            for g in range(6):
                awt, awb = aw[g % 2]
                k.dma("sync", awt[:], Li["ada_w"][:, g * 512:(g + 1) * 512].rearrange("(k p) n -> p k n", p=128), w=[awb])
                for v in range(2):
                    for kc in range(8):
                        h_mm(k, (pa[v][0][:], pa[v][1]), (rep[0][:, v * 8 + kc, :], rep[1]), (awt[:, kc, :], awb), start=(kc == 0), stop=(kc == 7))
                    h_tt(k, (mod[0][:, v, g * 512:(g + 1) * 512], mod[1]), (pa[v][0][:], pa[v][1]), (ab[0][:, g * 512:(g + 1) * 512], ab[1]), ALU.add)
            for v in range(2):
                h_stt(k, (Gt[0][:, v, :], Gt[1]), (mod[0][:, v, D:2 * D], mod[1]), 1.0, (ng[0][:], ng[1]), ALU.add, ALU.mult)
            k.end_phase()
        xt = [k.sb("xt%d" % i, [128, D], F32) for i in range(2)]
        tmp = [k.sb("tmp%d" % i, [128, D], F32) for i in range(2)]
        ss = [k.sb("ss%d" % i, [128, 4], F32) for i in range(2)]
        junk = k.sb("junk", [128, D], BF16)
        if has_prev:
            mt = [k.sb("mt%d" % i, [128, 8, 128], BF16) for i in range(2)]
            py = [k.ps("py%d" % i, [128, 512], F32) for i in range(2)]
        if not final:
            xm = [k.sb("xm%d" % i, [128, D], BF16) for i in range(2)]
            xmT = [k.sb("xmT%d" % i, [128, 8, 512], BF16) for i in range(2)]
            ptr = [k.ps("ptr%d" % i, [128, 8, 128], BF16) for i in range(2)]
            pu = [k.ps("pu%d" % i, [128, 512], F32) for i in range(2)]
            odt = BF16 if attn else F32
            ut = [k.sb("ut%d" % i, [128, 512], odt) for i in range(3)]
            if attn:
                vt = [k.sb("vt%d" % i, [128, 2, 65], BF16) for i in range(2)]
                for i in range(2):
                    h_memset(k, (vt[i][0][:], vt[i][1]), 1.0)
        bcount = 0
        ucount = 0
        nsb = (NTK + 511) // 512
        for sbi in range(nsb):
            t0 = sbi * 512
            ntok = min(512, NTK - t0)
            nblk = ntok // 128
            is_ctx = t0 >= n
            v = 1 if is_ctx else 0
            if not final:
                xTt, xTb = xmT[sbi % 2]
            for blk in range(nblk):
                b = t0 // 128 + blk
                X = (xt[bcount % 2][0][:], xt[bcount % 2][1])
                TT = (tmp[bcount % 2][0][:], tmp[bcount % 2][1])
                sst, ssb = ss[bcount % 2]
                if l <= 1:
                    src = ctx_d[(b - nlb) * 128:(b - nlb + 1) * 128, :] if is_ctx else x_d[b * 128:(b + 1) * 128, :]
                else:
                    src = xres[b * 128:(b + 1) * 128, :]
                k.dma("sync", X[0], src, w=[X[1]])
                if has_prev:
                    mtt, mtb = mt[bcount % 2]
                    k.dma("sync", mtt[:], mix_all[:, b * 128:(b + 1) * 128].rearrange("(k p) t -> p k t", p=128), w=[mtb])
                    for hh in range(2):
                        for kc in range(8):
                            h_mm(k, (py[hh][0][:], py[hh][1]), (mtt[:, kc, :], mtb), (wo[0][:, kc, hh * 512:(hh + 1) * 512], wo[1]), start=(kc == 0), stop=(kc == 7))
                        h_tt(k, (tmp[bcount % 2][0][:, hh * 512:(hh + 1) * 512], TT[1]), (py[hh][0][:], py[hh][1]), (gp[0][:, v, hh * 512:(hh + 1) * 512], gp[1]), ALU.mult)
                    h_tt(k, X, TT, X, ALU.add, eng="gpsimd")
                    if not final:
                        k.dma("sync", xres[b * 128:(b + 1) * 128, :], X[0], r=[X[1]])
                h_act(k, (junk[0][:], junk[1]), X, AF.Square, accum=(sst[:, 0:1], ssb))
                h_act(k, (sst[:, 1:2], ssb), (sst[:, 0:1], ssb), AF.Sqrt, scale=1.0 / D, bias=(eps_t[0][:], eps_t[1]))
                h_recip(k, (sst[:, 2:3], ssb), (sst[:, 1:2], ssb))
                if final:
                    if not is_ctx:
                        h_stt(k, TT, X, (sst[:, 2:3], ssb), (fg[0][:], fg[1]), ALU.mult, ALU.mult)
                        k.dma("sync", y_out[b * 128:(b + 1) * 128, :], TT[0], r=[TT[1]])
                    bcount += 1
                    continue
                XM = (xm[bcount % 2][0][:], xm[bcount % 2][1])
                h_stt(k, TT, X, (sst[:, 2:3], ssb), (Gt[0][:, v, :], Gt[1]), ALU.mult, ALU.mult)
                h_tt(k, XM, TT, (mod[0][:, v, 0:D], mod[1]), ALU.add, eng="gpsimd")
                ptt, ptb = ptr[bcount % 2]
                for kc in range(8):
                    h_tr(k, (ptt[:, kc, :], ptb), (xm[bcount % 2][0][:, kc * 128:(kc + 1) * 128], XM[1]), IDB)
                h_copy(k, (xTt[:, :, blk * 128:(blk + 1) * 128], xTb), (ptt[:], ptb), eng="scalar")
                if attn:
                    put, pub = pu[ucount % 2]
                    vtt, vtb = vt[ucount % 2]
                    ucount += 1
                    for kc in range(8):
                        h_mm(k, (put[:, 0:128], pub), (xTt[:, kc, blk * 128:(blk + 1) * 128], xTb), (wi[0][:, kc, 384:512], wi[1]), start=(kc == 0), stop=(kc == 7))
                    h_copy(k, (vtt[:, :, 0:64], vtb), (put[:, 0:128].rearrange("p (a c) -> p a c", a=2), pub))
                    k.dma("sync", va_s[:, b, :], vtt[:, 0, :], r=[vtb])
                    k.dma("sync", vb_s[:, b, :], vtt[:, 1, :], r=[vtb])
                bcount += 1
            if final:
                continue
            ncg = 3 if attn else 5
            for cg in range(ncg):
                put, pub = pu[ucount % 2]
                utt, utb = ut[ucount % 3]
                ucount += 1
                for kc in range(8):
                    h_mm(k, (put[:, 0:ntok], pub), (wi[0][:, kc, cg * 128:(cg + 1) * 128], wi[1]), (xTt[:, kc, 0:ntok], xTb), start=(kc == 0), stop=(kc == 7))
                h_copy(k, (utt[:, 0:ntok], utb), (put[:, 0:ntok], pub), eng=("vector" if cg % 2 == 0 else "scalar"))
                if attn:
                    names = [("qaT", "kaT"), ("gaT", "qbT"), ("kbT", "gbT")][cg]
                    k.dma("sync", A_s[names[0]][:, t0:t0 + ntok], utt[0:64, 0:ntok], r=[utb])
                    k.dma("sync", A_s[names[1]][:, t0:t0 + ntok], utt[64:128, 0:ntok], r=[utb])
                else:
                    pc = (1 + (t0 - n)) if is_ctx else (259 + t0)
                    if cg < 4:
                        k.dma("sync", fin_s[2 * cg, :, pc:pc + ntok], utt[0:64, 0:ntok], r=[utb])
                        k.dma("sync", fin_s[2 * cg + 1, :, pc:pc + ntok], utt[64:128, 0:ntok], r=[utb])
                    else:
                        k.dma("sync", lora_s[:, pc:pc + ntok], utt[:, 0:ntok], r=[utb])
        if not final:
            k.dma("sync", gates_s, mod[0][:, :, 2 * D:3 * D], r=[mod[1]])
        k.end_phase()

    def phase_attn(l):
        Li = L_in[l]
        k.begin_phase()
        qT = k.sb("qT", [64, NTK], BF16); kT = k.sb("kT", [64, NTK], BF16)
        vE = k.sb("vE", [128, NKB, 65], BF16)
        biasf = k.sb("biasf", [128, nvar, 128], F32); biasb = k.sb("biasb", [128, nvar, 128], BF16)
        sel = k.sb("sel_sb", [65, 64], F32); gains = k.sb("gains_sb", [64, 2], F32)
        rmf = k.sb("rmf", [64, 64], F32); rmb = k.sb("rmb", [64, 64], BF16)
        ones64 = k.sb("ones64", [64, 64], F32); eps_t = k.sb("eps_a", [64, 1], F32)
        k.dma("sync", biasf[0][:], Li["biasT"], w=[biasf[1]])
        k.dma("sync", sel[0][:], sel_d, w=[sel[1]])
        k.dma("sync", gains[0][:], Li["gains"], w=[gains[1]])
        k.dma("sync", rmf[0][:], rmat_d, w=[rmf[1]])
        h_copy(k, (biasb[0][:], biasb[1]), (biasf[0][:], biasf[1]))
        h_copy(k, (rmb[0][:], rmb[1]), (rmf[0][:], rmf[1]))
        h_memset(k, (ones64[0][:], ones64[1]), 1.0 / 64)
        h_memset(k, (eps_t[0][:], eps_t[1]), EPS)
        h_ts(k, (gains[0][:, 0:1], gains[1]), (gains[0][:, 0:1], gains[1]), 0.125, None, ALU.mult)
        pS = [k.ps("pS%d" % i, [128, 512], F32) for i in range(4)]
        pacc = [k.ps("pacc%d" % i, [65, 512], F32) for i in range(2)]
        pmisc = [k.ps("pm%d" % i, [64, 512], F32) for i in range(2)]
        PT = [k.sb("PT%d" % i, [128, 512], BF16) for i in range(4)]
        accs = [k.sb("accs%d" % i, [65, 512], F32) for i in range(2)]
        gt = [k.sb("gt%d" % i, [64, 512], BF16) for i in range(2)]
        w1 = [k.sb("w1_%d" % i, [64, 512], F32) for i in range(2)]
        w2 = [k.sb("w2_%d" % i, [64, 512], F32) for i in range(2)]
        ot = [k.sb("ot%d" % i, [64, 512], BF16) for i in range(2)]
        cin = [k.sb("cin%d" % i, [64, 512], BF16) for i in range(2)]
        ctab = [k.sb("ctab%d" % i, [64, 512], F32) for i in range(2)]
        stab = [k.sb("stab%d" % i, [64, 512], F32) for i in range(2)]
        qnb = [k.sb("qnb%d" % i, [64, 512], BF16) for i in range(2)]
        st = {"fin": 0, "S": 0, "pre": 0}

        def finalize(pa, g_d, row0, c0, nn):
            i = st["fin"] % 2
            st["fin"] += 1
            at = (accs[i][0][:, 0:nn], accs[i][1]); G = (gt[i][0][:, 0:nn], gt[i][1])
            a1 = (w1[i][0][:, 0:nn], w1[i][1]); a2 = (w2[i][0][:, 0:nn], w2[i][1])
            o = (ot[i][0][:, 0:nn], ot[i][1]); pm = (pmisc[i][0][:, 0:nn], pmisc[i][1])
            k.dma("sync", G[0], g_d[:, c0:c0 + nn], w=[G[1]])
            h_copy(k, at, (pa[0][:, 0:nn], pa[1]), eng="scalar")
            h_mm(k, pm, (sel[0][:], sel[1]), at)
            h_recip(k, a1, pm)
            h_tt(k, a1, a1, (accs[i][0][0:64, 0:nn], accs[i][1]), ALU.mult)
            h_silu(k, a2, G, a2)
            h_tt(k, o, a1, a2, ALU.mult)
            k.dma("sync", mix_send[row0:row0 + 64, c0:c0 + nn], o[0], r=[o[1]])

        def attend(qc0, nn, kbs, pa):
            pend = []
            nk = len(kbs)

            def pv(j, kbj, pj):
                h_mm(k, (pa[0][:, 0:nn], pa[1]), (vE[0][:, kbj, :], vE[1]), pj, start=(j == 0), stop=(j == nk - 1))
            for idx, kb in enumerate(kbs):
                s = st["S"]
                st["S"] += 1
                psx = (pS[s % 4][0][:, 0:nn], pS[s % 4][1])
                ptx = (PT[s % 4][0][:, 0:nn], PT[s % 4][1])
                h_mm(k, psx, (kT[0][:, kb * 128:(kb + 1) * 128], kT[1]), (qT[0][:, qc0:qc0 + nn], qT[1]))
                h_act(k, ptx, psx, AF.Exp)
                pend.append((idx, kb, ptx))
                if len(pend) > 2:
                    pv(*pend.pop(0))
            for it in pend:
                pv(*it)

        k.dma("sync", kT[0][:], A_s["kaT"], w=[kT[1]])
        k.dma("sync", vE[0][:], va_s, w=[vE[1]])
        k.dma("sync", qT[0][:], A_s["qaT"], w=[qT[1]])
        h_ts(k, (qT[0][:], qT[1]), (qT[0][:], qT[1]), 0.125, None, ALU.mult)
        ctxk = [nlb, nlb + 1]
        acc_i = 0
        for g0 in range(0, nlb, 4):
            pa = pacc[acc_i % 2]
            acc_i += 1
            nq = min(4, nlb - g0)
            for mi in range(nq):
                m = g0 + mi
                lst = plan[m]
                nreg = len(lst) + 2
                s = st["S"]
                st["S"] += 2
                banks = [(pS[s % 4], PT[s % 4]), (pS[(s + 1) % 4], PT[(s + 1) % 4])]
                regs = []
                for j, (kb, var) in enumerate(lst + [(ctxk[0], None), (ctxk[1], None)]):
                    (pt_, pb_), (tt_, tb_) = banks[j // 4]
                    jj = j % 4
                    h_mm(k, (pt_[:, jj * 128:(jj + 1) * 128], pb_), (kT[0][:, kb * 128:(kb + 1) * 128], kT[1]), (qT[0][:, m * 128:(m + 1) * 128], qT[1]), start=True, stop=(var is None))
                    if var is not None:
                        h_mm(k, (pt_[:, jj * 128:(jj + 1) * 128], pb_), IDB, (biasb[0][:, var, :], biasb[1]), start=False, stop=True)
                    regs.append((kb, tt_, tb_, jj))
                nA = min(4, nreg)
                h_act(k, (banks[0][1][0][:, 0:nA * 128], banks[0][1][1]), (banks[0][0][0][:, 0:nA * 128], banks[0][0][1]), AF.Exp)
                if nreg > 4:
                    nB = nreg - 4
                    h_act(k, (banks[1][1][0][:, 0:nB * 128], banks[1][1][1]), (banks[1][0][0][:, 0:nB * 128], banks[1][0][1]), AF.Exp)
                for j, (kb, tt_, tb_, jj) in enumerate(regs):
                    h_mm(k, (pa[0][:, mi * 128:(mi + 1) * 128], pa[1]), (vE[0][:, kb, :], vE[1]), (tt_[:, jj * 128:(jj + 1) * 128], tb_), start=(j == 0), stop=(j == nreg - 1))
            finalize(pa, A_s["gaT"], 0, g0 * 128, nq * 128)
        pa = pacc[acc_i % 2]
        acc_i += 1
        attend(nlb * 128, 256, ctxk, pa)
        finalize(pa, A_s["gaT"], 0, nlb * 128, 256)
        k.dma("sync", vE[0][:], vb_s, w=[vE[1]])

        def pre_chunk(src_d, dst, gcol, c0):
            nn = min(512, NTK - c0)
            i = st["pre"] % 2
            st["pre"] += 1
            ci = (cin[i][0][:, 0:nn], cin[i][1]); ct = (ctab[i][0][:, 0:nn], ctab[i][1]); sn = (stab[i][0][:, 0:nn], stab[i][1])
            qn = (qnb[i][0][:, 0:nn], qnb[i][1]); a1 = (w1[i][0][:, 0:nn], w1[i][1]); a2 = (w2[i][0][:, 0:nn], w2[i][1])
            pm = (pmisc[i][0][:, 0:nn], pmisc[i][1])
            k.dma("sync", ci[0], src_d[:, c0:c0 + nn], w=[ci[1]])
            k.dma("sync", ct[0], ropeC_d[:, c0:c0 + nn], w=[ct[1]])
            k.dma("sync", sn[0], ropeS_d[:, c0:c0 + nn], w=[sn[1]])
            h_tt(k, a1, ci, ci, ALU.mult)
            h_mm(k, pm, (ones64[0][:], ones64[1]), a1)
            h_act(k, a2, pm, AF.Sqrt, bias=(eps_t[0][:], eps_t[1]))
            h_recip(k, a2, a2)
            h_stt(k, qn, ci, (gains[0][:, gcol:gcol + 1], gains[1]), a2, ALU.mult, ALU.mult)
            h_mm(k, pm, (rmb[0][:], rmb[1]), qn)
            h_tt(k, a1, qn, ct, ALU.mult)
            h_tt(k, a2, pm, sn, ALU.mult)
            h_tt(k, (dst[0][:, c0:c0 + nn], dst[1]), a1, a2, ALU.add, eng="gpsimd")
        for c0 in range(0, NTK, 512):
            pre_chunk(A_s["kbT"], kT, 1, c0)
        for c0 in range(0, NTK, 512):
            pre_chunk(A_s["qbT"], qT, 0, c0)
        allk = list(range(NKB))
        for c0 in range(0, nlb * 128, 512):
            nn = min(512, nlb * 128 - c0)
            pa = pacc[acc_i % 2]
            acc_i += 1
            attend(c0, nn, allk, pa)
            finalize(pa, A_s["gbT"], 64, c0, nn)
        pa = pacc[acc_i % 2]
        acc_i += 1
        attend(nlb * 128, 256, ctxk, pa)
        finalize(pa, A_s["gbT"], 64, nlb * 128, 256)
        k.end_phase()

    def phase_rfeat(l):
        Li = L_in[l]
        k.begin_phase()
        pp = k.sb("pp_sb", [64, NPC], F32); npp = k.sb("npp_sb", [64, NPC], F32)
        mul = k.sb("mul_sb", [128, 1], F32); wx = k.sb("wx_sb", [128, 4, 64], F32)
        ones = k.sb("ones_sb", [64, 64], F32); tiny = k.sb("tiny_sb", [64, 1], F32)
        k.dma("sync", pp[0][:], Li["pp"], w=[pp[1]])
        k.dma("sync", mul[0][:], Li["mu_lora"], w=[mul[1]])
        k.dma("sync", wx[0][:], Li["wx"], w=[wx[1]])
        h_memset(k, (ones[0][:], ones[1]), 1.0)
        h_memset(k, (tiny[0][:], tiny[1]), 1e-12)
        h_ts(k, (npp[0][:], npp[1]), (pp[0][:], pp[1]), -1.0, None, ALU.mult)
        h_ts(k, (npp[0][:, PC["r_k"]:PC["r_k"] + 1], npp[1]), (pp[0][:, PC["r_k"]:PC["r_k"] + 1], pp[1]), 0.5, None, ALU.mult)

        def pcol(name):
            return (pp[0][:, PC[name]:PC[name] + 1], pp[1])

        def ncol(name):
            return (npp[0][:, PC[name]:PC[name] + 1], npp[1])
        fin = [[k.sb("fin%d_%d" % (i, j), [64, 514], F32) for j in range(2)] for i in range(NFI)]
        lor = [k.sb("lor%d" % j, [128, 514], F32) for j in range(2)]
        fo = [[k.sb("fo%d_%d" % (i, j), [64, 512], F32) for j in range(2)] for i in range(NFO)]
        tm = [k.sb("tm%d" % i, [64, 512], F32) for i in range(8)]
        lt = [k.sb("lt%d" % i, [128, 512], F32) for i in range(3)]
        pz = [k.ps("pz%d" % i, [64, 512], F32) for i in range(4)]
        ptk = [k.ps("ptk%d" % i, [128, 16, 64], F32) for i in range(1)]
        pfl = k.ps("pfl", [128, 512], F32)
        tok = [k.sb("tok%d" % i, [128, 9, 64], F32) for i in range(2)]
        tokb = [k.sb("tokb%d" % i, [128, 320], F32) for i in range(2)]
        vrev = [k.sb("vrev%d" % i, [64, 128], F32) for i in range(2)]
        tord = ["w_f", "nb_f", "k_f", "kkn", "r", "w_b", "nb_b", "k_b", "v"]
        blkc = [0]

        def chunk(ci, pc0, oc0, nn):
            j = ci % 2

            def I(name, lo=1):
                t, b = fin[FI[name]][j]
                return (t[:, lo:lo + nn], b)

            def O(name):
                t, b = fo[FO[name]][j]
                return (t[:, 0:nn], b)

            def Tm(i):
                return (tm[i][0][:, 0:nn], tm[i][1])

            def Lt(i):
                return (lt[i][0][:, 0:nn], lt[i][1])

            def Pz(i):
                return (pz[i][0][:, 0:nn], pz[i][1])
            for name, i in FI.items():
                t, b = fin[i][j]
                k.dma("sync", t[:, 0:nn + 2], fin_s[i, :, pc0:pc0 + nn + 2], w=[b])
            lt_, lb_ = lor[j]
            k.dma("sync", lt_[:, 0:nn + 2], lora_s[:, pc0:pc0 + nn + 2], w=[lb_])

            def shift(out, lo, mid, hi_, mu, t):
                h_tt(k, t, lo, hi_, ALU.add)
                h_stt(k, t, t, 0.5, mid, ALU.mult, ALU.subtract)
                h_stt(k, out, t, mu, mid, ALU.mult, ALU.add)
            rs, ks, vs = O("r"), Tm(0), O("v")
            shift(rs, I("r", 0), I("r", 1), I("r", 2), pcol("mu_r"), Tm(7))
            shift(ks, I("k", 0), I("k", 1), I("k", 2), pcol("mu_k"), Tm(7))
            shift(vs, I("v", 0), I("v", 1), I("v", 2), pcol("mu_v"), Tm(7))
            ls = Lt(0)
            shift(ls, (lt_[:, 0:nn], lb_), (lt_[:, 1:nn + 1], lb_), (lt_[:, 2:nn + 2], lb_), (mul[0][:], mul[1]), Lt(2))
            lth = Lt(1)
            h_act(k, lth, ls, AF.Tanh)
            kk = Tm(1)
            h_ts(k, kk, ks, pcol("k_k"), None, ALU.mult)
            h_tt(k, Tm(2), kk, kk, ALU.mult)
            h_mm(k, Pz(0), (ones[0][:], ones[1]), Tm(2))
            h_act(k, Tm(2), Pz(0), AF.Sqrt, bias=(tiny[0][:], tiny[1]))
            h_recip(k, Tm(2), Tm(2))
            h_tt(k, O("kkn"), kk, Tm(2), ALU.mult)
            for d, sfx in enumerate(("f", "b")):
                h_mm(k, Pz(1), (wx[0][:, d, :], wx[1]), lth)
                h_mm(k, Pz(2), (wx[0][:, 2 + d, :], wx[1]), ls)
                h_sigmoid(k, Tm(3), Pz(1), Tm(3), bias=ncol("w0_" + sfx))
                h_act(k, O("w_" + sfx), Tm(3), AF.Exp, scale=-float(np.exp(-0.5)))
                a = Tm(4)
                h_sigmoid(k, a, Pz(2), a, bias=ncol("a0_" + sfx))
                h_ts(k, Tm(5), a, -1.0, pcol("k_a"), ALU.add, ALU.mult)
                h_stt(k, O("k_" + sfx), Tm(5), 1.0, ks, ALU.add, ALU.mult)
                h_stt(k, O("nb_" + sfx), O("kkn"), -1.0, a, ALU.mult, ALU.mult)
            h_tt(k, Tm(5), O("k_f"), O("k_b"), ALU.add)
            h_stt(k, Tm(5), rs, ncol("r_k"), Tm(5), ALU.mult, ALU.mult)
            h_mm(k, Pz(3), (ones[0][:], ones[1]), Tm(5))
            h_tt(k, O("bonus"), Pz(3), vs, ALU.mult)
            h_silu(k, O("sg_rw"), I("g_rw", 1), Tm(6))
            h_silu(k, O("sg_hy"), I("g_hy", 1), Tm(6))
            for nm in ("v", "x1", "x2"):
                src = "h" + nm
                h_ts(k, Tm(6), I(src, 0), pcol("t0_" + nm), None, ALU.mult)
                h_stt(k, Tm(6), I(src, 1), pcol("t1_" + nm), Tm(6), ALU.mult, ALU.add)
                h_stt(k, O(src), I(src, 2), pcol("t2_" + nm), Tm(6), ALU.mult, ALU.add)
            for name in ("bonus", "sg_rw", "hv", "hx1", "hx2", "sg_hy"):
                t, b = fo[FO[name]][j]
                k.dma("sync", fo_s[FO[name], :, oc0:oc0 + nn], t[:, 0:nn], r=[b])
            k.dma("sync", vT2_s[0:64, oc0:oc0 + nn], fo[FO["v"]][j][0][:, 0:nn], r=[fo[FO["v"]][j][1]])
            for bi in range(nn // 128):
                tau0 = oc0 + bi * 128
                s_lo = (128 - tau0) if tau0 < 256 else (256 + n - 128 - (tau0 - 256))
                q = blkc[0] % 2
                blkc[0] += 1
                pk, pkb = ptk[0]
                for ai, nm in enumerate(tord):
                    t, b = fo[FO[nm]][j]
                    h_tr(k, (pk[:, ai, :], pkb), (t[:, bi * 128:(bi + 1) * 128], b), (identf[0][0:64, 0:64], identf[1]))
                tk = (tok[q][0][:], tok[q][1])
                h_copy(k, tk, (pk[:, 0:9, :], pkb), eng="scalar")
                k.dma("sync", bc_s[0, tau0:tau0 + 128, :], tok[q][0][:, 0:5, :], r=[tk[1]])
                h_mm(k, (pfl[0][:, 0:192], pfl[1]), (Jf[0][:], Jf[1]), (tok[q][0][:, 5:8, :], tk[1]))
                h_mm(k, (pfl[0][:, 192:320], pfl[1]), (Jf[0][:], Jf[1]), (tok[q][0][:, 3:5, :], tk[1]))
                h_copy(k, (tokb[q][0][:], tokb[q][1]), (pfl[0][:, 0:320], pfl[1]))
                k.dma("sync", bc_s[1, s_lo:s_lo + 128, :], tokb[q][0][:], r=[tokb[q][1]])
                h_mm(k, (pfl[0][0:64, 384:512], pfl[1]), (tok[q][0][:, 8, :], tk[1]), (Jf[0][:], Jf[1]))
                h_copy(k, (vrev[q][0][:], vrev[q][1]), (pfl[0][0:64, 384:512], pfl[1]), eng="gpsimd" if False else "vector")
                k.dma("sync", vT2_s[64:128, s_lo:s_lo + 128], vrev[q][0][:], r=[vrev[q][1]])
        for ci, (pc0, oc0, nn) in enumerate(seq_chunks(n)):
            chunk(ci, pc0, oc0, nn)
        k.end_phase()

    def phase_scan():
        k.begin_phase()
        S, S_b = k.sb("S", [128, 64], F32)
        tmp, _ = k.sb("stmp", [128, 64], F32)
        sa, _ = k.sb("sa", [128, 2], F32)
        NBUF = 4
        bt = [k.sb("bt%d" % i, [128, TC, 320], F32) for i in range(NBUF)]
        vt = [k.sb("svt%d" % i, [128, 512], F32) for i in range(2)]
        yt = [k.sb("syt%d" % i, [128, 512], F32) for i in range(2)]
        bt2 = [Buf() for _ in range(NBUF)]
        h_memset(k, (S[:], S_b), 0.0)
        nch = (T + TC - 1) // TC
        for c in range(nch):
            s0 = c * TC
            nst = min(TC, T - s0)
            btt, btb = bt[c % NBUF]
            btb2 = bt2[c % NBUF]
            q = "sync" if c % 2 == 0 else "scalar"
            k.dma(q, btt[0:64, 0:nst, :], bc_s[0, s0:s0 + nst, :].partition_broadcast(64), w=[btb])
            k.dma(q, btt[64:128, 0:nst, :], bc_s[1, s0:s0 + nst, :].partition_broadcast(64), w=[btb2])
            if s0 % 512 == 0:
                vi = (s0 // 512) % 2
                vtt, vtb = vt[vi]
                ytt, ytb = yt[vi]
                nv = min(512, T - s0)
                k.dma("gpsimd", vtt[:, 0:nv], vT2_s[:, s0:s0 + nv], w=[vtb])
            for i in range(nst):
                s = s0 + i
                sl = s % 512
                W = btt[:, i, 0:64]; NB_ = btt[:, i, 64:128]; K_ = btt[:, i, 128:192]; KK = btt[:, i, 192:256]; R_ = btt[:, i, 256:320]
                sac = sa[:, s % 2:s % 2 + 1]
                P.op("vector", lambda E, KK=KK, sac=sac: E.scalar_tensor_tensor(out=tmp[:], in0=S[:], scalar=1.0, in1=KK, op0=ALU.mult, op1=ALU.mult, accum_out=sac), [S_b, btb, btb2], [S_b])
                P.op("vector", lambda E, W=W: E.tensor_tensor(out=S[:], in0=S[:], in1=W, op=ALU.mult), [S_b], [S_b])
                P.op("vector", lambda E, NB_=NB_, sac=sac: E.scalar_tensor_tensor(out=S[:], in0=NB_, scalar=sac, in1=S[:], op0=ALU.mult, op1=ALU.add), [S_b], [S_b])
                P.op("vector", lambda E, K_=K_, vtt=vtt, sl=sl: E.scalar_tensor_tensor(out=S[:], in0=K_, scalar=vtt[:, sl:sl + 1], in1=S[:], op0=ALU.mult, op1=ALU.add), [S_b, vtb], [S_b])
                P.op("vector", lambda E, R_=R_, ytt=ytt, sl=sl: E.scalar_tensor_tensor(out=tmp[:], in0=S[:], scalar=1.0, in1=R_, op0=ALU.mult, op1=ALU.mult, accum_out=ytt[:, sl:sl + 1]), [S_b, btb, btb2], [S_b, ytb])
                if sl == 511 or s == T - 1:
                    c0 = s - sl
                    k.dma("gpsimd", yT2_s[:, c0:s + 1], ytt[:, 0:sl + 1], r=[ytb])
        k.end_phase()

    def phase_filt(l, Lf):
        Li = L_in[l]
        fc = fconst[Lf]
        k.begin_phase()
        w1 = k.sb("w1s", [33, 64], F32); w2 = k.sb("w2s", [64, 64], F32); w3 = k.sb("w3s", [64, 256], F32)
        bb = k.sb("bbs", [64, 2], F32); b3 = k.sb("b3s", [128, 2], F32); nd = k.sb("nds", [128, 1], F32)
        for t, d in ((w1, Li["hw1"]), (w2, Li["hw2"]), (w3, Li["hw3"]), (bb, Li["hb12"]), (b3, Li["hb3"]), (nd, nd_d)):
            k.dma("sync", t[0][:], d, w=[t[1]])
        nch = (Lf + 511) // 512
        part = k.sb("part", [128, 2 * nch], F32)
        h_memset(k, (part[0][:], part[1]), 0.0)
        b0 = k.sb("b0", [128, 1], F32)
        ft = [k.sb("ft%d" % i, [33, 512], F32) for i in range(2)]
        tv = [k.sb("tv%d" % i, [128, 512], F32) for i in range(2)]
        hx = [k.sb("hx%d" % i, [64, 512], F32) for i in range(2)]
        hi = k.sb("hi", [64, 512], I32); hf = k.sb("hf", [64, 512], F32)
        win = k.sb("win", [128, 512], F32)
        tF = [k.sb("tF%d" % i, [128, 512], F32) for i in range(2)]
        oF = [k.sb("oF%d" % i, [128, 512], BF16) for i in range(2)]
        junk = k.sb("junkf", [128, 512], F32)
        ph = [k.ps("ph%d" % i, [64, 512], F32) for i in range(2)]
        pt = [k.ps("pt%d" % i, [128, 512], F32) for i in range(2)]
        kl = kl_h[Lf].ap()

        def sin_layer(out, psum, bias, nn):
            x = (hf[0][:, 0:nn], hf[1]); ki = (hi[0][:, 0:nn], hi[1])
            h_ts(k, out, psum, bias, None, ALU.add)
            h_ts(k, ki, out, 1.0 / TWO_PI, None, ALU.mult)
            h_copy(k, x, ki)
            h_stt(k, out, x, -TWO_PI, out, ALU.mult, ALU.add)
            h_ts(k, out, out, -float(np.pi), float(np.pi), ALU.max, ALU.min)
            h_act(k, out, out, AF.Sin)
        cnt = 0
        for side in (1, 0):
            for c in range(nch):
                c0 = c * 512
                nn = min(512, Lf - c0)
                j = cnt % 2
                cnt += 1
                k.dma("sync", ft[j][0][:, 0:nn], (fc["featsTr"] if side else fc["featsT"])[:, c0:c0 + nn], w=[ft[j][1]])
                k.dma("sync", tv[j][0][:, 0:nn], (fc["tvr"] if side else fc["tv"])[:, c0:c0 + nn].partition_broadcast(128), w=[tv[j][1]])
                p0 = (ph[0][0][:, 0:nn], ph[0][1]); p1 = (ph[1][0][:, 0:nn], ph[1][1])
                h_mm(k, p0, (w1[0][:], w1[1]), (ft[j][0][:, 0:nn], ft[j][1]))
                h1 = (hx[0][0][:, 0:nn], hx[0][1])
                sin_layer(h1, p0, (bb[0][:, 0:1], bb[1]), nn)
                h_mm(k, p1, (w2[0][:], w2[1]), h1)
                h2 = (hx[1][0][:, 0:nn], hx[1][1])
                sin_layer(h2, p1, (bb[0][:, 1:2], bb[1]), nn)
                ptx = (pt[j][0][:, 0:nn], pt[j][1])
                h_mm(k, ptx, (w3[0][:, side * 128:(side + 1) * 128], w3[1]), h2)
                w_ = (win[0][:, 0:nn], win[1])
                h_act(k, w_, (tv[j][0][:, 0:nn], tv[j][1]), AF.Exp, scale=(nd[0][:], nd[1]))
                F_ = (tF[j][0][:, 0:nn], tF[j][1])
                h_stt(k, F_, ptx, (b3[0][:, side:side + 1], b3[1]), w_, ALU.add, ALU.mult)
                hi_ = nn
                if side == 1 and c == nch - 1:
                    h_copy(k, (b0[0][:], b0[1]), (tF[j][0][:, nn - 1:nn], tF[j][1]))
                    hi_ = nn - 1
                if side == 0 and c == 0:
                    h_tt(k, (tF[j][0][:, 0:1], tF[j][1]), (tF[j][0][:, 0:1], tF[j][1]), (b0[0][:], b0[1]), ALU.add)
                pcol_ = 2 * c + side
                if hi_ > 0:
                    h_act(k, (junk[0][:, 0:hi_], junk[1]), (tF[j][0][:, 0:hi_], tF[j][1]), AF.Square, accum=(part[0][:, pcol_:pcol_ + 1], part[1]))
                    h_copy(k, (oF[j][0][:, 0:hi_], oF[j][1]), (tF[j][0][:, 0:hi_], tF[j][1]), eng="gpsimd")
                    base = c0 if side == 1 else (Lf - 1 + c0)
                    k.dma("sync", kl[:, base:base + hi_], oF[j][0][:, 0:hi_], r=[oF[j][1]])
        tot = k.sb("tot", [128, 1], F32)
        k.V(lambda E: E.tensor_reduce(out=tot[0][:], in_=part[0][:], axis=mybir.AxisListType.X, op=ALU.add), [part[1]], [tot[1]])
        k.dma("sync", ssq_s[Lf].rearrange("o p -> p o"), tot[0][:], r=[tot[1]], allow_slow_non_contiguous=True)
        k.end_phase()

    def phase_hy(l, nb, col_fo, col_mix, readout):
        Li = L_in[l]
        Lf = 128 * nb
        W = 128 * (2 * nb - 1)
        k.begin_phase()
        nrm = k.sb("nrm", [128, 128], F32); skp = k.sb("skp", [128, 128], F32)
        k.dma("sync", nrm[0][:], ssq_s[Lf].partition_broadcast(128), w=[nrm[1]])
        k.dma("sync", skp[0][:], Li["skipT"].partition_broadcast(128), w=[skp[1]])
        h_act(k, (nrm[0][:], nrm[1]), (nrm[0][:], nrm[1]), AF.Sqrt)
        h_recip(k, (nrm[0][:], nrm[1]), (nrm[0][:], nrm[1]))
        if readout:
            k.begin_phase()
            pp = k.sb("pp_sb", [64, NPC], F32)
            k.dma("sync", pp[0][:], Li["pp"], w=[pp[1]])
            o64 = k.sb("o64", [64, 64], F32)
            h_memset(k, (o64[0][:], o64[1]), 1.0 / 64)
            geps = k.sb("geps", [64, 1], F32)
            h_memset(k, (geps[0][:], geps[1]), 64e-5)
            ra = [k.sb("ra%d" % i, [64, 512], F32) for i in range(2)]
            rbk = [k.sb("rbk%d" % i, [64, 128], F32) for i in range(2)]
            rbt = [k.sb("rbt%d" % i, [128, 64], F32) for i in range(2)]
            rc = [k.sb("rc%d" % i, [64, 512], F32) for i in range(2)]
            rd = [k.sb("rd%d" % i, [64, 512], F32) for i in range(2)]
            r1 = k.sb("r1", [64, 512], F32); r2 = k.sb("r2", [64, 512], F32)
            rob = [k.sb("rob%d" % i, [64, 512], BF16) for i in range(2)]
            pr = [k.ps("pr%d" % i, [64, 512], F32) for i in range(2)]
            prt = k.ps("prt", [128, 64], F32)
            prb = k.ps("prb", [64, 512], F32)
            bq = 0
            chunks = [(0, 256)] + [(256 + u0, min(512, n - u0)) for u0 in range(0, n, 512)]
            for ci, (c0, nn) in enumerate(chunks):
                j = ci % 2
                A = (ra[j][0][:, 0:nn], ra[j][1]); C = (rc[j][0][:, 0:nn], rc[j][1]); Dg = (rd[j][0][:, 0:nn], rd[j][1])
                R1 = (r1[0][:, 0:nn], r1[1]); R2 = (r2[0][:, 0:nn], r2[1])
                P0 = (pr[0][0][:, 0:nn], pr[0][1]); P1 = (pr[1][0][:, 0:nn], pr[1][1])
                k.dma("scalar", A[0], yT2_s[0:64, c0:c0 + nn], w=[A[1]])
                k.dma("scalar", C[0], fo_s[FO["bonus"], :, c0:c0 + nn], w=[C[1]])
                k.dma("scalar", Dg[0], fo_s[FO["sg_rw"], :, c0:c0 + nn], w=[Dg[1]])
                for bi in range(nn // 128):
                    tau0 = c0 + bi * 128
                    s_lo = (128 - tau0) if tau0 < 256 else (256 + n - 128 - (tau0 - 256))
                    q = bq % 2
                    bq += 1
                    k.dma("scalar", rbk[q][0][:], yT2_s[64:128, s_lo:s_lo + 128], w=[rbk[q][1]])
                    h_tr(k, (prt[0][:], prt[1]), (rbk[q][0][:], rbk[q][1]), (identf[0][0:64, 0:64], identf[1]))
                    h_copy(k, (rbt[q][0][:], rbt[q][1]), (prt[0][:], prt[1]), eng="scalar")
                    h_mm(k, (prb[0][:, bi * 128:(bi + 1) * 128], prb[1]), (rbt[q][0][:], rbt[q][1]), (Jf[0][:], Jf[1]))
                h_tt(k, A, A, (prb[0][:, 0:nn], prb[1]), ALU.add)
                h_mm(k, P0, (o64[0][:], o64[1]), A)
                h_tt(k, R1, A, P0, ALU.subtract)
                h_tt(k, R2, R1, R1, ALU.mult)
                h_mm(k, P1, (o64[0][:], o64[1]), R2)
                h_act(k, R2, P1, AF.Sqrt, bias=(geps[0][:], geps[1]))
                h_recip(k, R2, R2)
                h_tt(k, R1, R1, R2, ALU.mult)
                h_ts(k, R1, R1, (pp[0][:, PC["gn_w"]:PC["gn_w"] + 1], pp[1]), (pp[0][:, PC["gn_b"]:PC["gn_b"] + 1], pp[1]), ALU.mult, ALU.add)
                h_tt(k, R1, R1, C, ALU.add)
                OB = (rob[j][0][:, 0:nn], rob[j][1])
                h_tt(k, OB, R1, Dg, ALU.mult)
                mc = (n + c0) if c0 < 256 else (c0 - 256)
                k.dma("scalar", mix_send[0:64, mc:mc + nn], OB[0], r=[OB[1]])
            k.end_phase()
        ksh = [k.sb("ksh%d" % i, [128, W], BF16) for i in range(2)]
        ld = [[k.sb("ld%d_%d" % (a, i), [nb, 128], F32) for i in range(2)] for a in range(4)]
        zt = [k.sb("zt%d" % i, [128, nb], F32) for i in range(2)]
        x1t = [k.sb("x1t%d" % i, [128, nb], F32) for i in range(2)]
        x2t = [k.sb("x2t%d" % i, [128, nb], F32) for i in range(2)]
        sgt = [k.sb("sgt%d" % i, [128, nb], F32) for i in range(2)]
        zb = k.sb("zb", [128, nb], BF16); zf = k.sb("zf", [128, nb], BF16)
        z2 = k.sb("z2", [128, nb], F32); t1 = k.sb("t1", [128, nb], F32)
        ot = [k.sb("oth%d" % i, [128, nb], BF16) for i in range(2)]
        otT = [k.sb("otT%d" % i, [nb, 128], BF16) for i in range(2)]
        pin = [k.ps("pin%d" % i, [128, nb], F32) for i in range(2)]
        pf = k.ps("pf", [128, nb], F32)
        py = [k.ps("pyc%d" % i, [128, nb], F32) for i in range(2)]
        pot = k.ps("pot", [nb, 128], BF16)
        order = [0] + [d for d in range(-(nb - 1), nb) if d != 0]
        cnt = 0
        names = ["hv", "hx1", "hx2", "sg_hy"]
        for c in range(64):
            j = c % 2
            dst = [zt[j], x1t[j], x2t[j], sgt[j]]
            for a in range(4):
                lt_, lb_ = ld[a][j]
                k.dma("gpsimd", lt_[:], fo_s[FO[names[a]], c, col_fo:col_fo + Lf].rearrange("(s j) -> s j", j=128), w=[lb_])
                h_tr(k, (pin[a % 2][0][:], pin[a % 2][1]), (lt_[:], lb_), (identf[0][0:nb, 0:nb], identf[1]))
                h_copy(k, (dst[a][0][:], dst[a][1]), (pin[a % 2][0][:], pin[a % 2][1]), eng=("scalar" if a % 2 else "vector"))
            Z = (zt[j][0][:], zt[j][1]); X1 = (x1t[j][0][:], x1t[j][1]); X2 = (x2t[j][0][:], x2t[j][1]); SG = (sgt[j][0][:], sgt[j][1])
            zin = Z
            for o in range(2):
                row = o * 64 + c
                kt, kb_ = ksh[cnt % 2]
                q = "sync" if cnt % 2 == 0 else "scalar"
                cnt += 1
                src = bass.AP(kl_h[Lf], row * 2 * Lf, [[1, 128], [1, W]])
                k.dma(q, kt[:], src, w=[kb_])
                h_copy(k, (zb[0][:], zb[1]), zin, eng="gpsimd")
                h_mm(k, (pf[0][:], pf[1]), (Jb[0][:], Jb[1]), (zb[0][:], zb[1]))
                h_copy(k, (zf[0][:], zf[1]), (pf[0][:], pf[1]), eng="scalar")
                pyt, pyb = py[o]
                for ii, Dd in enumerate(order):
                    e = Dd + nb - 1
                    S0, S1 = max(0, -Dd), min(nb, nb - Dd)
                    h_mm(k, (pyt[:, S0 + Dd:S1 + Dd], pyb), (kt[:, 128 * e:128 * e + 128], kb_), (zf[0][:, S0:S1], zf[1]), start=(ii == 0), stop=(ii == len(order) - 1))
                ncol_ = (nrm[0][:, row:row + 1], nrm[1])
                scol = (skp[0][:, row:row + 1], skp[1])
                h_ts(k, (t1[0][:], t1[1]), (pyt[:], pyb), ncol_, None, ALU.mult)
                h_stt(k, (t1[0][:], t1[1]), zin, scol, (t1[0][:], t1[1]), ALU.mult, ALU.add)
                if o == 0:
                    h_tt(k, (z2[0][:], z2[1]), (t1[0][:], t1[1]), X1, ALU.mult)
                    zin = (z2[0][:], z2[1])
                else:
                    O_ = (ot[j][0][:], ot[j][1])
                    h_tt(k, (t1[0][:], t1[1]), (t1[0][:], t1[1]), X2, ALU.mult)
                    h_tt(k, O_, (t1[0][:], t1[1]), SG, ALU.mult, eng="gpsimd")
                    h_tr(k, (pot[0][:], pot[1]), O_, IDB)
                    h_copy(k, (otT[j][0][:], otT[j][1]), (pot[0][:], pot[1]), eng="scalar")
                    k.dma("gpsimd", mix_send[64 + c, col_mix:col_mix + Lf].rearrange("(s j) -> s j", j=128), otT[j][0][:], r=[otT[j][1]])
        k.end_phase()

    for l in range(4):
        phase_tok(l)
        if l % 2 == 0:
            phase_attn(l)
        else:
            phase_rfeat(l)
            phase_scan()
            phase_filt(l, n)
            phase_hy(l, nlb, 256, 0, True)
            if l < 3:
                phase_filt(l, 256)
                phase_hy(l, 2, 0, n, False)
        P.coll("AllGather", mybir.AluOpType.bypass, mix_send, mix_all)
        P.barrier()
    phase_tok(4, final=True)
    k.done()
    return nc


_FUSED_CACHE = {}


def _c_cols(c, c_ctx):
    return np.ascontiguousarray(np.concatenate([c.reshape(8, 128).T, c_ctx.reshape(8, 128).T], axis=1).astype(np.float32))


def fused_inputs(inp, n):
    nlb = n // 128
    x = np.ascontiguousarray(inp["x"][0], dtype=np.float32)
    ctx = np.ascontiguousarray(inp["ctx"][0], dtype=np.float32)
    C, S, RT = rope_tables(nlb)
    sel = np.zeros((65, 64), np.float32)
    sel[64] = 1.0
    perm = np.concatenate([np.concatenate([np.arange(64 * r, 64 * r + 64), np.arange(512 + 64 * r, 512 + 64 * r + 64)]) for r in range(8)])
    common = {"x": x, "ctx": ctx, "c_cols": _c_cols(inp["c"][0], inp["c_ctx"]), "ident": np.eye(128, dtype=np.float32),
              "Jmat": np.eye(128, dtype=np.float32)[::-1].copy(), "final_norm": inp["final_norm"].reshape(1, -1).astype(np.float32),
              "ropeC": C, "ropeS": S, "rmatT": RT, "sel": sel}
    for Lf in (n, 256):
        fT, tv, adel = filt_consts(Lf)
        common["featsT%d" % Lf] = fT
        common["featsTr%d" % Lf] = np.ascontiguousarray(fT[:, ::-1])
        common["tv%d" % Lf] = tv
        common["tvr%d" % Lf] = np.ascontiguousarray(tv[:, ::-1])
    maps = []
    for h in range(8):
        m = dict(common)
        kvh = h // 4
        sl = slice(64 * h, 64 * h + 64)
        m["negdelta"] = -np.concatenate([adel[sl], adel[sl]]).reshape(128, 1).astype(np.float32)
        for l in range(4):
            i = l // 2
            attn = l % 2 == 0
            pre = "attn" if attn else "rec"
            m["norm_g%d" % l] = inp[pre + "_norm"][i].reshape(1, -1)
            m["ada_w%d" % l] = inp[pre + "_ada_w"][i]
            m["ada_b%d" % l] = inp[pre + "_ada_b"][i].reshape(1, -1)
            m["w_out%d" % l] = np.ascontiguousarray(inp[pre + "_w_out"][i][perm])
            w_in = inp[pre + "_w_in"][i]
            if attn:
                starts = [64 * h, 512 + 64 * h, 1536 + 64 * h, 2048 + 64 * h, 2560 + 64 * kvh, 2816 + 64 * h, 1024 + 64 * h, 2688 + 64 * kvh]
                cols = np.concatenate([np.arange(s0, s0 + 64) for s0 in starts])
                m["w_in%d" % l] = np.ascontiguousarray(w_in[:, cols])
                m["biasT%d" % l] = na_bias_tables(inp["na_rpb"][i][h], nlb)
                m["gains%d" % l] = np.stack([inp["gqa_q_gain"][i], inp["gqa_k_gain"][i]], 1).astype(np.float32)
            else:
                starts = [64 * h, 512 + 64 * h, 1024 + 64 * h, 1664 + 64 * h, 2176 + 64 * h, 2688 + 64 * h, 3200 + 64 * h, 3712 + 64 * h]
                cols = np.concatenate([np.arange(s0, s0 + 64) for s0 in starts] + [np.arange(1536, 1664)])
                m["w_in%d" % l] = np.ascontiguousarray(w_in[:, cols])
                prm = {k_: inp[k_][i] for k_ in inp if k_.startswith("rwkv") or k_.startswith("hy")}
                ri = rfeat_inputs(np.zeros((1, 4224), np.float32), np.zeros((1, 4224), np.float32), h, prm)
                m["pp%d" % l] = ri["pp"]
                m["mu_lora%d" % l] = ri["mu_lora"]
                m["wx%d" % l] = ri["wx"]
                fi = filt_inputs(256, h, prm)
                m["hw1_%d" % l] = fi["w1"]
                m["hw2_%d" % l] = fi["w2"]
                m["hw3_%d" % l] = fi["w3c"]
                m["hb12_%d" % l] = fi["b12"]
                m["hb3_%d" % l] = fi["b3c"]
                m["skipT%d" % l] = np.concatenate([prm["hy_skip"][0][sl], prm["hy_skip"][1][sl]]).reshape(1, 128).astype(np.float32)
        maps.append(m)
    return maps


def kernel(**inp):
    inp = {k_: np.asarray(v) for k_, v in inp.items()}
    n = inp["x"].shape[1]
    if n not in _FUSED_CACHE:
        _FUSED_CACHE[n] = build_fused(n)
    nc = _FUSED_CACHE[n]
    res = run_bass_kernel_spmd(nc, fused_inputs(inp, n), core_ids=list(range(8)))
    tpc = n // 8
    out = np.concatenate([res.results[i]["y_out"][i * tpc:(i + 1) * tpc] for i in range(8)], 0)
    return out[None].astype(np.float32)
```

```python
import numpy as np
import concourse.bass as bass
import concourse.mybir as mybir
from concourse.bass_utils import run_bass_kernel_spmd

F32 = mybir.dt.float32
BF16 = mybir.dt.bfloat16
I32 = mybir.dt.int32
ALU = mybir.AluOpType
AF = mybir.ActivationFunctionType


class Buf:
    __slots__ = ("w", "r", "name")

    def __init__(self, name=""):
        self.w = None
        self.r = {}
        self.name = name


class Prog:
    ENGS = ("tensor", "vector", "scalar", "gpsimd", "sync")
    NDSEM = 6

    def __init__(self, nc):
        self.nc = nc
        self.lists = {e: [] for e in self.ENGS}
        self.count = {e: 0 for e in self.ENGS}
        self.waited = {e: {} for e in self.ENGS}
        self.sems = {}
        self._ctx = []
        for e in self.ENGS:
            cm = nc.semaphore("s_" + e)
            self.sems[e] = cm.__enter__()
            self._ctx.append(cm)
        self.dsem = {}
        self.dcount = {}
        self.dnext = {}
        for q in ("sync", "scalar", "gpsimd"):
            self.dsem[q] = []
            for i in range(self.NDSEM):
                cm = nc.semaphore("d_%s%d" % (q, i))
                self.dsem[q].append(cm.__enter__())
                self._ctx.append(cm)
                self.sems["d_%s%d" % (q, i)] = self.dsem[q][-1]
            self.dcount[q] = [0] * self.NDSEM
            self.dnext[q] = 0

    def _need(self, eng, evs, relax=False):
        best = {}
        for ev in evs:
            if ev is None:
                continue
            k, v = ev
            if best.get(k, 0) < v:
                best[k] = v
        for k, v in best.items():
            if relax and k == eng and v < self.count[eng]:
                continue
            if self.waited[eng].get(k, 0) >= v:
                continue
            self.waited[eng][k] = v
            sem = self.sems[k]
            self.lists[eng].append(lambda E, sem=sem, v=v: E.wait_ge(sem, v))

    def _deps(self, reads, writes):
        evs = []
        for b in reads:
            evs.append(b.w)
        for b in writes:
            evs.append(b.w)
            evs.extend(b.r.items())
        return evs

    def op(self, eng, fn, reads=(), writes=(), relax=False):
        self._need(eng, self._deps(reads, writes), relax)
        self.count[eng] += 1
        c = self.count[eng]
        sem = self.sems[eng]
        self.lists[eng].append(lambda E, fn=fn, sem=sem: fn(E).then_inc(sem, 1))
        ev = (eng, c)
        for b in reads:
            b.r[eng] = c
        for b in writes:
            b.w = ev
            b.r = {}
        return ev

    def dma(self, q, out, in_, reads=(), writes=(), **kw):
        i = self.dnext[q]
        self.dnext[q] = (i + 1) % self.NDSEM
        key = "d_%s%d" % (q, i)
        evs = self._deps(reads, writes)
        if self.dcount[q][i] > 0:
            evs.append((key, self.dcount[q][i]))
        self._need(q, evs)
        self.dcount[q][i] += 16
        v = self.dcount[q][i]
        sem = self.sems[key]
        self.lists[q].append(lambda E, out=out, in_=in_, sem=sem, kw=kw: E.dma_start(out=out, in_=in_, **kw).then_inc(sem, 16))
        ev = (key, v)
        for b in reads:
            b.r[key] = v
        for b in writes:
            b.w = ev
            b.r = {}
        return ev

    def coll(self, kind, op, ins_ap, outs_ap, reads=(), writes=()):
        if "cc" not in self.sems:
            cm = self.nc.semaphore("s_cc")
            self.sems["cc"] = cm.__enter__()
            self._ctx.append(cm)
            self.cccount = 0
        evs = self._deps(reads, writes)
        if self.cccount:
            evs.append(("cc", self.cccount))
        self._need("gpsimd", evs)
        self.cccount += 1
        v = self.cccount
        sem = self.sems["cc"]
        self.lists["gpsimd"].append(lambda E: E.collective_compute(kind, op, replica_groups=[list(range(8))], ins=[ins_ap.opt()], outs=[outs_ap.opt()]).then_inc(sem))
        ev = ("cc", v)
        for b in reads:
            b.r["cc"] = v
        for b in writes:
            b.w = ev
            b.r = {}
        return ev

    def barrier(self):
        evs = []
        for q in self.dsem:
            for i in range(self.NDSEM):
                if self.dcount[q][i]:
                    evs.append(("d_%s%d" % (q, i), self.dcount[q][i]))
        for e in self.ENGS:
            if self.count[e]:
                evs.append((e, self.count[e]))
        if "cc" in self.sems and self.cccount:
            evs.append(("cc", self.cccount))
        for e in self.ENGS:
            self._need(e, evs)

    def finish(self):
        evs = []
        for q in self.dsem:
            for i in range(self.NDSEM):
                if self.dcount[q][i]:
                    evs.append(("d_%s%d" % (q, i), self.dcount[q][i]))
        for e in self.ENGS:
            if e != "sync" and self.count[e]:
                evs.append((e, self.count[e]))
        if "cc" in self.sems:
            evs.append(("cc", self.cccount))
        self._need("sync", evs)
        nc = self.nc
        lists = self.lists
        with nc.Block() as block:
            @block.sync
            def _(E):
                for f in lists["sync"]:
                    f(E)

            @block.tensor
            def _(E):
                for f in lists["tensor"]:
                    f(E)

            @block.vector
            def _(E):
                for f in lists["vector"]:
                    f(E)

            @block.scalar
            def _(E):
                for f in lists["scalar"]:
                    f(E)

            @block.gpsimd
            def _(E):
                for f in lists["gpsimd"]:
                    f(E)
        for cm in reversed(self._ctx):
            cm.__exit__(None, None, None)


from contextlib import ExitStack

NB = 18
NT = NB * 128
D = 1024
EPS = 1e-6


class KB:
    def __init__(self, nc):
        self.nc = nc
        self.P = Prog(nc)
        self.es = ExitStack()

    def _uname(self, name):
        self._uid = getattr(self, "_uid", 0) + 1
        return "%s_%d" % (name, self._uid)

    def sb(self, name, shape, dt):
        return self.es.enter_context(self.nc.sbuf_tensor(self._uname(name), shape, dt)), Buf(name)

    def ps(self, name, shape, dt):
        return self.es.enter_context(self.nc.psum_tensor(self._uname(name), shape, dt)), Buf(name)

    def din(self, name, shape, dt):
        return self.nc.dram_tensor(name, list(shape), dt, kind="ExternalInput").ap()

    def dout(self, name, shape, dt):
        return self.nc.dram_tensor(name, list(shape), dt, kind="ExternalOutput").ap()

    def V(self, fn, r=(), w=()):
        return self.P.op("vector", fn, r, w)

    def A(self, fn, r=(), w=()):
        return self.P.op("scalar", fn, r, w)

    def G(self, fn, r=(), w=()):
        return self.P.op("gpsimd", fn, r, w)

    def T(self, fn, r=(), w=()):
        return self.P.op("tensor", fn, r, w)

    def dma(self, q, out, in_, r=(), w=(), **kw):
        return self.P.dma(q, out, in_, r, w, **kw)

    def begin_phase(self):
        self._saved_es = self.es
        self.es = ExitStack()

    def end_phase(self):
        self.P.barrier()
        self.es.close()
        self.es = self._saved_es

    def done(self):
        self.P.finish()
        self.es.close()


def load_w_bf16(k, w_dram, wt, wb, ncols, rows=8):
    first = True
    for kc in range(rows):
        c0 = 0
        while c0 < ncols:
            c1 = min(ncols, c0 + 2048)
            k.dma("gpsimd", wt[:, kc, c0:c1], w_dram[kc * 128:(kc + 1) * 128, c0:c1], w=[] if not first else [wb])
            first = False
            c0 = c1
    return


def build_tok(in_w, has_prev, final, u_dt, NB=18, NLAT=16):
    NT = NB * 128
    nc = bass.Bass("TRN2", target_bir_lowering=False)
    k = KB(nc)
    P = k.P
    x_in = k.din("x_in", [NT, D], F32)
    ident_d = k.din("ident", [128, 128], F32)
    if has_prev:
        mixT = k.din("mixT", [D, NT], BF16)
        w_out = k.din("w_out", [D, D], F32)
        gates_in = k.din("gates_in", [2, 128, D], F32)
        x_out = k.dout("x_out", [NT, D], F32)
    if final:
        fn_g = k.din("final_norm", [1, D], F32)
        y_out = k.dout("y_out", [NT, D], F32)
    else:
        c_cols = k.din("c_cols", [128, 16], F32)
        norm_g = k.din("norm_g", [1, D], F32)
        ada_w = k.din("ada_w", [D, 3 * D], F32)
        ada_b = k.din("ada_b", [1, 3 * D], F32)
        w_in = k.din("w_in", [D, in_w], F32)
        u_out = k.dout("u_out", [NT, in_w], u_dt)
        gates_out = k.dout("gates_out", [2, 128, D], F32)

    ident, ident_b = k.sb("ident_sb", [128, 128], F32)
    identb, identb_b = k.sb("identb_sb", [128, 128], BF16)
    k.dma("sync", ident[:], ident_d, w=[ident_b])
    k.V(lambda E: E.tensor_copy(out=identb[:], in_=ident[:]), [ident_b], [identb_b])
    eps_t, eps_b = k.sb("eps_t", [128, 1], F32)
    k.V(lambda E: E.memset(eps_t[:], EPS), [], [eps_b])
    if has_prev:
        wo, wo_b = k.sb("wo", [128, 8, D], BF16)
        wo_evs = []
        for kc in range(8):
            wo_evs.append(k.dma("gpsimd", wo[:, kc, :], w_out[kc * 128:(kc + 1) * 128, :]))
        gp, gp_b = k.sb("gp", [128, 2, D], F32)
        k.dma("sync", gp[:], gates_in.rearrange("v p d -> p v d"), w=[gp_b])
    if final:
        fg, fg_b = k.sb("fg", [128, D], F32)
        k.dma("sync", fg[:], fn_g.partition_broadcast(128), w=[fg_b])
    else:
        wi, wi_b = k.sb("wi", [128, 8, in_w], BF16)
        wi_evs = []
        for kc in range(8):
            c0 = 0
            while c0 < in_w:
                c1 = min(in_w, c0 + 2048)
                wi_evs.append(k.dma("gpsimd", wi[:, kc, c0:c1], w_in[kc * 128:(kc + 1) * 128, c0:c1]))
                c0 = c1
        mod, mod_b = k.sb("mod", [128, 2, 3 * D], F32)
        Gt, G_b = k.sb("Gt", [128, 2, D], F32)
        with ExitStack() as es1:
            cc, cc_b = es1.enter_context(nc.sbuf_tensor("cc", [128, 16], F32)), Buf()
            rep, rep_b = es1.enter_context(nc.sbuf_tensor("rep", [128, 16, 128], F32)), Buf()
            ones, ones_b = es1.enter_context(nc.sbuf_tensor("ones", [128, 128], F32)), Buf()
            ab, ab_b = es1.enter_context(nc.sbuf_tensor("ab", [128, 3 * D], F32)), Buf()
            ng, ng_b = es1.enter_context(nc.sbuf_tensor("ng", [128, D], F32)), Buf()
            aw = [(es1.enter_context(nc.sbuf_tensor("aw%d" % i, [128, 8, 512], F32)), Buf()) for i in range(2)]
            pa = [(es1.enter_context(nc.psum_tensor("pa%d" % i, [128, 512], F32)), Buf()) for i in range(2)]
            k.dma("sync", cc[:], c_cols, w=[cc_b])
            k.dma("sync", ab[:], ada_b.partition_broadcast(128), w=[ab_b])
            k.dma("sync", ng[:], norm_g.partition_broadcast(128), w=[ng_b])
            k.A(lambda E: E.activation(out=cc[:], in_=cc[:], func=AF.Silu), [cc_b], [cc_b])
            k.V(lambda E: E.memset(ones[:], 1.0), [], [ones_b])
            for j in range(16):
                k.V(lambda E, j=j: E.tensor_scalar(out=rep[:, j, :], in0=ones[:], scalar1=cc[:, j:j + 1], scalar2=None, op0=ALU.mult),
                    [ones_b, cc_b], [rep_b])
            for g in range(6):
                awt, awb = aw[g % 2]
                k.dma("sync", awt[:], ada_w[:, g * 512:(g + 1) * 512].rearrange("(k p) n -> p k n", p=128), w=[awb])
                for v in range(2):
                    pt, pb = pa[v]
                    for kc in range(8):
                        k.T(lambda E, pt=pt, v=v, kc=kc, awt=awt: E.matmul(pt[:], rep[:, v * 8 + kc, :], awt[:, kc, :], start=(kc == 0), stop=(kc == 7)),
                            [rep_b, awb], [pb])
                    k.V(lambda E, pt=pt, v=v, g=g: E.tensor_tensor(out=mod[:, v, g * 512:(g + 1) * 512], in0=pt[:], in1=ab[:, g * 512:(g + 1) * 512], op=ALU.add),
                        [pb, ab_b], [mod_b])
            for v in range(2):
                k.V(lambda E, v=v: E.scalar_tensor_tensor(out=Gt[:, v, :], in0=mod[:, v, D:2 * D], scalar=1.0, in1=ng[:], op0=ALU.add, op1=ALU.mult),
                    [mod_b, ng_b], [G_b])
            k.dma("sync", gates_out.rearrange("v p d -> p v d"), mod[:, :, 2 * D:3 * D], r=[mod_b])
            scope_bufs = [cc_b, rep_b, ones_b, ab_b, ng_b, aw[0][1], aw[1][1], pa[0][1], pa[1][1]]

    xt = [k.sb("xt%d" % i, [128, D], F32) for i in range(2)]
    tmp = [k.sb("tmp%d" % i, [128, D], F32) for i in range(2)]
    ss = [k.sb("ss%d" % i, [128, 4], F32) for i in range(2)]
    junk, junk_b = k.sb("junk", [128, D], BF16)
    if has_prev:
        mt = [k.sb("mt%d" % i, [128, 8, 128], BF16) for i in range(2)]
        py = [k.ps("py%d" % i, [128, 512], F32) for i in range(2)]
    if not final:
        xm = [k.sb("xm%d" % i, [128, D], BF16) for i in range(2)]
        xmT = [k.sb("xmT%d" % i, [128, 8, 128], BF16) for i in range(2)]
        ptr = [k.ps("ptr%d" % i, [128, 8, 128], BF16) for i in range(2)]
        pu = [k.ps("pu%d" % i, [128, 512], F32) for i in range(3)]
        ut = [k.sb("ut%d" % i, [128, 512], u_dt) for i in range(4)]
    if not final:
        fence = Buf("fence")
        for b in scope_bufs:
            if b.w:
                fence.r[b.w[0]] = max(fence.r.get(b.w[0], 0), b.w[1])
            for kk, vv in b.r.items():
                fence.r[kk] = max(fence.r.get(kk, 0), vv)
    else:
        fence = Buf("fence")
    fence_evs = list(fence.r.items())
    for e in ("vector", "scalar", "gpsimd", "tensor", "sync"):
        P._need(e, fence_evs)
    if has_prev:
        for e in ("tensor",):
            P._need(e, wo_evs)
    if not final:
        P._need("tensor", wi_evs)

    ngroups = (in_w + 511) // 512 if not final else 0
    ucount = 0
    for b in range(NB):
        v = 0 if b < NLAT else 1
        xtt, xtb = xt[b % 2]
        tt, tb = tmp[b % 2]
        sst, ssb = ss[b % 2]
        k.dma("sync", xtt[:], x_in[b * 128:(b + 1) * 128, :], w=[xtb])
        if has_prev:
            mtt, mtb = mt[b % 2]
            k.dma("sync", mtt[:], mixT[:, b * 128:(b + 1) * 128].rearrange("(k p) t -> p k t", p=128), w=[mtb])
            for h in range(2):
                pyt, pyb = py[h]
                for kc in range(8):
                    k.T(lambda E, pyt=pyt, mtt=mtt, kc=kc, h=h: E.matmul(pyt[:], mtt[:, kc, :], wo[:, kc, h * 512:(h + 1) * 512], start=(kc == 0), stop=(kc == 7)),
                        [mtb], [pyb])
                k.V(lambda E, pyt=pyt, tt=tt, h=h, v=v: E.tensor_tensor(out=tt[:, h * 512:(h + 1) * 512], in0=pyt[:], in1=gp[:, v, h * 512:(h + 1) * 512], op=ALU.mult),
                    [pyb, gp_b], [tb])
            k.G(lambda E, xtt=xtt, tt=tt: E.tensor_tensor(out=xtt[:], in0=tt[:], in1=xtt[:], op=ALU.add), [tb, xtb], [xtb])
            k.dma("sync", x_out[b * 128:(b + 1) * 128, :], xtt[:], r=[xtb])
        k.A(lambda E, xtt=xtt, sst=sst: E.activation(out=junk[:], in_=xtt[:], func=AF.Square, accum_out=sst[:, 0:1]), [xtb], [junk_b, ssb])
        k.A(lambda E, sst=sst: E.activation(out=sst[:, 1:2], in_=sst[:, 0:1], func=AF.Sqrt, scale=1.0 / D, bias=eps_t[:]), [ssb, eps_b], [ssb])
        k.V(lambda E, sst=sst: E.reciprocal(out=sst[:, 2:3], in_=sst[:, 1:2]), [ssb], [ssb])
        if final:
            k.V(lambda E, xtt=xtt, tt=tt, sst=sst: E.scalar_tensor_tensor(out=tt[:], in0=xtt[:], scalar=sst[:, 2:3], in1=fg[:], op0=ALU.mult, op1=ALU.mult),
                [xtb, ssb, fg_b], [tb])
            k.dma("sync", y_out[b * 128:(b + 1) * 128, :], tt[:], r=[tb])
            continue
        xmt, xmb = xm[b % 2]
        k.V(lambda E, xtt=xtt, tt=tt, sst=sst, v=v: E.scalar_tensor_tensor(out=tt[:], in0=xtt[:], scalar=sst[:, 2:3], in1=Gt[:, v, :], op0=ALU.mult, op1=ALU.mult),
            [xtb, ssb, G_b], [tb])
        k.G(lambda E, tt=tt, xmt=xmt, v=v: E.tensor_tensor(out=xmt[:], in0=tt[:], in1=mod[:, v, 0:D], op=ALU.add), [tb, mod_b], [xmb])
        ptt, ptb = ptr[b % 2]
        xTt, xTb = xmT[b % 2]
        for kc in range(8):
            k.T(lambda E, ptt=ptt, xmt=xmt, kc=kc: E.transpose(ptt[:, kc, :], xmt[:, kc * 128:(kc + 1) * 128], identb[:]), [xmb, identb_b], [ptb])
        k.A(lambda E, ptt=ptt, xTt=xTt: E.copy(out=xTt[:], in_=ptt[:]), [ptb], [xTb])
        for g in range(ngroups):
            c0 = g * 512
            c1 = min(in_w, c0 + 512)
            put, pub = pu[ucount % 3]
            utt, utb = ut[ucount % 4]
            for kc in range(8):
                k.T(lambda E, put=put, xTt=xTt, kc=kc, c0=c0, c1=c1: E.matmul(put[:, 0:c1 - c0], xTt[:, kc, :], wi[:, kc, c0:c1], start=(kc == 0), stop=(kc == 7)),
                    [xTb], [pub])
            if ucount % 2 == 0:
                k.V(lambda E, put=put, utt=utt, c0=c0, c1=c1: E.tensor_copy(out=utt[:, 0:c1 - c0], in_=put[:, 0:c1 - c0]), [pub], [utb])
            else:
                k.A(lambda E, put=put, utt=utt, c0=c0, c1=c1: E.copy(out=utt[:, 0:c1 - c0], in_=put[:, 0:c1 - c0]), [pub], [utb])
            k.dma("sync", u_out[b * 128:(b + 1) * 128, c0:c1], utt[:, 0:c1 - c0], r=[utb])
            ucount += 1
    k.done()
    return nc


def _sc(s):
    return (s[0], [s[1]]) if isinstance(s, tuple) else (s, [])


def h_tt(k, out, a, b, op, eng="vector"):
    oa, ob = out; aa, ab = a; ba, bb = b
    return k.P.op(eng, lambda E: E.tensor_tensor(out=oa, in0=aa, in1=ba, op=op), [ab, bb], [ob])


def h_stt(k, out, a, s, b, op0, op1, accum=None):
    oa, ob = out; aa, ab = a; ba, bb = b
    sv, sb_ = _sc(s)
    if accum is None:
        return k.P.op("vector", lambda E: E.scalar_tensor_tensor(out=oa, in0=aa, scalar=sv, in1=ba, op0=op0, op1=op1), [ab, bb] + sb_, [ob])
    ca, cb = accum
    return k.P.op("vector", lambda E: E.scalar_tensor_tensor(out=oa, in0=aa, scalar=sv, in1=ba, op0=op0, op1=op1, accum_out=ca), [ab, bb] + sb_, [ob, cb])


def h_ts(k, out, a, s1, s2, op0, op1=None, eng="vector"):
    oa, ob = out; aa, ab = a
    s1v, s1b = _sc(s1)
    s2v, s2b = _sc(s2) if s2 is not None else (None, [])
    if op1 is None:
        return k.P.op(eng, lambda E: E.tensor_scalar(out=oa, in0=aa, scalar1=s1v, scalar2=None, op0=op0), [ab] + s1b, [ob])
    return k.P.op(eng, lambda E: E.tensor_scalar(out=oa, in0=aa, scalar1=s1v, scalar2=s2v, op0=op0, op1=op1), [ab] + s1b + s2b, [ob])


def h_act(k, out, a, func, scale=1.0, bias=None, accum=None):
    oa, ob = out; aa, ab = a
    kw = {}
    rd = [ab]
    wr = [ob]
    if bias is not None:
        bv, bb = _sc(bias)
        kw["bias"] = bv
        rd += bb
    if isinstance(scale, tuple):
        rd.append(scale[1]); scale = scale[0]
    if accum is not None:
        kw["accum_out"] = accum[0]
        wr.append(accum[1])
    return k.P.op("scalar", lambda E: E.activation(out=oa, in_=aa, func=func, scale=scale, **kw), rd, wr)


def h_recip(k, out, a):
    oa, ob = out; aa, ab = a
    return k.P.op("vector", lambda E: E.reciprocal(out=oa, in_=aa), [ab], [ob])


def h_copy(k, out, a, eng="vector"):
    oa, ob = out; aa, ab = a
    if eng == "scalar":
        return k.P.op(eng, lambda E: E.copy(out=oa, in_=aa), [ab], [ob])
    return k.P.op(eng, lambda E: E.tensor_copy(out=oa, in_=aa), [ab], [ob])


def h_mm(k, out, lhsT, rhs, start=True, stop=True):
    oa, ob = out; la, lb = lhsT; ra, rb = rhs
    return k.P.op("tensor", lambda E: E.matmul(oa, la, ra, start=start, stop=stop), [lb, rb], [ob])


def h_memset(k, out, val, eng="vector"):
    oa, ob = out
    return k.P.op(eng, lambda E: E.memset(oa, val), [], [ob])


def h_sigmoid(k, out, a, tmp, scale=1.0, bias=None):
    h_act(k, tmp, a, AF.Exp, scale=-scale, bias=bias)
    h_ts(k, tmp, tmp, 1.0, None, ALU.add)
    h_recip(k, out, tmp)


def h_silu(k, out, a, tmp):
    h_sigmoid(k, tmp, a, tmp)
    h_tt(k, out, tmp, a, ALU.mult)


import numpy as np

NEG = -30000.0


def na_plan(nlb):
    rows_blocks = nlb
    plan = []
    variants = {}
    for m in range(nlb):
        r0 = min(max(2 * m - 4, 0), 2 * nlb - 8)
        r1 = min(max(2 * m + 1 - 4, 0), 2 * nlb - 8) + 7
        kb0, kb1 = r0 // 2, r1 // 2
        edge = (m < 2) or (m >= nlb - 2)
        lst = []
        for kb in range(kb0, kb1 + 1):
            key = (m if edge else "i", kb - m)
            if key not in variants:
                variants[key] = (len(variants), m, kb)
            lst.append((kb, variants[key][0]))
        plan.append(lst)
    return plan, variants


def na_bias_tables(rpb_h, nlb):
    plan, variants = na_plan(nlb)
    rows = 2 * nlb
    out = np.full((128, len(variants), 128), NEG, np.float32)
    for key, (vid, m, kb) in variants.items():
        q = m * 128 + np.arange(128)
        kk = kb * 128 + np.arange(128)
        qr, qc = q // 64, q % 64
        kr, kc = kk // 64, kk % 64
        row0 = np.clip(qr - 4, 0, rows - 8)
        col0 = np.clip(qc - 8, 0, 64 - 16)
        inwin = ((kr[:, None] >= row0[None]) & (kr[:, None] < row0[None] + 8) &
                 (kc[:, None] >= col0[None]) & (kc[:, None] < col0[None] + 16))
        rr = np.clip(kr[:, None] - qr[None] + 7, 0, 14)
        cc = np.clip(kc[:, None] - qc[None] + 15, 0, 30)
        out[:, vid, :] = np.where(inwin, rpb_h[rr, cc], NEG)
    return out


def build_attn(nlb=128):
    NTK = (nlb + 2) * 128
    NKB = nlb + 2
    plan, variants = na_plan(nlb)
    nvar = len(variants)
    nc = bass.Bass("TRN2", target_bir_lowering=False)
    k = KB(nc)
    P = k.P
    qa_d = k.din("qaT", [64, NTK], BF16)
    ka_d = k.din("kaT", [64, NTK], BF16)
    va_d = k.din("va", [128, NKB, 65], BF16)
    ga_d = k.din("gaT", [64, NTK], BF16)
    qb_d = k.din("qbT", [64, NTK], BF16)
    kb_d = k.din("kbT", [64, NTK], BF16)
    vb_d = k.din("vb", [128, NKB, 65], BF16)
    gb_d = k.din("gbT", [64, NTK], BF16)
    bias_d = k.din("biasT", [128, nvar, 128], F32)
    cs_d = k.din("ropeC", [64, NTK], F32)
    sn_d = k.din("ropeS", [64, NTK], F32)
    gains_d = k.din("gains", [64, 2], F32)
    rmat_d = k.din("rmatT", [64, 64], F32)
    ident_d = k.din("ident", [128, 128], F32)
    sel_d = k.din("sel", [65, 64], F32)
    out_d = k.dout("mixT", [128, NTK], BF16)

    qT, qT_b = k.sb("qT", [64, NTK], BF16)
    kT, kT_b = k.sb("kT", [64, NTK], BF16)
    vE, vE_b = k.sb("vE", [128, NKB, 65], BF16)
    biasf, biasf_b = k.sb("biasf", [128, nvar, 128], F32)
    biasb, biasb_b = k.sb("biasb", [128, nvar, 128], BF16)
    identf, identf_b = k.sb("identf", [128, 128], F32)
    identb, identb_b = k.sb("identb", [128, 128], BF16)
    sel, sel_b = k.sb("sel_sb", [65, 64], F32)
    gains, gains_b = k.sb("gains_sb", [64, 2], F32)
    rmf, rmf_b = k.sb("rmf", [64, 64], F32)
    rmb, rmb_b = k.sb("rmb", [64, 64], BF16)
    ones64, ones64_b = k.sb("ones64", [64, 64], F32)
    eps_t, eps_b = k.sb("eps_t", [64, 1], F32)
    k.dma("sync", biasf[:], bias_d, w=[biasf_b])
    k.dma("sync", identf[:], ident_d, w=[identf_b])
    k.dma("sync", sel[:], sel_d, w=[sel_b])
    k.dma("sync", gains[:], gains_d, w=[gains_b])
    k.dma("sync", rmf[:], rmat_d, w=[rmf_b])
    k.V(lambda E: E.tensor_copy(out=biasb[:], in_=biasf[:]), [biasf_b], [biasb_b])
    k.V(lambda E: E.tensor_copy(out=identb[:], in_=identf[:]), [identf_b], [identb_b])
    k.V(lambda E: E.tensor_copy(out=rmb[:], in_=rmf[:]), [rmf_b], [rmb_b])
    k.V(lambda E: E.memset(ones64[:], 1.0 / 64), [], [ones64_b])
    k.V(lambda E: E.memset(eps_t[:], EPS), [], [eps_b])
    k.V(lambda E: E.tensor_scalar(out=gains[:, 0:1], in0=gains[:, 0:1], scalar1=0.125, scalar2=None, op0=ALU.mult), [gains_b], [gains_b])

    pS = [k.ps("pS%d" % i, [128, 512], F32) for i in range(4)]
    pacc = [k.ps("pacc%d" % i, [65, 512], F32) for i in range(2)]
    pmisc = [k.ps("pm%d" % i, [64, 512], F32) for i in range(2)]
    PT = [k.sb("PT%d" % i, [128, 512], BF16) for i in range(4)]
    accs = [k.sb("accs%d" % i, [65, 512], F32) for i in range(2)]
    gt = [k.sb("gt%d" % i, [64, 512], BF16) for i in range(2)]
    w1 = [k.sb("w1_%d" % i, [64, 512], F32) for i in range(2)]
    w2 = [k.sb("w2_%d" % i, [64, 512], F32) for i in range(2)]
    ot = [k.sb("ot%d" % i, [64, 512], BF16) for i in range(2)]
    cin = [k.sb("cin%d" % i, [64, 512], BF16) for i in range(2)]
    ctab = [k.sb("ctab%d" % i, [64, 512], F32) for i in range(2)]
    stab = [k.sb("stab%d" % i, [64, 512], F32) for i in range(2)]
    qnb = [k.sb("qnb%d" % i, [64, 512], BF16) for i in range(2)]

    state = {"fin": 0, "S": 0, "pre": 0}

    def finalize(pa, pab, g_d, row0, c0, n):
        i = state["fin"] % 2
        state["fin"] += 1
        at, ab = accs[i]
        gtt, gtb = gt[i]
        a1, a1b = w1[i]
        a2, a2b = w2[i]
        o, ob = ot[i]
        pm, pmb = pmisc[i]
        k.dma("sync", gtt[:, 0:n], g_d[:, c0:c0 + n], w=[gtb])
        k.A(lambda E: E.copy(out=at[:, 0:n], in_=pa[:, 0:n]), [pab], [ab])
        k.T(lambda E: E.matmul(pm[:, 0:n], sel[:], at[:, 0:n], start=True, stop=True), [sel_b, ab], [pmb])
        k.V(lambda E: E.reciprocal(out=a1[:, 0:n], in_=pm[:, 0:n]), [pmb], [a1b])
        k.V(lambda E: E.tensor_tensor(out=a1[:, 0:n], in0=a1[:, 0:n], in1=at[0:64, 0:n], op=ALU.mult), [a1b, ab], [a1b])
        k.A(lambda E: E.activation(out=a2[:, 0:n], in_=gtt[:, 0:n], func=AF.Exp, scale=-1.0), [gtb], [a2b])
        k.V(lambda E: E.tensor_scalar(out=a2[:, 0:n], in0=a2[:, 0:n], scalar1=1.0, scalar2=None, op0=ALU.add), [a2b], [a2b])
        k.V(lambda E: E.reciprocal(out=a2[:, 0:n], in_=a2[:, 0:n]), [a2b], [a2b])
        k.V(lambda E: E.tensor_tensor(out=a2[:, 0:n], in0=a2[:, 0:n], in1=gtt[:, 0:n], op=ALU.mult), [a2b, gtb], [a2b])
        k.V(lambda E: E.tensor_tensor(out=o[:, 0:n], in0=a1[:, 0:n], in1=a2[:, 0:n], op=ALU.mult), [a1b, a2b], [ob])
        k.dma("sync", out_d[row0:row0 + 64, c0:c0 + n], o[:, 0:n], r=[ob])

    def attend(qc0, n, kbs, pa, pab, first=True, last=True):
        pend = []
        nk = len(kbs)
        for idx, kb in enumerate(kbs):
            s = state["S"]
            state["S"] += 1
            pst, psb = pS[s % 4]
            ptt, ptb = PT[s % 4]
            k.T(lambda E, pst=pst, kb=kb: E.matmul(pst[:, 0:n], kT[:, kb * 128:(kb + 1) * 128], qT[:, qc0:qc0 + n], start=True, stop=True),
                [kT_b, qT_b], [psb])
            k.A(lambda E, pst=pst, ptt=ptt: E.activation(out=ptt[:, 0:n], in_=pst[:, 0:n], func=AF.Exp), [psb], [ptb])
            pend.append((idx, kb, ptt, ptb))
            if len(pend) > 2:
                j, kbj, pj, pjb = pend.pop(0)
                k.T(lambda E, kbj=kbj, pj=pj, j=j: E.matmul(pa[:, 0:n], vE[:, kbj, :], pj[:, 0:n], start=(first and j == 0), stop=(last and j == nk - 1)),
                    [vE_b, pjb], [pab])
        for (j, kbj, pj, pjb) in pend:
            k.T(lambda E, kbj=kbj, pj=pj, j=j: E.matmul(pa[:, 0:n], vE[:, kbj, :], pj[:, 0:n], start=(first and j == 0), stop=(last and j == nk - 1)),
                [vE_b, pjb], [pab])

    k.dma("sync", kT[:], ka_d, w=[kT_b])
    k.dma("sync", vE[:], va_d, w=[vE_b])
    k.dma("sync", qT[:], qa_d, w=[qT_b])
    k.V(lambda E: E.tensor_scalar(out=qT[:], in0=qT[:], scalar1=0.125, scalar2=None, op0=ALU.mult), [qT_b], [qT_b])
    ctxk = [nlb, nlb + 1]
    acc_i = 0
    for g0 in range(0, nlb, 4):
        pa, pab = pacc[acc_i % 2]
        acc_i += 1
        nq = min(4, nlb - g0)
        for mi in range(nq):
            m = g0 + mi
            lst = plan[m]
            nreg = len(lst) + 2
            s = state["S"]
            state["S"] += 2
            pA, pAb = pS[s % 4]
            pB, pBb = pS[(s + 1) % 4]
            tA, tAb = PT[s % 4]
            tB, tBb = PT[(s + 1) % 4]
            regs = []
            for j, (kb, var) in enumerate(lst + [(ctxk[0], None), (ctxk[1], None)]):
                pt_, pb_, tt_, tb_ = (pA, pAb, tA, tAb) if j < 4 else (pB, pBb, tB, tBb)
                jj = j % 4
                k.T(lambda E, pt_=pt_, kb=kb, jj=jj, m=m, var=var: E.matmul(pt_[:, jj * 128:(jj + 1) * 128], kT[:, kb * 128:(kb + 1) * 128], qT[:, m * 128:(m + 1) * 128], start=True, stop=(var is None)),
                    [kT_b, qT_b], [pb_])
                if var is not None:
                    k.T(lambda E, pt_=pt_, jj=jj, var=var: E.matmul(pt_[:, jj * 128:(jj + 1) * 128], identb[:], biasb[:, var, :], start=False, stop=True),
                        [identb_b, biasb_b], [pb_])
                regs.append((kb, tt_, tb_, jj))
            nA = min(4, nreg)
            k.A(lambda E, pA=pA, tA=tA, nA=nA: E.activation(out=tA[:, 0:nA * 128], in_=pA[:, 0:nA * 128], func=AF.Exp), [pAb], [tAb])
            if nreg > 4:
                nB = nreg - 4
                k.A(lambda E, pB=pB, tB=tB, nB=nB: E.activation(out=tB[:, 0:nB * 128], in_=pB[:, 0:nB * 128], func=AF.Exp), [pBb], [tBb])
            for j, (kb, tt_, tb_, jj) in enumerate(regs):
                k.T(lambda E, pa=pa, kb=kb, tt_=tt_, jj=jj, mi=mi, j=j, nreg=nreg: E.matmul(pa[:, mi * 128:(mi + 1) * 128], vE[:, kb, :], tt_[:, jj * 128:(jj + 1) * 128], start=(j == 0), stop=(j == nreg - 1)),
                    [vE_b, tb_], [pab])
        finalize(pa, pab, ga_d, 0, g0 * 128, nq * 128)
    pa, pab = pacc[acc_i % 2]
    acc_i += 1
    attend(nlb * 128, 256, ctxk, pa, pab)
    finalize(pa, pab, ga_d, 0, nlb * 128, 256)

    k.dma("sync", vE[:], vb_d, w=[vE_b])

    def prepass(src_d, dst, dst_b, gcol):
        for c0_ in range(0, NTK, 512):
            pre_chunk(src_d, dst, dst_b, gcol, c0_)

    def pre_chunk(src_d, dst, dst_b, gcol, c0):
        if True:
            n = min(512, NTK - c0)
            i = state["pre"] % 2
            state["pre"] += 1
            ci, cib = cin[i]
            ct, ctb = ctab[i]
            st_, stb = stab[i]
            qn, qnb_ = qnb[i]
            a1, a1b = w1[i]
            a2, a2b = w2[i]
            pm, pmb = pmisc[i]
            k.dma("sync", ci[:, 0:n], src_d[:, c0:c0 + n], w=[cib])
            k.dma("sync", ct[:, 0:n], cs_d[:, c0:c0 + n], w=[ctb])
            k.dma("sync", st_[:, 0:n], sn_d[:, c0:c0 + n], w=[stb])
            k.V(lambda E: E.tensor_tensor(out=a1[:, 0:n], in0=ci[:, 0:n], in1=ci[:, 0:n], op=ALU.mult), [cib], [a1b])
            k.T(lambda E: E.matmul(pm[:, 0:n], ones64[:], a1[:, 0:n], start=True, stop=True), [ones64_b, a1b], [pmb])
            k.A(lambda E: E.activation(out=a2[:, 0:n], in_=pm[:, 0:n], func=AF.Sqrt, bias=eps_t[:]), [pmb, eps_b], [a2b])
            k.V(lambda E: E.reciprocal(out=a2[:, 0:n], in_=a2[:, 0:n]), [a2b], [a2b])
            k.V(lambda E: E.scalar_tensor_tensor(out=qn[:, 0:n], in0=ci[:, 0:n], scalar=gains[:, gcol:gcol + 1], in1=a2[:, 0:n], op0=ALU.mult, op1=ALU.mult),
                [cib, gains_b, a2b], [qnb_])
            k.T(lambda E: E.matmul(pm[:, 0:n], rmb[:], qn[:, 0:n], start=True, stop=True), [rmb_b, qnb_], [pmb])
            k.V(lambda E: E.tensor_tensor(out=a1[:, 0:n], in0=qn[:, 0:n], in1=ct[:, 0:n], op=ALU.mult), [qnb_, ctb], [a1b])
            k.V(lambda E: E.tensor_tensor(out=a2[:, 0:n], in0=pm[:, 0:n], in1=st_[:, 0:n], op=ALU.mult), [pmb, stb], [a2b])
            k.G(lambda E: E.tensor_tensor(out=dst[:, c0:c0 + n], in0=a1[:, 0:n], in1=a2[:, 0:n], op=ALU.add), [a1b, a2b], [dst_b])

    prepass(kb_d, kT, kT_b, 1)
    prepass(qb_d, qT, qT_b, 0)
    allk = list(range(NKB))
    for c0 in range(0, nlb * 128, 512):
        n = min(512, nlb * 128 - c0)
        pa, pab = pacc[acc_i % 2]
        acc_i += 1
        attend(c0, n, allk, pa, pab)
        finalize(pa, pab, gb_d, 64, c0, n)
    pa, pab = pacc[acc_i % 2]
    acc_i += 1
    attend(nlb * 128, 256, ctxk, pa, pab)
    finalize(pa, pab, gb_d, 64, nlb * 128, 256)
    k.done()
    return nc


def rope_tables(nlb):
    n = nlb * 128
    t = np.arange(n)
    pos = np.stack([t // 64, t % 64], -1).astype(np.float32)
    inv = (10000.0 ** (-np.arange(16, dtype=np.float32) / 16)).astype(np.float32)
    ang = pos[:, :, None] * inv
    cos, sin = np.cos(ang), np.sin(ang)
    C = np.ones((64, n + 256), np.float32)
    S = np.zeros((64, n + 256), np.float32)
    for a in range(2):
        for hf in range(2):
            C[a * 32 + hf * 16:a * 32 + hf * 16 + 16, :n] = cos[:, a, :].T
            S[a * 32 + hf * 16:a * 32 + hf * 16 + 16, :n] = sin[:, a, :].T
    R = np.zeros((64, 64), np.float32)
    for a in range(2):
        for f in range(16):
            R[a * 32 + f, a * 32 + 16 + f] = -1.0
            R[a * 32 + 16 + f, a * 32 + f] = 1.0
    return C, S, np.ascontiguousarray(R.T)


import numpy as np

PC = {n: i for i, n in enumerate(
    ["mu_r", "mu_k", "mu_v", "k_k", "k_a", "w0_f", "w0_b", "a0_f", "a0_b", "r_k",
     "t0_v", "t1_v", "t2_v", "t0_x1", "t1_x1", "t2_x1", "t0_x2", "t1_x2", "t2_x2",
     "gn_w", "gn_b", "skip0", "skip1"])}
NPC = len(PC)
FO = {n: i for i, n in enumerate(
    ["w_f", "w_b", "kkn", "nb_f", "nb_b", "k_f", "k_b", "r", "v", "bonus", "sg_rw", "hv", "hx1", "hx2", "sg_hy"])}
NFO = len(FO)
FI = {n: i for i, n in enumerate(["r", "k", "v", "g_rw", "hv", "hx1", "hx2", "g_hy"])}
NFI = len(FI)


def seq_chunks(n_lat):
    ch = [(0, 0, 256)]
    for c0 in range(0, n_lat, 512):
        n = min(512, n_lat - c0)
        ch.append((258 + c0, 256 + c0, n))
    return ch


def build_rfeat(n_lat):
    T = 256 + n_lat
    TP = T + 4
    nc = bass.Bass("TRN2", target_bir_lowering=False)
    k = KB(nc)
    fin_d = k.din("fin", [NFI, 64, TP], F32)
    lora_d = k.din("lora", [128, TP], F32)
    pp_d = k.din("pp", [64, NPC], F32)
    mul_d = k.din("mu_lora", [128, 1], F32)
    wx_d = k.din("wx", [128, 4, 64], F32)
    fo_d = k.dout("fo", [NFO, 64, T], F32)

    pp = k.sb("pp_sb", [64, NPC], F32)
    npp = k.sb("npp_sb", [64, NPC], F32)
    mul = k.sb("mul_sb", [128, 1], F32)
    wx = k.sb("wx_sb", [128, 4, 64], F32)
    ones = k.sb("ones_sb", [64, 64], F32)
    tiny = k.sb("tiny_sb", [64, 1], F32)
    k.dma("sync", pp[0][:], pp_d, w=[pp[1]])
    k.dma("sync", mul[0][:], mul_d, w=[mul[1]])
    k.dma("sync", wx[0][:], wx_d, w=[wx[1]])
    h_memset(k, (ones[0][:], ones[1]), 1.0)
    h_memset(k, (tiny[0][:], tiny[1]), 1e-12)
    h_ts(k, (npp[0][:], npp[1]), (pp[0][:], pp[1]), -1.0, None, ALU.mult)
    h_ts(k, (npp[0][:, PC["r_k"]:PC["r_k"] + 1], npp[1]), (pp[0][:, PC["r_k"]:PC["r_k"] + 1], pp[1]), 0.5, None, ALU.mult)

    def pcol(name):
        return (pp[0][:, PC[name]:PC[name] + 1], pp[1])

    def ncol(name):
        return (npp[0][:, PC[name]:PC[name] + 1], npp[1])

    fin = [[k.sb("fin%d_%d" % (i, j), [64, 514], F32) for j in range(2)] for i in range(NFI)]
    lor = [k.sb("lor%d" % j, [128, 514], F32) for j in range(2)]
    fo = [[k.sb("fo%d_%d" % (i, j), [64, 512], F32) for j in range(2)] for i in range(NFO)]
    tm = [k.sb("tm%d" % i, [64, 512], F32) for i in range(8)]
    lt = [k.sb("lt%d" % i, [128, 512], F32) for i in range(3)]
    pz = [k.ps("pz%d" % i, [64, 512], F32) for i in range(6)]

    def chunk(ci, pc0, oc0, n):
        j = ci % 2

        def I(name, lo=1):
            t, b = fin[FI[name]][j]
            return (t[:, lo:lo + n], b)

        def O(name):
            t, b = fo[FO[name]][j]
            return (t[:, 0:n], b)

        def Tm(i):
            return (tm[i][0][:, 0:n], tm[i][1])

        def Lt(i):
            return (lt[i][0][:, 0:n], lt[i][1])

        def Pz(i):
            return (pz[i][0][:, 0:n], pz[i][1])

        for name, i in FI.items():
            t, b = fin[i][j]
            k.dma("sync", t[:, 0:n + 2], fin_d[i, :, pc0:pc0 + n + 2], w=[b])
        lt_, lb_ = lor[j]
        k.dma("sync", lt_[:, 0:n + 2], lora_d[:, pc0:pc0 + n + 2], w=[lb_])

        def shift(out, src_lo, src_mid, src_hi, mu, t):
            h_tt(k, t, src_lo, src_hi, ALU.add)
            h_stt(k, t, t, 0.5, src_mid, ALU.mult, ALU.subtract)
            h_stt(k, out, t, mu, src_mid, ALU.mult, ALU.add)

        rs, ks, vs = O("r"), Tm(0), O("v")
        shift(rs, I("r", 0), I("r", 1), I("r", 2), pcol("mu_r"), Tm(7))
        shift(ks, I("k", 0), I("k", 1), I("k", 2), pcol("mu_k"), Tm(7))
        shift(vs, I("v", 0), I("v", 1), I("v", 2), pcol("mu_v"), Tm(7))
        ls = Lt(0)
        shift(ls, (lt_[:, 0:n], lb_), (lt_[:, 1:n + 1], lb_), (lt_[:, 2:n + 2], lb_), (mul[0][:], mul[1]), Lt(2))
        lth = Lt(1)
        h_act(k, lth, ls, AF.Tanh)
        h_mm(k, Pz(0), (wx[0][:, 0, :], wx[1]), lth)
        h_mm(k, Pz(1), (wx[0][:, 1, :], wx[1]), lth)
        h_mm(k, Pz(2), (wx[0][:, 2, :], wx[1]), ls)
        h_mm(k, Pz(3), (wx[0][:, 3, :], wx[1]), ls)
        kk = Tm(1)
        h_ts(k, kk, ks, pcol("k_k"), None, ALU.mult)
        h_tt(k, Tm(2), kk, kk, ALU.mult)
        h_mm(k, Pz(4), (ones[0][:], ones[1]), Tm(2))
        h_act(k, Tm(2), Pz(4), AF.Sqrt, bias=(tiny[0][:], tiny[1]))
        h_recip(k, Tm(2), Tm(2))
        h_tt(k, O("kkn"), kk, Tm(2), ALU.mult)
        for d, sfx in enumerate(("f", "b")):
            h_sigmoid(k, Tm(3), Pz(d), Tm(3), bias=ncol("w0_" + sfx))
            h_act(k, O("w_" + sfx), Tm(3), AF.Exp, scale=-float(np.exp(-0.5)))
            a = Tm(4)
            h_sigmoid(k, a, Pz(2 + d), a, bias=ncol("a0_" + sfx))
            h_ts(k, Tm(5), a, -1.0, pcol("k_a"), ALU.add, ALU.mult)
            h_stt(k, O("k_" + sfx), Tm(5), 1.0, ks, ALU.add, ALU.mult)
            h_stt(k, O("nb_" + sfx), O("kkn"), -1.0, a, ALU.mult, ALU.mult)
        h_tt(k, Tm(5), O("k_f"), O("k_b"), ALU.add)
        h_stt(k, Tm(5), rs, ncol("r_k"), Tm(5), ALU.mult, ALU.mult)
        h_mm(k, Pz(5), (ones[0][:], ones[1]), Tm(5))
        h_tt(k, O("bonus"), Pz(5), vs, ALU.mult)
        h_silu(k, O("sg_rw"), I("g_rw", 1), Tm(6))
        h_silu(k, O("sg_hy"), I("g_hy", 1), Tm(6))
        for nm in ("v", "x1", "x2"):
            src = "h" + nm
            h_ts(k, Tm(6), I(src, 0), pcol("t0_" + nm), None, ALU.mult)
            h_stt(k, Tm(6), I(src, 1), pcol("t1_" + nm), Tm(6), ALU.mult, ALU.add)
            h_stt(k, O(src), I(src, 2), pcol("t2_" + nm), Tm(6), ALU.mult, ALU.add)
        for name, i in FO.items():
            t, b = fo[i][j]
            k.dma("sync", fo_d[i, :, oc0:oc0 + n], t[:, 0:n], r=[b])

    for ci, (pc0, oc0, n) in enumerate(seq_chunks(n_lat)):
        chunk(ci, pc0, oc0, n)
    k.done()
    return nc


import numpy as np

TWO_PI = float(2 * np.pi)


def build_filt(L):
    nc = bass.Bass("TRN2", target_bir_lowering=False)
    k = KB(nc)
    feats_d = k.din("featsT", [33, L], F32)
    tv_d = k.din("tvals", [1, L], F32)
    w1_d = k.din("w1", [33, 64], F32)
    w2_d = k.din("w2", [64, 64], F32)
    w3_d = k.din("w3c", [64, 256], F32)
    bb_d = k.din("b12", [64, 2], F32)
    b3_d = k.din("b3c", [128, 2], F32)
    nd_d = k.din("negdelta", [128, 1], F32)
    tf_d = k.dout("tapsF", [128, L], BF16)
    tb_d = k.dout("tapsB", [128, L], BF16)
    ssq_d = k.dout("ssq", [128, 1], F32)
    w1 = k.sb("w1s", [33, 64], F32); w2 = k.sb("w2s", [64, 64], F32); w3 = k.sb("w3s", [64, 256], F32)
    bb = k.sb("bbs", [64, 2], F32); b3 = k.sb("b3s", [128, 2], F32); nd = k.sb("nds", [128, 1], F32)
    for t, d in ((w1, w1_d), (w2, w2_d), (w3, w3_d), (bb, bb_d), (b3, b3_d), (nd, nd_d)):
        k.dma("sync", t[0][:], d, w=[t[1]])
    nch = (L + 511) // 512
    part = k.sb("part", [128, 2 * nch], F32)
    h_memset(k, (part[0][:], part[1]), 0.0)
    ft = [k.sb("ft%d" % i, [33, 512], F32) for i in range(2)]
    tv = [k.sb("tv%d" % i, [128, 512], F32) for i in range(2)]
    hx = [k.sb("hx%d" % i, [64, 512], F32) for i in range(2)]
    hi = k.sb("hi", [64, 512], I32)
    hf = k.sb("hf", [64, 512], F32)
    win = k.sb("win", [128, 512], F32)
    tF = [k.sb("tF%d" % i, [128, 512], F32) for i in range(2)]
    tB = [k.sb("tB%d" % i, [128, 512], F32) for i in range(2)]
    oF = [k.sb("oF%d" % i, [128, 512], BF16) for i in range(2)]
    oB = [k.sb("oB%d" % i, [128, 512], BF16) for i in range(2)]
    junk = k.sb("junkf", [128, 512], F32)
    ph = [k.ps("ph%d" % i, [64, 512], F32) for i in range(2)]
    pt = [k.ps("pt%d" % i, [128, 512], F32) for i in range(2)]

    def sin_layer(out, psum, bias, n):
        x = (hf[0][:, 0:n], hf[1])
        ki = (hi[0][:, 0:n], hi[1])
        h_ts(k, out, psum, bias, None, ALU.add)
        h_ts(k, ki, out, 1.0 / TWO_PI, None, ALU.mult)
        h_copy(k, x, ki)
        h_stt(k, out, x, -TWO_PI, out, ALU.mult, ALU.add)
        h_ts(k, out, out, -float(np.pi), float(np.pi), ALU.max, ALU.min)
        h_act(k, out, out, AF.Sin)

    for c in range(nch):
        c0 = c * 512
        n = min(512, L - c0)
        j = c % 2
        k.dma("sync", ft[j][0][:, 0:n], feats_d[:, c0:c0 + n], w=[ft[j][1]])
        k.dma("sync", tv[j][0][:, 0:n], tv_d[:, c0:c0 + n].partition_broadcast(128), w=[tv[j][1]])
        h_mm(k, (ph[0][0][:, 0:n], ph[0][1]), (w1[0][:], w1[1]), (ft[j][0][:, 0:n], ft[j][1]))
        h1 = (hx[0][0][:, 0:n], hx[0][1])
        sin_layer(h1, (ph[0][0][:, 0:n], ph[0][1]), (bb[0][:, 0:1], bb[1]), n)
        h_mm(k, (ph[1][0][:, 0:n], ph[1][1]), (w2[0][:], w2[1]), h1)
        h2 = (hx[1][0][:, 0:n], hx[1][1])
        sin_layer(h2, (ph[1][0][:, 0:n], ph[1][1]), (bb[0][:, 1:2], bb[1]), n)
        h_mm(k, (pt[0][0][:, 0:n], pt[0][1]), (w3[0][:, 0:128], w3[1]), h2)
        h_mm(k, (pt[1][0][:, 0:n], pt[1][1]), (w3[0][:, 128:256], w3[1]), h2)
        w_ = (win[0][:, 0:n], win[1])
        h_act(k, w_, (tv[j][0][:, 0:n], tv[j][1]), AF.Exp, scale=(nd[0][:], nd[1]))
        F_ = (tF[j][0][:, 0:n], tF[j][1])
        B_ = (tB[j][0][:, 0:n], tB[j][1])
        h_stt(k, F_, (pt[0][0][:, 0:n], pt[0][1]), (b3[0][:, 0:1], b3[1]), w_, ALU.add, ALU.mult)
        h_stt(k, B_, (pt[1][0][:, 0:n], pt[1][1]), (b3[0][:, 1:2], b3[1]), w_, ALU.add, ALU.mult)
        lo = 0
        if c == 0:
            h_tt(k, (tF[j][0][:, 0:1], tF[j][1]), (tF[j][0][:, 0:1], tF[j][1]), (tB[j][0][:, 0:1], tB[j][1]), ALU.add)
            lo = 1
        h_act(k, (junk[0][:, 0:n], junk[1]), F_, AF.Square, accum=(part[0][:, 2 * c:2 * c + 1], part[1]))
        h_act(k, (junk[0][:, lo:n], junk[1]), (tB[j][0][:, lo:n], tB[j][1]), AF.Square, accum=(part[0][:, 2 * c + 1:2 * c + 2], part[1]))
        h_copy(k, (oF[j][0][:, 0:n], oF[j][1]), F_)
        h_copy(k, (oB[j][0][:, 0:n], oB[j][1]), B_, eng="gpsimd")
        k.dma("sync", tf_d[:, c0:c0 + n], oF[j][0][:, 0:n], r=[oF[j][1]])
        k.dma("sync", tb_d[:, c0:c0 + n], oB[j][0][:, 0:n], r=[oB[j][1]])
    tot = k.sb("tot", [128, 1], F32)
    k.V(lambda E: E.tensor_reduce(out=tot[0][:], in_=part[0][:], axis=mybir.AxisListType.X, op=ALU.add), [part[1]], [tot[1]])
    k.dma("sync", ssq_d, tot[0][:], r=[tot[1]])
    k.done()
    return nc


def filt_consts(L):
    t_idx = np.arange(L, dtype=np.float32)
    t = (t_idx / np.float32(max(L - 1, 1))).astype(np.float32)
    bands = np.linspace(1e-4, 15, 16, dtype=np.float32)
    ang = (np.float32(2.0 * np.pi) * bands[None, :] * t_idx[:, None] / np.float32(L)).astype(np.float32)
    feats = np.concatenate([t[:, None], np.cos(ang), -np.sin(ang)], -1).astype(np.float32)
    deltas = np.linspace(np.log(1e-2) / 0.3, np.log(1e-2) / 1.5, 512, dtype=np.float32)
    return np.ascontiguousarray(feats.T), t.reshape(1, L).copy(), np.abs(deltas)


def filt_inputs(L, h, prm):
    featsT, tv, adel = filt_consts(L)
    sl = slice(64 * h, 64 * h + 64)
    w3 = prm["hy_w3"]; b3 = prm["hy_b3"]
    cols = []
    for side in range(2):
        for o in range(2):
            cols.append(np.arange(side * 1024 + o * 512 + 64 * h, side * 1024 + o * 512 + 64 * h + 64))
    cols = np.concatenate(cols)
    nd = -np.concatenate([adel[sl], adel[sl]]).reshape(128, 1).astype(np.float32)
    return {"featsT": featsT, "tvals": tv, "w1": prm["hy_w1"], "w2": prm["hy_w2"], "w3c": np.ascontiguousarray(w3[:, cols]),
            "b12": np.stack([prm["hy_b1"], prm["hy_b2"]], 1).astype(np.float32),
            "b3c": np.stack([b3[cols[:128]], b3[cols[128:]]], 1).astype(np.float32), "negdelta": nd}


def toeplitz_src(tapsF, tapsB):
    L = tapsF.shape[1]
    KL = np.zeros((128, 2 * L), tapsF.dtype)
    KL[:, 0:L - 1] = tapsB[:, :0:-1]
    KL[:, L - 1:2 * L - 1] = tapsF
    return KL


def build_hy(nb, T_read=0):
    L = 128 * nb
    W = 128 * (2 * nb - 1)
    nc = bass.Bass("TRN2", target_bir_lowering=False)
    k = KB(nc)
    kl_h = nc.dram_tensor("KL", [128, 2 * L], BF16, kind="ExternalInput")
    ssq_d = k.din("ssqT", [1, 128], F32)
    skip_d = k.din("skipT", [1, 128], F32)
    z_d = k.din("z1", [64, 128, nb], F32)
    x1_d = k.din("x1g", [64, 128, nb], F32)
    x2_d = k.din("x2g", [64, 128, nb], F32)
    sg_d = k.din("sgh", [64, 128, nb], F32)
    J_d = k.din("Jmat", [128, 128], F32)
    y_d = k.dout("yhy", [64, 128, nb], BF16)
    Jf = k.sb("Jf", [128, 128], F32); Jb = k.sb("Jb", [128, 128], BF16)
    nrm = k.sb("nrm", [128, 128], F32); skp = k.sb("skp", [128, 128], F32)
    k.dma("sync", Jf[0][:], J_d, w=[Jf[1]])
    h_copy(k, (Jb[0][:], Jb[1]), (Jf[0][:], Jf[1]))
    k.dma("sync", nrm[0][:], ssq_d.partition_broadcast(128), w=[nrm[1]])
    k.dma("sync", skp[0][:], skip_d.partition_broadcast(128), w=[skp[1]])
    h_act(k, (nrm[0][:], nrm[1]), (nrm[0][:], nrm[1]), AF.Sqrt)
    h_recip(k, (nrm[0][:], nrm[1]), (nrm[0][:], nrm[1]))

    if T_read:
        pp_d = k.din("pp", [64, NPC], F32)
        yf_d = k.din("yf", [64, T_read], F32)
        yb_d = k.din("yb", [64, T_read], F32)
        bo_d = k.din("bonus", [64, T_read], F32)
        sgr_d = k.din("sgr", [64, T_read], F32)
        mr_d = k.dout("mixrw", [64, T_read], BF16)
        pp = k.sb("pp_sb", [64, NPC], F32)
        k.dma("sync", pp[0][:], pp_d, w=[pp[1]])
        o64 = k.sb("o64", [64, 64], F32)
        h_memset(k, (o64[0][:], o64[1]), 1.0 / 64)
        geps = k.sb("geps", [64, 1], F32)
        h_memset(k, (geps[0][:], geps[1]), 64e-5)
        ra = [k.sb("ra%d" % i, [64, 512], F32) for i in range(2)]
        rb = [k.sb("rb%d" % i, [64, 512], F32) for i in range(2)]
        rc = [k.sb("rc%d" % i, [64, 512], F32) for i in range(2)]
        rd = [k.sb("rd%d" % i, [64, 512], F32) for i in range(2)]
        r1 = k.sb("r1", [64, 512], F32); r2 = k.sb("r2", [64, 512], F32)
        rob = [k.sb("rob%d" % i, [64, 512], BF16) for i in range(2)]
        pr = [k.ps("pr%d" % i, [64, 512], F32) for i in range(2)]
        for ci, c0 in enumerate(range(0, T_read, 512)):
            n = min(512, T_read - c0)
            j = ci % 2
            A = (ra[j][0][:, 0:n], ra[j][1]); B = (rb[j][0][:, 0:n], rb[j][1])
            C = (rc[j][0][:, 0:n], rc[j][1]); D = (rd[j][0][:, 0:n], rd[j][1])
            R1 = (r1[0][:, 0:n], r1[1]); R2 = (r2[0][:, 0:n], r2[1])
            P0 = (pr[0][0][:, 0:n], pr[0][1]); P1 = (pr[1][0][:, 0:n], pr[1][1])
            k.dma("scalar", A[0], yf_d[:, c0:c0 + n], w=[A[1]])
            k.dma("scalar", B[0], yb_d[:, c0:c0 + n], w=[B[1]])
            k.dma("scalar", C[0], bo_d[:, c0:c0 + n], w=[C[1]])
            k.dma("scalar", D[0], sgr_d[:, c0:c0 + n], w=[D[1]])
            h_tt(k, A, A, B, ALU.add)
            h_mm(k, P0, (o64[0][:], o64[1]), A)
            h_tt(k, R1, A, P0, ALU.subtract)
            h_tt(k, R2, R1, R1, ALU.mult)
            h_mm(k, P1, (o64[0][:], o64[1]), R2)
            h_act(k, R2, P1, AF.Sqrt, bias=(geps[0][:], geps[1]))
            h_recip(k, R2, R2)
            h_tt(k, R1, R1, R2, ALU.mult)
            h_ts(k, R1, R1, (pp[0][:, PC["gn_w"]:PC["gn_w"] + 1], pp[1]), (pp[0][:, PC["gn_b"]:PC["gn_b"] + 1], pp[1]), ALU.mult, ALU.add)
            h_tt(k, R1, R1, C, ALU.add)
            OB = (rob[j][0][:, 0:n], rob[j][1])
            h_tt(k, OB, R1, D, ALU.mult)
            k.dma("scalar", mr_d[:, c0:c0 + n], OB[0], r=[OB[1]])

    ksh = [k.sb("ksh%d" % i, [128, W], BF16) for i in range(2)]
    zt = [k.sb("zt%d" % i, [128, nb], F32) for i in range(2)]
    x1t = [k.sb("x1t%d" % i, [128, nb], F32) for i in range(2)]
    x2t = [k.sb("x2t%d" % i, [128, nb], F32) for i in range(2)]
    sgt = [k.sb("sgt%d" % i, [128, nb], F32) for i in range(2)]
    zb = k.sb("zb", [128, nb], BF16)
    zf = k.sb("zf", [128, nb], BF16)
    z2 = k.sb("z2", [128, nb], F32)
    t1 = k.sb("t1", [128, nb], F32)
    ot = [k.sb("oth%d" % i, [128, nb], BF16) for i in range(2)]
    pf = k.ps("pf", [128, nb], F32)
    py = [k.ps("pyc%d" % i, [128, nb], F32) for i in range(2)]
    order = [0] + [d for d in range(-(nb - 1), nb) if d != 0]
    cnt = 0
    for c in range(64):
        j = c % 2
        Z = (zt[j][0][:], zt[j][1]); X1 = (x1t[j][0][:], x1t[j][1]); X2 = (x2t[j][0][:], x2t[j][1]); SG = (sgt[j][0][:], sgt[j][1])
        k.dma("gpsimd", Z[0], z_d[c], w=[Z[1]])
        k.dma("gpsimd", X1[0], x1_d[c], w=[X1[1]])
        k.dma("gpsimd", X2[0], x2_d[c], w=[X2[1]])
        k.dma("gpsimd", SG[0], sg_d[c], w=[SG[1]])
        zin = Z
        for o in range(2):
            row = o * 64 + c
            kt, kb_ = ksh[cnt % 2]
            q = "sync" if cnt % 2 == 0 else "scalar"
            cnt += 1
            src = bass.AP(kl_h, row * 2 * L, [[1, 128], [1, W]])
            k.dma(q, kt[:], src, w=[kb_])
            h_copy(k, (zb[0][:], zb[1]), zin, eng="gpsimd")
            h_mm(k, (pf[0][:], pf[1]), (Jb[0][:], Jb[1]), (zb[0][:], zb[1]))
            h_copy(k, (zf[0][:], zf[1]), (pf[0][:], pf[1]), eng="scalar")
            pyt, pyb = py[o]
            for ii, Dd in enumerate(order):
                e = Dd + nb - 1
                S0, S1 = max(0, -Dd), min(nb, nb - Dd)
                k.T(lambda E, pyt=pyt, kt=kt, e=e, S0=S0, S1=S1, Dd=Dd, ii=ii: E.matmul(pyt[:, S0 + Dd:S1 + Dd], kt[:, 128 * e:128 * e + 128], zf[0][:, S0:S1], start=(ii == 0), stop=(ii == len(order) - 1)),
                    [kb_, zf[1]], [pyb])
            ncol = (nrm[0][:, row:row + 1], nrm[1])
            scol = (skp[0][:, row:row + 1], skp[1])
            h_ts(k, (t1[0][:], t1[1]), (pyt[:], pyb), ncol, None, ALU.mult)
            h_stt(k, (t1[0][:], t1[1]), zin, scol, (t1[0][:], t1[1]), ALU.mult, ALU.add)
            if o == 0:
                h_tt(k, (z2[0][:], z2[1]), (t1[0][:], t1[1]), X1, ALU.mult)
                zin = (z2[0][:], z2[1])
            else:
                O_ = (ot[j][0][:], ot[j][1])
                h_tt(k, (t1[0][:], t1[1]), (t1[0][:], t1[1]), X2, ALU.mult)
                h_tt(k, O_, (t1[0][:], t1[1]), SG, ALU.mult, eng="gpsimd")
                k.dma("gpsimd", y_d[c], O_[0], r=[O_[1]])
    k.done()
    return nc


import numpy as np


def rfeat_inputs(u_ctx, u_lat, h, prm):
    n = u_lat.shape[0]
    def fm_pad(c0, w=64):
        a = np.zeros((w, 256 + n + 4), np.float32)
        a[:, 1:257] = u_ctx[:, c0:c0 + w].T
        a[:, 259:259 + n] = u_lat[:, c0:c0 + w].T
        return a
    cols = {"r": 64 * h, "k": 512 + 64 * h, "v": 1024 + 64 * h, "g_rw": 1664 + 64 * h,
            "hv": 2176 + 64 * h, "hx1": 2688 + 64 * h, "hx2": 3200 + 64 * h, "g_hy": 3712 + 64 * h}
    fin = np.stack([fm_pad(cols[nm]) for nm in FI], 0)
    lora = fm_pad(1536, 128)
    mu = prm["rwkv_mu"]
    sl = slice(64 * h, 64 * h + 64)
    pp = np.zeros((64, NPC), np.float32)
    pp[:, PC["mu_r"]] = mu[sl]
    pp[:, PC["mu_k"]] = mu[512 + 64 * h:512 + 64 * h + 64]
    pp[:, PC["mu_v"]] = mu[1024 + 64 * h:1024 + 64 * h + 64]
    pp[:, PC["k_k"]] = prm["rwkv_k_k"][sl]
    pp[:, PC["k_a"]] = prm["rwkv_k_a"][sl]
    pp[:, PC["w0_f"]] = prm["rwkv_w0"][0][sl]
    pp[:, PC["w0_b"]] = prm["rwkv_w0"][1][sl]
    pp[:, PC["a0_f"]] = prm["rwkv_a0"][0][sl]
    pp[:, PC["a0_b"]] = prm["rwkv_a0"][1][sl]
    pp[:, PC["r_k"]] = prm["rwkv_r_k"][h]
    for ai, nm in enumerate(("v", "x1", "x2")):
        for t in range(3):
            pp[:, PC["t%d_%s" % (t, nm)]] = prm["hy_short"][t][512 * ai + 64 * h:512 * ai + 64 * h + 64]
    pp[:, PC["gn_w"]] = prm["rwkv_gn_w"][sl]
    pp[:, PC["gn_b"]] = prm["rwkv_gn_b"][sl]
    pp[:, PC["skip0"]] = prm["hy_skip"][0][sl]
    pp[:, PC["skip1"]] = prm["hy_skip"][1][sl]
    wx = np.zeros((128, 4, 64), np.float32)
    wx[0:32, 0] = prm["rwkv_w_up"][0][:, sl]
    wx[32:64, 1] = prm["rwkv_w_up"][1][:, sl]
    wx[64:96, 2] = prm["rwkv_a_up"][0][:, sl]
    wx[96:128, 3] = prm["rwkv_a_up"][1][:, sl]
    return {"fin": fin, "lora": lora, "pp": pp, "mu_lora": mu[1536:1664].reshape(128, 1).copy(), "wx": wx}


import numpy as np

TC = 16


def h_tr(k, out, a, ident):
    oa, ob = out; aa, ab = a; ia, ib = ident
    return k.P.op("tensor", lambda E: E.transpose(oa, aa, ia), [ab, ib], [ob])


def build_fused(n):
    nlb = n // 128
    NKB = nlb + 2
    NTK = NKB * 128
    T = 256 + n
    TP = T + 4
    nc = bass.Bass("TRN2", target_bir_lowering=False)
    k = KB(nc)
    P = k.P
    plan, variants = na_plan(nlb)
    nvar = len(variants)

    def scratch(name, shape, dt):
        return nc.dram_tensor(name, list(shape), dt).ap()

    x_d = k.din("x", [n, D], F32)
    ctx_d = k.din("ctx", [256, D], F32)
    ccols_d = k.din("c_cols", [128, 16], F32)
    ident_d = k.din("ident", [128, 128], F32)
    J_d = k.din("Jmat", [128, 128], F32)
    fng_d = k.din("final_norm", [1, D], F32)
    L_in = []
    for l in range(4):
        attn = l % 2 == 0
        d = {"norm_g": k.din("norm_g%d" % l, [1, D], F32), "ada_w": k.din("ada_w%d" % l, [D, 3 * D], F32),
             "ada_b": k.din("ada_b%d" % l, [1, 3 * D], F32), "w_in": k.din("w_in%d" % l, [D, 512 if attn else 640], F32),
             "w_out": k.din("w_out%d" % l, [D, D], F32)}
        if attn:
            d["biasT"] = k.din("biasT%d" % l, [128, nvar, 128], F32)
            d["gains"] = k.din("gains%d" % l, [64, 2], F32)
        else:
            d["pp"] = k.din("pp%d" % l, [64, NPC], F32)
            d["mu_lora"] = k.din("mu_lora%d" % l, [128, 1], F32)
            d["wx"] = k.din("wx%d" % l, [128, 4, 64], F32)
            d["hw1"] = k.din("hw1_%d" % l, [33, 64], F32)
            d["hw2"] = k.din("hw2_%d" % l, [64, 64], F32)
            d["hw3"] = k.din("hw3_%d" % l, [64, 256], F32)
            d["hb12"] = k.din("hb12_%d" % l, [64, 2], F32)
            d["hb3"] = k.din("hb3_%d" % l, [128, 2], F32)
            d["skipT"] = k.din("skipT%d" % l, [1, 128], F32)
        L_in.append(d)
    ropeC_d = k.din("ropeC", [64, NTK], F32)
    ropeS_d = k.din("ropeS", [64, NTK], F32)
    rmat_d = k.din("rmatT", [64, 64], F32)
    sel_d = k.din("sel", [65, 64], F32)
    fconst = {}
    for Lf in (n, 256):
        fconst[Lf] = {"featsT": k.din("featsT%d" % Lf, [33, Lf], F32), "featsTr": k.din("featsTr%d" % Lf, [33, Lf], F32),
                      "tv": k.din("tv%d" % Lf, [1, Lf], F32), "tvr": k.din("tvr%d" % Lf, [1, Lf], F32)}
    nd_d = k.din("negdelta", [128, 1], F32)
    y_out = k.dout("y_out", [n, D], F32)

    xres = scratch("xres", [NTK, D], F32)
    gates_s = scratch("gates_s", [128, 2, D], F32)
    mix_send = scratch("mix_send", [128, NTK], BF16)
    mix_all = scratch("mix_all", [1024, NTK], BF16)
    A_s = {nm: scratch("a_" + nm, [64, NTK], BF16) for nm in ("qaT", "kaT", "gaT", "qbT", "kbT", "gbT")}
    va_s = scratch("va_s", [128, NKB, 65], BF16)
    vb_s = scratch("vb_s", [128, NKB, 65], BF16)
    fin_s = scratch("fin_s", [NFI, 64, TP], F32)
    lora_s = scratch("lora_s", [128, TP], F32)
    fo_s = scratch("fo_s", [NFO, 64, T], F32)
    bc_s = scratch("bc_s", [2, T, 320], F32)
    vT2_s = scratch("vT2_s", [128, T], F32)
    yT2_s = scratch("yT2_s", [128, T], F32)
    kl_h = {Lf: nc.dram_tensor("KL%d" % Lf, [128, 2 * Lf], BF16) for Lf in (n, 256)}
    ssq_s = {Lf: scratch("ssq%d" % Lf, [1, 128], F32) for Lf in (n, 256)}

    identf = k.sb("identf", [128, 128], F32)
    identb = k.sb("identb", [128, 128], BF16)
    Jf = k.sb("Jf", [128, 128], F32)
    Jb = k.sb("Jb", [128, 128], BF16)
    zer = k.sb("zer", [128, 8], F32)
    k.dma("sync", identf[0][:], ident_d, w=[identf[1]])
    k.dma("sync", Jf[0][:], J_d, w=[Jf[1]])
    h_copy(k, (identb[0][:], identb[1]), (identf[0][:], identf[1]))
    h_copy(k, (Jb[0][:], Jb[1]), (Jf[0][:], Jf[1]))
    h_memset(k, (zer[0][:], zer[1]), 0.0)
    ID = (identf[0][:], identf[1])
    IDB = (identb[0][:], identb[1])
    for col in (0, 257, 258, TP - 1):
        for i in range(NFI):
            k.dma("sync", fin_s[i, :, col:col + 1], zer[0][0:64, 0:1], r=[zer[1]], allow_slow_non_contiguous=True)
        k.dma("sync", lora_s[:, col:col + 1], zer[0][:, 0:1], r=[zer[1]], allow_slow_non_contiguous=True)

    def phase_tok(l, final=False):
        has_prev = l > 0
        attn = (l % 2 == 0) and not final
        k.begin_phase()
        eps_t = k.sb("eps_t", [128, 1], F32)
        h_memset(k, (eps_t[0][:], eps_t[1]), EPS)
        if has_prev:
            wo = k.sb("wo", [128, 8, D], BF16)
            for kc in range(8):
                k.dma("gpsimd", wo[0][:, kc, :], L_in[l - 1]["w_out"][kc * 128:(kc + 1) * 128, :], w=[wo[1]])
            gp = k.sb("gp", [128, 2, D], F32)
            k.dma("sync", gp[0][:], gates_s, w=[gp[1]])
        if final:
            fg = k.sb("fg", [128, D], F32)
            k.dma("sync", fg[0][:], fng_d.partition_broadcast(128), w=[fg[1]])
        else:
            Li = L_in[l]
            ncol = 512 if attn else 640
            wi = k.sb("wi", [128, 8, ncol], BF16)
            for kc in range(8):
                k.dma("gpsimd", wi[0][:, kc, :], Li["w_in"][kc * 128:(kc + 1) * 128, :], w=[wi[1]])
            mod = k.sb("mod", [128, 2, 3 * D], F32)
            Gt = k.sb("Gt", [128, 2, D], F32)
            k.begin_phase()
            cc = k.sb("cc", [128, 16], F32)
            rep = k.sb("rep", [128, 16, 128], F32)
            ones = k.sb("ones", [128, 128], F32)
            ab = k.sb("ab", [128, 3 * D], F32)
            ng = k.sb("ng", [128, D], F32)
            aw = [k.sb("aw%d" % i, [128, 8, 512], F32) for i in range(2)]
            pa = [k.ps("pa%d" % i, [128, 512], F32) for i in range(2)]
            k.dma("sync", cc[0][:], ccols_d, w=[cc[1]])
            k.dma("sync", ab[0][:], Li["ada_b"].partition_broadcast(128), w=[ab[1]])
            k.dma("sync", ng[0][:], Li["norm_g"].partition_broadcast(128), w=[ng[1]])
            h_act(k, (cc[0][:], cc[1]), (cc[0][:], cc[1]), AF.Silu)
            h_memset(k, (ones[0][:], ones[1]), 1.0)
            for j in range(16):
                h_ts(k, (rep[0][:, j, :], rep[1]), (ones[0][:], ones[1]), (cc[0][:, j:j + 1], cc[1]), None, ALU.mult)
            for g in range(6):
                awt, awb = aw[g % 2]
                k.dma("sync", awt[:], Li["ada_w"][:, g * 512:(g + 1) * 512].rearrange("(k p) n -> p k n", p=128), w=[awb])
                for v in range(2):
                    for kc in range(8):
                        h_mm(k, (pa[v][0][:], pa[v][1]), (rep[0][:, v * 8 + kc, :], rep[1]), (awt[:, kc, :], awb), start=(kc == 0), stop=(kc == 7))
                    h_tt(k, (mod[0][:, v, g * 512:(g + 1) * 512], mod[1]), (pa[v][0][:], pa[v][1]), (ab[0][:, g * 512:(g + 1) * 512], ab[1]), ALU.add)
            for v in range(2):
                h_stt(k, (Gt[0][:, v, :], Gt[1]), (mod[0][:, v, D:2 * D], mod[1]), 1.0, (ng[0][:], ng[1]), ALU.add, ALU.mult)
            k.end_phase()
        xt = [k.sb("xt%d" % i, [128, D], F32) for i in range(2)]
        tmp = [k.sb("tmp%d" % i, [128, D], F32) for i in range(2)]
        ss = [k.sb("ss%d" % i, [128, 4], F32) for i in range(2)]
        junk = k.sb("junk", [128, D], BF16)
        if has_prev:
            mt = [k.sb("mt%d" % i, [128, 8, 128], BF16) for i in range(2)]
            py = [k.ps("py%d" % i, [128, 512], F32) for i in range(2)]
        if not final:
            xm = [k.sb("xm%d" % i, [128, D], BF16) for i in range(2)]
            xmT = [k.sb("xmT%d" % i, [128, 8, 512], BF16) for i in range(2)]
            ptr = [k.ps("ptr%d" % i, [128, 8, 128], BF16) for i in range(2)]
            pu = [k.ps("pu%d" % i, [128, 512], F32) for i in range(2)]
            odt = BF16 if attn else F32
            ut = [k.sb("ut%d" % i, [128, 512], odt) for i in range(3)]
            if attn:
                vt = [k.sb("vt%d" % i, [128, 2, 65], BF16) for i in range(2)]
                for i in range(2):
                    h_memset(k, (vt[i][0][:], vt[i][1]), 1.0)
        bcount = 0
        ucount = 0
        nsb = (NTK + 511) // 512
        for sbi in range(nsb):
            t0 = sbi * 512
            ntok = min(512, NTK - t0)
            nblk = ntok // 128
            is_ctx = t0 >= n
            v = 1 if is_ctx else 0
            if not final:
                xTt, xTb = xmT[sbi % 2]
            for blk in range(nblk):
                b = t0 // 128 + blk
                X = (xt[bcount % 2][0][:], xt[bcount % 2][1])
                TT = (tmp[bcount % 2][0][:], tmp[bcount % 2][1])
                sst, ssb = ss[bcount % 2]
                if l <= 1:
                    src = ctx_d[(b - nlb) * 128:(b - nlb + 1) * 128, :] if is_ctx else x_d[b * 128:(b + 1) * 128, :]
                else:
                    src = xres[b * 128:(b + 1) * 128, :]
                k.dma("sync", X[0], src, w=[X[1]])
                if has_prev:
                    mtt, mtb = mt[bcount % 2]
                    k.dma("sync", mtt[:], mix_all[:, b * 128:(b + 1) * 128].rearrange("(k p) t -> p k t", p=128), w=[mtb])
                    for hh in range(2):
                        for kc in range(8):
                            h_mm(k, (py[hh][0][:], py[hh][1]), (mtt[:, kc, :], mtb), (wo[0][:, kc, hh * 512:(hh + 1) * 512], wo[1]), start=(kc == 0), stop=(kc == 7))
                        h_tt(k, (tmp[bcount % 2][0][:, hh * 512:(hh + 1) * 512], TT[1]), (py[hh][0][:], py[hh][1]), (gp[0][:, v, hh * 512:(hh + 1) * 512], gp[1]), ALU.mult)
                    h_tt(k, X, TT, X, ALU.add, eng="gpsimd")
                    if not final:
                        k.dma("sync", xres[b * 128:(b + 1) * 128, :], X[0], r=[X[1]])
                h_act(k, (junk[0][:], junk[1]), X, AF.Square, accum=(sst[:, 0:1], ssb))
                h_act(k, (sst[:, 1:2], ssb), (sst[:, 0:1], ssb), AF.Sqrt, scale=1.0 / D, bias=(eps_t[0][:], eps_t[1]))
                h_recip(k, (sst[:, 2:3], ssb), (sst[:, 1:2], ssb))
                if final:
                    if not is_ctx:
                        h_stt(k, TT, X, (sst[:, 2:3], ssb), (fg[0][:], fg[1]), ALU.mult, ALU.mult)
                        k.dma("sync", y_out[b * 128:(b + 1) * 128, :], TT[0], r=[TT[1]])
                    bcount += 1
                    continue
                XM = (xm[bcount % 2][0][:], xm[bcount % 2][1])
                h_stt(k, TT, X, (sst[:, 2:3], ssb), (Gt[0][:, v, :], Gt[1]), ALU.mult, ALU.mult)
                h_tt(k, XM, TT, (mod[0][:, v, 0:D], mod[1]), ALU.add, eng="gpsimd")
                ptt, ptb = ptr[bcount % 2]
                for kc in range(8):
                    h_tr(k, (ptt[:, kc, :], ptb), (xm[bcount % 2][0][:, kc * 128:(kc + 1) * 128], XM[1]), IDB)
                h_copy(k, (xTt[:, :, blk * 128:(blk + 1) * 128], xTb), (ptt[:], ptb), eng="scalar")
                if attn:
                    put, pub = pu[ucount % 2]
                    vtt, vtb = vt[ucount % 2]
                    ucount += 1
                    for kc in range(8):
                        h_mm(k, (put[:, 0:128], pub), (xTt[:, kc, blk * 128:(blk + 1) * 128], xTb), (wi[0][:, kc, 384:512], wi[1]), start=(kc == 0), stop=(kc == 7))
                    h_copy(k, (vtt[:, :, 0:64], vtb), (put[:, 0:128].rearrange("p (a c) -> p a c", a=2), pub))
                    k.dma("sync", va_s[:, b, :], vtt[:, 0, :], r=[vtb])
                    k.dma("sync", vb_s[:, b, :], vtt[:, 1, :], r=[vtb])
                bcount += 1
            if final:
                continue
            ncg = 3 if attn else 5
            for cg in range(ncg):
                put, pub = pu[ucount % 2]
                utt, utb = ut[ucount % 3]
                ucount += 1
                for kc in range(8):
                    h_mm(k, (put[:, 0:ntok], pub), (wi[0][:, kc, cg * 128:(cg + 1) * 128], wi[1]), (xTt[:, kc, 0:ntok], xTb), start=(kc == 0), stop=(kc == 7))
                h_copy(k, (utt[:, 0:ntok], utb), (put[:, 0:ntok], pub), eng=("vector" if cg % 2 == 0 else "scalar"))
                if attn:
                    names = [("qaT", "kaT"), ("gaT", "qbT"), ("kbT", "gbT")][cg]
                    k.dma("sync", A_s[names[0]][:, t0:t0 + ntok], utt[0:64, 0:ntok], r=[utb])
                    k.dma("sync", A_s[names[1]][:, t0:t0 + ntok], utt[64:128, 0:ntok], r=[utb])
                else:
                    pc = (1 + (t0 - n)) if is_ctx else (259 + t0)
                    if cg < 4:
                        k.dma("sync", fin_s[2 * cg, :, pc:pc + ntok], utt[0:64, 0:ntok], r=[utb])
                        k.dma("sync", fin_s[2 * cg + 1, :, pc:pc + ntok], utt[64:128, 0:ntok], r=[utb])
                    else:
                        k.dma("sync", lora_s[:, pc:pc + ntok], utt[:, 0:ntok], r=[utb])
        if not final:
            k.dma("sync", gates_s, mod[0][:, :, 2 * D:3 * D], r=[mod[1]])
        k.end_phase()

    def phase_attn(l):
        Li = L_in[l]
        k.begin_phase()
        qT = k.sb("qT", [64, NTK], BF16); kT = k.sb("kT", [64, NTK], BF16)
        vE = k.sb("vE", [128, NKB, 65], BF16)
        biasf = k.sb("biasf", [128, nvar, 128], F32); biasb = k.sb("biasb", [128, nvar, 128], BF16)
        sel = k.sb("sel_sb", [65, 64], F32); gains = k.sb("gains_sb", [64, 2], F32)
        rmf = k.sb("rmf", [64, 64], F32); rmb = k.sb("rmb", [64, 64], BF16)
        ones64 = k.sb("ones64", [64, 64], F32); eps_t = k.sb("eps_a", [64, 1], F32)
        k.dma("sync", biasf[0][:], Li["biasT"], w=[biasf[1]])
        k.dma("sync", sel[0][:], sel_d, w=[sel[1]])
        k.dma("sync", gains[0][:], Li["gains"], w=[gains[1]])
        k.dma("sync", rmf[0][:], rmat_d, w=[rmf[1]])
        h_copy(k, (biasb[0][:], biasb[1]), (biasf[0][:], biasf[1]))
        h_copy(k, (rmb[0][:], rmb[1]), (rmf[0][:], rmf[1]))
        h_memset(k, (ones64[0][:], ones64[1]), 1.0 / 64)
        h_memset(k, (eps_t[0][:], eps_t[1]), EPS)
        h_ts(k, (gains[0][:, 0:1], gains[1]), (gains[0][:, 0:1], gains[1]), 0.125, None, ALU.mult)
        pS = [k.ps("pS%d" % i, [128, 512], F32) for i in range(4)]
        pacc = [k.ps("pacc%d" % i, [65, 512], F32) for i in range(2)]
        pmisc = [k.ps("pm%d" % i, [64, 512], F32) for i in range(2)]
        PT = [k.sb("PT%d" % i, [128, 512], BF16) for i in range(4)]
        accs = [k.sb("accs%d" % i, [65, 512], F32) for i in range(2)]
        gt = [k.sb("gt%d" % i, [64, 512], BF16) for i in range(2)]
        w1 = [k.sb("w1_%d" % i, [64, 512], F32) for i in range(2)]
        w2 = [k.sb("w2_%d" % i, [64, 512], F32) for i in range(2)]
        ot = [k.sb("ot%d" % i, [64, 512], BF16) for i in range(2)]
        cin = [k.sb("cin%d" % i, [64, 512], BF16) for i in range(2)]
        ctab = [k.sb("ctab%d" % i, [64, 512], F32) for i in range(2)]
        stab = [k.sb("stab%d" % i, [64, 512], F32) for i in range(2)]
        qnb = [k.sb("qnb%d" % i, [64, 512], BF16) for i in range(2)]
        st = {"fin": 0, "S": 0, "pre": 0}

        def finalize(pa, g_d, row0, c0, nn):
            i = st["fin"] % 2
            st["fin"] += 1
            at = (accs[i][0][:, 0:nn], accs[i][1]); G = (gt[i][0][:, 0:nn], gt[i][1])
            a1 = (w1[i][0][:, 0:nn], w1[i][1]); a2 = (w2[i][0][:, 0:nn], w2[i][1])
            o = (ot[i][0][:, 0:nn], ot[i][1]); pm = (pmisc[i][0][:, 0:nn], pmisc[i][1])
            k.dma("sync", G[0], g_d[:, c0:c0 + nn], w=[G[1]])
            h_copy(k, at, (pa[0][:, 0:nn], pa[1]), eng="scalar")
            h_mm(k, pm, (sel[0][:], sel[1]), at)
            h_recip(k, a1, pm)
            h_tt(k, a1, a1, (accs[i][0][0:64, 0:nn], accs[i][1]), ALU.mult)
            h_silu(k, a2, G, a2)
            h_tt(k, o, a1, a2, ALU.mult)
            k.dma("sync", mix_send[row0:row0 + 64, c0:c0 + nn], o[0], r=[o[1]])

        def attend(qc0, nn, kbs, pa):
            pend = []
            nk = len(kbs)

            def pv(j, kbj, pj):
                h_mm(k, (pa[0][:, 0:nn], pa[1]), (vE[0][:, kbj, :], vE[1]), pj, start=(j == 0), stop=(j == nk - 1))
            for idx, kb in enumerate(kbs):
                s = st["S"]
                st["S"] += 1
                psx = (pS[s % 4][0][:, 0:nn], pS[s % 4][1])
                ptx = (PT[s % 4][0][:, 0:nn], PT[s % 4][1])
                h_mm(k, psx, (kT[0][:, kb * 128:(kb + 1) * 128], kT[1]), (qT[0][:, qc0:qc0 + nn], qT[1]))
                h_act(k, ptx, psx, AF.Exp)
                pend.append((idx, kb, ptx))
                if len(pend) > 2:
                    pv(*pend.pop(0))
            for it in pend:
                pv(*it)

        k.dma("sync", kT[0][:], A_s["kaT"], w=[kT[1]])
        k.dma("sync", vE[0][:], va_s, w=[vE[1]])
        k.dma("sync", qT[0][:], A_s["qaT"], w=[qT[1]])
        h_ts(k, (qT[0][:], qT[1]), (qT[0][:], qT[1]), 0.125, None, ALU.mult)
        ctxk = [nlb, nlb + 1]
        acc_i = 0
        for g0 in range(0, nlb, 4):
            pa = pacc[acc_i % 2]
            acc_i += 1
            nq = min(4, nlb - g0)
            for mi in range(nq):
                m = g0 + mi
                lst = plan[m]
                nreg = len(lst) + 2
                s = st["S"]
                st["S"] += 2
                banks = [(pS[s % 4], PT[s % 4]), (pS[(s + 1) % 4], PT[(s + 1) % 4])]
                regs = []
                for j, (kb, var) in enumerate(lst + [(ctxk[0], None), (ctxk[1], None)]):
                    (pt_, pb_), (tt_, tb_) = banks[j // 4]
                    jj = j % 4
                    h_mm(k, (pt_[:, jj * 128:(jj + 1) * 128], pb_), (kT[0][:, kb * 128:(kb + 1) * 128], kT[1]), (qT[0][:, m * 128:(m + 1) * 128], qT[1]), start=True, stop=(var is None))
                    if var is not None:
                        h_mm(k, (pt_[:, jj * 128:(jj + 1) * 128], pb_), IDB, (biasb[0][:, var, :], biasb[1]), start=False, stop=True)
                    regs.append((kb, tt_, tb_, jj))
                nA = min(4, nreg)
                h_act(k, (banks[0][1][0][:, 0:nA * 128], banks[0][1][1]), (banks[0][0][0][:, 0:nA * 128], banks[0][0][1]), AF.Exp)
                if nreg > 4:
                    nB = nreg - 4
                    h_act(k, (banks[1][1][0][:, 0:nB * 128], banks[1][1][1]), (banks[1][0][0][:, 0:nB * 128], banks[1][0][1]), AF.Exp)
                for j, (kb, tt_, tb_, jj) in enumerate(regs):
                    h_mm(k, (pa[0][:, mi * 128:(mi + 1) * 128], pa[1]), (vE[0][:, kb, :], vE[1]), (tt_[:, jj * 128:(jj + 1) * 128], tb_), start=(j == 0), stop=(j == nreg - 1))
            finalize(pa, A_s["gaT"], 0, g0 * 128, nq * 128)
        pa = pacc[acc_i % 2]
        acc_i += 1
        attend(nlb * 128, 256, ctxk, pa)
        finalize(pa, A_s["gaT"], 0, nlb * 128, 256)
        k.dma("sync", vE[0][:], vb_s, w=[vE[1]])

        def pre_chunk(src_d, dst, gcol, c0):
            nn = min(512, NTK - c0)
            i = st["pre"] % 2
            st["pre"] += 1
            ci = (cin[i][0][:, 0:nn], cin[i][1]); ct = (ctab[i][0][:, 0:nn], ctab[i][1]); sn = (stab[i][0][:, 0:nn], stab[i][1])
            qn = (qnb[i][0][:, 0:nn], qnb[i][1]); a1 = (w1[i][0][:, 0:nn], w1[i][1]); a2 = (w2[i][0][:, 0:nn], w2[i][1])
            pm = (pmisc[i][0][:, 0:nn], pmisc[i][1])
            k.dma("sync", ci[0], src_d[:, c0:c0 + nn], w=[ci[1]])
            k.dma("sync", ct[0], ropeC_d[:, c0:c0 + nn], w=[ct[1]])
            k.dma("sync", sn[0], ropeS_d[:, c0:c0 + nn], w=[sn[1]])
            h_tt(k, a1, ci, ci, ALU.mult)
            h_mm(k, pm, (ones64[0][:], ones64[1]), a1)
            h_act(k, a2, pm, AF.Sqrt, bias=(eps_t[0][:], eps_t[1]))
            h_recip(k, a2, a2)
            h_stt(k, qn, ci, (gains[0][:, gcol:gcol + 1], gains[1]), a2, ALU.mult, ALU.mult)
            h_mm(k, pm, (rmb[0][:], rmb[1]), qn)
            h_tt(k, a1, qn, ct, ALU.mult)
            h_tt(k, a2, pm, sn, ALU.mult)
            h_tt(k, (dst[0][:, c0:c0 + nn], dst[1]), a1, a2, ALU.add, eng="gpsimd")
        for c0 in range(0, NTK, 512):
            pre_chunk(A_s["kbT"], kT, 1, c0)
        for c0 in range(0, NTK, 512):
            pre_chunk(A_s["qbT"], qT, 0, c0)
        allk = list(range(NKB))
        for c0 in range(0, nlb * 128, 512):
            nn = min(512, nlb * 128 - c0)
            pa = pacc[acc_i % 2]
            acc_i += 1
            attend(c0, nn, allk, pa)
            finalize(pa, A_s["gbT"], 64, c0, nn)
        pa = pacc[acc_i % 2]
        acc_i += 1
        attend(nlb * 128, 256, ctxk, pa)
        finalize(pa, A_s["gbT"], 64, nlb * 128, 256)
        k.end_phase()

    def phase_rfeat(l):
        Li = L_in[l]
        k.begin_phase()
        pp = k.sb("pp_sb", [64, NPC], F32); npp = k.sb("npp_sb", [64, NPC], F32)
        mul = k.sb("mul_sb", [128, 1], F32); wx = k.sb("wx_sb", [128, 4, 64], F32)
        ones = k.sb("ones_sb", [64, 64], F32); tiny = k.sb("tiny_sb", [64, 1], F32)
        k.dma("sync", pp[0][:], Li["pp"], w=[pp[1]])
        k.dma("sync", mul[0][:], Li["mu_lora"], w=[mul[1]])
        k.dma("sync", wx[0][:], Li["wx"], w=[wx[1]])
        h_memset(k, (ones[0][:], ones[1]), 1.0)
        h_memset(k, (tiny[0][:], tiny[1]), 1e-12)
        h_ts(k, (npp[0][:], npp[1]), (pp[0][:], pp[1]), -1.0, None, ALU.mult)
        h_ts(k, (npp[0][:, PC["r_k"]:PC["r_k"] + 1], npp[1]), (pp[0][:, PC["r_k"]:PC["r_k"] + 1], pp[1]), 0.5, None, ALU.mult)

        def pcol(name):
            return (pp[0][:, PC[name]:PC[name] + 1], pp[1])

        def ncol(name):
            return (npp[0][:, PC[name]:PC[name] + 1], npp[1])
        fin = [[k.sb("fin%d_%d" % (i, j), [64, 514], F32) for j in range(2)] for i in range(NFI)]
        lor = [k.sb("lor%d" % j, [128, 514], F32) for j in range(2)]
        fo = [[k.sb("fo%d_%d" % (i, j), [64, 512], F32) for j in range(2)] for i in range(NFO)]
        tm = [k.sb("tm%d" % i, [64, 512], F32) for i in range(8)]
        lt = [k.sb("lt%d" % i, [128, 512], F32) for i in range(3)]
        pz = [k.ps("pz%d" % i, [64, 512], F32) for i in range(4)]
        ptk = [k.ps("ptk%d" % i, [128, 16, 64], F32) for i in range(1)]
        pfl = k.ps("pfl", [128, 512], F32)
        tok = [k.sb("tok%d" % i, [128, 9, 64], F32) for i in range(2)]
        tokb = [k.sb("tokb%d" % i, [128, 320], F32) for i in range(2)]
        vrev = [k.sb("vrev%d" % i, [64, 128], F32) for i in range(2)]
        tord = ["w_f", "nb_f", "k_f", "kkn", "r", "w_b", "nb_b", "k_b", "v"]
        blkc = [0]

        def chunk(ci, pc0, oc0, nn):
            j = ci % 2

            def I(name, lo=1):
                t, b = fin[FI[name]][j]
                return (t[:, lo:lo + nn], b)

            def O(name):
                t, b = fo[FO[name]][j]
                return (t[:, 0:nn], b)

            def Tm(i):
                return (tm[i][0][:, 0:nn], tm[i][1])

            def Lt(i):
                return (lt[i][0][:, 0:nn], lt[i][1])

            def Pz(i):
                return (pz[i][0][:, 0:nn], pz[i][1])
            for name, i in FI.items():
                t, b = fin[i][j]
                k.dma("sync", t[:, 0:nn + 2], fin_s[i, :, pc0:pc0 + nn + 2], w=[b])
            lt_, lb_ = lor[j]
            k.dma("sync", lt_[:, 0:nn + 2], lora_s[:, pc0:pc0 + nn + 2], w=[lb_])

            def shift(out, lo, mid, hi_, mu, t):
                h_tt(k, t, lo, hi_, ALU.add)
                h_stt(k, t, t, 0.5, mid, ALU.mult, ALU.subtract)
                h_stt(k, out, t, mu, mid, ALU.mult, ALU.add)
            rs, ks, vs = O("r"), Tm(0), O("v")
            shift(rs, I("r", 0), I("r", 1), I("r", 2), pcol("mu_r"), Tm(7))
            shift(ks, I("k", 0), I("k", 1), I("k", 2), pcol("mu_k"), Tm(7))
            shift(vs, I("v", 0), I("v", 1), I("v", 2), pcol("mu_v"), Tm(7))
            ls = Lt(0)
            shift(ls, (lt_[:, 0:nn], lb_), (lt_[:, 1:nn + 1], lb_), (lt_[:, 2:nn + 2], lb_), (mul[0][:], mul[1]), Lt(2))
            lth = Lt(1)
            h_act(k, lth, ls, AF.Tanh)
            kk = Tm(1)
            h_ts(k, kk, ks, pcol("k_k"), None, ALU.mult)
            h_tt(k, Tm(2), kk, kk, ALU.mult)
            h_mm(k, Pz(0), (ones[0][:], ones[1]), Tm(2))
            h_act(k, Tm(2), Pz(0), AF.Sqrt, bias=(tiny[0][:], tiny[1]))
            h_recip(k, Tm(2), Tm(2))
            h_tt(k, O("kkn"), kk, Tm(2), ALU.mult)
            for d, sfx in enumerate(("f", "b")):
                h_mm(k, Pz(1), (wx[0][:, d, :], wx[1]), lth)
                h_mm(k, Pz(2), (wx[0][:, 2 + d, :], wx[1]), ls)
                h_sigmoid(k, Tm(3), Pz(1), Tm(3), bias=ncol("w0_" + sfx))
                h_act(k, O("w_" + sfx), Tm(3), AF.Exp, scale=-float(np.exp(-0.5)))
                a = Tm(4)
                h_sigmoid(k, a, Pz(2), a, bias=ncol("a0_" + sfx))
                h_ts(k, Tm(5), a, -1.0, pcol("k_a"), ALU.add, ALU.mult)
                h_stt(k, O("k_" + sfx), Tm(5), 1.0, ks, ALU.add, ALU.mult)
                h_stt(k, O("nb_" + sfx), O("kkn"), -1.0, a, ALU.mult, ALU.mult)
            h_tt(k, Tm(5), O("k_f"), O("k_b"), ALU.add)
            h_stt(k, Tm(5), rs, ncol("r_k"), Tm(5), ALU.mult, ALU.mult)
            h_mm(k, Pz(3), (ones[0][:], ones[1]), Tm(5))
            h_tt(k, O("bonus"), Pz(3), vs, ALU.mult)
            h_silu(k, O("sg_rw"), I("g_rw", 1), Tm(6))
            h_silu(k, O("sg_hy"), I("g_hy", 1), Tm(6))
            for nm in ("v", "x1", "x2"):
                src = "h" + nm
                h_ts(k, Tm(6), I(src, 0), pcol("t0_" + nm), None, ALU.mult)
                h_stt(k, Tm(6), I(src, 1), pcol("t1_" + nm), Tm(6), ALU.mult, ALU.add)
                h_stt(k, O(src), I(src, 2), pcol("t2_" + nm), Tm(6), ALU.mult, ALU.add)
            for name in ("bonus", "sg_rw", "hv", "hx1", "hx2", "sg_hy"):
                t, b = fo[FO[name]][j]
                k.dma("sync", fo_s[FO[name], :, oc0:oc0 + nn], t[:, 0:nn], r=[b])
            k.dma("sync", vT2_s[0:64, oc0:oc0 + nn], fo[FO["v"]][j][0][:, 0:nn], r=[fo[FO["v"]][j][1]])
            for bi in range(nn // 128):
                tau0 = oc0 + bi * 128
                s_lo = (128 - tau0) if tau0 < 256 else (256 + n - 128 - (tau0 - 256))
                q = blkc[0] % 2
                blkc[0] += 1
                pk, pkb = ptk[0]
                for ai, nm in enumerate(tord):
                    t, b = fo[FO[nm]][j]
                    h_tr(k, (pk[:, ai, :], pkb), (t[:, bi * 128:(bi + 1) * 128], b), (identf[0][0:64, 0:64], identf[1]))
                tk = (tok[q][0][:], tok[q][1])
                h_copy(k, tk, (pk[:, 0:9, :], pkb), eng="scalar")
                k.dma("sync", bc_s[0, tau0:tau0 + 128, :], tok[q][0][:, 0:5, :], r=[tk[1]])
                h_mm(k, (pfl[0][:, 0:192], pfl[1]), (Jf[0][:], Jf[1]), (tok[q][0][:, 5:8, :], tk[1]))
                h_mm(k, (pfl[0][:, 192:320], pfl[1]), (Jf[0][:], Jf[1]), (tok[q][0][:, 3:5, :], tk[1]))
                h_copy(k, (tokb[q][0][:], tokb[q][1]), (pfl[0][:, 0:320], pfl[1]))
                k.dma("sync", bc_s[1, s_lo:s_lo + 128, :], tokb[q][0][:], r=[tokb[q][1]])
                h_mm(k, (pfl[0][0:64, 384:512], pfl[1]), (tok[q][0][:, 8, :], tk[1]), (Jf[0][:], Jf[1]))
                h_copy(k, (vrev[q][0][:], vrev[q][1]), (pfl[0][0:64, 384:512], pfl[1]), eng="gpsimd" if False else "vector")
                k.dma("sync", vT2_s[64:128, s_lo:s_lo + 128], vrev[q][0][:], r=[vrev[q][1]])
        for ci, (pc0, oc0, nn) in enumerate(seq_chunks(n)):
            chunk(ci, pc0, oc0, nn)
        k.end_phase()

    def phase_scan():
        k.begin_phase()
        S, S_b = k.sb("S", [128, 64], F32)
        tmp, _ = k.sb("stmp", [128, 64], F32)
        sa, _ = k.sb("sa", [128, 2], F32)
        NBUF = 4
        bt = [k.sb("bt%d" % i, [128, TC, 320], F32) for i in range(NBUF)]
        vt = [k.sb("svt%d" % i, [128, 512], F32) for i in range(2)]
        yt = [k.sb("syt%d" % i, [128, 512], F32) for i in range(2)]
        A_b = Buf()
        bt2 = [Buf() for _ in range(NBUF)]
        h_memset(k, (S[:], S_b), 0.0)
        nch = (T + TC - 1) // TC
        for c in range(nch):
            s0 = c * TC
            nst = min(TC, T - s0)
            btt, btb = bt[c % NBUF]
            btb2 = bt2[c % NBUF]
            q = "sync" if c % 2 == 0 else "scalar"
            k.dma(q, btt[0:64, 0:nst, :], bc_s[0, s0:s0 + nst, :].partition_broadcast(64), w=[btb])
            k.dma(q, btt[64:128, 0:nst, :], bc_s[1, s0:s0 + nst, :].partition_broadcast(64), w=[btb2])
            if s0 % 512 == 0:
                vi = (s0 // 512) % 2
                vtt, vtb = vt[vi]
                ytt, ytb = yt[vi]
                nv = min(512, T - s0)
                k.dma("gpsimd", vtt[:, 0:nv], vT2_s[:, s0:s0 + nv], w=[vtb])
            for i in range(nst):
                s = s0 + i
                sl = s % 512
                W = btt[:, i, 0:64]; NB_ = btt[:, i, 64:128]; K_ = btt[:, i, 128:192]; KK = btt[:, i, 192:256]; R_ = btt[:, i, 256:320]
                sac = sa[:, s % 2:s % 2 + 1]
                P.op("vector", lambda E, KK=KK, sac=sac: E.scalar_tensor_tensor(out=tmp[:], in0=S[:], scalar=1.0, in1=KK, op0=ALU.mult, op1=ALU.mult, accum_out=sac), [btb, btb2], [A_b], relax=True)
                P.op("vector", lambda E, W=W: E.tensor_tensor(out=S[:], in0=S[:], in1=W, op=ALU.mult), [S_b], [S_b], relax=True)
                P.op("vector", lambda E, NB_=NB_, sac=sac: E.scalar_tensor_tensor(out=S[:], in0=NB_, scalar=sac, in1=S[:], op0=ALU.mult, op1=ALU.add), [S_b, A_b], [S_b], relax=True)
                P.op("vector", lambda E, K_=K_, vtt=vtt, sl=sl: E.scalar_tensor_tensor(out=S[:], in0=K_, scalar=vtt[:, sl:sl + 1], in1=S[:], op0=ALU.mult, op1=ALU.add), [S_b, vtb], [S_b], relax=True)
                P.op("vector", lambda E, R_=R_, ytt=ytt, sl=sl: E.scalar_tensor_tensor(out=tmp[:], in0=S[:], scalar=1.0, in1=R_, op0=ALU.mult, op1=ALU.mult, accum_out=ytt[:, sl:sl + 1]), [S_b, btb, btb2], [ytb], relax=True)
                if sl == 511 or s == T - 1:
                    c0 = s - sl
                    k.dma("gpsimd", yT2_s[:, c0:s + 1], ytt[:, 0:sl + 1], r=[ytb])
        k.end_phase()

    def phase_filt(l, Lf):
        Li = L_in[l]
        fc = fconst[Lf]
        k.begin_phase()
        w1 = k.sb("w1s", [33, 64], F32); w2 = k.sb("w2s", [64, 64], F32); w3 = k.sb("w3s", [64, 256], F32)
        bb = k.sb("bbs", [64, 2], F32); b3 = k.sb("b3s", [128, 2], F32); nd = k.sb("nds", [128, 1], F32)
        for t, d in ((w1, Li["hw1"]), (w2, Li["hw2"]), (w3, Li["hw3"]), (bb, Li["hb12"]), (b3, Li["hb3"]), (nd, nd_d)):
            k.dma("sync", t[0][:], d, w=[t[1]])
        nch = (Lf + 511) // 512
        part = k.sb("part", [128, 2 * nch], F32)
        h_memset(k, (part[0][:], part[1]), 0.0)
        b0 = k.sb("b0", [128, 1], F32)
        ft = [k.sb("ft%d" % i, [33, 512], F32) for i in range(2)]
        tv = [k.sb("tv%d" % i, [128, 512], F32) for i in range(2)]
        hx = [k.sb("hx%d" % i, [64, 512], F32) for i in range(2)]
        hi = k.sb("hi", [64, 512], I32); hf = k.sb("hf", [64, 512], F32)
        win = k.sb("win", [128, 512], F32)
        tF = [k.sb("tF%d" % i, [128, 512], F32) for i in range(2)]
        oF = [k.sb("oF%d" % i, [128, 512], BF16) for i in range(2)]
        junk = k.sb("junkf", [128, 512], F32)
        ph = [k.ps("ph%d" % i, [64, 512], F32) for i in range(2)]
        pt = [k.ps("pt%d" % i, [128, 512], F32) for i in range(2)]
        kl = kl_h[Lf].ap()

        def sin_layer(out, psum, bias, nn):
            x = (hf[0][:, 0:nn], hf[1]); ki = (hi[0][:, 0:nn], hi[1])
            h_ts(k, out, psum, bias, None, ALU.add)
            h_ts(k, ki, out, 1.0 / TWO_PI, None, ALU.mult)
            h_copy(k, x, ki)
            h_stt(k, out, x, -TWO_PI, out, ALU.mult, ALU.add)
            h_ts(k, out, out, -float(np.pi), float(np.pi), ALU.max, ALU.min)
            h_act(k, out, out, AF.Sin)
        cnt = 0
        for side in (1, 0):
            for c in range(nch):
                c0 = c * 512
                nn = min(512, Lf - c0)
                j = cnt % 2
                cnt += 1
                k.dma("sync", ft[j][0][:, 0:nn], (fc["featsTr"] if side else fc["featsT"])[:, c0:c0 + nn], w=[ft[j][1]])
                k.dma("sync", tv[j][0][:, 0:nn], (fc["tvr"] if side else fc["tv"])[:, c0:c0 + nn].partition_broadcast(128), w=[tv[j][1]])
                p0 = (ph[0][0][:, 0:nn], ph[0][1]); p1 = (ph[1][0][:, 0:nn], ph[1][1])
                h_mm(k, p0, (w1[0][:], w1[1]), (ft[j][0][:, 0:nn], ft[j][1]))
                h1 = (hx[0][0][:, 0:nn], hx[0][1])
                sin_layer(h1, p0, (bb[0][:, 0:1], bb[1]), nn)
                h_mm(k, p1, (w2[0][:], w2[1]), h1)
                h2 = (hx[1][0][:, 0:nn], hx[1][1])
                sin_layer(h2, p1, (bb[0][:, 1:2], bb[1]), nn)
                ptx = (pt[j][0][:, 0:nn], pt[j][1])
                h_mm(k, ptx, (w3[0][:, side * 128:(side + 1) * 128], w3[1]), h2)
                w_ = (win[0][:, 0:nn], win[1])
                h_act(k, w_, (tv[j][0][:, 0:nn], tv[j][1]), AF.Exp, scale=(nd[0][:], nd[1]))
                F_ = (tF[j][0][:, 0:nn], tF[j][1])
                h_stt(k, F_, ptx, (b3[0][:, side:side + 1], b3[1]), w_, ALU.add, ALU.mult)
                hi_ = nn
                if side == 1 and c == nch - 1:
                    h_copy(k, (b0[0][:], b0[1]), (tF[j][0][:, nn - 1:nn], tF[j][1]))
                    hi_ = nn - 1
                if side == 0 and c == 0:
                    h_tt(k, (tF[j][0][:, 0:1], tF[j][1]), (tF[j][0][:, 0:1], tF[j][1]), (b0[0][:], b0[1]), ALU.add)
                pcol_ = 2 * c + side
                if hi_ > 0:
                    h_act(k, (junk[0][:, 0:hi_], junk[1]), (tF[j][0][:, 0:hi_], tF[j][1]), AF.Square, accum=(part[0][:, pcol_:pcol_ + 1], part[1]))
                    h_copy(k, (oF[j][0][:, 0:hi_], oF[j][1]), (tF[j][0][:, 0:hi_], tF[j][1]), eng="gpsimd")
                    base = c0 if side == 1 else (Lf - 1 + c0)
                    k.dma("sync", kl[:, base:base + hi_], oF[j][0][:, 0:hi_], r=[oF[j][1]])
        tot = k.sb("tot", [128, 1], F32)
        k.V(lambda E: E.tensor_reduce(out=tot[0][:], in_=part[0][:], axis=mybir.AxisListType.X, op=ALU.add), [part[1]], [tot[1]])
        k.dma("sync", ssq_s[Lf].rearrange("o p -> p o"), tot[0][:], r=[tot[1]], allow_slow_non_contiguous=True)
        k.end_phase()

    def phase_hy(l, nb, col_fo, col_mix, readout):
        Li = L_in[l]
        Lf = 128 * nb
        W = 128 * (2 * nb - 1)
        k.begin_phase()
        nrm = k.sb("nrm", [128, 128], F32); skp = k.sb("skp", [128, 128], F32)
        k.dma("sync", nrm[0][:], ssq_s[Lf].partition_broadcast(128), w=[nrm[1]])
        k.dma("sync", skp[0][:], Li["skipT"].partition_broadcast(128), w=[skp[1]])
        h_act(k, (nrm[0][:], nrm[1]), (nrm[0][:], nrm[1]), AF.Sqrt)
        h_recip(k, (nrm[0][:], nrm[1]), (nrm[0][:], nrm[1]))
        if readout:
            k.begin_phase()
            pp = k.sb("pp_sb", [64, NPC], F32)
            k.dma("sync", pp[0][:], Li["pp"], w=[pp[1]])
            o64 = k.sb("o64", [64, 64], F32)
            h_memset(k, (o64[0][:], o64[1]), 1.0 / 64)
            geps = k.sb("geps", [64, 1], F32)
            h_memset(k, (geps[0][:], geps[1]), 64e-5)
            ra = [k.sb("ra%d" % i, [64, 512], F32) for i in range(2)]
            rbk = [k.sb("rbk%d" % i, [64, 128], F32) for i in range(2)]
            rbt = [k.sb("rbt%d" % i, [128, 64], F32) for i in range(2)]
            rc = [k.sb("rc%d" % i, [64, 512], F32) for i in range(2)]
            rd = [k.sb("rd%d" % i, [64, 512], F32) for i in range(2)]
            r1 = k.sb("r1", [64, 512], F32); r2 = k.sb("r2", [64, 512], F32)
            rob = [k.sb("rob%d" % i, [64, 512], BF16) for i in range(2)]
            pr = [k.ps("pr%d" % i, [64, 512], F32) for i in range(2)]
            prt = k.ps("prt", [128, 64], F32)
            prb = k.ps("prb", [64, 512], F32)
            bq = 0
            chunks = [(0, 256)] + [(256 + u0, min(512, n - u0)) for u0 in range(0, n, 512)]
            for ci, (c0, nn) in enumerate(chunks):
                j = ci % 2
                A = (ra[j][0][:, 0:nn], ra[j][1]); C = (rc[j][0][:, 0:nn], rc[j][1]); Dg = (rd[j][0][:, 0:nn], rd[j][1])
                R1 = (r1[0][:, 0:nn], r1[1]); R2 = (r2[0][:, 0:nn], r2[1])
                P0 = (pr[0][0][:, 0:nn], pr[0][1]); P1 = (pr[1][0][:, 0:nn], pr[1][1])
                k.dma("scalar", A[0], yT2_s[0:64, c0:c0 + nn], w=[A[1]])
                k.dma("scalar", C[0], fo_s[FO["bonus"], :, c0:c0 + nn], w=[C[1]])
                k.dma("scalar", Dg[0], fo_s[FO["sg_rw"], :, c0:c0 + nn], w=[Dg[1]])
                for bi in range(nn // 128):
                    tau0 = c0 + bi * 128
                    s_lo = (128 - tau0) if tau0 < 256 else (256 + n - 128 - (tau0 - 256))
                    q = bq % 2
                    bq += 1
                    k.dma("scalar", rbk[q][0][:], yT2_s[64:128, s_lo:s_lo + 128], w=[rbk[q][1]])
                    h_tr(k, (prt[0][:], prt[1]), (rbk[q][0][:], rbk[q][1]), (identf[0][0:64, 0:64], identf[1]))
                    h_copy(k, (rbt[q][0][:], rbt[q][1]), (prt[0][:], prt[1]), eng="scalar")
                    h_mm(k, (prb[0][:, bi * 128:(bi + 1) * 128], prb[1]), (rbt[q][0][:], rbt[q][1]), (Jf[0][:], Jf[1]))
                h_tt(k, A, A, (prb[0][:, 0:nn], prb[1]), ALU.add)
                h_mm(k, P0, (o64[0][:], o64[1]), A)
                h_tt(k, R1, A, P0, ALU.subtract)
                h_tt(k, R2, R1, R1, ALU.mult)
                h_mm(k, P1, (o64[0][:], o64[1]), R2)
                h_act(k, R2, P1, AF.Sqrt, bias=(geps[0][:], geps[1]))
                h_recip(k, R2, R2)
                h_tt(k, R1, R1, R2, ALU.mult)
                h_ts(k, R1, R1, (pp[0][:, PC["gn_w"]:PC["gn_w"] + 1], pp[1]), (pp[0][:, PC["gn_b"]:PC["gn_b"] + 1], pp[1]), ALU.mult, ALU.add)
                h_tt(k, R1, R1, C, ALU.add)
                OB = (rob[j][0][:, 0:nn], rob[j][1])
                h_tt(k, OB, R1, Dg, ALU.mult)
                mc = (n + c0) if c0 < 256 else (c0 - 256)
                k.dma("scalar", mix_send[0:64, mc:mc + nn], OB[0], r=[OB[1]])
            k.end_phase()
        ksh = [k.sb("ksh%d" % i, [128, W], BF16) for i in range(2)]
        ld = [[k.sb("ld%d_%d" % (a, i), [nb, 128], F32) for i in range(2)] for a in range(4)]
        zt = [k.sb("zt%d" % i, [128, nb], F32) for i in range(2)]
        x1t = [k.sb("x1t%d" % i, [128, nb], F32) for i in range(2)]
        x2t = [k.sb("x2t%d" % i, [128, nb], F32) for i in range(2)]
        sgt = [k.sb("sgt%d" % i, [128, nb], F32) for i in range(2)]
        zb = k.sb("zb", [128, nb], BF16); zf = k.sb("zf", [128, nb], BF16)
        z2 = k.sb("z2", [128, nb], F32); t1 = k.sb("t1", [128, nb], F32)
        ot = [k.sb("oth%d" % i, [128, nb], BF16) for i in range(2)]
        otT = [k.sb("otT%d" % i, [nb, 128], BF16) for i in range(2)]
        pin = [k.ps("pin%d" % i, [128, nb], F32) for i in range(2)]
        pf = k.ps("pf", [128, nb], F32)
        py = [k.ps("pyc%d" % i, [128, nb], F32) for i in range(2)]
        pot = k.ps("pot", [nb, 128], BF16)
        order = [0] + [d for d in range(-(nb - 1), nb) if d != 0]
        cnt = 0
        names = ["hv", "hx1", "hx2", "sg_hy"]
        for c in range(64):
            j = c % 2
            dst = [zt[j], x1t[j], x2t[j], sgt[j]]
            for a in range(4):
                lt_, lb_ = ld[a][j]
                k.dma("gpsimd", lt_[:], fo_s[FO[names[a]], c, col_fo:col_fo + Lf].rearrange("(s j) -> s j", j=128), w=[lb_])
                h_tr(k, (pin[a % 2][0][:], pin[a % 2][1]), (lt_[:], lb_), (identf[0][0:nb, 0:nb], identf[1]))
                h_copy(k, (dst[a][0][:], dst[a][1]), (pin[a % 2][0][:], pin[a % 2][1]), eng=("scalar" if a % 2 else "vector"))
            Z = (zt[j][0][:], zt[j][1]); X1 = (x1t[j][0][:], x1t[j][1]); X2 = (x2t[j][0][:], x2t[j][1]); SG = (sgt[j][0][:], sgt[j][1])
            zin = Z
            for o in range(2):
                row = o * 64 + c
                kt, kb_ = ksh[cnt % 2]
                q = "sync" if cnt % 2 == 0 else "scalar"
                cnt += 1
                src = bass.AP(kl_h[Lf], row * 2 * Lf, [[1, 128], [1, W]])
                k.dma(q, kt[:], src, w=[kb_])
                h_copy(k, (zb[0][:], zb[1]), zin, eng="gpsimd")
                h_mm(k, (pf[0][:], pf[1]), (Jb[0][:], Jb[1]), (zb[0][:], zb[1]))
                h_copy(k, (zf[0][:], zf[1]), (pf[0][:], pf[1]), eng="scalar")
                pyt, pyb = py[o]
                for ii, Dd in enumerate(order):
                    e = Dd + nb - 1
                    S0, S1 = max(0, -Dd), min(nb, nb - Dd)
                    h_mm(k, (pyt[:, S0 + Dd:S1 + Dd], pyb), (kt[:, 128 * e:128 * e + 128], kb_), (zf[0][:, S0:S1], zf[1]), start=(ii == 0), stop=(ii == len(order) - 1))
                ncol_ = (nrm[0][:, row:row + 1], nrm[1])
                scol = (skp[0][:, row:row + 1], skp[1])
                h_ts(k, (t1[0][:], t1[1]), (pyt[:], pyb), ncol_, None, ALU.mult)
                h_stt(k, (t1[0][:], t1[1]), zin, scol, (t1[0][:], t1[1]), ALU.mult, ALU.add)
                if o == 0:
                    h_tt(k, (z2[0][:], z2[1]), (t1[0][:], t1[1]), X1, ALU.mult)
                    zin = (z2[0][:], z2[1])
                else:
                    O_ = (ot[j][0][:], ot[j][1])
                    h_tt(k, (t1[0][:], t1[1]), (t1[0][:], t1[1]), X2, ALU.mult)
                    h_tt(k, O_, (t1[0][:], t1[1]), SG, ALU.mult, eng="gpsimd")
                    h_tr(k, (pot[0][:], pot[1]), O_, IDB)
                    h_copy(k, (otT[j][0][:], otT[j][1]), (pot[0][:], pot[1]), eng="scalar")
                    k.dma("gpsimd", mix_send[64 + c, col_mix:col_mix + Lf].rearrange("(s j) -> s j", j=128), otT[j][0][:], r=[otT[j][1]])
        k.end_phase()

    for l in range(4):
        phase_tok(l)
        if l % 2 == 0:
            phase_attn(l)
        else:
            phase_rfeat(l)
            phase_scan()
            phase_filt(l, n)
            phase_hy(l, nlb, 256, 0, True)
            if l < 3:
                phase_filt(l, 256)
                phase_hy(l, 2, 0, n, False)
        P.coll("AllGather", mybir.AluOpType.bypass, mix_send, mix_all)
        P.barrier()
    phase_tok(4, final=True)
    k.done()
    return nc


_FUSED_CACHE = {}


def _c_cols(c, c_ctx):
    return np.ascontiguousarray(np.concatenate([c.reshape(8, 128).T, c_ctx.reshape(8, 128).T], axis=1).astype(np.float32))


def fused_inputs(inp, n):
    nlb = n // 128
    x = np.ascontiguousarray(inp["x"][0], dtype=np.float32)
    ctx = np.ascontiguousarray(inp["ctx"][0], dtype=np.float32)
    C, S, RT = rope_tables(nlb)
    sel = np.zeros((65, 64), np.float32)
    sel[64] = 1.0
    perm = np.concatenate([np.concatenate([np.arange(64 * r, 64 * r + 64), np.arange(512 + 64 * r, 512 + 64 * r + 64)]) for r in range(8)])
    common = {"x": x, "ctx": ctx, "c_cols": _c_cols(inp["c"][0], inp["c_ctx"]), "ident": np.eye(128, dtype=np.float32),
              "Jmat": np.eye(128, dtype=np.float32)[::-1].copy(), "final_norm": inp["final_norm"].reshape(1, -1).astype(np.float32),
              "ropeC": C, "ropeS": S, "rmatT": RT, "sel": sel}
    for Lf in (n, 256):
        fT, tv, adel = filt_consts(Lf)
        common["featsT%d" % Lf] = fT
        common["featsTr%d" % Lf] = np.ascontiguousarray(fT[:, ::-1])
        common["tv%d" % Lf] = tv
        common["tvr%d" % Lf] = np.ascontiguousarray(tv[:, ::-1])
    maps = []
    for h in range(8):
        m = dict(common)
        kvh = h // 4
        sl = slice(64 * h, 64 * h + 64)
        m["negdelta"] = -np.concatenate([adel[sl], adel[sl]]).reshape(128, 1).astype(np.float32)
        for l in range(4):
            i = l // 2
            attn = l % 2 == 0
            pre = "attn" if attn else "rec"
            m["norm_g%d" % l] = inp[pre + "_norm"][i].reshape(1, -1)
            m["ada_w%d" % l] = inp[pre + "_ada_w"][i]
            m["ada_b%d" % l] = inp[pre + "_ada_b"][i].reshape(1, -1)
            m["w_out%d" % l] = np.ascontiguousarray(inp[pre + "_w_out"][i][perm])
            w_in = inp[pre + "_w_in"][i]
            if attn:
                starts = [64 * h, 512 + 64 * h, 1536 + 64 * h, 2048 + 64 * h, 2560 + 64 * kvh, 2816 + 64 * h, 1024 + 64 * h, 2688 + 64 * kvh]
                cols = np.concatenate([np.arange(s0, s0 + 64) for s0 in starts])
                m["w_in%d" % l] = np.ascontiguousarray(w_in[:, cols])
                m["biasT%d" % l] = na_bias_tables(inp["na_rpb"][i][h], nlb)
                m["gains%d" % l] = np.stack([inp["gqa_q_gain"][i], inp["gqa_k_gain"][i]], 1).astype(np.float32)
            else:
                starts = [64 * h, 512 + 64 * h, 1024 + 64 * h, 1664 + 64 * h, 2176 + 64 * h, 2688 + 64 * h, 3200 + 64 * h, 3712 + 64 * h]
                cols = np.concatenate([np.arange(s0, s0 + 64) for s0 in starts] + [np.arange(1536, 1664)])
                m["w_in%d" % l] = np.ascontiguousarray(w_in[:, cols])
                prm = {k_: inp[k_][i] for k_ in inp if k_.startswith("rwkv") or k_.startswith("hy")}
                ri = rfeat_inputs(np.zeros((1, 4224), np.float32), np.zeros((1, 4224), np.float32), h, prm)
                m["pp%d" % l] = ri["pp"]
                m["mu_lora%d" % l] = ri["mu_lora"]
                m["wx%d" % l] = ri["wx"]
                fi = filt_inputs(256, h, prm)
                m["hw1_%d" % l] = fi["w1"]
                m["hw2_%d" % l] = fi["w2"]
                m["hw3_%d" % l] = fi["w3c"]
                m["hb12_%d" % l] = fi["b12"]
                m["hb3_%d" % l] = fi["b3c"]
                m["skipT%d" % l] = np.concatenate([prm["hy_skip"][0][sl], prm["hy_skip"][1][sl]]).reshape(1, 128).astype(np.float32)
        maps.append(m)
    return maps


def kernel(**inp):
    inp = {k_: np.asarray(v) for k_, v in inp.items()}
    n = inp["x"].shape[1]
    if n not in _FUSED_CACHE:
        _FUSED_CACHE[n] = build_fused(n)
    nc = _FUSED_CACHE[n]
    res = run_bass_kernel_spmd(nc, fused_inputs(inp, n), core_ids=list(range(8)))
    tpc = n // 8
    out = np.concatenate([res.results[i]["y_out"][i * tpc:(i + 1) * tpc] for i in range(8)], 0)
    return out[None].astype(np.float32)
```
